# Optimizing a Trainium2 kernel written in Bass

```python
import math
import jax, jax.numpy as jnp
from jax import lax
import numpy as np

D_MODEL = 2048
BATCH = 1
SEQ = 8192
DEPTH = 1

ATT_HEADS = 8
ATT_HEAD_DIM = 128
ATT_WIDTH = ATT_HEADS * ATT_HEAD_DIM
MOBA_BLOCK = 256
MOBA_TOPK = 3
Q_CHUNK = 128
ROPE_THETA = 500000.0
ROPE_DIM = ATT_HEAD_DIM // 4
RWKV_HEAD_DIM = 64
RWKV_WIDTH = 1024
RWKV_HEADS = RWKV_WIDTH // RWKV_HEAD_DIM
DECAY_LORA = 96
AAA_LORA = 96
GATE_LORA = 256
GN_EPS = 64e-5
IN_WIDTH = 3 * ATT_WIDTH + 3 * RWKV_WIDTH + 2 * D_MODEL
N_GROUPS = 8
EXPERTS_PER_GROUP = 8
N_EXPERTS = N_GROUPS * EXPERTS_PER_GROUP
TOP_K = 2
EXPERT_FF = 512
EXPERT_BLOCK = 128
RMS_EPS = 1e-6
NEG = -1e30

kernel_name = "hybrid_moba_rwkv7_hmoe_block"


def rms_norm(z, g):
    zf = z.astype(jnp.float32)
    zf = zf * lax.rsqrt(jnp.mean(zf * zf, axis=-1, keepdims=True) + RMS_EPS)
    return zf.astype(z.dtype) * g


def shift_right(z):
    return jnp.pad(z, ((0, 0), (1, 0), (0, 0)))[:, :-1]


def partial_rope(z, pos):
    half = ROPE_DIM // 2
    inv = ROPE_THETA ** (-jnp.arange(half, dtype=jnp.float32) / half)
    ang = pos.astype(jnp.float32)[:, None] * inv[None, :]
    cos = jnp.cos(ang).astype(z.dtype)
    sin = jnp.sin(ang).astype(z.dtype)
    z1 = z[..., :half]
    z2 = z[..., half:ROPE_DIM]
    rot = jnp.concatenate([z1 * cos - z2 * sin, z2 * cos + z1 * sin], axis=-1)
    return jnp.concatenate([rot, z[..., ROPE_DIM:]], axis=-1)


def moba_attention(q, k, v):
    B, H, S, Dh = q.shape
    nb = -(-S // MOBA_BLOCK)
    sp = nb * MOBA_BLOCK
    pad = ((0, 0), (0, 0), (0, sp - S), (0, 0))
    q, k, v = jnp.pad(q, pad), jnp.pad(k, pad), jnp.pad(v, pad)
    kb = k.reshape(B, H, nb, MOBA_BLOCK, Dh)
    vb = v.reshape(B, H, nb, MOBA_BLOCK, Dh)
    kmean = jnp.mean(kb.astype(jnp.float32), axis=3)
    nc = sp // Q_CHUNK
    qc = q.reshape(B, H, nc, Q_CHUNK, Dh).transpose(2, 0, 1, 3, 4)
    ntop = min(MOBA_TOPK, nb)
    scale = ATT_HEAD_DIM ** -0.5
    bi = jnp.arange(B)[:, None, None, None]
    hi = jnp.arange(H)[None, :, None, None]

    def one_chunk(args):
        ci, qx = args
        qblk = (ci * Q_CHUNK) // MOBA_BLOCK
        qpos = ci * Q_CHUNK + jnp.arange(Q_CHUNK)
        gate = jnp.einsum('bhqd,bhnd->bhqn', qx.astype(jnp.float32), kmean)
        gate = jnp.where(jnp.arange(nb) < qblk, gate, NEG)
        _, sel = lax.top_k(gate, ntop)
        valid = sel < qblk
        k_own = lax.dynamic_index_in_dim(kb, qblk, axis=2, keepdims=False)
        v_own = lax.dynamic_index_in_dim(vb, qblk, axis=2, keepdims=False)
        kpos = qblk * MOBA_BLOCK + jnp.arange(MOBA_BLOCK)
        s_own = jnp.einsum('bhqd,bhkd->bhqk', qx, k_own).astype(jnp.float32) * scale
        s_own = jnp.where(kpos[None, :] <= qpos[:, None], s_own, NEG)
        k_sel = kb[bi, hi, sel]
        v_sel = vb[bi, hi, sel]
        s_sel = jnp.einsum('bhqd,bhqnkd->bhqnk', qx, k_sel).astype(jnp.float32) * scale
        s_sel = jnp.where(valid[..., None], s_sel, NEG)
        s_all = jnp.concatenate([s_own, s_sel.reshape(B, H, Q_CHUNK, ntop * MOBA_BLOCK)], axis=-1)
        p = jax.nn.softmax(s_all, axis=-1).astype(v.dtype)
        p_own = p[..., :MOBA_BLOCK]
        p_sel = p[..., MOBA_BLOCK:].reshape(B, H, Q_CHUNK, ntop, MOBA_BLOCK)
        return (jnp.einsum('bhqk,bhkd->bhqd', p_own, v_own)
                + jnp.einsum('bhqnk,bhqnkd->bhqd', p_sel, v_sel))

    out = lax.map(one_chunk, (jnp.arange(nc), qc))
    out = out.transpose(1, 2, 0, 3, 4).reshape(B, H, sp, Dh)
    return out[:, :, :S]


def rwkv7_time_mix(h, r, k, v, mu_w, mu_a, mu_g, w0, w_w1, w_w2, a0, w_a1, w_a2,
                   w_g1, w_g2, k_k, k_a, r_k, gn_w, gn_b):
    B, S, _ = h.shape
    H, N = RWKV_HEADS, RWKV_HEAD_DIM
    f32 = jnp.float32
    dh = shift_right(h) - h
    xw = h + dh * mu_w
    xa = h + dh * mu_a
    xg = h + dh * mu_g
    d = (w0 + jnp.tanh(xw @ w_w1) @ w_w2).astype(f32)
    w = jnp.exp(-math.exp(-0.5) * jax.nn.sigmoid(d))
    a = jax.nn.sigmoid((a0 + (xa @ w_a1) @ w_a2).astype(f32))
    g = jax.nn.sigmoid(xg @ w_g1) @ w_g2
    rf, kf, vf = r.astype(f32), k.astype(f32), v.astype(f32)
    kk = (kf * k_k).reshape(B, S, H, N)
    kk = kk / jnp.maximum(jnp.sqrt(jnp.sum(kk * kk, axis=-1, keepdims=True)), 1e-12)
    kt = kf * (1.0 + (a - 1.0) * k_a)
    rh, wh, kth, vh, ah = (z.reshape(B, S, H, N) for z in (rf, w, kt, vf, a))
    xs = tuple(jnp.moveaxis(z, 1, 0) for z in (rh, wh, kth, vh, kk, ah))

    def step(state, inp):
        r_t, w_t, k_t, v_t, kk_t, a_t = inp
        sa = jnp.einsum('bhvk,bhk->bhv', state, -kk_t)
        state = (state * w_t[:, :, None, :]
                 + sa[..., None] * (kk_t * a_t)[:, :, None, :]
                 + v_t[..., None] * k_t[:, :, None, :])
        return state, jnp.einsum('bhvk,bhk->bhv', state, r_t)

    _, ys = lax.scan(step, jnp.zeros((B, H, N, N), f32), xs)
    y = jnp.moveaxis(ys, 0, 1)
    mean = jnp.mean(y, axis=-1, keepdims=True)
    var = jnp.mean(jnp.square(y - mean), axis=-1, keepdims=True)
    yn = ((y - mean) * lax.rsqrt(var + GN_EPS)).reshape(B, S, RWKV_WIDTH) * gn_w + gn_b
    bonus = (jnp.sum(rh * kth * r_k, axis=-1, keepdims=True) * vh).reshape(B, S, RWKV_WIDTH)
    return ((yn + bonus) * g).astype(h.dtype)


def token_mixer(h, w_in, mu_r, mu_k, mu_v, mu_w, mu_a, mu_g, w0, w_w1, w_w2, a0, w_a1, w_a2,
                w_g1, w_g2, k_k, k_a, r_k, gn_w, gn_b, w_up_att, w_up_rwkv, w_o):
    B, S, _ = h.shape
    proj = h @ w_in
    q, k, v, rkv, gates = jnp.split(
        proj, [ATT_WIDTH, 2 * ATT_WIDTH, 3 * ATT_WIDTH, 3 * ATT_WIDTH + 3 * RWKV_WIDTH], axis=-1)
    gate_att, gate_rwkv = jnp.split(gates, 2, axis=-1)
    pos = jnp.arange(S)
    def heads(z):
        return z.reshape(B, S, ATT_HEADS, ATT_HEAD_DIM).transpose(0, 2, 1, 3)
    qh = partial_rope(heads(q), pos)
    kh = partial_rope(heads(k), pos)
    o_att = moba_attention(qh, kh, heads(v)).transpose(0, 2, 1, 3).reshape(B, S, ATT_WIDTH)
    mu_rkv = jnp.concatenate([mu_r, mu_k, mu_v])
    rkv = rkv + (shift_right(rkv) - rkv) * mu_rkv
    r, kr, vr = jnp.split(rkv, 3, axis=-1)
    o_rwkv = rwkv7_time_mix(h, r, kr, vr, mu_w, mu_a, mu_g, w0, w_w1, w_w2, a0, w_a1, w_a2,
                            w_g1, w_g2, k_k, k_a, r_k, gn_w, gn_b)
    mix = (jax.nn.sigmoid(gate_att) * (o_att @ w_up_att)
           + jax.nn.sigmoid(gate_rwkv) * (o_rwkv @ w_up_rwkv))
    return mix @ w_o


def hierarchical_moe(h, w_rg, b_rg, w_re, b_re, w_gate_e, w_up_e, w_down_e):
    B, S, D = h.shape
    N = B * S
    t = h.reshape(N, D)
    pg = jax.nn.softmax((t @ w_rg).astype(jnp.float32) + b_rg, axis=-1)
    pg_top, g_idx = lax.top_k(pg, 1)
    le = ((t @ w_re).astype(jnp.float32) + b_re).reshape(N, N_GROUPS, EXPERTS_PER_GROUP)
    le = jnp.take_along_axis(le, g_idx[:, :, None], axis=1)[:, 0]
    pe = jax.nn.softmax(le, axis=-1)
    pe_top, e_local = lax.top_k(pe, TOP_K)
    wts = pg_top * pe_top / jnp.sum(pe_top, axis=-1, keepdims=True)
    e_idx = g_idx * EXPERTS_PER_GROUP + e_local
    P = N * TOP_K
    n_blk = (P + N_EXPERTS * (EXPERT_BLOCK - 1) + EXPERT_BLOCK - 1) // EXPERT_BLOCK
    e_flat = e_idx.reshape(P)
    tok_flat = jnp.repeat(jnp.arange(N, dtype=jnp.int32), TOP_K)
    w_flat = wts.reshape(P)
    order = jnp.argsort(e_flat)
    e_s, tok_s, w_s = e_flat[order], tok_flat[order], w_flat[order]
    counts = jnp.zeros((N_EXPERTS,), jnp.int32).at[e_flat].add(1)
    start = jnp.cumsum(counts) - counts
    pcounts = (counts + EXPERT_BLOCK - 1) // EXPERT_BLOCK * EXPERT_BLOCK
    pend = jnp.cumsum(pcounts)
    pstart = pend - pcounts
    dest = pstart[e_s] + (jnp.arange(P, dtype=jnp.int32) - start[e_s])
    buf_tok = jnp.zeros((n_blk * EXPERT_BLOCK,), jnp.int32).at[dest].set(tok_s)
    buf_w = jnp.zeros((n_blk * EXPERT_BLOCK,), jnp.float32).at[dest].set(w_s)
    blk_e = jnp.clip(jnp.searchsorted(pend, jnp.arange(n_blk, dtype=jnp.int32) * EXPERT_BLOCK,
                                      side='right'), 0, N_EXPERTS - 1)
    x_buf = t[buf_tok].reshape(n_blk, EXPERT_BLOCK, D)

    def run_block(args):
        xb, e = args
        hid = jax.nn.silu(xb @ w_gate_e[e]) * (xb @ w_up_e[e])
        return hid @ w_down_e[e]

    y_buf = lax.map(run_block, (x_buf, blk_e)).reshape(n_blk * EXPERT_BLOCK, D)
    out = jax.ops.segment_sum(y_buf * buf_w[:, None], buf_tok, num_segments=N)
    return out.astype(h.dtype).reshape(B, S, D)


def setup_inputs(seed: int = 0) -> dict:
    key = jax.random.key(seed)
    ks = iter(jax.random.split(key, 64))
    L, D = DEPTH, D_MODEL
    def nrm(shape, scale):
        return jax.random.normal(next(ks), shape, jnp.float32) * scale
    def uni(shape, lo, hi):
        return jax.random.uniform(next(ks), shape, jnp.float32, lo, hi)
    return {
        "x": nrm((BATCH, SEQ, D), 1.0),
        "c": nrm((BATCH, D), 1.0),
        "w_ada": nrm((L, D, 6 * D), 0.1 * D ** -0.5),
        "b_ada": nrm((L, 6 * D), 0.02),
        "g_pre_mix": 1.0 + nrm((L, D), 0.02),
        "g_post_mix": 1.0 + nrm((L, D), 0.02),
        "g_pre_ffn": 1.0 + nrm((L, D), 0.02),
        "g_post_ffn": 1.0 + nrm((L, D), 0.02),
        "w_in": nrm((L, D, IN_WIDTH), D ** -0.5),
        "mu_r": uni((L, RWKV_WIDTH), 0.0, 1.0),
        "mu_k": uni((L, RWKV_WIDTH), 0.0, 1.0),
        "mu_v": uni((L, RWKV_WIDTH), 0.0, 1.0),
        "mu_w": uni((L, D), 0.0, 1.0),
        "mu_a": uni((L, D), 0.0, 1.0),
        "mu_g": uni((L, D), 0.0, 1.0),
        "w0": uni((L, RWKV_WIDTH), -3.0, 1.0),
        "w_w1": nrm((L, D, DECAY_LORA), D ** -0.5),
        "w_w2": nrm((L, DECAY_LORA, RWKV_WIDTH), DECAY_LORA ** -0.5),
        "a0": nrm((L, RWKV_WIDTH), 0.5),
        "w_a1": nrm((L, D, AAA_LORA), D ** -0.5),
        "w_a2": nrm((L, AAA_LORA, RWKV_WIDTH), AAA_LORA ** -0.5),
        "w_g1": nrm((L, D, GATE_LORA), D ** -0.5),
        "w_g2": nrm((L, GATE_LORA, RWKV_WIDTH), GATE_LORA ** -0.5),
        "k_k": 0.85 + nrm((L, RWKV_WIDTH), 0.05),
        "k_a": 1.0 + nrm((L, RWKV_WIDTH), 0.05),
        "r_k": nrm((L, RWKV_HEADS, RWKV_HEAD_DIM), 0.1),
        "gn_w": 1.0 + nrm((L, RWKV_WIDTH), 0.02),
        "gn_b": nrm((L, RWKV_WIDTH), 0.02),
        "w_up_att": nrm((L, ATT_WIDTH, D), ATT_WIDTH ** -0.5),
        "w_up_rwkv": nrm((L, RWKV_WIDTH, D), RWKV_WIDTH ** -0.5),
        "w_o": nrm((L, D, D), D ** -0.5),
        "w_rg": nrm((L, D, N_GROUPS), D ** -0.5),
        "b_rg": nrm((L, N_GROUPS), 0.01),
        "w_re": nrm((L, D, N_EXPERTS), D ** -0.5),
        "b_re": nrm((L, N_EXPERTS), 0.01),
        "w_gate_e": nrm((L, N_EXPERTS, D, EXPERT_FF), D ** -0.5),
        "w_up_e": nrm((L, N_EXPERTS, D, EXPERT_FF), D ** -0.5),
        "w_down_e": nrm((L, N_EXPERTS, EXPERT_FF, D), EXPERT_FF ** -0.5),
    }


def reference(x, c, w_ada, b_ada, g_pre_mix, g_post_mix, g_pre_ffn, g_post_ffn, w_in,
              mu_r, mu_k, mu_v, mu_w, mu_a, mu_g, w0, w_w1, w_w2, a0, w_a1, w_a2,
              w_g1, w_g2, k_k, k_a, r_k, gn_w, gn_b, w_up_att, w_up_rwkv, w_o,
              w_rg, b_rg, w_re, b_re, w_gate_e, w_up_e, w_down_e):
    for l in range(DEPTH):
        ada = (c @ w_ada[l] + b_ada[l])[:, None, :]
        sh1, sc1, gt1, sh2, sc2, gt2 = jnp.split(ada, 6, axis=-1)
        h = rms_norm(x, g_pre_mix[l]) * (1.0 + sc1) + sh1
        y = token_mixer(h, w_in[l], mu_r[l], mu_k[l], mu_v[l], mu_w[l], mu_a[l], mu_g[l],
                        w0[l], w_w1[l], w_w2[l], a0[l], w_a1[l], w_a2[l], w_g1[l], w_g2[l],
                        k_k[l], k_a[l], r_k[l], gn_w[l], gn_b[l],
                        w_up_att[l], w_up_rwkv[l], w_o[l])
        x = x + gt1 * rms_norm(y, g_post_mix[l])
        h = rms_norm(x, g_pre_ffn[l]) * (1.0 + sc2) + sh2
        y = hierarchical_moe(h, w_rg[l], b_rg[l], w_re[l], b_re[l],
                             w_gate_e[l], w_up_e[l], w_down_e[l])
        x = x + gt2 * rms_norm(y, g_post_ffn[l])
    return x
```

```python
import numpy as np
import concourse.bass as bass
import concourse.mybir as mybir
from concourse.bass_utils import run_bass_kernel_spmd

F32 = mybir.dt.float32
F32R = mybir.dt.float32r
BF16 = mybir.dt.bfloat16
I32 = mybir.dt.int32
U32 = mybir.dt.uint32
AF = mybir.ActivationFunctionType
ALU = mybir.AluOpType
AX = mybir.AxisListType

NDMA_SLOTS = 6


class Prog:
    def __init__(self, nc):
        self.nc = nc
        self.ops = []
        self.last_w = {}
        self.readers = {}
        self.engs = {"pe": nc.tensor, "act": nc.scalar, "dve": nc.vector,
                     "pool": nc.gpsimd, "sp": nc.sync}

    def op(self, eng, fn, reads=(), writes=(), dma=False, force=False):
        deps = set()
        raw = set()
        for k in reads:
            if k in self.last_w:
                deps.add(self.last_w[k])
                raw.add(self.last_w[k])
        for k in writes:
            if k in self.last_w:
                deps.add(self.last_w[k])
                raw.add(self.last_w[k])
            for r in self.readers.get(k, ()):
                deps.add(r)
        idx = len(self.ops)
        self.ops.append(dict(eng=eng, fn=fn, deps=deps, raw=raw, dma=dma, force=force))
        for k in reads:
            self.readers.setdefault(k, []).append(idx)
        for k in writes:
            self.last_w[k] = idx
            self.readers[k] = []
        return idx

    def dma(self, q, out, in_, reads=(), writes=(), **kw):
        return self.op(q, lambda e: e.dma_start(out=out, in_=in_, **kw), reads, writes, dma=True)

    def emit(self, stack):
        nc = self.nc
        ops = self.ops
        need = [False] * len(ops)
        for i, o in enumerate(ops):
            nd = set()
            for d in o["deps"]:
                od = ops[d]
                if od["dma"] or o["dma"] or o["force"] or od["eng"] != o["eng"] or (d in o["raw"] and o["eng"] != "pe"):
                    nd.add(d)
            o["xdeps"] = nd
            for d in nd:
                need[d] = True
        for i, o in enumerate(ops):
            if o["dma"]:
                need[i] = True
        esem = {e: stack.enter_context(nc.semaphore("es_" + e)) for e in self.engs}
        dsem = {e: [stack.enter_context(nc.semaphore(f"ds_{e}_{k}")) for k in range(NDMA_SLOTS)]
                for e in ("sp", "act", "pool")}
        ecount = {e: 0 for e in self.engs}
        dcount = {e: 0 for e in dsem}
        signal = [None] * len(ops)
        waited = {}
        nwaits = 0
        for i, o in enumerate(ops):
            e = o["eng"]
            eng = self.engs[e]
            wl = {}
            for d in o["xdeps"]:
                s, v = signal[d]
                key = id(s)
                if waited.get((e, key), 0) >= v:
                    continue
                if key not in wl or wl[key][1] < v:
                    wl[key] = (s, v)
            if o["dma"]:
                j = dcount[e]
                slot = j % NDMA_SLOTS
                s = dsem[e][slot]
                prev = 16 * (j // NDMA_SLOTS)
                if prev > 0 and waited.get((e, id(s)), 0) < prev:
                    if id(s) not in wl or wl[id(s)][1] < prev:
                        wl[id(s)] = (s, prev)
            for key, (s, v) in wl.items():
                eng.wait_ge(s, v)
                waited[(e, key)] = v
                nwaits += 1
            inst = o["fn"](eng)
            if o["dma"]:
                j = dcount[e]
                dcount[e] += 1
                s = dsem[e][j % NDMA_SLOTS]
                v = 16 * (j // NDMA_SLOTS + 1)
                inst.then_inc(s, 16)
                signal[i] = (s, v)
            elif need[i]:
                ecount[e] += 1
                inst.then_inc(esem[e], 1)
                signal[i] = (esem[e], ecount[e])
        for e in dsem:
            n = dcount[e]
            for slot in range(min(n, NDMA_SLOTS)):
                uses = (n - slot + NDMA_SLOTS - 1) // NDMA_SLOTS
                self.engs[e].wait_ge(dsem[e][slot], 16 * uses)
        self.stats = dict(n_ops=len(ops), n_waits=nwaits, ecount=ecount, dcount=dcount)
        return self.stats

from contextlib import ExitStack

S = 8192
D = 2048
EPS = 1e-6


def _run(build, in_maps):
    nc = bass.Bass("TRN2", target_bir_lowering=False)
    with ExitStack() as st:
        p = Prog(nc)
        build(nc, p, st)
        p.emit(st)
    r = run_bass_kernel_spmd(nc, in_maps, core_ids=list(range(8)))
    return r.results


def _sb(nc, st, name, shape, dt=F32):
    return st.enter_context(nc.sbuf_tensor(name, shape, dt))


def _ps(nc, st, name, shape=(128, 512), dt=F32):
    return st.enter_context(nc.psum_tensor(name, list(shape), dt))


def build_ada(nc, p, st):
    w = nc.dram_tensor("w", [2048, 1536], F32, kind="ExternalInput").ap()
    c = nc.dram_tensor("c", [128, 16], F32, kind="ExternalInput").ap()
    b = nc.dram_tensor("b", [1, 1536], F32, kind="ExternalInput").ap()
    y = nc.dram_tensor("y", [1, 1536], F32, kind="ExternalOutput").ap()
    wt = [_sb(nc, st, f"wt{i}", [128, 1536]) for i in range(2)]
    ct = _sb(nc, st, "ct", [128, 16])
    bt = _sb(nc, st, "bt", [1, 1536])
    acc = _sb(nc, st, "acc", [128, 1536])
    ones = _sb(nc, st, "ones", [128, 1])
    res = _sb(nc, st, "res", [1, 1536])
    ps = [_ps(nc, st, f"ps{i}", (1, 512)) for i in range(2)]
    p.dma("sp", ct[:], c, writes=["ct"])
    p.dma("sp", bt[:], b, writes=["bt"])
    p.op("dve", lambda e: e.memset(ones[:], 1.0), writes=["ones"])
    for kc in range(16):
        i = kc % 2
        p.dma("sp", wt[i][:], w[kc * 128:(kc + 1) * 128, :], writes=[f"wt{i}"])
        if kc == 0:
            p.op("dve", lambda e, i=i, kc=kc: e.tensor_scalar(out=acc[:], in0=wt[i][:], scalar1=ct[:, kc:kc + 1], scalar2=None, op0=ALU.mult),
                 reads=[f"wt{i}", "ct"], writes=["acc"])
        else:
            p.op("dve", lambda e, i=i, kc=kc: e.scalar_tensor_tensor(out=acc[:], in0=wt[i][:], scalar=ct[:, kc:kc + 1], in1=acc[:], op0=ALU.mult, op1=ALU.add),
                 reads=[f"wt{i}", "ct", "acc"], writes=["acc"])
    for j in range(3):
        pj = ps[j % 2]
        p.op("pe", lambda e, j=j, pj=pj: e.matmul(pj[:], lhsT=ones[:], rhs=acc[:, j * 512:(j + 1) * 512], start=True, stop=True),
             reads=["acc", "ones"], writes=[f"ps{j%2}"])
        p.op("dve", lambda e, j=j, pj=pj: e.tensor_tensor(out=res[:, j * 512:(j + 1) * 512], in0=pj[:], in1=bt[:, j * 512:(j + 1) * 512], op=ALU.add),
             reads=[f"ps{j%2}", "bt"], writes=[f"res{j}"])
    p.dma("sp", y, res[:], reads=["res0", "res1", "res2"])


def launch_ada(c, w_ada, b_ada):
    in_maps = [{"w": np.ascontiguousarray(w_ada[:, i * 1536:(i + 1) * 1536]),
                "c": np.ascontiguousarray(c.reshape(16, 128).T),
                "b": np.ascontiguousarray(b_ada[None, i * 1536:(i + 1) * 1536])} for i in range(8)]
    res = _run(build_ada, in_maps)
    return np.concatenate([r["y"][0] for r in res])


def make_build_norm(add_one, has_b, has_base, ntile=8):
    def build(nc, p, st):
        n = ntile * 128
        y = nc.dram_tensor("y", [n, D], F32, kind="ExternalInput").ap()
        g = nc.dram_tensor("g", [1, D], F32, kind="ExternalInput").ap()
        s = nc.dram_tensor("s", [1, D], F32, kind="ExternalInput").ap()
        bv = nc.dram_tensor("bv", [1, D], F32, kind="ExternalInput").ap() if has_b else None
        base = nc.dram_tensor("base", [n, D], F32, kind="ExternalInput").ap() if has_base else None
        o = nc.dram_tensor("o", [n, D], F32, kind="ExternalOutput").ap()
        gb = _sb(nc, st, "gb", [128, D])
        A = _sb(nc, st, "A", [128, D])
        bb = _sb(nc, st, "bb", [128, D]) if has_b else None
        p.dma("sp", gb[:], g.partition_broadcast(128), writes=["gb"])
        p.dma("sp", A[:], s.partition_broadcast(128), writes=["A"])
        if has_b:
            p.dma("sp", bb[:], bv.partition_broadcast(128), writes=["bb"])
        p.op("dve", lambda e: e.scalar_tensor_tensor(out=A[:], in0=A[:], scalar=float(add_one), in1=gb[:], op0=ALU.add, op1=ALU.mult),
             reads=["gb", "A"], writes=["A"])
        yt = [_sb(nc, st, f"yt{i}", [128, D]) for i in range(2)]
        bt = [_sb(nc, st, f"bt{i}", [128, D]) for i in range(2)] if has_base else None
        junk = _sb(nc, st, "junk", [128, D])
        ot = [_sb(nc, st, f"ot{i}", [128, D]) for i in range(2)]
        ss = [_sb(nc, st, f"ss{i}", [128, 4]) for i in range(2)]
        for t in range(ntile):
            i = t % 2
            rows = slice(t * 128, (t + 1) * 128)
            p.dma("sp", yt[i][:], y[rows, :], writes=[f"yt{i}"])
            if has_base:
                p.dma("sp", bt[i][:], base[rows, :], writes=[f"bt{i}"])
            p.op("act", lambda e, i=i: e.activation(out=junk[:], in_=yt[i][:], func=AF.Square, accum_out=ss[i][:, 0:1]),
                 reads=[f"yt{i}"], writes=["junk", f"ss{i}"])
            p.op("dve", lambda e, i=i: e.tensor_scalar(out=ss[i][:, 1:2], in0=ss[i][:, 0:1], scalar1=1.0 / D, scalar2=EPS, op0=ALU.mult, op1=ALU.add),
                 reads=[f"ss{i}"], writes=[f"ss{i}"])
            p.op("act", lambda e, i=i: e.activation(out=ss[i][:, 2:3], in_=ss[i][:, 1:2], func=AF.Sqrt),
                 reads=[f"ss{i}"], writes=[f"ss{i}"])
            p.op("dve", lambda e, i=i: e.reciprocal(out=ss[i][:, 3:4], in_=ss[i][:, 2:3]),
                 reads=[f"ss{i}"], writes=[f"ss{i}"])
            p.op("dve", lambda e, i=i: e.scalar_tensor_tensor(out=ot[i][:], in0=yt[i][:], scalar=ss[i][:, 3:4], in1=A[:], op0=ALU.mult, op1=ALU.mult),
                 reads=[f"yt{i}", f"ss{i}", "A"], writes=[f"ot{i}"], force=True)
            if has_b:
                p.op("pool", lambda e, i=i: e.tensor_tensor(out=ot[i][:], in0=ot[i][:], in1=bb[:], op=ALU.add),
                     reads=[f"ot{i}", "bb"], writes=[f"ot{i}"])
            if has_base:
                p.op("pool", lambda e, i=i: e.tensor_tensor(out=ot[i][:], in0=ot[i][:], in1=bt[i][:], op=ALU.add),
                     reads=[f"ot{i}", f"bt{i}"], writes=[f"ot{i}"])
            p.dma("pool", o[rows, :], ot[i][:], reads=[f"ot{i}"])
    return build


def launch_norm(y, g, s, add_one, bv=None, base=None):
    n = y.shape[0] // 8
    in_maps = []
    for i in range(8):
        m = {"y": np.ascontiguousarray(y[i * n:(i + 1) * n]), "g": np.ascontiguousarray(g[None, :]),
             "s": np.ascontiguousarray(s[None, :])}
        if bv is not None:
            m["bv"] = np.ascontiguousarray(bv[None, :])
        if base is not None:
            m["base"] = np.ascontiguousarray(base[i * n:(i + 1) * n])
        in_maps.append(m)
    res = _run(make_build_norm(add_one, bv is not None, base is not None, n // 128), in_maps)
    return np.concatenate([r["o"] for r in res], axis=0)


LC_OUTS = ["qT", "kT", "vT", "rT", "krT", "vrT", "wdec", "nkk", "kka", "kt", "g", "bonus"]
WDECAY = 0.6065306597126334


def build_inproj(nc, p, st, ntt=16):
    T = ntt * 512
    hTp = nc.dram_tensor("hTp", [D, T + 1], F32, kind="ExternalInput").ap()
    ws_d = nc.dram_tensor("ws", [D, 448], F32, kind="ExternalInput").ap()
    wd_d = nc.dram_tensor("wd", [D, 384], F32, kind="ExternalInput").ap()
    mucol_d = nc.dram_tensor("mucol", [1, 384], F32, kind="ExternalInput").ap()
    wl_d = nc.dram_tensor("wl", [D, 448], F32, kind="ExternalInput").ap()
    murow_d = nc.dram_tensor("murow", [128, 16, 3], F32, kind="ExternalInput").ap()
    w2w_d = nc.dram_tensor("w2w", [96, 128], F32, kind="ExternalInput").ap()
    w2a_d = nc.dram_tensor("w2a", [96, 128], F32, kind="ExternalInput").ap()
    w2g_d = nc.dram_tensor("w2g", [128, 2, 128], F32, kind="ExternalInput").ap()
    vecs_d = nc.dram_tensor("vecs", [128, 5], F32, kind="ExternalInput").ap()
    cos_d = nc.dram_tensor("cos", [32, T], F32, kind="ExternalInput").ap()
    sin_d = nc.dram_tensor("sin", [32, T], F32, kind="ExternalInput").ap()
    blk_d = nc.dram_tensor("blk", [128, 128], F32, kind="ExternalInput").ap()
    outs = {n: nc.dram_tensor(n, [128, T], F32, kind="ExternalOutput").ap() for n in LC_OUTS}

    w0 = _sb(nc, st, "w0", [128, 16, 448])
    wa = _sb(nc, st, "wa", [128, 16, 448])
    wb = _sb(nc, st, "wb", [128, 16, 448])
    hb = [_sb(nc, st, f"hb{i}", [128, 16, 514]) for i in range(2)]
    mucol = _sb(nc, st, "mucol_sb", [128, 384])
    murow = _sb(nc, st, "murow_sb", [128, 16, 3])
    w2w = _sb(nc, st, "w2w_sb", [96, 128])
    w2a = _sb(nc, st, "w2a_sb", [96, 128])
    w2g = _sb(nc, st, "w2g_sb", [128, 2, 128])
    vecs = _sb(nc, st, "vecs_sb", [128, 5])
    blk = _sb(nc, st, "blk_sb", [128, 128])
    cs = [_sb(nc, st, f"cs{i}", [32, 2, 512]) for i in range(2)]
    ps = [_ps(nc, st, f"ps{i}") for i in range(8)]
    NOB = 6
    ob = [_sb(nc, st, f"ob{i}", [128, 512]) for i in range(NOB)]
    obi = [0]
    psi = [0]

    def nps():
        i = psi[0] % 8
        psi[0] += 1
        return ps[i], f"ps{i}"

    def nob():
        i = obi[0] % NOB
        obi[0] += 1
        return ob[i], f"ob{i}"

    hview = hTp.rearrange("(c p) t -> p c t", p=128)
    r32 = lambda ap: ap.bitcast(F32R)

    for dst, src, k in [(mucol[:], mucol_d.partition_broadcast(128), "mucol"), (murow[:], murow_d, "murow"),
                        (w2w[:], w2w_d, "w2w"), (w2a[:], w2a_d, "w2a"), (w2g[:], w2g_d, "w2g"),
                        (vecs[:], vecs_d, "vecs"), (blk[:], blk_d, "blk")]:
        p.dma("sp", dst, src, writes=[k])

    def load_h(tt):
        i = tt % 2
        p.dma("pool", r32(hb[i][:, :, 0:513]), r32(hview[:, :, tt * 512:tt * 512 + 513]), writes=[f"hb{i}"])

    def gemm(tt, kind, co, M, pst, psk):
        i = tt % 2
        for c in range(16):
            cur = hb[i][:, c, 1:513]
            prev = hb[i][:, c, 0:512]
            if kind == "single":
                p.op("pe", lambda e, c=c, cur=cur: e.matmul(pst[:M, :], lhsT=r32(w0[:, c, co:co + M]), rhs=r32(cur), start=(c == 0), stop=(c == 15)),
                     reads=["w0", f"hb{i}"], writes=[psk])
            else:
                p.op("pe", lambda e, c=c, cur=cur: e.matmul(pst[:M, :], lhsT=r32(wa[:, c, co:co + M]), rhs=r32(cur), start=(c == 0), stop=False),
                     reads=["wa", f"hb{i}"], writes=[psk])
                p.op("pe", lambda e, c=c, prev=prev: e.matmul(pst[:M, :], lhsT=r32(wb[:, c, co:co + M]), rhs=r32(prev), start=False, stop=(c == 15)),
                     reads=["wb", f"hb{i}"], writes=[psk])

    p.dma("pool", r32(w0[:]), r32(ws_d.rearrange("(c p) n -> p c n", p=128)), writes=["w0"])
    p.dma("pool", r32(wa[:, :, 0:384]), r32(wd_d.rearrange("(c p) n -> p c n", p=128)), writes=["wa"])
    for c in range(16):
        p.op("dve", lambda e, c=c: e.tensor_tensor(out=r32(wb[:, c, 0:384]), in0=wa[:, c, 0:384], in1=mucol[:], op=ALU.mult),
             reads=["wa", "mucol"], writes=["wb"])
    p.op("dve", lambda e: e.tensor_tensor(out=r32(wa[:, :, 0:384]), in0=wa[:, :, 0:384], in1=wb[:, :, 0:384], op=ALU.subtract),
         reads=["wa", "wb"], writes=["wa"])
    load_h(0)
    for tt in range(ntt):
        if tt + 1 < ntt:
            load_h(tt + 1)
        tsl = slice(tt * 512, (tt + 1) * 512)
        ci = tt % 2
        p.dma("sp", cs[ci][:, 0, :], cos_d[:, tsl], writes=[f"cs{ci}"])
        p.dma("sp", cs[ci][:, 1, :], sin_d[:, tsl], writes=[f"cs{ci}"])
        for name, co in (("qT", 0), ("kT", 160)):
            pq, pqk = nps()
            gemm(tt, "single", co, 128, pq, pqk)
            psw, pswk = nps()
            gemm(tt, "single", co + 128, 32, psw, pswk)
            o, ok = nob()
            p.op("act", lambda e, o=o, pq=pq: e.copy(out=o[:], in_=pq[:]), reads=[pqk], writes=[ok, ok + "hi"])
            t1, t1k = nob()
            p.op("dve", lambda e, t1=t1, psw=psw, ci=ci: e.tensor_tensor(out=t1[0:32, :], in0=psw[0:32, :], in1=cs[ci][:, 1, :], op=ALU.mult),
                 reads=[pswk, f"cs{ci}"], writes=[t1k])
            p.op("dve", lambda e, o=o, pq=pq, ci=ci: e.tensor_tensor(out=o[0:32, :], in0=pq[0:32, :], in1=cs[ci][:, 0, :], op=ALU.mult),
                 reads=[pqk, f"cs{ci}"], writes=[ok])
            p.op("dve", lambda e, o=o, t1=t1: e.tensor_tensor(out=o[0:32, :], in0=o[0:32, :], in1=t1[0:32, :], op=ALU.add),
                 reads=[ok, t1k], writes=[ok])
            p.dma("pool", outs[name][:, tsl], o[:], reads=[ok, ok + "hi"], writes=[f"d_{name}_{tt}"])
        pv, pvk = nps()
        gemm(tt, "single", 320, 128, pv, pvk)
        o, ok = nob()
        p.op("act", lambda e, o=o, pv=pv: e.copy(out=o[:], in_=pv[:]), reads=[pvk], writes=[ok, ok + "hi"])
        p.dma("pool", outs["vT"][:, tsl], o[:], reads=[ok, ok + "hi"], writes=[f"d_vT_{tt}"])
        for j, name in enumerate(("rT", "krT", "vrT")):
            pr, prk = nps()
            gemm(tt, "dual", j * 128, 128, pr, prk)
            o, ok = nob()
            p.op("act", lambda e, o=o, pr=pr: e.copy(out=o[:], in_=pr[:]), reads=[prk], writes=[ok, ok + "hi"])
            p.dma("pool", outs[name][:, tsl], o[:], reads=[ok, ok + "hi"], writes=[f"d_{name}_{tt}"])

    p.dma("pool", r32(w0[:]), r32(wl_d.rearrange("(c p) n -> p c n", p=128)), writes=["w0"])
    for c in range(16):
        for j, (lo, hi) in enumerate(((0, 96), (96, 192), (192, 448))):
            p.op("dve", lambda e, c=c, j=j, lo=lo, hi=hi: e.tensor_scalar(out=r32(wb[:, c, lo:hi]), in0=w0[:, c, lo:hi], scalar1=murow[:, c, j:j + 1], scalar2=None, op0=ALU.mult),
                 reads=["w0", "murow"], writes=["wb"])
    p.op("dve", lambda e: e.tensor_tensor(out=r32(wa[:]), in0=w0[:], in1=wb[:], op=ALU.subtract),
         reads=["w0", "wb"], writes=["wa"])
    tw = _sb(nc, st, "tw", [96, 512])
    ta = _sb(nc, st, "ta", [96, 512])
    tg = _sb(nc, st, "tg", [128, 2, 512])
    rin = [_sb(nc, st, f"rin{i}", [128, 3, 512]) for i in range(1)]
    tmp = {n: _sb(nc, st, "tmp_" + n, [128, 512]) for n in ["a", "kkr", "sq", "nrm", "rn", "u", "rk"]}
    load_h(0)
    for tt in range(ntt):
        if tt + 1 < ntt:
            load_h(tt + 1)
        tsl = slice(tt * 512, (tt + 1) * 512)
        ri = 0
        for j, name in enumerate(("rT", "krT", "vrT")):
            p.dma("sp", rin[ri][:, j, :], outs[name][:, tsl], reads=[f"d_{name}_{tt}"], writes=[f"rin{ri}_{j}"])
        R_, KR, VR = rin[ri][:, 0, :], rin[ri][:, 1, :], rin[ri][:, 2, :]
        rk_, krk, vrk = f"rin{ri}_0", f"rin{ri}_1", f"rin{ri}_2"
        pw, pwk = nps()
        gemm(tt, "dual", 0, 96, pw, pwk)
        p.op("act", lambda e, pw=pw: e.activation(out=tw[:], in_=pw[:96, :], func=AF.Tanh), reads=[pwk], writes=["tw"])
        pa, pak = nps()
        gemm(tt, "dual", 96, 96, pa, pak)
        p.op("act", lambda e, pa=pa: e.copy(out=ta[:], in_=pa[:96, :]), reads=[pak], writes=["ta"])
        for h in range(2):
            pg, pgk = nps()
            gemm(tt, "dual", 192 + h * 128, 128, pg, pgk)
            p.op("act", lambda e, pg=pg, h=h: e.activation(out=tg[:, h, :], in_=pg[:], func=AF.Sigmoid), reads=[pgk], writes=[f"tg{h}"])
        pd, pdk = nps()
        p.op("pe", lambda e, pd=pd: e.matmul(pd[:], lhsT=w2w[:], rhs=tw[:], start=True, stop=True), reads=["w2w", "tw"], writes=[pdk])
        o_w, o_wk = nob()
        p.op("act", lambda e, pd=pd: e.activation(out=tmp["sq"][:], in_=pd[:], func=AF.Sigmoid, bias=vecs[:, 0:1]), reads=[pdk, "vecs"], writes=["t_sq"])
        p.op("act", lambda e, o_w=o_w: e.activation(out=o_w[:], in_=tmp["sq"][:], func=AF.Exp, scale=-WDECAY), reads=["t_sq"], writes=[o_wk])
        p.dma("pool", outs["wdec"][:, tsl], o_w[:], reads=[o_wk])
        pa2, pa2k = nps()
        p.op("pe", lambda e, pa2=pa2: e.matmul(pa2[:], lhsT=w2a[:], rhs=ta[:], start=True, stop=True), reads=["w2a", "ta"], writes=[pa2k])
        p.op("act", lambda e, pa2=pa2: e.activation(out=tmp["a"][:], in_=pa2[:], func=AF.Sigmoid, bias=vecs[:, 1:2]), reads=[pa2k, "vecs"], writes=["t_a"])
        pg2, pg2k = nps()
        for h in range(2):
            p.op("pe", lambda e, pg2=pg2, h=h: e.matmul(pg2[:], lhsT=w2g[:, h, :], rhs=tg[:, h, :], start=(h == 0), stop=(h == 1)),
                 reads=["w2g", f"tg{h}"], writes=[pg2k])
        o_g, o_gk = nob()
        p.op("act", lambda e, o_g=o_g, pg2=pg2: e.copy(out=o_g[:], in_=pg2[:]), reads=[pg2k], writes=[o_gk])
        p.dma("pool", outs["g"][:, tsl], o_g[:], reads=[o_gk])
        p.op("dve", lambda e, KR=KR: e.tensor_scalar(out=tmp["kkr"][:], in0=KR, scalar1=vecs[:, 2:3], scalar2=None, op0=ALU.mult),
             reads=[krk, "vecs"], writes=["t_kkr"])
        p.op("pool", lambda e: e.tensor_tensor(out=tmp["sq"][:], in0=tmp["kkr"][:], in1=tmp["kkr"][:], op=ALU.mult),
             reads=["t_kkr", "t_sq"], writes=["t_sq"])
        pn, pnk = nps()
        p.op("pe", lambda e, pn=pn: e.matmul(pn[:], lhsT=blk[:], rhs=tmp["sq"][:], start=True, stop=True), reads=["blk", "t_sq"], writes=[pnk])
        p.op("act", lambda e, pn=pn: e.activation(out=tmp["nrm"][:], in_=pn[:], func=AF.Sqrt), reads=[pnk], writes=["t_nrm"])
        p.op("dve", lambda e: e.tensor_scalar(out=tmp["nrm"][:], in0=tmp["nrm"][:], scalar1=1e-12, scalar2=None, op0=ALU.max),
             reads=["t_nrm"], writes=["t_nrm"])
        p.op("dve", lambda e: e.reciprocal(out=tmp["rn"][:], in_=tmp["nrm"][:]), reads=["t_nrm"], writes=["t_rn"])
        o_n, o_nk = nob()
        p.op("dve", lambda e, o_n=o_n: e.scalar_tensor_tensor(out=o_n[:], in0=tmp["kkr"][:], scalar=-1.0, in1=tmp["rn"][:], op0=ALU.mult, op1=ALU.mult),
             reads=["t_kkr", "t_rn"], writes=[o_nk])
        p.dma("pool", outs["nkk"][:, tsl], o_n[:], reads=[o_nk])
        o_ka, o_kak = nob()
        p.op("dve", lambda e, o_n=o_n, o_ka=o_ka: e.scalar_tensor_tensor(out=o_ka[:], in0=o_n[:], scalar=-1.0, in1=tmp["a"][:], op0=ALU.mult, op1=ALU.mult),
             reads=[o_nk, "t_a"], writes=[o_kak])
        p.dma("pool", outs["kka"][:, tsl], o_ka[:], reads=[o_kak])
        p.op("dve", lambda e: e.tensor_scalar(out=tmp["u"][:], in0=tmp["a"][:], scalar1=-1.0, scalar2=vecs[:, 3:4], op0=ALU.add, op1=ALU.mult),
             reads=["t_a", "vecs"], writes=["t_u"])
        o_kt, o_ktk = nob()
        p.op("dve", lambda e, o_kt=o_kt, KR=KR: e.scalar_tensor_tensor(out=o_kt[:], in0=tmp["u"][:], scalar=1.0, in1=KR, op0=ALU.add, op1=ALU.mult),
             reads=["t_u", krk], writes=[o_ktk])
        p.dma("pool", outs["kt"][:, tsl], o_kt[:], reads=[o_ktk])
        p.op("dve", lambda e, o_kt=o_kt, R_=R_: e.scalar_tensor_tensor(out=tmp["rk"][:], in0=R_, scalar=vecs[:, 4:5], in1=o_kt[:], op0=ALU.mult, op1=ALU.mult),
             reads=[rk_, "vecs", o_ktk], writes=["t_rk"])
        pb, pbk = nps()
        p.op("pe", lambda e, pb=pb: e.matmul(pb[:], lhsT=blk[:], rhs=tmp["rk"][:], start=True, stop=True), reads=["blk", "t_rk"], writes=[pbk])
        o_b, o_bk = nob()
        p.op("dve", lambda e, o_b=o_b, pb=pb, VR=VR: e.tensor_tensor(out=o_b[:], in0=pb[:], in1=VR, op=ALU.mult),
             reads=[pbk, vrk], writes=[o_bk])
        p.dma("pool", outs["bonus"][:, tsl], o_b[:], reads=[o_bk])


def _rope_tables(T):
    half = 16
    inv = (500000.0 ** (-np.arange(half, dtype=np.float32) / half)).astype(np.float32)
    ang = np.arange(T, dtype=np.float32)[:, None] * inv[None, :]
    cos = np.cos(ang).astype(np.float32).T
    sin = np.sin(ang).astype(np.float32).T
    COS = np.concatenate([cos, cos], 0)
    SIN = np.concatenate([-sin, sin], 0)
    return np.ascontiguousarray(COS), np.ascontiguousarray(SIN)


def launch_inproj(h, I):
    T = h.shape[0]
    hTp = np.zeros((D, T + 1), np.float32)
    hTp[:, 1:] = h.T
    w_in = I["w_in"][0]
    COS, SIN = _rope_tables(T)
    swp = np.concatenate([np.arange(16, 32), np.arange(0, 16)])
    blk = np.zeros((128, 128), np.float32)
    blk[:64, :64] = 1
    blk[64:, 64:] = 1
    murow = np.stack([I["mu_w"][0], I["mu_a"][0], I["mu_g"][0]], -1).reshape(16, 128, 3).transpose(1, 0, 2)
    wl = np.concatenate([I["w_w1"][0], I["w_a1"][0], I["w_g1"][0]], 1)
    in_maps = []
    for i in range(8):
        cq = slice(i * 128, (i + 1) * 128)
        q = w_in[:, 0:1024][:, cq]
        k = w_in[:, 1024:2048][:, cq]
        v = w_in[:, 2048:3072][:, cq]
        ws = np.concatenate([q, q[:, swp], k, k[:, swp], v], 1)
        r = w_in[:, 3072:4096][:, cq]
        kr = w_in[:, 4096:5120][:, cq]
        vr = w_in[:, 5120:6144][:, cq]
        wd = np.concatenate([r, kr, vr], 1)
        mucol = np.concatenate([I["mu_r"][0][cq], I["mu_k"][0][cq], I["mu_v"][0][cq]])[None, :]
        vecs = np.stack([I["w0"][0][cq], I["a0"][0][cq], I["k_k"][0][cq], I["k_a"][0][cq], I["r_k"][0].reshape(-1)[cq]], -1)
        in_maps.append({
            "hTp": hTp, "ws": np.ascontiguousarray(ws), "wd": np.ascontiguousarray(wd), "mucol": np.ascontiguousarray(mucol),
            "wl": np.ascontiguousarray(wl), "murow": np.ascontiguousarray(murow),
            "w2w": np.ascontiguousarray(I["w_w2"][0][:, cq]), "w2a": np.ascontiguousarray(I["w_a2"][0][:, cq]),
            "w2g": np.ascontiguousarray(I["w_g2"][0][:, cq].reshape(2, 128, 128).transpose(1, 0, 2)),
            "vecs": np.ascontiguousarray(vecs), "cos": COS, "sin": SIN, "blk": blk})
    ntt = T // 512
    res = _run(lambda nc, p, st: build_inproj(nc, p, st, ntt), in_maps)
    return res


GN_EPS = 64e-5
TCH = 32


def build_rwkv(nc, p, st, T=S):
    nch = T // TCH
    bcin_d = nc.dram_tensor("bcin", [2, nch, 5, TCH, 64], F32, kind="ExternalInput").ap()
    vT_d = nc.dram_tensor("vT", [128, T], F32, kind="ExternalInput").ap()
    g_d = nc.dram_tensor("g", [128, T], F32, kind="ExternalInput").ap()
    bonus_d = nc.dram_tensor("bonus", [128, T], F32, kind="ExternalInput").ap()
    gnv_d = nc.dram_tensor("gnv", [128, 2], F32, kind="ExternalInput").ap()
    sel_d = nc.dram_tensor("sel", [128, 128], F32, kind="ExternalInput").ap()
    blk_d = nc.dram_tensor("blk", [128, 128], F32, kind="ExternalInput").ap()
    o_d = nc.dram_tensor("o", [128, T], F32, kind="ExternalOutput").ap()
    r32 = lambda ap: ap.bitcast(F32R)

    vT = _sb(nc, st, "vT_sb", [128, T])
    yT = _sb(nc, st, "yT_sb", [128, T])
    Sst = _sb(nc, st, "S_sb", [128, 64])
    junk = _sb(nc, st, "junk", [128, 64])
    sa = _sb(nc, st, "sa", [128, 1])
    sel = _sb(nc, st, "sel_sb", [128, 128])
    blk = _sb(nc, st, "blk_sb", [128, 128])
    gnv = _sb(nc, st, "gnv_sb", [128, 2])
    bc = [_sb(nc, st, f"bc{i}", [128, 5, TCH, 64]) for i in range(2)]
    ps = [_ps(nc, st, f"ps{i}") for i in range(8)]
    p.dma("pool", r32(sel[:]), r32(sel_d), writes=["sel"])
    p.dma("sp", blk[:], blk_d, writes=["blk"])
    p.dma("sp", gnv[:], gnv_d, writes=["gnv"])
    p.dma("sp", vT[:], vT_d, writes=["vT"])
    p.op("dve", lambda e: e.memset(Sst[:], 0.0), writes=["S"])
    zer_d = nc.dram_tensor("zer", [126, 5, TCH, 64], F32, kind="ExternalInput").ap()
    for i in range(2):
        p.dma("pool", r32(bc[i][2:128]), r32(zer_d), writes=[f"bc{i}"])

    def load_bc(c):
        i = c % 2
        p.dma("pool", r32(bc[i][0:2]), r32(bcin_d[:, c]), writes=[f"bc{i}"])

    load_bc(0)
    grp = 0
    for c in range(nch):
        if c + 1 < nch:
            load_bc(c + 1)
        bi = c % 2
        for g4 in range(TCH // 4):
            base = (grp % 2) * 3
            grp += 1
            views = []
            for j in range(5):
                bank = ps[base + j // 2]
                bk = f"ps{base + j // 2}"
                half = bank[:, (j % 2) * 256:(j % 2) * 256 + 256]
                p.op("pe", lambda e, half=half, j=j, g4=g4, bi=bi: e.matmul(half, lhsT=r32(sel[:]), rhs=r32(bc[bi][:, j, g4 * 4:(g4 + 1) * 4, :]), start=True, stop=True),
                     reads=["sel", f"bc{bi}"], writes=[bk + f"h{j%2}"])
                views.append((half, bk + f"h{j%2}"))
            for tl in range(4):
                t = c * TCH + g4 * 4 + tl
                cs = slice(tl * 64, (tl + 1) * 64)
                wv, nv, kav, ktv, rv = [(v[0][:, cs], v[1]) for v in views]
                p.op("dve", lambda e, nv=nv: e.scalar_tensor_tensor(out=junk[:], in0=Sst[:], scalar=1.0, in1=nv[0], op0=ALU.mult, op1=ALU.mult, accum_out=sa[:]),
                     reads=["S", nv[1]], writes=["junk", "sa"])
                p.op("dve", lambda e, wv=wv: e.tensor_tensor(out=Sst[:], in0=Sst[:], in1=wv[0], op=ALU.mult),
                     reads=["S", wv[1]], writes=["S"])
                p.op("dve", lambda e, kav=kav: e.scalar_tensor_tensor(out=Sst[:], in0=kav[0], scalar=sa[:, 0:1], in1=Sst[:], op0=ALU.mult, op1=ALU.add),
                     reads=["S", "sa", kav[1]], writes=["S"], force=True)
                p.op("dve", lambda e, ktv=ktv, t=t: e.scalar_tensor_tensor(out=Sst[:], in0=ktv[0], scalar=vT[:, t:t + 1], in1=Sst[:], op0=ALU.mult, op1=ALU.add),
                     reads=["S", "vT", ktv[1]], writes=["S"])
                p.op("dve", lambda e, rv=rv, t=t: e.scalar_tensor_tensor(out=junk[:], in0=Sst[:], scalar=1.0, in1=rv[0], op0=ALU.mult, op1=ALU.mult, accum_out=yT[:, t:t + 1]),
                     reads=["S", rv[1]], writes=["junk", f"yT{t // 512}"])
    gt = [_sb(nc, st, f"g_sb{i}", [128, 512]) for i in range(2)]
    bt = [_sb(nc, st, f"b_sb{i}", [128, 512]) for i in range(2)]
    yc = _sb(nc, st, "yc", [128, 512])
    sq = _sb(nc, st, "sq", [128, 512])
    rs = _sb(nc, st, "rs", [128, 512])
    ot = [_sb(nc, st, f"ot{i}", [128, 512]) for i in range(2)]
    for tt in range(T // 512):
        i = tt % 2
        tsl = slice(tt * 512, (tt + 1) * 512)
        p.dma("sp", gt[i][:], g_d[:, tsl], writes=[f"gt{i}"])
        p.dma("sp", bt[i][:], bonus_d[:, tsl], writes=[f"bt{i}"])
        pm, pmk = ps[6], "ps6"
        p.op("pe", lambda e, tsl=tsl: e.matmul(ps[6][:], lhsT=blk[:], rhs=yT[:, tsl], start=True, stop=True), reads=["blk", f"yT{tt}"], writes=["ps6"])
        p.op("dve", lambda e, tsl=tsl: e.scalar_tensor_tensor(out=yc[:], in0=ps[6][:], scalar=-1.0 / 64, in1=yT[:, tsl], op0=ALU.mult, op1=ALU.add),
             reads=["ps6", f"yT{tt}"], writes=["yc"])
        p.op("act", lambda e: e.activation(out=sq[:], in_=yc[:], func=AF.Square), reads=["yc"], writes=["sq"])
        p.op("pe", lambda e: e.matmul(ps[7][:], lhsT=blk[:], rhs=sq[:], start=True, stop=True), reads=["blk", "sq"], writes=["ps7"])
        p.op("dve", lambda e: e.tensor_scalar(out=rs[:], in0=ps[7][:], scalar1=1.0 / 64, scalar2=GN_EPS, op0=ALU.mult, op1=ALU.add), reads=["ps7"], writes=["rs"])
        p.op("act", lambda e: e.activation(out=rs[:], in_=rs[:], func=AF.Sqrt), reads=["rs"], writes=["rs"])
        p.op("dve", lambda e: e.reciprocal(out=rs[:], in_=rs[:]), reads=["rs"], writes=["rs"])
        p.op("dve", lambda e: e.tensor_tensor(out=yc[:], in0=yc[:], in1=rs[:], op=ALU.mult), reads=["yc", "rs"], writes=["yc"])
        p.op("dve", lambda e: e.tensor_scalar(out=yc[:], in0=yc[:], scalar1=gnv[:, 0:1], scalar2=gnv[:, 1:2], op0=ALU.mult, op1=ALU.add), reads=["yc", "gnv"], writes=["yc"])
        p.op("dve", lambda e, i=i: e.tensor_tensor(out=yc[:], in0=yc[:], in1=bt[i][:], op=ALU.add), reads=["yc", f"bt{i}"], writes=["yc"])
        p.op("dve", lambda e, i=i: e.tensor_tensor(out=ot[i][:], in0=yc[:], in1=gt[i][:], op=ALU.mult), reads=["yc", f"gt{i}"], writes=[f"ot{i}"])
        p.dma("sp", o_d[:, tsl], ot[i][:], reads=[f"ot{i}"])


def launch_rwkv(lc, I, T=S):
    sel = np.zeros((128, 128), np.float32)
    sel[0, :64] = 1
    sel[1, 64:] = 1
    blk = np.zeros((128, 128), np.float32)
    blk[:64, :64] = 1
    blk[64:, 64:] = 1
    in_maps = []
    nch = T // TCH
    for i in range(8):
        cq = slice(i * 128, (i + 1) * 128)
        q5 = np.stack([lc[i][n][:, :T] for n in ("wdec", "nkk", "kka", "kt", "rT")], 0)
        q5 = q5.reshape(5, 2, 64, nch, TCH).transpose(1, 3, 0, 4, 2)
        gnv = np.stack([I["gn_w"][0][cq], I["gn_b"][0][cq]], -1)
        in_maps.append({"bcin": np.ascontiguousarray(q5), "vT": np.ascontiguousarray(lc[i]["vrT"][:, :T]),
                        "g": np.ascontiguousarray(lc[i]["g"][:, :T]), "bonus": np.ascontiguousarray(lc[i]["bonus"][:, :T]),
                        "gnv": np.ascontiguousarray(gnv), "sel": sel, "blk": blk, "zer": np.zeros((126, 5, TCH, 64), np.float32)})
    res = _run(lambda nc, p, st: build_rwkv(nc, p, st, T), in_maps)
    return np.concatenate([r["o"].T for r in res], axis=1)


NEGB = 30000.0


def build_moba(nc, p, st, T=S):
    nb = T // 256
    nkt = T // 128
    qT_d = nc.dram_tensor("qT", [128, T], F32, kind="ExternalInput").ap()
    kT_d = nc.dram_tensor("kT", [128, T], F32, kind="ExternalInput").ap()
    va_d = nc.dram_tensor("va", [128, nkt, 129], F32, kind="ExternalInput").ap()
    E_d = nc.dram_tensor("E", [32, T], F32, kind="ExternalInput").ap()
    cm_d = nc.dram_tensor("cm", [128, 128], F32, kind="ExternalInput").ap()
    id_d = nc.dram_tensor("ident", [128, 128], F32, kind="ExternalInput").ap()
    o_d = nc.dram_tensor("o", [T, 128], F32, kind="ExternalOutput").ap()
    r32 = lambda ap: ap.bitcast(F32R)
    qT = _sb(nc, st, "qT_sb", [128, T])
    kT = _sb(nc, st, "kT_sb", [128, T])
    va = _sb(nc, st, "va_sb", [128, nkt, 129])
    E = _sb(nc, st, "E_sb", [32, T])
    cm = _sb(nc, st, "cm_sb", [128, 128])
    ident = _sb(nc, st, "id_sb", [128, 128])
    kmean = _sb(nc, st, "kmean", [128, 32])
    gsb = _sb(nc, st, "gsb", [128, 32])
    m8 = _sb(nc, st, "m8", [128, 8])
    bias = _sb(nc, st, "bias", [128, 32])
    biasT = [_sb(nc, st, f"biasT{i}", [32, 256]) for i in range(2)]
    pT = [_sb(nc, st, f"pT{i}", [128, 256]) for i in range(3)]
    osb = [_sb(nc, st, f"osb{i}", [128, 128]) for i in range(2)]
    rden = _sb(nc, st, "rden", [128, 2])
    s_ps = [_ps(nc, st, f"s_ps{i}") for i in range(2)]
    o_ps = [_ps(nc, st, f"o_ps{i}") for i in range(4)]
    g_ps = _ps(nc, st, "g_ps")
    t_ps = _ps(nc, st, "t_ps")
    p.dma("pool", r32(qT[:]), r32(qT_d), writes=["qT"])
    p.dma("pool", r32(kT[:]), r32(kT_d), writes=["kT"])
    p.dma("sp", va[:], va_d, writes=["va"])
    p.dma("sp", E[:], E_d, writes=["E"])
    p.dma("sp", cm[:], cm_d, writes=["cm"])
    p.dma("sp", ident[:], id_d, writes=["ident"])
    p.op("dve", lambda e: e.tensor_reduce(out=kmean[:, 0:nb], in_=kT[:].rearrange("p (n k) -> p n k", k=256), axis=AX.X, op=ALU.add),
         reads=["kT"], writes=["kmean"])
    p.op("dve", lambda e: e.tensor_scalar(out=kmean[:, 0:nb], in0=kmean[:, 0:nb], scalar1=1.0 / 256, scalar2=None, op0=ALU.mult),
         reads=["kmean"], writes=["kmean"])
    p.op("dve", lambda e: e.memset(gsb[:], -1e30), writes=["gsb"])
    scale = 128 ** -0.5
    pti = 0
    si = 0
    for b in range(nb):
        bT = biasT[b % 2]
        bTk = f"biasT{b % 2}"
        for j in range(2):
            qs = slice(b * 256 + j * 128, b * 256 + (j + 1) * 128)
            if b > 3:
                p.op("pe", lambda e, qs=qs: e.matmul(g_ps[:, 0:nb], lhsT=qT[:, qs], rhs=kmean[:, 0:nb], start=True, stop=True),
                     reads=["qT", "kmean"], writes=["g_ps"])
                p.op("dve", lambda e, b=b: e.tensor_copy(out=gsb[:, 0:b], in_=g_ps[:, 0:b]), reads=["g_ps", "gsb"], writes=["gsb"])
                p.op("dve", lambda e: e.max(out=m8[:], in_=gsb[:]), reads=["gsb"], writes=["m8"])
                p.op("dve", lambda e: e.tensor_scalar(out=bias[:], in0=gsb[:], scalar1=m8[:, 2:3], scalar2=None, op0=ALU.is_ge),
                     reads=["gsb", "m8"], writes=["bias"], force=True)
                p.op("dve", lambda e: e.tensor_scalar(out=bias[:], in0=bias[:], scalar1=NEGB, scalar2=-NEGB, op0=ALU.mult, op1=ALU.add),
                     reads=["bias"], writes=["bias"])
            else:
                p.op("dve", lambda e: e.memset(bias[:], -NEGB), reads=["bias"], writes=["bias"])
                if b > 0:
                    p.op("dve", lambda e, b=b: e.memset(bias[:, 0:b], 0.0), reads=["bias"], writes=["bias"])
            p.op("dve", lambda e, b=b: e.memset(bias[:, b:b + 1], 0.0), reads=["bias"], writes=["bias"])
            p.op("pe", lambda e: e.transpose(t_ps[0:32, 0:128], bias[:], ident[:]), reads=["bias", "ident"], writes=["t_ps"])
            p.op("act", lambda e, bT=bT, j=j: e.copy(out=bT[:, j * 128:(j + 1) * 128], in_=t_ps[0:32, 0:128]), reads=["t_ps"], writes=[bTk])
        ob = (b % 2) * 2
        for kt in range(2 * b + 2):
            sp_ = s_ps[si % 2]
            spk = f"s_ps{si % 2}"
            si += 1
            ks = slice(kt * 128, (kt + 1) * 128)
            own = kt >= 2 * b
            p.op("pe", lambda e, sp_=sp_, ks=ks, b=b: e.matmul(sp_[:, 0:256], lhsT=r32(kT[:, ks]), rhs=r32(qT[:, b * 256:(b + 1) * 256]), start=True, stop=False),
                 reads=["kT", "qT"], writes=[spk])
            p.op("pe", lambda e, sp_=sp_, ks=ks, bT=bT, own=own: e.matmul(sp_[:, 0:256], lhsT=E[:, ks], rhs=bT[:], start=False, stop=(not own)),
                 reads=["E", bTk], writes=[spk])
            if own:
                half = 0 if kt == 2 * b else 1
                p.op("pe", lambda e, sp_=sp_, half=half: e.matmul(sp_[:, half * 128:(half + 1) * 128], lhsT=ident[:], rhs=cm[:], start=False, stop=True),
                     reads=["ident", "cm"], writes=[spk])
            pt = pT[pti % 3]
            ptk = f"pT{pti % 3}"
            pti += 1
            p.op("act", lambda e, pt=pt, sp_=sp_: e.activation(out=pt[:], in_=sp_[:, 0:256], func=AF.Exp, scale=scale), reads=[spk], writes=[ptk])
            for j in range(2):
                if kt == 2 * b + 1 and j == 0:
                    continue
                last = (kt == 2 * b) if j == 0 else (kt == 2 * b + 1)
                p.op("pe", lambda e, pt=pt, j=j, kt=kt, last=last, ob=ob: e.matmul(o_ps[ob + j][:, 0:129], lhsT=pt[:, j * 128:(j + 1) * 128], rhs=va[:, kt, :], start=(kt == 0), stop=last),
                     reads=[ptk, "va"], writes=[f"o_ps{ob + j}"])
        for j in range(2):
            op_ = o_ps[ob + j]
            p.op("dve", lambda e, op_=op_, j=j: e.reciprocal(out=rden[:, j:j + 1], in_=op_[:, 128:129]), reads=[f"o_ps{ob + j}"], writes=[f"rden{j}"])
            p.op("act", lambda e, op_=op_, j=j: e.activation(out=osb[j][:], in_=op_[:, 0:128], func=AF.Copy, scale=rden[:, j:j + 1]),
                 reads=[f"o_ps{ob + j}", f"rden{j}"], writes=[f"osb{j}"])
            p.dma("sp", o_d[b * 256 + j * 128:b * 256 + (j + 1) * 128, :], osb[j][:], reads=[f"osb{j}"])


def launch_moba(lc, T=S):
    nkt = T // 128
    E = np.zeros((32, T), np.float32)
    for n in range(T // 256):
        E[n, n * 256:(n + 1) * 256] = 1
    kk = np.arange(128)
    cm = np.where(kk[:, None] <= kk[None, :], 0.0, -NEGB).astype(np.float32)
    ident = np.eye(128, dtype=np.float32)
    in_maps = []
    for i in range(8):
        v = lc[i]["vT"][:, :T].T
        va = np.concatenate([v, np.ones((T, 1), np.float32)], 1).reshape(nkt, 128, 129).transpose(1, 0, 2)
        in_maps.append({"qT": np.ascontiguousarray(lc[i]["qT"][:, :T]), "kT": np.ascontiguousarray(lc[i]["kT"][:, :T]),
                        "va": np.ascontiguousarray(va), "E": E, "cm": cm, "ident": ident})
    res = _run(lambda nc, p, st: build_moba(nc, p, st, T), in_maps)
    return np.concatenate([r["o"] for r in res], axis=1)


def build_merge(nc, p, st, nunit=2):
    TT = 512
    NT = nunit * TT
    hT_d = nc.dram_tensor("hT", [D, NT], F32, kind="ExternalInput").ap()
    oa_d = nc.dram_tensor("oaT", [1024, NT], F32, kind="ExternalInput").ap()
    or_d = nc.dram_tensor("orT", [1024, NT], F32, kind="ExternalInput").ap()
    wg_d = nc.dram_tensor("wg", [D, 4096], F32, kind="ExternalInput").ap()
    wua_d = nc.dram_tensor("wua", [1024, D], F32, kind="ExternalInput").ap()
    wur_d = nc.dram_tensor("wur", [1024, D], F32, kind="ExternalInput").ap()
    wo_d = nc.dram_tensor("wo", [D, D], F32, kind="ExternalInput").ap()
    y_d = nc.dram_tensor("yT", [D, NT], F32, kind="ExternalOutput").ap()
    r32 = lambda ap: ap.bitcast(F32R)
    hT = _sb(nc, st, "hT_sb", [128, 16, TT])
    oa = _sb(nc, st, "oa_sb", [128, 8, TT])
    orr = _sb(nc, st, "or_sb", [128, 8, TT])
    mix = _sb(nc, st, "mix_sb", [128, 16, TT])
    wb = [_sb(nc, st, f"wb{i}", [128, 48, 128]) for i in range(2)]
    wob = [_sb(nc, st, f"wob{i}", [128, 16, 128]) for i in range(2)]
    sg = [_sb(nc, st, f"sg{i}", [128, TT]) for i in range(2)]
    m12 = [_sb(nc, st, f"m12_{i}", [128, TT]) for i in range(2)]
    yo = [_sb(nc, st, f"yo{i}", [128, TT]) for i in range(2)]
    ps = [_ps(nc, st, f"ps{i}") for i in range(8)]
    wgv = wg_d.rearrange("(c p) n -> p c n", p=128)
    wuav = wua_d.rearrange("(c p) n -> p c n", p=128)
    wurv = wur_d.rearrange("(c p) n -> p c n", p=128)
    wov = wo_d.rearrange("(c p) n -> p c n", p=128)
    wcount = 0
    for u in range(nunit):
        ts_ = slice(u * TT, (u + 1) * TT)
        p.dma("pool", r32(hT[:]), r32(hT_d.rearrange("(c p) t -> p c t", p=128)[:, :, ts_]), writes=["hT"])
        p.dma("pool", r32(oa[:]), r32(oa_d.rearrange("(c p) t -> p c t", p=128)[:, :, ts_]), writes=["oa"])
        p.dma("pool", r32(orr[:]), r32(or_d.rearrange("(c p) t -> p c t", p=128)[:, :, ts_]), writes=["or"])
        for n in range(16):
            wi = wcount % 2
            wcount += 1
            W = wb[wi]
            wk = f"wb{wi}"
            ns = slice(n * 128, (n + 1) * 128)
            ns2 = slice(2048 + n * 128, 2048 + (n + 1) * 128)
            p.dma("pool", r32(W[:, 0:8, :]), r32(wgv[:, 0:8, ns]), writes=[wk + "a"])
            p.dma("pool", r32(W[:, 8:16, :]), r32(wgv[:, 8:16, ns]), writes=[wk + "b"])
            p.dma("pool", r32(W[:, 16:24, :]), r32(wgv[:, 0:8, ns2]), writes=[wk + "c"])
            p.dma("pool", r32(W[:, 24:32, :]), r32(wgv[:, 8:16, ns2]), writes=[wk + "d"])
            p.dma("pool", r32(W[:, 32:40, :]), r32(wuav[:, :, ns]), writes=[wk + "e"])
            p.dma("pool", r32(W[:, 40:48, :]), r32(wurv[:, :, ns]), writes=[wk + "f"])
            wkeys = [wk + x for x in "abcdef"]
            b0 = (n % 2) * 4
            pga, pgr, pua, pur = ps[b0], ps[b0 + 1], ps[b0 + 2], ps[b0 + 3]
            for c in range(16):
                p.op("pe", lambda e, c=c, W=W, pga=pga: e.matmul(pga[:], lhsT=r32(W[:, c, :]), rhs=r32(hT[:, c, :]), start=(c == 0), stop=(c == 15)),
                     reads=wkeys + ["hT"], writes=[f"ps{b0}"])
            for c in range(16):
                p.op("pe", lambda e, c=c, W=W, pgr=pgr: e.matmul(pgr[:], lhsT=r32(W[:, 16 + c, :]), rhs=r32(hT[:, c, :]), start=(c == 0), stop=(c == 15)),
                     reads=wkeys + ["hT"], writes=[f"ps{b0 + 1}"])
            for c in range(8):
                p.op("pe", lambda e, c=c, W=W, pua=pua: e.matmul(pua[:], lhsT=r32(W[:, 32 + c, :]), rhs=r32(oa[:, c, :]), start=(c == 0), stop=(c == 7)),
                     reads=wkeys + ["oa"], writes=[f"ps{b0 + 2}"])
            for c in range(8):
                p.op("pe", lambda e, c=c, W=W, pur=pur: e.matmul(pur[:], lhsT=r32(W[:, 40 + c, :]), rhs=r32(orr[:, c, :]), start=(c == 0), stop=(c == 7)),
                     reads=wkeys + ["or"], writes=[f"ps{b0 + 3}"])
            p.op("act", lambda e, pga=pga: e.activation(out=sg[0][:], in_=pga[:], func=AF.Sigmoid), reads=[f"ps{b0}"], writes=["sg0"])
            p.op("act", lambda e, pgr=pgr: e.activation(out=sg[1][:], in_=pgr[:], func=AF.Sigmoid), reads=[f"ps{b0 + 1}"], writes=["sg1"])
            p.op("dve", lambda e, pua=pua: e.tensor_tensor(out=m12[0][:], in0=pua[:], in1=sg[0][:], op=ALU.mult), reads=[f"ps{b0 + 2}", "sg0"], writes=["m0"])
            p.op("dve", lambda e, pur=pur: e.tensor_tensor(out=m12[1][:], in0=pur[:], in1=sg[1][:], op=ALU.mult), reads=[f"ps{b0 + 3}", "sg1"], writes=["m1"])
            p.op("dve", lambda e, n=n: e.tensor_tensor(out=r32(mix[:, n, :]), in0=m12[0][:], in1=m12[1][:], op=ALU.add), reads=["m0", "m1"], writes=[f"mix{n}"])
        for m in range(16):
            wi = m % 2
            ms = slice(m * 128, (m + 1) * 128)
            p.dma("pool", r32(wob[wi][:, 0:8, :]), r32(wov[:, 0:8, ms]), writes=[f"wob{wi}a"])
            p.dma("pool", r32(wob[wi][:, 8:16, :]), r32(wov[:, 8:16, ms]), writes=[f"wob{wi}b"])
            py = ps[m % 2]
            for c in range(16):
                p.op("pe", lambda e, c=c, wi=wi, py=py: e.matmul(py[:], lhsT=r32(wob[wi][:, c, :]), rhs=r32(mix[:, c, :]), start=(c == 0), stop=(c == 15)),
                     reads=[f"wob{wi}a", f"wob{wi}b", f"mix{c}"], writes=[f"ps{m % 2}"])
            p.op("act", lambda e, py=py, wi=wi: e.copy(out=yo[wi][:], in_=py[:]), reads=[f"ps{m % 2}"], writes=[f"yo{wi}"])
            p.dma("pool", y_d[ms, ts_], yo[wi][:], reads=[f"yo{wi}"])


def launch_merge(h1, o_att, o_rwkv, I):
    wg = np.ascontiguousarray(I["w_in"][0][:, 6144:10240])
    in_maps = []
    for i in range(8):
        ts_ = slice(i * 1024, (i + 1) * 1024)
        in_maps.append({"hT": np.ascontiguousarray(h1[ts_].T), "oaT": np.ascontiguousarray(o_att[ts_].T),
                        "orT": np.ascontiguousarray(o_rwkv[ts_].T), "wg": wg,
                        "wua": I["w_up_att"][0], "wur": I["w_up_rwkv"][0], "wo": I["w_o"][0]})
    res = _run(lambda nc, p, st: build_merge(nc, p, st, 2), in_maps)
    return np.concatenate([r["yT"].T for r in res], axis=0)


def build_router(nc, p, st, ntile=8):
    NT = ntile * 128
    hT_d = nc.dram_tensor("hT", [D, NT], F32, kind="ExternalInput").ap()
    wr_d = nc.dram_tensor("wr", [D, 72], F32, kind="ExternalInput").ap()
    br_d = nc.dram_tensor("br", [1, 72], F32, kind="ExternalInput").ap()
    o_d = nc.dram_tensor("o", [NT, 4], F32, kind="ExternalOutput").ap()
    hT = _sb(nc, st, "hT_sb", [128, 16, NT])
    wr = _sb(nc, st, "wr_sb", [128, 16, 72])
    br = _sb(nc, st, "br_sb", [128, 72])
    ps = [_ps(nc, st, f"ps{i}") for i in range(2)]
    p.dma("sp", hT[:], hT_d.rearrange("(c p) t -> p c t", p=128), writes=["hT"])
    p.dma("sp", wr[:], wr_d.rearrange("(c p) n -> p c n", p=128), writes=["wr"])
    p.dma("sp", br[:], br_d.partition_broadcast(128), writes=["br"])
    l_sb = _sb(nc, st, "l_sb", [128, 72])
    lem = _sb(nc, st, "lem", [128, 64])
    m8g = _sb(nc, st, "m8g", [128, 8])
    m8e = _sb(nc, st, "m8e", [128, 8])
    idx = _sb(nc, st, "idx", [128, 8], U32)
    sm = _sb(nc, st, "sm", [128, 8])
    junk = _sb(nc, st, "junk", [128, 8])
    pen = _sb(nc, st, "pen", [128, 8])
    res = [_sb(nc, st, f"res{i}", [128, 4]) for i in range(2)]
    for t in range(ntile):
        pt = ps[t % 2]
        ptk = f"ps{t % 2}"
        rs_ = res[t % 2]
        rk = f"res{t % 2}"
        for c in range(16):
            p.op("pe", lambda e, c=c, t=t, pt=pt: e.matmul(pt[:, 0:72], lhsT=hT[:, c, t * 128:(t + 1) * 128], rhs=wr[:, c, :], start=(c == 0), stop=(c == 15)),
                 reads=["hT", "wr"], writes=[ptk])
        p.op("dve", lambda e, pt=pt: e.tensor_tensor(out=l_sb[:], in0=pt[:, 0:72], in1=br[:], op=ALU.add), reads=[ptk, "br"], writes=["l"])
        p.op("dve", lambda e: e.max(out=m8g[:], in_=l_sb[:, 0:8]), reads=["l"], writes=["m8g"])
        p.op("dve", lambda e: e.tensor_scalar(out=sm[:, 0:1], in0=m8g[:, 0:1], scalar1=-1.0, scalar2=None, op0=ALU.mult), reads=["m8g"], writes=["sm0"])
        p.op("act", lambda e: e.activation(out=junk[:], in_=l_sb[:, 0:8], func=AF.Exp, bias=sm[:, 0:1], accum_out=sm[:, 1:2]), reads=["l", "sm0"], writes=["junk", "sm1"])
        p.op("dve", lambda e: e.reciprocal(out=sm[:, 2:3], in_=sm[:, 1:2]), reads=["sm1"], writes=["sm2"])
        p.op("dve", lambda e: e.tensor_scalar(out=pen[:], in0=l_sb[:, 0:8], scalar1=m8g[:, 0:1], scalar2=None, op0=ALU.is_ge), reads=["l", "m8g"], writes=["pen"], force=True)
        p.op("dve", lambda e: e.tensor_scalar(out=pen[:], in0=pen[:], scalar1=1e30, scalar2=-1e30, op0=ALU.mult, op1=ALU.add), reads=["pen"], writes=["pen"])
        for g in range(8):
            p.op("dve", lambda e, g=g: e.tensor_scalar(out=lem[:, g * 8:(g + 1) * 8], in0=l_sb[:, 8 + g * 8:16 + g * 8], scalar1=pen[:, g:g + 1], scalar2=None, op0=ALU.add),
                 reads=["l", "pen"], writes=["lem"], force=(g == 0))
        p.op("dve", lambda e: e.max(out=m8e[:], in_=lem[:]), reads=["lem"], writes=["m8e"])
        p.op("dve", lambda e: e.max_index(out=idx[:], in_max=m8e[:], in_values=lem[:]), reads=["m8e", "lem"], writes=["idx"], force=True)
        p.op("dve", lambda e, rs_=rs_: e.tensor_copy(out=rs_[:, 0:2], in_=idx[:, 0:2]), reads=["idx"], writes=[rk + "a"], force=True)
        p.op("dve", lambda e: e.tensor_tensor(out=sm[:, 3:4], in0=m8e[:, 0:1], in1=m8e[:, 1:2], op=ALU.subtract), reads=["m8e"], writes=["sm3"], force=True)
        p.op("act", lambda e: e.activation(out=sm[:, 4:5], in_=sm[:, 3:4], func=AF.Sigmoid), reads=["sm3"], writes=["sm4"])
        p.op("dve", lambda e, rs_=rs_: e.tensor_tensor(out=rs_[:, 2:3], in0=sm[:, 4:5], in1=sm[:, 2:3], op=ALU.mult), reads=["sm4", "sm2"], writes=[rk + "b"], force=True)
        p.op("dve", lambda e, rs_=rs_: e.tensor_tensor(out=rs_[:, 3:4], in0=sm[:, 2:3], in1=rs_[:, 2:3], op=ALU.subtract), reads=["sm2", rk + "b"], writes=[rk + "c"], force=True)
        p.dma("sp", o_d[t * 128:(t + 1) * 128, :], rs_[:], reads=[rk + "a", rk + "b", rk + "c"])


def launch_router(h2, I):
    wr = np.ascontiguousarray(np.concatenate([I["w_rg"][0], I["w_re"][0]], 1))
    br = np.ascontiguousarray(np.concatenate([I["b_rg"][0], I["b_re"][0]])[None, :])
    in_maps = [{"hT": np.ascontiguousarray(h2[i * 1024:(i + 1) * 1024].T), "wr": wr, "br": br} for i in range(8)]
    res = _run(lambda nc, p, st: build_router(nc, p, st, 8), in_maps)
    o = np.concatenate([r["o"] for r in res], axis=0)
    return o[:, 0:2].astype(np.int64), o[:, 2:4]


def build_experts(nc, p, st, cap):
    xT_d = nc.dram_tensor("xT", [8, D, cap], F32, kind="ExternalInput").ap()
    wg_d = nc.dram_tensor("wg", [8, D, 512], F32, kind="ExternalInput").ap()
    wu_d = nc.dram_tensor("wu", [8, D, 512], F32, kind="ExternalInput").ap()
    wd_d = nc.dram_tensor("wd", [8, 512, D], F32, kind="ExternalInput").ap()
    y_d = nc.dram_tensor("yT", [8, D, cap], F32, kind="ExternalOutput").ap()
    r32 = lambda ap: ap.bitcast(F32R)
    xT = _sb(nc, st, "xT_sb", [128, 16, cap])
    Wg = _sb(nc, st, "Wg_sb", [128, 16, 512])
    Wu = _sb(nc, st, "Wu_sb", [128, 16, 512])
    Wd = _sb(nc, st, "Wd_sb", [128, 4, D])
    hid = _sb(nc, st, "hid_sb", [128, 4, cap])
    sg = [_sb(nc, st, f"sg{i}", [128, cap]) for i in range(2)]
    yo = [_sb(nc, st, f"yo{i}", [128, cap]) for i in range(3)]
    ps = [_ps(nc, st, f"ps{i}") for i in range(8)]
    for ex in range(8):
        xv = xT_d[ex].rearrange("(c p) t -> p c t", p=128)
        for h in range(4):
            p.dma("pool", r32(xT[:, h * 4:(h + 1) * 4, :]), r32(xv[:, h * 4:(h + 1) * 4, :]), writes=[f"xT{h}"])
        gv = wg_d[ex].rearrange("(c p) n -> p c n", p=128)
        uv = wu_d[ex].rearrange("(c p) n -> p c n", p=128)
        dv = wd_d[ex].rearrange("(c p) n -> p c n", p=128)
        for h in range(8):
            p.dma("pool", r32(Wg[:, h * 2:(h + 1) * 2, :]), r32(gv[:, h * 2:(h + 1) * 2, :]), writes=[f"Wg{h}"])
        for h in range(8):
            p.dma("pool", r32(Wu[:, h * 2:(h + 1) * 2, :]), r32(uv[:, h * 2:(h + 1) * 2, :]), writes=[f"Wu{h}"])
        for h in range(4):
            p.dma("pool", r32(Wd[:, h, 0:1024]), r32(dv[:, h, 0:1024]), writes=[f"Wd{h}a"])
            p.dma("pool", r32(Wd[:, h, 1024:2048]), r32(dv[:, h, 1024:2048]), writes=[f"Wd{h}b"])
        for f in range(4):
            pg = ps[(f % 2) * 2]
            pu = ps[(f % 2) * 2 + 1]
            pgk = f"ps{(f % 2) * 2}"
            puk = f"ps{(f % 2) * 2 + 1}"
            fs = slice(f * 128, (f + 1) * 128)
            for c in range(16):
                p.op("pe", lambda e, c=c, pg=pg, fs=fs: e.matmul(pg[:, 0:cap], lhsT=r32(Wg[:, c, fs]), rhs=r32(xT[:, c, :]), start=(c == 0), stop=(c == 15)),
                     reads=[f"Wg{c // 2}", f"xT{c // 4}"], writes=[pgk])
            for c in range(16):
                p.op("pe", lambda e, c=c, pu=pu, fs=fs: e.matmul(pu[:, 0:cap], lhsT=r32(Wu[:, c, fs]), rhs=r32(xT[:, c, :]), start=(c == 0), stop=(c == 15)),
                     reads=[f"Wu{c // 2}", f"xT{c // 4}"], writes=[puk])
            s_ = sg[f % 2]
            p.op("act", lambda e, s_=s_, pg=pg: e.activation(out=s_[:], in_=pg[:, 0:cap], func=AF.Silu), reads=[pgk], writes=[f"sg{f % 2}"])
            p.op("dve", lambda e, s_=s_, pu=pu, f=f: e.tensor_tensor(out=r32(hid[:, f, :]), in0=pu[:, 0:cap], in1=s_[:], op=ALU.mult),
                 reads=[puk, f"sg{f % 2}"], writes=[f"hid{f}"])
        for d in range(16):
            py = ps[4 + d % 4]
            pyk = f"ps{4 + d % 4}"
            ds_ = slice(d * 128, (d + 1) * 128)
            for f in range(4):
                p.op("pe", lambda e, f=f, py=py, ds_=ds_: e.matmul(py[:, 0:cap], lhsT=r32(Wd[:, f, ds_]), rhs=r32(hid[:, f, :]), start=(f == 0), stop=(f == 3)),
                     reads=[f"Wd{f}a", f"Wd{f}b", f"hid{f}"], writes=[pyk])
            yb = yo[d % 3]
            p.op("act" if d % 2 else "dve", (lambda e, yb=yb, py=py: e.copy(out=yb[:], in_=py[:, 0:cap])) if d % 2 else (lambda e, yb=yb, py=py: e.tensor_copy(out=yb[:], in_=py[:, 0:cap])),
                 reads=[pyk], writes=[f"yo{d % 3}"])
            p.dma("sp", y_d[ex, ds_, :], yb[:], reads=[f"yo{d % 3}"])


def launch_experts(h2, eidx, I):
    N = h2.shape[0]
    flat_e = eidx.reshape(-1)
    flat_t = np.repeat(np.arange(N), 2)
    order = np.argsort(flat_e, kind="stable")
    counts = np.bincount(flat_e, minlength=64)
    cap = int(max(256, -(-counts.max() // 128) * 128))
    starts = np.cumsum(counts) - counts
    xT = np.zeros((64, D, cap), np.float32)
    pos_of = np.zeros(2 * N, np.int64)
    for e in range(64):
        sl = order[starts[e]:starts[e] + counts[e]]
        xT[e, :, :counts[e]] = h2[flat_t[sl]].T
        pos_of[sl] = np.arange(counts[e])
    in_maps = [{"xT": np.ascontiguousarray(xT[g * 8:(g + 1) * 8]), "wg": np.ascontiguousarray(I["w_gate_e"][0][g * 8:(g + 1) * 8]),
                "wu": np.ascontiguousarray(I["w_up_e"][0][g * 8:(g + 1) * 8]), "wd": np.ascontiguousarray(I["w_down_e"][0][g * 8:(g + 1) * 8])} for g in range(8)]
    res = _run(lambda nc, p, st: build_experts(nc, p, st, cap), in_maps)
    yT = np.concatenate([r["yT"] for r in res], axis=0)
    yflat = yT[flat_e, :, pos_of]
    yflat = yflat.reshape(N, 2, D)
    return np.ascontiguousarray(yflat[:, 0]), np.ascontiguousarray(yflat[:, 1])


def build_combine(nc, p, st, ntile=8):
    n = ntile * 128
    ya_d = nc.dram_tensor("ya", [n, D], F32, kind="ExternalInput").ap()
    yb_d = nc.dram_tensor("yb", [n, D], F32, kind="ExternalInput").ap()
    w_d = nc.dram_tensor("w", [n, 2], F32, kind="ExternalInput").ap()
    g = nc.dram_tensor("g", [1, D], F32, kind="ExternalInput").ap()
    s = nc.dram_tensor("s", [1, D], F32, kind="ExternalInput").ap()
    base = nc.dram_tensor("base", [n, D], F32, kind="ExternalInput").ap()
    o = nc.dram_tensor("o", [n, D], F32, kind="ExternalOutput").ap()
    gb = _sb(nc, st, "gb", [128, D])
    A = _sb(nc, st, "A", [128, D])
    p.dma("sp", gb[:], g.partition_broadcast(128), writes=["gb"])
    p.dma("sp", A[:], s.partition_broadcast(128), writes=["A"])
    p.op("dve", lambda e: e.tensor_tensor(out=A[:], in0=A[:], in1=gb[:], op=ALU.mult), reads=["gb", "A"], writes=["A"])
    ya = [_sb(nc, st, f"ya{i}", [128, D]) for i in range(2)]
    yb = [_sb(nc, st, f"yb{i}", [128, D]) for i in range(2)]
    bt = [_sb(nc, st, f"bt{i}", [128, D]) for i in range(2)]
    wt = [_sb(nc, st, f"wt{i}", [128, 2]) for i in range(2)]
    junk = _sb(nc, st, "junk", [128, D])
    ot = [_sb(nc, st, f"ot{i}", [128, D]) for i in range(2)]
    ss = [_sb(nc, st, f"ss{i}", [128, 4]) for i in range(2)]
    for t in range(ntile):
        i = t % 2
        rows = slice(t * 128, (t + 1) * 128)
        p.dma("sp", ya[i][:], ya_d[rows, :], writes=[f"ya{i}"])
        p.dma("sp", yb[i][:], yb_d[rows, :], writes=[f"yb{i}"])
        p.dma("sp", bt[i][:], base[rows, :], writes=[f"bt{i}"])
        p.dma("sp", wt[i][:], w_d[rows, :], writes=[f"wt{i}"])
        p.op("dve", lambda e, i=i: e.tensor_scalar(out=ya[i][:], in0=ya[i][:], scalar1=wt[i][:, 0:1], scalar2=None, op0=ALU.mult),
             reads=[f"ya{i}", f"wt{i}"], writes=[f"ya{i}"])
        p.op("dve", lambda e, i=i: e.scalar_tensor_tensor(out=ya[i][:], in0=yb[i][:], scalar=wt[i][:, 1:2], in1=ya[i][:], op0=ALU.mult, op1=ALU.add),
             reads=[f"ya{i}", f"yb{i}", f"wt{i}"], writes=[f"ya{i}"])
        p.op("act", lambda e, i=i: e.activation(out=junk[:], in_=ya[i][:], func=AF.Square, accum_out=ss[i][:, 0:1]),
             reads=[f"ya{i}"], writes=["junk", f"ss{i}"])
        p.op("dve", lambda e, i=i: e.tensor_scalar(out=ss[i][:, 1:2], in0=ss[i][:, 0:1], scalar1=1.0 / D, scalar2=EPS, op0=ALU.mult, op1=ALU.add),
             reads=[f"ss{i}"], writes=[f"ss{i}"])
        p.op("act", lambda e, i=i: e.activation(out=ss[i][:, 2:3], in_=ss[i][:, 1:2], func=AF.Sqrt), reads=[f"ss{i}"], writes=[f"ss{i}"])
        p.op("dve", lambda e, i=i: e.reciprocal(out=ss[i][:, 3:4], in_=ss[i][:, 2:3]), reads=[f"ss{i}"], writes=[f"ss{i}"])
        p.op("dve", lambda e, i=i: e.scalar_tensor_tensor(out=ot[i][:], in0=ya[i][:], scalar=ss[i][:, 3:4], in1=A[:], op0=ALU.mult, op1=ALU.mult),
             reads=[f"ya{i}", f"ss{i}", "A"], writes=[f"ot{i}"], force=True)
        p.op("pool", lambda e, i=i: e.tensor_tensor(out=ot[i][:], in0=ot[i][:], in1=bt[i][:], op=ALU.add),
             reads=[f"ot{i}", f"bt{i}"], writes=[f"ot{i}"])
        p.dma("pool", o[rows, :], ot[i][:], reads=[f"ot{i}"])


def launch_combine(ya, yb, w, g, s, base):
    n = ya.shape[0] // 8
    in_maps = [{"ya": np.ascontiguousarray(ya[i * n:(i + 1) * n]), "yb": np.ascontiguousarray(yb[i * n:(i + 1) * n]),
                "w": np.ascontiguousarray(w[i * n:(i + 1) * n]), "g": np.ascontiguousarray(g[None, :]),
                "s": np.ascontiguousarray(s[None, :]), "base": np.ascontiguousarray(base[i * n:(i + 1) * n])} for i in range(8)]
    res = _run(lambda nc, p, st: build_combine(nc, p, st, n // 128), in_maps)
    return np.concatenate([r["o"] for r in res], axis=0)


def kernel(**inputs):
    I = {k: np.asarray(v) for k, v in inputs.items()}
    x = I["x"][0]
    ada = launch_ada(I["c"][0], I["w_ada"][0], I["b_ada"][0])
    sh1, sc1, gt1, sh2, sc2, gt2 = np.split(ada, 6)
    h1 = launch_norm(x, I["g_pre_mix"][0], sc1, 1.0, bv=sh1)
    lc = launch_inproj(h1, I)
    o_att = launch_moba(lc)
    o_rwkv = launch_rwkv(lc, I)
    y1 = launch_merge(h1, o_att, o_rwkv, I)
    x1 = launch_norm(y1, I["g_post_mix"][0], gt1, 0.0, base=x)
    h2 = launch_norm(x1, I["g_pre_ffn"][0], sc2, 1.0, bv=sh2)
    eidx, ew = launch_router(h2, I)
    ya, yb = launch_experts(h2, eidx, I)
    out = launch_combine(ya, yb, ew, I["g_post_ffn"][0], gt2, x1)
    return out[None].astype(np.float32)
```

```python
import numpy as np
import concourse.bass as bass
import concourse.mybir as mybir
from concourse.bass_utils import run_bass_kernel_spmd

F32 = mybir.dt.float32
F32R = mybir.dt.float32r
BF16 = mybir.dt.bfloat16
I32 = mybir.dt.int32
U32 = mybir.dt.uint32
AF = mybir.ActivationFunctionType
ALU = mybir.AluOpType
AX = mybir.AxisListType

NDMA_SLOTS = 6


class Prog:
    def __init__(self, nc):
        self.nc = nc
        self.ops = []
        self.last_w = {}
        self.readers = {}
        self.engs = {"pe": nc.tensor, "act": nc.scalar, "dve": nc.vector,
                     "pool": nc.gpsimd, "sp": nc.sync}

    def op(self, eng, fn, reads=(), writes=(), dma=False, force=False, inc=16):
        deps = set()
        raw = set()
        for k in reads:
            if k in self.last_w:
                deps.add(self.last_w[k])
                raw.add(self.last_w[k])
        for k in writes:
            if k in self.last_w:
                deps.add(self.last_w[k])
                raw.add(self.last_w[k])
            for r in self.readers.get(k, ()):
                deps.add(r)
        idx = len(self.ops)
        self.ops.append(dict(eng=eng, fn=fn, deps=deps, raw=raw, dma=dma, force=force, inc=inc))
        for k in reads:
            self.readers.setdefault(k, []).append(idx)
        for k in writes:
            self.last_w[k] = idx
            self.readers[k] = []
        return idx

    def dma(self, q, out, in_, reads=(), writes=(), **kw):
        return self.op(q, lambda e: e.dma_start(out=out, in_=in_, **kw), reads, writes, dma=True)

    def emit(self, stack):
        nc = self.nc
        ops = self.ops
        need = [False] * len(ops)
        for i, o in enumerate(ops):
            nd = set()
            for d in o["deps"]:
                od = ops[d]
                if od["dma"] or o["dma"] or o["force"] or od["eng"] != o["eng"] or (d in o["raw"] and o["eng"] != "pe"):
                    nd.add(d)
            o["xdeps"] = nd
            for d in nd:
                need[d] = True
        for i, o in enumerate(ops):
            if o["dma"]:
                need[i] = True
        esem = {e: stack.enter_context(nc.semaphore("es_" + e)) for e in self.engs}
        dsem = {e: [stack.enter_context(nc.semaphore(f"ds_{e}_{k}")) for k in range(NDMA_SLOTS)]
                for e in ("sp", "act", "pool")}
        ecount = {e: 0 for e in self.engs}
        dcount = {e: 0 for e in dsem}
        signal = [None] * len(ops)
        waited = {}
        nwaits = 0
        actions = {e: [] for e in self.engs}
        for i, o in enumerate(ops):
            e = o["eng"]
            wl = {}
            for d in o["xdeps"]:
                s_, v = signal[d]
                key = id(s_)
                if waited.get((e, key), 0) >= v:
                    continue
                if key not in wl or wl[key][1] < v:
                    wl[key] = (s_, v)
            if o["dma"]:
                j = dcount[e]
                slot = j % NDMA_SLOTS
                s_ = dsem[e][slot]
                prev = o.get("prev_total", None)
                prev = self._slot_total.get((e, slot), 0) if hasattr(self, "_slot_total") else 0
                if prev > 0 and waited.get((e, id(s_)), 0) < prev:
                    if id(s_) not in wl or wl[id(s_)][1] < prev:
                        wl[id(s_)] = (s_, prev)
            for key, (s_, v) in wl.items():
                waited[(e, key)] = v
                nwaits += 1
            sem = None
            inc = 0
            if o["dma"]:
                if not hasattr(self, "_slot_total"):
                    self._slot_total = {}
                j = dcount[e]
                dcount[e] += 1
                slot = j % NDMA_SLOTS
                sem = dsem[e][slot]
                inc = o.get("inc", 16)
                tot = self._slot_total.get((e, slot), 0) + inc
                self._slot_total[(e, slot)] = tot
                signal[i] = (sem, tot)
            elif need[i]:
                ecount[e] += 1
                sem = esem[e]
                inc = 1
                signal[i] = (sem, ecount[e])
            actions[e].append((list(wl.values()), o["fn"], sem, inc))
        finals = {e: [] for e in self.engs}
        for e in dsem:
            for slot in range(NDMA_SLOTS):
                tot = getattr(self, "_slot_total", {}).get((e, slot), 0)
                if tot > 0:
                    finals[e].append((dsem[e][slot], tot))
        bnames = {"pe": "tensor", "act": "scalar", "dve": "vector", "pool": "gpsimd", "sp": "sync"}
        with nc.Block() as block:
            for e in self.engs:
                if not actions[e] and not finals[e]:
                    continue

                def body(eng, e=e):
                    for waits, fn, sem, inc in actions[e]:
                        for s_, v in waits:
                            eng.wait_ge(s_, v)
                        inst = fn(eng)
                        if sem is not None:
                            inst.then_inc(sem, inc)
                    for s_, v in finals[e]:
                        eng.wait_ge(s_, v)
                getattr(block, bnames[e])(body)
        self.stats = dict(n_ops=len(ops), n_waits=nwaits, ecount=ecount, dcount=dcount)
        return self.stats


from contextlib import ExitStack

S = 8192
D = 2048
EPS = 1e-6
_TRACE = False


def _run(build, in_maps):
    nc = bass.Bass("TRN2", target_bir_lowering=False)
    with ExitStack() as st:
        p = Prog(nc)
        build(nc, p, st)
        p.emit(st)
    if _TRACE:
        r = run_bass_kernel_spmd(nc, in_maps, core_ids=list(range(8)), trace=True)
        print("EXEC_NS", getattr(build, "__name__", "?"), r.exec_time_ns, p.stats, flush=True)
    else:
        r = run_bass_kernel_spmd(nc, in_maps, core_ids=list(range(8)))
    return r.results


def _sb(nc, st, name, shape, dt=F32):
    return st.enter_context(nc.sbuf_tensor(name, shape, dt))


def _ps(nc, st, name, shape=(128, 512), dt=F32):
    return st.enter_context(nc.psum_tensor(name, list(shape), dt))


def build_ada(nc, p, st):
    w = nc.dram_tensor("w", [2048, 1536], F32, kind="ExternalInput").ap()
    c = nc.dram_tensor("c", [128, 16], F32, kind="ExternalInput").ap()
    b = nc.dram_tensor("b", [1, 1536], F32, kind="ExternalInput").ap()
    y = nc.dram_tensor("y", [1, 1536], F32, kind="ExternalOutput").ap()
    wt = [_sb(nc, st, f"wt{i}", [128, 1536]) for i in range(2)]
    ct = _sb(nc, st, "ct", [128, 16])
    bt = _sb(nc, st, "bt", [1, 1536])
    acc = _sb(nc, st, "acc", [128, 1536])
    ones = _sb(nc, st, "ones", [128, 1])
    res = _sb(nc, st, "res", [1, 1536])
    ps = [_ps(nc, st, f"ps{i}", (1, 512)) for i in range(2)]
    p.dma("sp", ct[:], c, writes=["ct"])
    p.dma("sp", bt[:], b, writes=["bt"])
    p.op("dve", lambda e: e.memset(ones[:], 1.0), writes=["ones"])
    for kc in range(16):
        i = kc % 2
        p.dma("sp", wt[i][:], w[kc * 128:(kc + 1) * 128, :], writes=[f"wt{i}"])
        if kc == 0:
            p.op("dve", lambda e, i=i, kc=kc: e.tensor_scalar(out=acc[:], in0=wt[i][:], scalar1=ct[:, kc:kc + 1], scalar2=None, op0=ALU.mult),
                 reads=[f"wt{i}", "ct"], writes=["acc"])
        else:
            p.op("dve", lambda e, i=i, kc=kc: e.scalar_tensor_tensor(out=acc[:], in0=wt[i][:], scalar=ct[:, kc:kc + 1], in1=acc[:], op0=ALU.mult, op1=ALU.add),
                 reads=[f"wt{i}", "ct", "acc"], writes=["acc"])
    for j in range(3):
        pj = ps[j % 2]
        p.op("pe", lambda e, j=j, pj=pj: e.matmul(pj[:], lhsT=ones[:], rhs=acc[:, j * 512:(j + 1) * 512], start=True, stop=True),
             reads=["acc", "ones"], writes=[f"ps{j%2}"])
        p.op("dve", lambda e, j=j, pj=pj: e.tensor_tensor(out=res[:, j * 512:(j + 1) * 512], in0=pj[:], in1=bt[:, j * 512:(j + 1) * 512], op=ALU.add),
             reads=[f"ps{j%2}", "bt"], writes=[f"res{j}"])
    p.dma("sp", y, res[:], reads=["res0", "res1", "res2"])


def launch_ada(c, w_ada, b_ada):
    in_maps = [{"w": np.ascontiguousarray(w_ada[:, i * 1536:(i + 1) * 1536]),
                "c": np.ascontiguousarray(c.reshape(16, 128).T),
                "b": np.ascontiguousarray(b_ada[None, i * 1536:(i + 1) * 1536])} for i in range(8)]
    res = _run(build_ada, in_maps)
    return np.concatenate([r["y"][0] for r in res])


def make_build_norm(add_one, has_b, has_base, ntile=8):
    def build(nc, p, st):
        n = ntile * 128
        y = nc.dram_tensor("y", [n, D], F32, kind="ExternalInput").ap()
        g = nc.dram_tensor("g", [1, D], F32, kind="ExternalInput").ap()
        s = nc.dram_tensor("s", [1, D], F32, kind="ExternalInput").ap()
        bv = nc.dram_tensor("bv", [1, D], F32, kind="ExternalInput").ap() if has_b else None
        base = nc.dram_tensor("base", [n, D], F32, kind="ExternalInput").ap() if has_base else None
        o = nc.dram_tensor("o", [n, D], F32, kind="ExternalOutput").ap()
        gb = _sb(nc, st, "gb", [128, D])
        A = _sb(nc, st, "A", [128, D])
        bb = _sb(nc, st, "bb", [128, D]) if has_b else None
        p.dma("sp", gb[:], g.partition_broadcast(128), writes=["gb"])
        p.dma("sp", A[:], s.partition_broadcast(128), writes=["A"])
        if has_b:
            p.dma("sp", bb[:], bv.partition_broadcast(128), writes=["bb"])
        p.op("dve", lambda e: e.scalar_tensor_tensor(out=A[:], in0=A[:], scalar=float(add_one), in1=gb[:], op0=ALU.add, op1=ALU.mult),
             reads=["gb", "A"], writes=["A"])
        yt = [_sb(nc, st, f"yt{i}", [128, D]) for i in range(2)]
        bt = [_sb(nc, st, f"bt{i}", [128, D]) for i in range(2)] if has_base else None
        junk = _sb(nc, st, "junk", [128, D])
        ot = [_sb(nc, st, f"ot{i}", [128, D]) for i in range(2)]
        ss = [_sb(nc, st, f"ss{i}", [128, 4]) for i in range(2)]
        for t in range(ntile):
            i = t % 2
            rows = slice(t * 128, (t + 1) * 128)
            p.dma("sp", yt[i][:], y[rows, :], writes=[f"yt{i}"])
            if has_base:
                p.dma("sp", bt[i][:], base[rows, :], writes=[f"bt{i}"])
            p.op("act", lambda e, i=i: e.activation(out=junk[:], in_=yt[i][:], func=AF.Square, accum_out=ss[i][:, 0:1]),
                 reads=[f"yt{i}"], writes=["junk", f"ss{i}"])
            p.op("dve", lambda e, i=i: e.tensor_scalar(out=ss[i][:, 1:2], in0=ss[i][:, 0:1], scalar1=1.0 / D, scalar2=EPS, op0=ALU.mult, op1=ALU.add),
                 reads=[f"ss{i}"], writes=[f"ss{i}"])
            p.op("act", lambda e, i=i: e.activation(out=ss[i][:, 2:3], in_=ss[i][:, 1:2], func=AF.Sqrt),
                 reads=[f"ss{i}"], writes=[f"ss{i}"])
            p.op("dve", lambda e, i=i: e.reciprocal(out=ss[i][:, 3:4], in_=ss[i][:, 2:3]),
                 reads=[f"ss{i}"], writes=[f"ss{i}"])
            p.op("dve", lambda e, i=i: e.scalar_tensor_tensor(out=ot[i][:], in0=yt[i][:], scalar=ss[i][:, 3:4], in1=A[:], op0=ALU.mult, op1=ALU.mult),
                 reads=[f"yt{i}", f"ss{i}", "A"], writes=[f"ot{i}"], force=True)
            if has_b:
                p.op("pool", lambda e, i=i: e.tensor_tensor(out=ot[i][:], in0=ot[i][:], in1=bb[:], op=ALU.add),
                     reads=[f"ot{i}", "bb"], writes=[f"ot{i}"])
            if has_base:
                p.op("pool", lambda e, i=i: e.tensor_tensor(out=ot[i][:], in0=ot[i][:], in1=bt[i][:], op=ALU.add),
                     reads=[f"ot{i}", f"bt{i}"], writes=[f"ot{i}"])
            p.dma("pool", o[rows, :], ot[i][:], reads=[f"ot{i}"])
    return build


def launch_norm(y, g, s, add_one, bv=None, base=None):
    n = y.shape[0] // 8
    in_maps = []
    for i in range(8):
        m = {"y": np.ascontiguousarray(y[i * n:(i + 1) * n]), "g": np.ascontiguousarray(g[None, :]),
             "s": np.ascontiguousarray(s[None, :])}
        if bv is not None:
            m["bv"] = np.ascontiguousarray(bv[None, :])
        if base is not None:
            m["base"] = np.ascontiguousarray(base[i * n:(i + 1) * n])
        in_maps.append(m)
    res = _run(make_build_norm(add_one, bv is not None, base is not None, n // 128), in_maps)
    return np.concatenate([r["o"] for r in res], axis=0)


LC_OUTS = ["qT", "kT", "vT", "rT", "krT", "vrT", "wdec", "nkk", "kka", "kt", "g", "bonus"]
WDECAY = 0.6065306597126334


def build_inproj(nc, p, st, ntt=16):
    T = ntt * 512
    hTp = nc.dram_tensor("hTp", [D, T + 1], F32, kind="ExternalInput").ap()
    ws_d = nc.dram_tensor("ws", [D, 448], F32, kind="ExternalInput").ap()
    wd_d = nc.dram_tensor("wd", [D, 384], F32, kind="ExternalInput").ap()
    mucol_d = nc.dram_tensor("mucol", [1, 384], F32, kind="ExternalInput").ap()
    wl_d = nc.dram_tensor("wl", [D, 448], F32, kind="ExternalInput").ap()
    murow_d = nc.dram_tensor("murow", [128, 16, 3], F32, kind="ExternalInput").ap()
    w2w_d = nc.dram_tensor("w2w", [96, 128], F32, kind="ExternalInput").ap()
    w2a_d = nc.dram_tensor("w2a", [96, 128], F32, kind="ExternalInput").ap()
    w2g_d = nc.dram_tensor("w2g", [128, 2, 128], F32, kind="ExternalInput").ap()
    vecs_d = nc.dram_tensor("vecs", [128, 5], F32, kind="ExternalInput").ap()
    cos_d = nc.dram_tensor("cos", [32, T], F32, kind="ExternalInput").ap()
    sin_d = nc.dram_tensor("sin", [32, T], F32, kind="ExternalInput").ap()
    blk_d = nc.dram_tensor("blk", [128, 128], F32, kind="ExternalInput").ap()
    outs = {n: nc.dram_tensor(n, [128, T], F32, kind="ExternalOutput").ap() for n in LC_OUTS}

    w0 = _sb(nc, st, "w0", [128, 16, 448])
    wa = _sb(nc, st, "wa", [128, 16, 448])
    wb = _sb(nc, st, "wb", [128, 16, 448])
    hb = [_sb(nc, st, f"hb{i}", [128, 16, 514]) for i in range(2)]
    mucol = _sb(nc, st, "mucol_sb", [128, 384])
    murow = _sb(nc, st, "murow_sb", [128, 16, 3])
    w2w = _sb(nc, st, "w2w_sb", [96, 128])
    w2a = _sb(nc, st, "w2a_sb", [96, 128])
    w2g = _sb(nc, st, "w2g_sb", [128, 2, 128])
    vecs = _sb(nc, st, "vecs_sb", [128, 5])
    blk = _sb(nc, st, "blk_sb", [128, 128])
    cs = [_sb(nc, st, f"cs{i}", [32, 2, 512]) for i in range(2)]
    ps = [_ps(nc, st, f"ps{i}") for i in range(8)]
    NOB = 6
    ob = [_sb(nc, st, f"ob{i}", [128, 512]) for i in range(NOB)]
    obi = [0]
    psi = [0]

    def nps():
        i = psi[0] % 8
        psi[0] += 1
        return ps[i], f"ps{i}"

    def nob():
        i = obi[0] % NOB
        obi[0] += 1
        return ob[i], f"ob{i}"

    hview = hTp.rearrange("(c p) t -> p c t", p=128)
    r32 = lambda ap: ap.bitcast(F32R)

    for dst, src, k in [(mucol[:], mucol_d.partition_broadcast(128), "mucol"), (murow[:], murow_d, "murow"),
                        (w2w[:], w2w_d, "w2w"), (w2a[:], w2a_d, "w2a"), (w2g[:], w2g_d, "w2g"),
                        (vecs[:], vecs_d, "vecs"), (blk[:], blk_d, "blk")]:
        p.dma("sp", dst, src, writes=[k])

    def load_h(tt):
        i = tt % 2
        p.dma("pool", r32(hb[i][:, :, 0:513]), r32(hview[:, :, tt * 512:tt * 512 + 513]), writes=[f"hb{i}"])

    def gemm(tt, kind, co, M, pst, psk):
        i = tt % 2
        for c in range(16):
            cur = hb[i][:, c, 1:513]
            prev = hb[i][:, c, 0:512]
            if kind == "single":
                p.op("pe", lambda e, c=c, cur=cur: e.matmul(pst[:M, :], lhsT=r32(w0[:, c, co:co + M]), rhs=r32(cur), start=(c == 0), stop=(c == 15)),
                     reads=["w0", f"hb{i}"], writes=[psk])
            else:
                p.op("pe", lambda e, c=c, cur=cur: e.matmul(pst[:M, :], lhsT=r32(wa[:, c, co:co + M]), rhs=r32(cur), start=(c == 0), stop=False),
                     reads=["wa", f"hb{i}"], writes=[psk])
                p.op("pe", lambda e, c=c, prev=prev: e.matmul(pst[:M, :], lhsT=r32(wb[:, c, co:co + M]), rhs=r32(prev), start=False, stop=(c == 15)),
                     reads=["wb", f"hb{i}"], writes=[psk])

    p.dma("pool", r32(w0[:]), r32(ws_d.rearrange("(c p) n -> p c n", p=128)), writes=["w0"])
    p.dma("pool", r32(wa[:, :, 0:384]), r32(wd_d.rearrange("(c p) n -> p c n", p=128)), writes=["wa"])
    for c in range(16):
        p.op("dve", lambda e, c=c: e.tensor_tensor(out=r32(wb[:, c, 0:384]), in0=wa[:, c, 0:384], in1=mucol[:], op=ALU.mult),
             reads=["wa", "mucol"], writes=["wb"])
    p.op("dve", lambda e: e.tensor_tensor(out=r32(wa[:, :, 0:384]), in0=wa[:, :, 0:384], in1=wb[:, :, 0:384], op=ALU.subtract),
         reads=["wa", "wb"], writes=["wa"])
    load_h(0)
    for tt in range(ntt):
        if tt + 1 < ntt:
            load_h(tt + 1)
        tsl = slice(tt * 512, (tt + 1) * 512)
        ci = tt % 2
        p.dma("sp", cs[ci][:, 0, :], cos_d[:, tsl], writes=[f"cs{ci}"])
        p.dma("sp", cs[ci][:, 1, :], sin_d[:, tsl], writes=[f"cs{ci}"])
        for name, co in (("qT", 0), ("kT", 160)):
            pq, pqk = nps()
            gemm(tt, "single", co, 128, pq, pqk)
            psw, pswk = nps()
            gemm(tt, "single", co + 128, 32, psw, pswk)
            o, ok = nob()
            p.op("act", lambda e, o=o, pq=pq: e.copy(out=o[:], in_=pq[:]), reads=[pqk], writes=[ok, ok + "hi"])
            t1, t1k = nob()
            p.op("dve", lambda e, t1=t1, psw=psw, ci=ci: e.tensor_tensor(out=t1[0:32, :], in0=psw[0:32, :], in1=cs[ci][:, 1, :], op=ALU.mult),
                 reads=[pswk, f"cs{ci}"], writes=[t1k])
            p.op("dve", lambda e, o=o, pq=pq, ci=ci: e.tensor_tensor(out=o[0:32, :], in0=pq[0:32, :], in1=cs[ci][:, 0, :], op=ALU.mult),
                 reads=[pqk, f"cs{ci}"], writes=[ok])
            p.op("dve", lambda e, o=o, t1=t1: e.tensor_tensor(out=o[0:32, :], in0=o[0:32, :], in1=t1[0:32, :], op=ALU.add),
                 reads=[ok, t1k], writes=[ok])
            p.dma("pool", outs[name][:, tsl], o[:], reads=[ok, ok + "hi"], writes=[f"d_{name}_{tt}"])
        pv, pvk = nps()
        gemm(tt, "single", 320, 128, pv, pvk)
        o, ok = nob()
        p.op("act", lambda e, o=o, pv=pv: e.copy(out=o[:], in_=pv[:]), reads=[pvk], writes=[ok, ok + "hi"])
        p.dma("pool", outs["vT"][:, tsl], o[:], reads=[ok, ok + "hi"], writes=[f"d_vT_{tt}"])
        for j, name in enumerate(("rT", "krT", "vrT")):
            pr, prk = nps()
            gemm(tt, "dual", j * 128, 128, pr, prk)
            o, ok = nob()
            p.op("act", lambda e, o=o, pr=pr: e.copy(out=o[:], in_=pr[:]), reads=[prk], writes=[ok, ok + "hi"])
            p.dma("pool", outs[name][:, tsl], o[:], reads=[ok, ok + "hi"], writes=[f"d_{name}_{tt}"])

    p.dma("pool", r32(w0[:]), r32(wl_d.rearrange("(c p) n -> p c n", p=128)), writes=["w0"])
    for c in range(16):
        for j, (lo, hi) in enumerate(((0, 96), (96, 192), (192, 448))):
            p.op("dve", lambda e, c=c, j=j, lo=lo, hi=hi: e.tensor_scalar(out=r32(wb[:, c, lo:hi]), in0=w0[:, c, lo:hi], scalar1=murow[:, c, j:j + 1], scalar2=None, op0=ALU.mult),
                 reads=["w0", "murow"], writes=["wb"])
    p.op("dve", lambda e: e.tensor_tensor(out=r32(wa[:]), in0=w0[:], in1=wb[:], op=ALU.subtract),
         reads=["w0", "wb"], writes=["wa"])
    tw = _sb(nc, st, "tw", [96, 512])
    ta = _sb(nc, st, "ta", [96, 512])
    tg = _sb(nc, st, "tg", [128, 2, 512])
    rin = [_sb(nc, st, f"rin{i}", [128, 3, 512]) for i in range(1)]
    tmp = {n: _sb(nc, st, "tmp_" + n, [128, 512]) for n in ["a", "kkr", "sq", "nrm", "rn", "u", "rk"]}
    load_h(0)
    for tt in range(ntt):
        if tt + 1 < ntt:
            load_h(tt + 1)
        tsl = slice(tt * 512, (tt + 1) * 512)
        ri = 0
        for j, name in enumerate(("rT", "krT", "vrT")):
            p.dma("sp", rin[ri][:, j, :], outs[name][:, tsl], reads=[f"d_{name}_{tt}"], writes=[f"rin{ri}_{j}"])
        R_, KR, VR = rin[ri][:, 0, :], rin[ri][:, 1, :], rin[ri][:, 2, :]
        rk_, krk, vrk = f"rin{ri}_0", f"rin{ri}_1", f"rin{ri}_2"
        pw, pwk = nps()
        gemm(tt, "dual", 0, 96, pw, pwk)
        p.op("act", lambda e, pw=pw: e.activation(out=tw[:], in_=pw[:96, :], func=AF.Tanh), reads=[pwk], writes=["tw"])
        pa, pak = nps()
        gemm(tt, "dual", 96, 96, pa, pak)
        p.op("act", lambda e, pa=pa: e.copy(out=ta[:], in_=pa[:96, :]), reads=[pak], writes=["ta"])
        for h in range(2):
            pg, pgk = nps()
            gemm(tt, "dual", 192 + h * 128, 128, pg, pgk)
            p.op("act", lambda e, pg=pg, h=h: e.activation(out=tg[:, h, :], in_=pg[:], func=AF.Sigmoid), reads=[pgk], writes=[f"tg{h}"])
        pd, pdk = nps()
        p.op("pe", lambda e, pd=pd: e.matmul(pd[:], lhsT=w2w[:], rhs=tw[:], start=True, stop=True), reads=["w2w", "tw"], writes=[pdk])
        o_w, o_wk = nob()
        p.op("act", lambda e, pd=pd: e.activation(out=tmp["sq"][:], in_=pd[:], func=AF.Sigmoid, bias=vecs[:, 0:1]), reads=[pdk, "vecs"], writes=["t_sq"])
        p.op("act", lambda e, o_w=o_w: e.activation(out=o_w[:], in_=tmp["sq"][:], func=AF.Exp, scale=-WDECAY), reads=["t_sq"], writes=[o_wk])
        p.dma("pool", outs["wdec"][:, tsl], o_w[:], reads=[o_wk])
        pa2, pa2k = nps()
        p.op("pe", lambda e, pa2=pa2: e.matmul(pa2[:], lhsT=w2a[:], rhs=ta[:], start=True, stop=True), reads=["w2a", "ta"], writes=[pa2k])
        p.op("act", lambda e, pa2=pa2: e.activation(out=tmp["a"][:], in_=pa2[:], func=AF.Sigmoid, bias=vecs[:, 1:2]), reads=[pa2k, "vecs"], writes=["t_a"])
        pg2, pg2k = nps()
        for h in range(2):
            p.op("pe", lambda e, pg2=pg2, h=h: e.matmul(pg2[:], lhsT=w2g[:, h, :], rhs=tg[:, h, :], start=(h == 0), stop=(h == 1)),
                 reads=["w2g", f"tg{h}"], writes=[pg2k])
        o_g, o_gk = nob()
        p.op("act", lambda e, o_g=o_g, pg2=pg2: e.copy(out=o_g[:], in_=pg2[:]), reads=[pg2k], writes=[o_gk])
        p.dma("pool", outs["g"][:, tsl], o_g[:], reads=[o_gk])
        p.op("dve", lambda e, KR=KR: e.tensor_scalar(out=tmp["kkr"][:], in0=KR, scalar1=vecs[:, 2:3], scalar2=None, op0=ALU.mult),
             reads=[krk, "vecs"], writes=["t_kkr"])
        p.op("pool", lambda e: e.tensor_tensor(out=tmp["sq"][:], in0=tmp["kkr"][:], in1=tmp["kkr"][:], op=ALU.mult),
             reads=["t_kkr", "t_sq"], writes=["t_sq"])
        pn, pnk = nps()
        p.op("pe", lambda e, pn=pn: e.matmul(pn[:], lhsT=blk[:], rhs=tmp["sq"][:], start=True, stop=True), reads=["blk", "t_sq"], writes=[pnk])
        p.op("act", lambda e, pn=pn: e.activation(out=tmp["nrm"][:], in_=pn[:], func=AF.Sqrt), reads=[pnk], writes=["t_nrm"])
        p.op("dve", lambda e: e.tensor_scalar(out=tmp["nrm"][:], in0=tmp["nrm"][:], scalar1=1e-12, scalar2=None, op0=ALU.max),
             reads=["t_nrm"], writes=["t_nrm"])
        p.op("dve", lambda e: e.reciprocal(out=tmp["rn"][:], in_=tmp["nrm"][:]), reads=["t_nrm"], writes=["t_rn"])
        o_n, o_nk = nob()
        p.op("dve", lambda e, o_n=o_n: e.scalar_tensor_tensor(out=o_n[:], in0=tmp["kkr"][:], scalar=-1.0, in1=tmp["rn"][:], op0=ALU.mult, op1=ALU.mult),
             reads=["t_kkr", "t_rn"], writes=[o_nk])
        p.dma("pool", outs["nkk"][:, tsl], o_n[:], reads=[o_nk])
        o_ka, o_kak = nob()
        p.op("dve", lambda e, o_n=o_n, o_ka=o_ka: e.scalar_tensor_tensor(out=o_ka[:], in0=o_n[:], scalar=-1.0, in1=tmp["a"][:], op0=ALU.mult, op1=ALU.mult),
             reads=[o_nk, "t_a"], writes=[o_kak])
        p.dma("pool", outs["kka"][:, tsl], o_ka[:], reads=[o_kak])
        p.op("dve", lambda e: e.tensor_scalar(out=tmp["u"][:], in0=tmp["a"][:], scalar1=-1.0, scalar2=vecs[:, 3:4], op0=ALU.add, op1=ALU.mult),
             reads=["t_a", "vecs"], writes=["t_u"])
        o_kt, o_ktk = nob()
        p.op("dve", lambda e, o_kt=o_kt, KR=KR: e.scalar_tensor_tensor(out=o_kt[:], in0=tmp["u"][:], scalar=1.0, in1=KR, op0=ALU.add, op1=ALU.mult),
             reads=["t_u", krk], writes=[o_ktk])
        p.dma("pool", outs["kt"][:, tsl], o_kt[:], reads=[o_ktk])
        p.op("dve", lambda e, o_kt=o_kt, R_=R_: e.scalar_tensor_tensor(out=tmp["rk"][:], in0=R_, scalar=vecs[:, 4:5], in1=o_kt[:], op0=ALU.mult, op1=ALU.mult),
             reads=[rk_, "vecs", o_ktk], writes=["t_rk"])
        pb, pbk = nps()
        p.op("pe", lambda e, pb=pb: e.matmul(pb[:], lhsT=blk[:], rhs=tmp["rk"][:], start=True, stop=True), reads=["blk", "t_rk"], writes=[pbk])
        o_b, o_bk = nob()
        p.op("dve", lambda e, o_b=o_b, pb=pb, VR=VR: e.tensor_tensor(out=o_b[:], in0=pb[:], in1=VR, op=ALU.mult),
             reads=[pbk, vrk], writes=[o_bk])
        p.dma("pool", outs["bonus"][:, tsl], o_b[:], reads=[o_bk])


def _rope_tables(T):
    half = 16
    inv = (500000.0 ** (-np.arange(half, dtype=np.float32) / half)).astype(np.float32)
    ang = np.arange(T, dtype=np.float32)[:, None] * inv[None, :]
    cos = np.cos(ang).astype(np.float32).T
    sin = np.sin(ang).astype(np.float32).T
    COS = np.concatenate([cos, cos], 0)
    SIN = np.concatenate([-sin, sin], 0)
    return np.ascontiguousarray(COS), np.ascontiguousarray(SIN)


def launch_inproj(h, I):
    T = h.shape[0]
    hTp = np.zeros((D, T + 1), np.float32)
    hTp[:, 1:] = h.T
    w_in = I["w_in"][0]
    COS, SIN = _rope_tables(T)
    swp = np.concatenate([np.arange(16, 32), np.arange(0, 16)])
    blk = np.zeros((128, 128), np.float32)
    blk[:64, :64] = 1
    blk[64:, 64:] = 1
    murow = np.stack([I["mu_w"][0], I["mu_a"][0], I["mu_g"][0]], -1).reshape(16, 128, 3).transpose(1, 0, 2)
    wl = np.concatenate([I["w_w1"][0], I["w_a1"][0], I["w_g1"][0]], 1)
    in_maps = []
    for i in range(8):
        cq = slice(i * 128, (i + 1) * 128)
        q = w_in[:, 0:1024][:, cq]
        k = w_in[:, 1024:2048][:, cq]
        v = w_in[:, 2048:3072][:, cq]
        ws = np.concatenate([q, q[:, swp], k, k[:, swp], v], 1)
        r = w_in[:, 3072:4096][:, cq]
        kr = w_in[:, 4096:5120][:, cq]
        vr = w_in[:, 5120:6144][:, cq]
        wd = np.concatenate([r, kr, vr], 1)
        mucol = np.concatenate([I["mu_r"][0][cq], I["mu_k"][0][cq], I["mu_v"][0][cq]])[None, :]
        vecs = np.stack([I["w0"][0][cq], I["a0"][0][cq], I["k_k"][0][cq], I["k_a"][0][cq], I["r_k"][0].reshape(-1)[cq]], -1)
        in_maps.append({
            "hTp": hTp, "ws": np.ascontiguousarray(ws), "wd": np.ascontiguousarray(wd), "mucol": np.ascontiguousarray(mucol),
            "wl": np.ascontiguousarray(wl), "murow": np.ascontiguousarray(murow),
            "w2w": np.ascontiguousarray(I["w_w2"][0][:, cq]), "w2a": np.ascontiguousarray(I["w_a2"][0][:, cq]),
            "w2g": np.ascontiguousarray(I["w_g2"][0][:, cq].reshape(2, 128, 128).transpose(1, 0, 2)),
            "vecs": np.ascontiguousarray(vecs), "cos": COS, "sin": SIN, "blk": blk})
    ntt = T // 512
    res = _run(lambda nc, p, st: build_inproj(nc, p, st, ntt), in_maps)
    return res


GN_EPS = 64e-5
TCH = 32


def build_rwkv(nc, p, st, T=S):
    nch = T // TCH
    bcin_d = nc.dram_tensor("bcin", [2, nch, 5, TCH, 64], F32, kind="ExternalInput").ap()
    vT_d = nc.dram_tensor("vT", [128, T], F32, kind="ExternalInput").ap()
    g_d = nc.dram_tensor("g", [128, T], F32, kind="ExternalInput").ap()
    bonus_d = nc.dram_tensor("bonus", [128, T], F32, kind="ExternalInput").ap()
    gnv_d = nc.dram_tensor("gnv", [128, 2], F32, kind="ExternalInput").ap()
    sel_d = nc.dram_tensor("sel", [128, 128], F32, kind="ExternalInput").ap()
    blk_d = nc.dram_tensor("blk", [128, 128], F32, kind="ExternalInput").ap()
    o_d = nc.dram_tensor("o", [128, T], F32, kind="ExternalOutput").ap()
    r32 = lambda ap: ap.bitcast(F32R)

    vT = _sb(nc, st, "vT_sb", [128, T])
    yT = _sb(nc, st, "yT_sb", [128, T])
    Sst = _sb(nc, st, "S_sb", [128, 64])
    junk = _sb(nc, st, "junk", [128, 64])
    sa = _sb(nc, st, "sa", [128, 1])
    sel = _sb(nc, st, "sel_sb", [128, 128])
    blk = _sb(nc, st, "blk_sb", [128, 128])
    gnv = _sb(nc, st, "gnv_sb", [128, 2])
    bc = [_sb(nc, st, f"bc{i}", [128, 5, TCH, 64]) for i in range(2)]
    ps = [_ps(nc, st, f"ps{i}") for i in range(8)]
    p.dma("pool", r32(sel[:]), r32(sel_d), writes=["sel"])
    p.dma("sp", blk[:], blk_d, writes=["blk"])
    p.dma("sp", gnv[:], gnv_d, writes=["gnv"])
    p.dma("sp", vT[:], vT_d, writes=["vT"])
    p.op("dve", lambda e: e.memset(Sst[:], 0.0), writes=["S"])
    zer_d = nc.dram_tensor("zer", [126, 5, TCH, 64], F32, kind="ExternalInput").ap()
    for i in range(2):
        p.dma("pool", r32(bc[i][2:128]), r32(zer_d), writes=[f"bc{i}"])

    def load_bc(c):
        i = c % 2
        p.dma("pool", r32(bc[i][0:2]), r32(bcin_d[:, c]), writes=[f"bc{i}"])

    load_bc(0)
    grp = 0
    for c in range(nch):
        if c + 1 < nch:
            load_bc(c + 1)
        bi = c % 2
        for g4 in range(TCH // 4):
            base = (grp % 2) * 3
            grp += 1
            views = []
            for j in range(5):
                bank = ps[base + j // 2]
                bk = f"ps{base + j // 2}"
                half = bank[:, (j % 2) * 256:(j % 2) * 256 + 256]
                p.op("pe", lambda e, half=half, j=j, g4=g4, bi=bi: e.matmul(half, lhsT=r32(sel[:]), rhs=r32(bc[bi][:, j, g4 * 4:(g4 + 1) * 4, :]), start=True, stop=True),
                     reads=["sel", f"bc{bi}"], writes=[bk + f"h{j%2}"])
                views.append((half, bk + f"h{j%2}"))
            for tl in range(4):
                t = c * TCH + g4 * 4 + tl
                cs = slice(tl * 64, (tl + 1) * 64)
                wv, nv, kav, ktv, rv = [(v[0][:, cs], v[1]) for v in views]
                p.op("dve", lambda e, nv=nv: e.scalar_tensor_tensor(out=junk[:], in0=Sst[:], scalar=1.0, in1=nv[0], op0=ALU.mult, op1=ALU.mult, accum_out=sa[:]),
                     reads=["S", nv[1]], writes=["junk", "sa"])
                p.op("dve", lambda e, wv=wv: e.tensor_tensor(out=Sst[:], in0=Sst[:], in1=wv[0], op=ALU.mult),
                     reads=["S", wv[1]], writes=["S"])
                p.op("dve", lambda e, kav=kav: e.scalar_tensor_tensor(out=Sst[:], in0=kav[0], scalar=sa[:, 0:1], in1=Sst[:], op0=ALU.mult, op1=ALU.add),
                     reads=["S", "sa", kav[1]], writes=["S"], force=True)
                p.op("dve", lambda e, ktv=ktv, t=t: e.scalar_tensor_tensor(out=Sst[:], in0=ktv[0], scalar=vT[:, t:t + 1], in1=Sst[:], op0=ALU.mult, op1=ALU.add),
                     reads=["S", "vT", ktv[1]], writes=["S"])
                p.op("dve", lambda e, rv=rv, t=t: e.scalar_tensor_tensor(out=junk[:], in0=Sst[:], scalar=1.0, in1=rv[0], op0=ALU.mult, op1=ALU.mult, accum_out=yT[:, t:t + 1]),
                     reads=["S", rv[1]], writes=["junk", f"yT{t // 512}"])
    gt = [_sb(nc, st, f"g_sb{i}", [128, 512]) for i in range(2)]
    bt = [_sb(nc, st, f"b_sb{i}", [128, 512]) for i in range(2)]
    yc = _sb(nc, st, "yc", [128, 512])
    sq = _sb(nc, st, "sq", [128, 512])
    rs = _sb(nc, st, "rs", [128, 512])
    ot = [_sb(nc, st, f"ot{i}", [128, 512]) for i in range(2)]
    for tt in range(T // 512):
        i = tt % 2
        tsl = slice(tt * 512, (tt + 1) * 512)
        p.dma("sp", gt[i][:], g_d[:, tsl], writes=[f"gt{i}"])
        p.dma("sp", bt[i][:], bonus_d[:, tsl], writes=[f"bt{i}"])
        pm, pmk = ps[6], "ps6"
        p.op("pe", lambda e, tsl=tsl: e.matmul(ps[6][:], lhsT=blk[:], rhs=yT[:, tsl], start=True, stop=True), reads=["blk", f"yT{tt}"], writes=["ps6"])
        p.op("dve", lambda e, tsl=tsl: e.scalar_tensor_tensor(out=yc[:], in0=ps[6][:], scalar=-1.0 / 64, in1=yT[:, tsl], op0=ALU.mult, op1=ALU.add),
             reads=["ps6", f"yT{tt}"], writes=["yc"])
        p.op("act", lambda e: e.activation(out=sq[:], in_=yc[:], func=AF.Square), reads=["yc"], writes=["sq"])
        p.op("pe", lambda e: e.matmul(ps[7][:], lhsT=blk[:], rhs=sq[:], start=True, stop=True), reads=["blk", "sq"], writes=["ps7"])
        p.op("dve", lambda e: e.tensor_scalar(out=rs[:], in0=ps[7][:], scalar1=1.0 / 64, scalar2=GN_EPS, op0=ALU.mult, op1=ALU.add), reads=["ps7"], writes=["rs"])
        p.op("act", lambda e: e.activation(out=rs[:], in_=rs[:], func=AF.Sqrt), reads=["rs"], writes=["rs"])
        p.op("dve", lambda e: e.reciprocal(out=rs[:], in_=rs[:]), reads=["rs"], writes=["rs"])
        p.op("dve", lambda e: e.tensor_tensor(out=yc[:], in0=yc[:], in1=rs[:], op=ALU.mult), reads=["yc", "rs"], writes=["yc"])
        p.op("dve", lambda e: e.tensor_scalar(out=yc[:], in0=yc[:], scalar1=gnv[:, 0:1], scalar2=gnv[:, 1:2], op0=ALU.mult, op1=ALU.add), reads=["yc", "gnv"], writes=["yc"])
        p.op("dve", lambda e, i=i: e.tensor_tensor(out=yc[:], in0=yc[:], in1=bt[i][:], op=ALU.add), reads=["yc", f"bt{i}"], writes=["yc"])
        p.op("dve", lambda e, i=i: e.tensor_tensor(out=ot[i][:], in0=yc[:], in1=gt[i][:], op=ALU.mult), reads=["yc", f"gt{i}"], writes=[f"ot{i}"])
        p.dma("sp", o_d[:, tsl], ot[i][:], reads=[f"ot{i}"])


def launch_rwkv(lc, I, T=S):
    sel = np.zeros((128, 128), np.float32)
    sel[0, :64] = 1
    sel[1, 64:] = 1
    blk = np.zeros((128, 128), np.float32)
    blk[:64, :64] = 1
    blk[64:, 64:] = 1
    in_maps = []
    nch = T // TCH
    for i in range(8):
        cq = slice(i * 128, (i + 1) * 128)
        q5 = np.stack([lc[i][n][:, :T] for n in ("wdec", "nkk", "kka", "kt", "rT")], 0)
        q5 = q5.reshape(5, 2, 64, nch, TCH).transpose(1, 3, 0, 4, 2)
        gnv = np.stack([I["gn_w"][0][cq], I["gn_b"][0][cq]], -1)
        in_maps.append({"bcin": np.ascontiguousarray(q5), "vT": np.ascontiguousarray(lc[i]["vrT"][:, :T]),
                        "g": np.ascontiguousarray(lc[i]["g"][:, :T]), "bonus": np.ascontiguousarray(lc[i]["bonus"][:, :T]),
                        "gnv": np.ascontiguousarray(gnv), "sel": sel, "blk": blk, "zer": np.zeros((126, 5, TCH, 64), np.float32)})
    res = _run(lambda nc, p, st: build_rwkv(nc, p, st, T), in_maps)
    return np.concatenate([r["o"].T for r in res], axis=1)


NEGB = 30000.0


def build_moba(nc, p, st, T=S):
    nb = T // 256
    nkt = T // 128
    qT_d = nc.dram_tensor("qT", [128, T], F32, kind="ExternalInput").ap()
    kT_d = nc.dram_tensor("kT", [128, T], F32, kind="ExternalInput").ap()
    va_d = nc.dram_tensor("va", [128, nkt, 129], F32, kind="ExternalInput").ap()
    E_d = nc.dram_tensor("E", [32, T], F32, kind="ExternalInput").ap()
    cm_d = nc.dram_tensor("cm", [128, 128], F32, kind="ExternalInput").ap()
    id_d = nc.dram_tensor("ident", [128, 128], F32, kind="ExternalInput").ap()
    o_d = nc.dram_tensor("o", [T, 128], F32, kind="ExternalOutput").ap()
    r32 = lambda ap: ap.bitcast(F32R)
    qT = _sb(nc, st, "qT_sb", [128, T])
    kT = _sb(nc, st, "kT_sb", [128, T])
    va = _sb(nc, st, "va_sb", [128, nkt, 129])
    E = _sb(nc, st, "E_sb", [32, T])
    cm = _sb(nc, st, "cm_sb", [128, 128])
    ident = _sb(nc, st, "id_sb", [128, 128])
    kmean = _sb(nc, st, "kmean", [128, 32])
    gsb = _sb(nc, st, "gsb", [128, 32])
    m8 = _sb(nc, st, "m8", [128, 8])
    bias = _sb(nc, st, "bias", [128, 32])
    biasT = [_sb(nc, st, f"biasT{i}", [32, 256]) for i in range(2)]
    pT = [_sb(nc, st, f"pT{i}", [128, 256]) for i in range(3)]
    osb = [_sb(nc, st, f"osb{i}", [128, 128]) for i in range(2)]
    rden = _sb(nc, st, "rden", [128, 2])
    s_ps = [_ps(nc, st, f"s_ps{i}") for i in range(2)]
    o_ps = [_ps(nc, st, f"o_ps{i}") for i in range(4)]
    g_ps = _ps(nc, st, "g_ps")
    t_ps = _ps(nc, st, "t_ps")
    p.dma("pool", r32(qT[:]), r32(qT_d), writes=["qT"])
    p.dma("pool", r32(kT[:]), r32(kT_d), writes=["kT"])
    p.dma("sp", va[:], va_d, writes=["va"])
    p.dma("sp", E[:], E_d, writes=["E"])
    p.dma("sp", cm[:], cm_d, writes=["cm"])
    p.dma("sp", ident[:], id_d, writes=["ident"])
    p.op("dve", lambda e: e.tensor_reduce(out=kmean[:, 0:nb], in_=kT[:].rearrange("p (n k) -> p n k", k=256), axis=AX.X, op=ALU.add),
         reads=["kT"], writes=["kmean"])
    p.op("dve", lambda e: e.tensor_scalar(out=kmean[:, 0:nb], in0=kmean[:, 0:nb], scalar1=1.0 / 256, scalar2=None, op0=ALU.mult),
         reads=["kmean"], writes=["kmean"])
    p.op("dve", lambda e: e.memset(gsb[:], -1e30), writes=["gsb"])
    scale = 128 ** -0.5
    pti = 0
    si = 0
    for b in range(nb):
        bT = biasT[b % 2]
        bTk = f"biasT{b % 2}"
        for j in range(2):
            qs = slice(b * 256 + j * 128, b * 256 + (j + 1) * 128)
            if b > 3:
                p.op("pe", lambda e, qs=qs: e.matmul(g_ps[:, 0:nb], lhsT=qT[:, qs], rhs=kmean[:, 0:nb], start=True, stop=True),
                     reads=["qT", "kmean"], writes=["g_ps"])
                p.op("dve", lambda e, b=b: e.tensor_copy(out=gsb[:, 0:b], in_=g_ps[:, 0:b]), reads=["g_ps", "gsb"], writes=["gsb"])
                p.op("dve", lambda e: e.max(out=m8[:], in_=gsb[:]), reads=["gsb"], writes=["m8"])
                p.op("dve", lambda e: e.tensor_scalar(out=bias[:], in0=gsb[:], scalar1=m8[:, 2:3], scalar2=None, op0=ALU.is_ge),
                     reads=["gsb", "m8"], writes=["bias"], force=True)
                p.op("dve", lambda e: e.tensor_scalar(out=bias[:], in0=bias[:], scalar1=NEGB, scalar2=-NEGB, op0=ALU.mult, op1=ALU.add),
                     reads=["bias"], writes=["bias"])
            else:
                p.op("dve", lambda e: e.memset(bias[:], -NEGB), reads=["bias"], writes=["bias"])
                if b > 0:
                    p.op("dve", lambda e, b=b: e.memset(bias[:, 0:b], 0.0), reads=["bias"], writes=["bias"])
            p.op("dve", lambda e, b=b: e.memset(bias[:, b:b + 1], 0.0), reads=["bias"], writes=["bias"])
            p.op("pe", lambda e: e.transpose(t_ps[0:32, 0:128], bias[:], ident[:]), reads=["bias", "ident"], writes=["t_ps"])
            p.op("act", lambda e, bT=bT, j=j: e.copy(out=bT[:, j * 128:(j + 1) * 128], in_=t_ps[0:32, 0:128]), reads=["t_ps"], writes=[bTk])
        ob = (b % 2) * 2
        for kt in range(2 * b + 2):
            sp_ = s_ps[si % 2]
            spk = f"s_ps{si % 2}"
            si += 1
            ks = slice(kt * 128, (kt + 1) * 128)
            own = kt >= 2 * b
            p.op("pe", lambda e, sp_=sp_, ks=ks, b=b: e.matmul(sp_[:, 0:256], lhsT=r32(kT[:, ks]), rhs=r32(qT[:, b * 256:(b + 1) * 256]), start=True, stop=False),
                 reads=["kT", "qT"], writes=[spk])
            p.op("pe", lambda e, sp_=sp_, ks=ks, bT=bT, own=own: e.matmul(sp_[:, 0:256], lhsT=E[:, ks], rhs=bT[:], start=False, stop=(not own)),
                 reads=["E", bTk], writes=[spk])
            if own:
                half = 0 if kt == 2 * b else 1
                p.op("pe", lambda e, sp_=sp_, half=half: e.matmul(sp_[:, half * 128:(half + 1) * 128], lhsT=ident[:], rhs=cm[:], start=False, stop=True),
                     reads=["ident", "cm"], writes=[spk])
            pt = pT[pti % 3]
            ptk = f"pT{pti % 3}"
            pti += 1
            p.op("act", lambda e, pt=pt, sp_=sp_: e.activation(out=pt[:], in_=sp_[:, 0:256], func=AF.Exp, scale=scale), reads=[spk], writes=[ptk])
            for j in range(2):
                if kt == 2 * b + 1 and j == 0:
                    continue
                last = (kt == 2 * b) if j == 0 else (kt == 2 * b + 1)
                p.op("pe", lambda e, pt=pt, j=j, kt=kt, last=last, ob=ob: e.matmul(o_ps[ob + j][:, 0:129], lhsT=pt[:, j * 128:(j + 1) * 128], rhs=va[:, kt, :], start=(kt == 0), stop=last),
                     reads=[ptk, "va"], writes=[f"o_ps{ob + j}"])
        for j in range(2):
            op_ = o_ps[ob + j]
            p.op("dve", lambda e, op_=op_, j=j: e.reciprocal(out=rden[:, j:j + 1], in_=op_[:, 128:129]), reads=[f"o_ps{ob + j}"], writes=[f"rden{j}"])
            p.op("act", lambda e, op_=op_, j=j: e.activation(out=osb[j][:], in_=op_[:, 0:128], func=AF.Copy, scale=rden[:, j:j + 1]),
                 reads=[f"o_ps{ob + j}", f"rden{j}"], writes=[f"osb{j}"])
            p.dma("sp", o_d[b * 256 + j * 128:b * 256 + (j + 1) * 128, :], osb[j][:], reads=[f"osb{j}"])


def launch_moba(lc, T=S):
    nkt = T // 128
    E = np.zeros((32, T), np.float32)
    for n in range(T // 256):
        E[n, n * 256:(n + 1) * 256] = 1
    kk = np.arange(128)
    cm = np.where(kk[:, None] <= kk[None, :], 0.0, -NEGB).astype(np.float32)
    ident = np.eye(128, dtype=np.float32)
    in_maps = []
    for i in range(8):
        v = lc[i]["vT"][:, :T].T
        va = np.concatenate([v, np.ones((T, 1), np.float32)], 1).reshape(nkt, 128, 129).transpose(1, 0, 2)
        in_maps.append({"qT": np.ascontiguousarray(lc[i]["qT"][:, :T]), "kT": np.ascontiguousarray(lc[i]["kT"][:, :T]),
                        "va": np.ascontiguousarray(va), "E": E, "cm": cm, "ident": ident})
    res = _run(lambda nc, p, st: build_moba(nc, p, st, T), in_maps)
    return np.concatenate([r["o"] for r in res], axis=1)


def build_merge(nc, p, st, nunit=2):
    TT = 512
    NT = nunit * TT
    hT_d = nc.dram_tensor("hT", [D, NT], F32, kind="ExternalInput").ap()
    oa_d = nc.dram_tensor("oaT", [1024, NT], F32, kind="ExternalInput").ap()
    or_d = nc.dram_tensor("orT", [1024, NT], F32, kind="ExternalInput").ap()
    wg_d = nc.dram_tensor("wg", [D, 4096], F32, kind="ExternalInput").ap()
    wua_d = nc.dram_tensor("wua", [1024, D], F32, kind="ExternalInput").ap()
    wur_d = nc.dram_tensor("wur", [1024, D], F32, kind="ExternalInput").ap()
    wo_d = nc.dram_tensor("wo", [D, D], F32, kind="ExternalInput").ap()
    y_d = nc.dram_tensor("yT", [D, NT], F32, kind="ExternalOutput").ap()
    r32 = lambda ap: ap.bitcast(F32R)
    hT = _sb(nc, st, "hT_sb", [128, 16, TT])
    oa = _sb(nc, st, "oa_sb", [128, 8, TT])
    orr = _sb(nc, st, "or_sb", [128, 8, TT])
    mix = _sb(nc, st, "mix_sb", [128, 16, TT])
    wb = [_sb(nc, st, f"wb{i}", [128, 48, 128]) for i in range(2)]
    wob = [_sb(nc, st, f"wob{i}", [128, 16, 128]) for i in range(2)]
    sg = [_sb(nc, st, f"sg{i}", [128, TT]) for i in range(2)]
    m12 = [_sb(nc, st, f"m12_{i}", [128, TT]) for i in range(2)]
    yo = [_sb(nc, st, f"yo{i}", [128, TT]) for i in range(2)]
    ps = [_ps(nc, st, f"ps{i}") for i in range(8)]
    wgv = wg_d.rearrange("(c p) n -> p c n", p=128)
    wuav = wua_d.rearrange("(c p) n -> p c n", p=128)
    wurv = wur_d.rearrange("(c p) n -> p c n", p=128)
    wov = wo_d.rearrange("(c p) n -> p c n", p=128)
    wcount = 0
    for u in range(nunit):
        ts_ = slice(u * TT, (u + 1) * TT)
        p.dma("pool", r32(hT[:]), r32(hT_d.rearrange("(c p) t -> p c t", p=128)[:, :, ts_]), writes=["hT"])
        p.dma("pool", r32(oa[:]), r32(oa_d.rearrange("(c p) t -> p c t", p=128)[:, :, ts_]), writes=["oa"])
        p.dma("pool", r32(orr[:]), r32(or_d.rearrange("(c p) t -> p c t", p=128)[:, :, ts_]), writes=["or"])
        for n in range(16):
            wi = wcount % 2
            wcount += 1
            W = wb[wi]
            wk = f"wb{wi}"
            ns = slice(n * 128, (n + 1) * 128)
            ns2 = slice(2048 + n * 128, 2048 + (n + 1) * 128)
            p.dma("pool", r32(W[:, 0:8, :]), r32(wgv[:, 0:8, ns]), writes=[wk + "a"])
            p.dma("pool", r32(W[:, 8:16, :]), r32(wgv[:, 8:16, ns]), writes=[wk + "b"])
            p.dma("pool", r32(W[:, 16:24, :]), r32(wgv[:, 0:8, ns2]), writes=[wk + "c"])
            p.dma("pool", r32(W[:, 24:32, :]), r32(wgv[:, 8:16, ns2]), writes=[wk + "d"])
            p.dma("pool", r32(W[:, 32:40, :]), r32(wuav[:, :, ns]), writes=[wk + "e"])
            p.dma("pool", r32(W[:, 40:48, :]), r32(wurv[:, :, ns]), writes=[wk + "f"])
            wkeys = [wk + x for x in "abcdef"]
            b0 = (n % 2) * 4
            pga, pgr, pua, pur = ps[b0], ps[b0 + 1], ps[b0 + 2], ps[b0 + 3]
            for c in range(16):
                p.op("pe", lambda e, c=c, W=W, pga=pga: e.matmul(pga[:], lhsT=r32(W[:, c, :]), rhs=r32(hT[:, c, :]), start=(c == 0), stop=(c == 15)),
                     reads=wkeys + ["hT"], writes=[f"ps{b0}"])
            for c in range(16):
                p.op("pe", lambda e, c=c, W=W, pgr=pgr: e.matmul(pgr[:], lhsT=r32(W[:, 16 + c, :]), rhs=r32(hT[:, c, :]), start=(c == 0), stop=(c == 15)),
                     reads=wkeys + ["hT"], writes=[f"ps{b0 + 1}"])
            for c in range(8):
                p.op("pe", lambda e, c=c, W=W, pua=pua: e.matmul(pua[:], lhsT=r32(W[:, 32 + c, :]), rhs=r32(oa[:, c, :]), start=(c == 0), stop=(c == 7)),
                     reads=wkeys + ["oa"], writes=[f"ps{b0 + 2}"])
            for c in range(8):
                p.op("pe", lambda e, c=c, W=W, pur=pur: e.matmul(pur[:], lhsT=r32(W[:, 40 + c, :]), rhs=r32(orr[:, c, :]), start=(c == 0), stop=(c == 7)),
                     reads=wkeys + ["or"], writes=[f"ps{b0 + 3}"])
            p.op("act", lambda e, pga=pga: e.activation(out=sg[0][:], in_=pga[:], func=AF.Sigmoid), reads=[f"ps{b0}"], writes=["sg0"])
            p.op("act", lambda e, pgr=pgr: e.activation(out=sg[1][:], in_=pgr[:], func=AF.Sigmoid), reads=[f"ps{b0 + 1}"], writes=["sg1"])
            p.op("dve", lambda e, pua=pua: e.tensor_tensor(out=m12[0][:], in0=pua[:], in1=sg[0][:], op=ALU.mult), reads=[f"ps{b0 + 2}", "sg0"], writes=["m0"])
            p.op("dve", lambda e, pur=pur: e.tensor_tensor(out=m12[1][:], in0=pur[:], in1=sg[1][:], op=ALU.mult), reads=[f"ps{b0 + 3}", "sg1"], writes=["m1"])
            p.op("dve", lambda e, n=n: e.tensor_tensor(out=r32(mix[:, n, :]), in0=m12[0][:], in1=m12[1][:], op=ALU.add), reads=["m0", "m1"], writes=[f"mix{n}"])
        for m in range(16):
            wi = m % 2
            ms = slice(m * 128, (m + 1) * 128)
            p.dma("pool", r32(wob[wi][:, 0:8, :]), r32(wov[:, 0:8, ms]), writes=[f"wob{wi}a"])
            p.dma("pool", r32(wob[wi][:, 8:16, :]), r32(wov[:, 8:16, ms]), writes=[f"wob{wi}b"])
            py = ps[m % 2]
            for c in range(16):
                p.op("pe", lambda e, c=c, wi=wi, py=py: e.matmul(py[:], lhsT=r32(wob[wi][:, c, :]), rhs=r32(mix[:, c, :]), start=(c == 0), stop=(c == 15)),
                     reads=[f"wob{wi}a", f"wob{wi}b", f"mix{c}"], writes=[f"ps{m % 2}"])
            p.op("act", lambda e, py=py, wi=wi: e.copy(out=yo[wi][:], in_=py[:]), reads=[f"ps{m % 2}"], writes=[f"yo{wi}"])
            p.dma("pool", y_d[ms, ts_], yo[wi][:], reads=[f"yo{wi}"])


def launch_merge(h1, o_att, o_rwkv, I):
    wg = np.ascontiguousarray(I["w_in"][0][:, 6144:10240])
    in_maps = []
    for i in range(8):
        ts_ = slice(i * 1024, (i + 1) * 1024)
        in_maps.append({"hT": np.ascontiguousarray(h1[ts_].T), "oaT": np.ascontiguousarray(o_att[ts_].T),
                        "orT": np.ascontiguousarray(o_rwkv[ts_].T), "wg": wg,
                        "wua": I["w_up_att"][0], "wur": I["w_up_rwkv"][0], "wo": I["w_o"][0]})
    res = _run(lambda nc, p, st: build_merge(nc, p, st, 2), in_maps)
    return np.concatenate([r["yT"].T for r in res], axis=0)


def build_router(nc, p, st, ntile=8):
    NT = ntile * 128
    hT_d = nc.dram_tensor("hT", [D, NT], F32, kind="ExternalInput").ap()
    wr_d = nc.dram_tensor("wr", [D, 72], F32, kind="ExternalInput").ap()
    br_d = nc.dram_tensor("br", [1, 72], F32, kind="ExternalInput").ap()
    o_d = nc.dram_tensor("o", [NT, 4], F32, kind="ExternalOutput").ap()
    hT = _sb(nc, st, "hT_sb", [128, 16, NT])
    wr = _sb(nc, st, "wr_sb", [128, 16, 72])
    br = _sb(nc, st, "br_sb", [128, 72])
    ps = [_ps(nc, st, f"ps{i}") for i in range(2)]
    p.dma("sp", hT[:], hT_d.rearrange("(c p) t -> p c t", p=128), writes=["hT"])
    p.dma("sp", wr[:], wr_d.rearrange("(c p) n -> p c n", p=128), writes=["wr"])
    p.dma("sp", br[:], br_d.partition_broadcast(128), writes=["br"])
    l_sb = _sb(nc, st, "l_sb", [128, 72])
    lem = _sb(nc, st, "lem", [128, 64])
    m8g = _sb(nc, st, "m8g", [128, 8])
    m8e = _sb(nc, st, "m8e", [128, 8])
    idx = _sb(nc, st, "idx", [128, 8], U32)
    sm = _sb(nc, st, "sm", [128, 8])
    junk = _sb(nc, st, "junk", [128, 8])
    pen = _sb(nc, st, "pen", [128, 8])
    res = [_sb(nc, st, f"res{i}", [128, 4]) for i in range(2)]
    for t in range(ntile):
        pt = ps[t % 2]
        ptk = f"ps{t % 2}"
        rs_ = res[t % 2]
        rk = f"res{t % 2}"
        for c in range(16):
            p.op("pe", lambda e, c=c, t=t, pt=pt: e.matmul(pt[:, 0:72], lhsT=hT[:, c, t * 128:(t + 1) * 128], rhs=wr[:, c, :], start=(c == 0), stop=(c == 15)),
                 reads=["hT", "wr"], writes=[ptk])
        p.op("dve", lambda e, pt=pt: e.tensor_tensor(out=l_sb[:], in0=pt[:, 0:72], in1=br[:], op=ALU.add), reads=[ptk, "br"], writes=["l"])
        p.op("dve", lambda e: e.max(out=m8g[:], in_=l_sb[:, 0:8]), reads=["l"], writes=["m8g"])
        p.op("dve", lambda e: e.tensor_scalar(out=sm[:, 0:1], in0=m8g[:, 0:1], scalar1=-1.0, scalar2=None, op0=ALU.mult), reads=["m8g"], writes=["sm0"])
        p.op("act", lambda e: e.activation(out=junk[:], in_=l_sb[:, 0:8], func=AF.Exp, bias=sm[:, 0:1], accum_out=sm[:, 1:2]), reads=["l", "sm0"], writes=["junk", "sm1"])
        p.op("dve", lambda e: e.reciprocal(out=sm[:, 2:3], in_=sm[:, 1:2]), reads=["sm1"], writes=["sm2"])
        p.op("dve", lambda e: e.tensor_scalar(out=pen[:], in0=l_sb[:, 0:8], scalar1=m8g[:, 0:1], scalar2=None, op0=ALU.is_ge), reads=["l", "m8g"], writes=["pen"], force=True)
        p.op("dve", lambda e: e.tensor_scalar(out=pen[:], in0=pen[:], scalar1=1e30, scalar2=-1e30, op0=ALU.mult, op1=ALU.add), reads=["pen"], writes=["pen"])
        for g in range(8):
            p.op("dve", lambda e, g=g: e.tensor_scalar(out=lem[:, g * 8:(g + 1) * 8], in0=l_sb[:, 8 + g * 8:16 + g * 8], scalar1=pen[:, g:g + 1], scalar2=None, op0=ALU.add),
                 reads=["l", "pen"], writes=["lem"], force=(g == 0))
        p.op("dve", lambda e: e.max(out=m8e[:], in_=lem[:]), reads=["lem"], writes=["m8e"])
        p.op("dve", lambda e: e.max_index(out=idx[:], in_max=m8e[:], in_values=lem[:]), reads=["m8e", "lem"], writes=["idx"], force=True)
        p.op("dve", lambda e, rs_=rs_: e.tensor_copy(out=rs_[:, 0:2], in_=idx[:, 0:2]), reads=["idx"], writes=[rk + "a"], force=True)
        p.op("dve", lambda e: e.tensor_tensor(out=sm[:, 3:4], in0=m8e[:, 0:1], in1=m8e[:, 1:2], op=ALU.subtract), reads=["m8e"], writes=["sm3"], force=True)
        p.op("act", lambda e: e.activation(out=sm[:, 4:5], in_=sm[:, 3:4], func=AF.Sigmoid), reads=["sm3"], writes=["sm4"])
        p.op("dve", lambda e, rs_=rs_: e.tensor_tensor(out=rs_[:, 2:3], in0=sm[:, 4:5], in1=sm[:, 2:3], op=ALU.mult), reads=["sm4", "sm2"], writes=[rk + "b"], force=True)
        p.op("dve", lambda e, rs_=rs_: e.tensor_tensor(out=rs_[:, 3:4], in0=sm[:, 2:3], in1=rs_[:, 2:3], op=ALU.subtract), reads=["sm2", rk + "b"], writes=[rk + "c"], force=True)
        p.dma("sp", o_d[t * 128:(t + 1) * 128, :], rs_[:], reads=[rk + "a", rk + "b", rk + "c"])


def launch_router(h2, I):
    wr = np.ascontiguousarray(np.concatenate([I["w_rg"][0], I["w_re"][0]], 1))
    br = np.ascontiguousarray(np.concatenate([I["b_rg"][0], I["b_re"][0]])[None, :])
    in_maps = [{"hT": np.ascontiguousarray(h2[i * 1024:(i + 1) * 1024].T), "wr": wr, "br": br} for i in range(8)]
    res = _run(lambda nc, p, st: build_router(nc, p, st, 8), in_maps)
    o = np.concatenate([r["o"] for r in res], axis=0)
    return o[:, 0:2].astype(np.int64), o[:, 2:4]


def build_experts(nc, p, st, cap):
    xT_d = nc.dram_tensor("xT", [8, D, cap], F32, kind="ExternalInput").ap()
    wg_d = nc.dram_tensor("wg", [8, D, 512], F32, kind="ExternalInput").ap()
    wu_d = nc.dram_tensor("wu", [8, D, 512], F32, kind="ExternalInput").ap()
    wd_d = nc.dram_tensor("wd", [8, 512, D], F32, kind="ExternalInput").ap()
    y_d = nc.dram_tensor("yT", [8, D, cap], F32, kind="ExternalOutput").ap()
    r32 = lambda ap: ap.bitcast(F32R)
    xT = _sb(nc, st, "xT_sb", [128, 16, cap])
    Wg = _sb(nc, st, "Wg_sb", [128, 16, 512])
    Wu = _sb(nc, st, "Wu_sb", [128, 16, 512])
    Wd = _sb(nc, st, "Wd_sb", [128, 4, D])
    hid = _sb(nc, st, "hid_sb", [128, 4, cap])
    sg = [_sb(nc, st, f"sg{i}", [128, cap]) for i in range(2)]
    yo = [_sb(nc, st, f"yo{i}", [128, cap]) for i in range(3)]
    ps = [_ps(nc, st, f"ps{i}") for i in range(8)]
    for ex in range(8):
        xv = xT_d[ex].rearrange("(c p) t -> p c t", p=128)
        for h in range(4):
            p.dma("pool", r32(xT[:, h * 4:(h + 1) * 4, :]), r32(xv[:, h * 4:(h + 1) * 4, :]), writes=[f"xT{h}"])
        gv = wg_d[ex].rearrange("(c p) n -> p c n", p=128)
        uv = wu_d[ex].rearrange("(c p) n -> p c n", p=128)
        dv = wd_d[ex].rearrange("(c p) n -> p c n", p=128)
        for h in range(8):
            p.dma("pool", r32(Wg[:, h * 2:(h + 1) * 2, :]), r32(gv[:, h * 2:(h + 1) * 2, :]), writes=[f"Wg{h}"])
        for h in range(8):
            p.dma("pool", r32(Wu[:, h * 2:(h + 1) * 2, :]), r32(uv[:, h * 2:(h + 1) * 2, :]), writes=[f"Wu{h}"])
        for h in range(4):
            p.dma("pool", r32(Wd[:, h, 0:1024]), r32(dv[:, h, 0:1024]), writes=[f"Wd{h}a"])
            p.dma("pool", r32(Wd[:, h, 1024:2048]), r32(dv[:, h, 1024:2048]), writes=[f"Wd{h}b"])
        for f in range(4):
            pg = ps[(f % 2) * 2]
            pu = ps[(f % 2) * 2 + 1]
            pgk = f"ps{(f % 2) * 2}"
            puk = f"ps{(f % 2) * 2 + 1}"
            fs = slice(f * 128, (f + 1) * 128)
            for c in range(16):
                p.op("pe", lambda e, c=c, pg=pg, fs=fs: e.matmul(pg[:, 0:cap], lhsT=r32(Wg[:, c, fs]), rhs=r32(xT[:, c, :]), start=(c == 0), stop=(c == 15)),
                     reads=[f"Wg{c // 2}", f"xT{c // 4}"], writes=[pgk])
            for c in range(16):
                p.op("pe", lambda e, c=c, pu=pu, fs=fs: e.matmul(pu[:, 0:cap], lhsT=r32(Wu[:, c, fs]), rhs=r32(xT[:, c, :]), start=(c == 0), stop=(c == 15)),
                     reads=[f"Wu{c // 2}", f"xT{c // 4}"], writes=[puk])
            s_ = sg[f % 2]
            p.op("act", lambda e, s_=s_, pg=pg: e.activation(out=s_[:], in_=pg[:, 0:cap], func=AF.Silu), reads=[pgk], writes=[f"sg{f % 2}"])
            p.op("dve", lambda e, s_=s_, pu=pu, f=f: e.tensor_tensor(out=r32(hid[:, f, :]), in0=pu[:, 0:cap], in1=s_[:], op=ALU.mult),
                 reads=[puk, f"sg{f % 2}"], writes=[f"hid{f}"])
        for d in range(16):
            py = ps[4 + d % 4]
            pyk = f"ps{4 + d % 4}"
            ds_ = slice(d * 128, (d + 1) * 128)
            for f in range(4):
                p.op("pe", lambda e, f=f, py=py, ds_=ds_: e.matmul(py[:, 0:cap], lhsT=r32(Wd[:, f, ds_]), rhs=r32(hid[:, f, :]), start=(f == 0), stop=(f == 3)),
                     reads=[f"Wd{f}a", f"Wd{f}b", f"hid{f}"], writes=[pyk])
            yb = yo[d % 3]
            p.op("act" if d % 2 else "dve", (lambda e, yb=yb, py=py: e.copy(out=yb[:], in_=py[:, 0:cap])) if d % 2 else (lambda e, yb=yb, py=py: e.tensor_copy(out=yb[:], in_=py[:, 0:cap])),
                 reads=[pyk], writes=[f"yo{d % 3}"])
            p.dma("sp", y_d[ex, ds_, :], yb[:], reads=[f"yo{d % 3}"])


def launch_experts(h2, eidx, I):
    N = h2.shape[0]
    flat_e = eidx.reshape(-1)
    flat_t = np.repeat(np.arange(N), 2)
    order = np.argsort(flat_e, kind="stable")
    counts = np.bincount(flat_e, minlength=64)
    cap = int(max(256, -(-counts.max() // 128) * 128))
    starts = np.cumsum(counts) - counts
    xT = np.zeros((64, D, cap), np.float32)
    pos_of = np.zeros(2 * N, np.int64)
    for e in range(64):
        sl = order[starts[e]:starts[e] + counts[e]]
        xT[e, :, :counts[e]] = h2[flat_t[sl]].T
        pos_of[sl] = np.arange(counts[e])
    in_maps = [{"xT": np.ascontiguousarray(xT[g * 8:(g + 1) * 8]), "wg": np.ascontiguousarray(I["w_gate_e"][0][g * 8:(g + 1) * 8]),
                "wu": np.ascontiguousarray(I["w_up_e"][0][g * 8:(g + 1) * 8]), "wd": np.ascontiguousarray(I["w_down_e"][0][g * 8:(g + 1) * 8])} for g in range(8)]
    res = _run(lambda nc, p, st: build_experts(nc, p, st, cap), in_maps)
    yT = np.concatenate([r["yT"] for r in res], axis=0)
    yflat = yT[flat_e, :, pos_of]
    yflat = yflat.reshape(N, 2, D)
    return np.ascontiguousarray(yflat[:, 0]), np.ascontiguousarray(yflat[:, 1])


def build_combine(nc, p, st, ntile=8):
    n = ntile * 128
    ya_d = nc.dram_tensor("ya", [n, D], F32, kind="ExternalInput").ap()
    yb_d = nc.dram_tensor("yb", [n, D], F32, kind="ExternalInput").ap()
    w_d = nc.dram_tensor("w", [n, 2], F32, kind="ExternalInput").ap()
    g = nc.dram_tensor("g", [1, D], F32, kind="ExternalInput").ap()
    s = nc.dram_tensor("s", [1, D], F32, kind="ExternalInput").ap()
    base = nc.dram_tensor("base", [n, D], F32, kind="ExternalInput").ap()
    o = nc.dram_tensor("o", [n, D], F32, kind="ExternalOutput").ap()
    gb = _sb(nc, st, "gb", [128, D])
    A = _sb(nc, st, "A", [128, D])
    p.dma("sp", gb[:], g.partition_broadcast(128), writes=["gb"])
    p.dma("sp", A[:], s.partition_broadcast(128), writes=["A"])
    p.op("dve", lambda e: e.tensor_tensor(out=A[:], in0=A[:], in1=gb[:], op=ALU.mult), reads=["gb", "A"], writes=["A"])
    ya = [_sb(nc, st, f"ya{i}", [128, D]) for i in range(2)]
    yb = [_sb(nc, st, f"yb{i}", [128, D]) for i in range(2)]
    bt = [_sb(nc, st, f"bt{i}", [128, D]) for i in range(2)]
    wt = [_sb(nc, st, f"wt{i}", [128, 2]) for i in range(2)]
    junk = _sb(nc, st, "junk", [128, D])
    ot = [_sb(nc, st, f"ot{i}", [128, D]) for i in range(2)]
    ss = [_sb(nc, st, f"ss{i}", [128, 4]) for i in range(2)]
    for t in range(ntile):
        i = t % 2
        rows = slice(t * 128, (t + 1) * 128)
        p.dma("sp", ya[i][:], ya_d[rows, :], writes=[f"ya{i}"])
        p.dma("sp", yb[i][:], yb_d[rows, :], writes=[f"yb{i}"])
        p.dma("sp", bt[i][:], base[rows, :], writes=[f"bt{i}"])
        p.dma("sp", wt[i][:], w_d[rows, :], writes=[f"wt{i}"])
        p.op("dve", lambda e, i=i: e.tensor_scalar(out=ya[i][:], in0=ya[i][:], scalar1=wt[i][:, 0:1], scalar2=None, op0=ALU.mult),
             reads=[f"ya{i}", f"wt{i}"], writes=[f"ya{i}"])
        p.op("dve", lambda e, i=i: e.scalar_tensor_tensor(out=ya[i][:], in0=yb[i][:], scalar=wt[i][:, 1:2], in1=ya[i][:], op0=ALU.mult, op1=ALU.add),
             reads=[f"ya{i}", f"yb{i}", f"wt{i}"], writes=[f"ya{i}"])
        p.op("act", lambda e, i=i: e.activation(out=junk[:], in_=ya[i][:], func=AF.Square, accum_out=ss[i][:, 0:1]),
             reads=[f"ya{i}"], writes=["junk", f"ss{i}"])
        p.op("dve", lambda e, i=i: e.tensor_scalar(out=ss[i][:, 1:2], in0=ss[i][:, 0:1], scalar1=1.0 / D, scalar2=EPS, op0=ALU.mult, op1=ALU.add),
             reads=[f"ss{i}"], writes=[f"ss{i}"])
        p.op("act", lambda e, i=i: e.activation(out=ss[i][:, 2:3], in_=ss[i][:, 1:2], func=AF.Sqrt), reads=[f"ss{i}"], writes=[f"ss{i}"])
        p.op("dve", lambda e, i=i: e.reciprocal(out=ss[i][:, 3:4], in_=ss[i][:, 2:3]), reads=[f"ss{i}"], writes=[f"ss{i}"])
        p.op("dve", lambda e, i=i: e.scalar_tensor_tensor(out=ot[i][:], in0=ya[i][:], scalar=ss[i][:, 3:4], in1=A[:], op0=ALU.mult, op1=ALU.mult),
             reads=[f"ya{i}", f"ss{i}", "A"], writes=[f"ot{i}"], force=True)
        p.op("pool", lambda e, i=i: e.tensor_tensor(out=ot[i][:], in0=ot[i][:], in1=bt[i][:], op=ALU.add),
             reads=[f"ot{i}", f"bt{i}"], writes=[f"ot{i}"])
        p.dma("pool", o[rows, :], ot[i][:], reads=[f"ot{i}"])


def launch_combine(ya, yb, w, g, s, base):
    n = ya.shape[0] // 8
    in_maps = [{"ya": np.ascontiguousarray(ya[i * n:(i + 1) * n]), "yb": np.ascontiguousarray(yb[i * n:(i + 1) * n]),
                "w": np.ascontiguousarray(w[i * n:(i + 1) * n]), "g": np.ascontiguousarray(g[None, :]),
                "s": np.ascontiguousarray(s[None, :]), "base": np.ascontiguousarray(base[i * n:(i + 1) * n])} for i in range(8)]
    res = _run(lambda nc, p, st: build_combine(nc, p, st, n // 128), in_maps)
    return np.concatenate([r["o"] for r in res], axis=0)


def kernel(**inputs):
    I = {k: np.asarray(v) for k, v in inputs.items()}
    x = I["x"][0]
    ada = launch_ada(I["c"][0], I["w_ada"][0], I["b_ada"][0])
    sh1, sc1, gt1, sh2, sc2, gt2 = np.split(ada, 6)
    h1 = launch_norm(x, I["g_pre_mix"][0], sc1, 1.0, bv=sh1)
    lc = launch_inproj(h1, I)
    o_att = launch_moba(lc)
    o_rwkv = launch_rwkv_chunked(lc, I)
    y1 = launch_merge(h1, o_att, o_rwkv, I)
    x1 = launch_norm(y1, I["g_post_mix"][0], gt1, 0.0, base=x)
    h2 = launch_norm(x1, I["g_pre_ffn"][0], sc2, 1.0, bv=sh2)
    eidx, ew = launch_router(h2, I)
    ya, yb = launch_experts(h2, eidx, I)
    out = launch_combine(ya, yb, ew, I["g_post_ffn"][0], gt2, x1)
    return out[None].astype(np.float32)


CH_C = 64
SEG = 512


def build_rwkv_chunked(nc, p, st, T=S):
    nseg = T // SEG
    cps = SEG // CH_C
    F_d = nc.dram_tensor("F", [64, 6, 2, T], F32, kind="ExternalInput").ap()
    gb_d = nc.dram_tensor("gb", [64, 2, 2, T], F32, kind="ExternalInput").ap()
    gnv_d = nc.dram_tensor("gnv", [64, 2, 2], F32, kind="ExternalInput").ap()
    id_d = nc.dram_tensor("ident", [128, 128], F32, kind="ExternalInput").ap()
    msk_d = nc.dram_tensor("msk", [64, 10, 64], F32, kind="ExternalInput").ap()
    rm_d = nc.dram_tensor("rmask", [64, 2 * SEG], F32, kind="ExternalInput").ap()
    on_d = nc.dram_tensor("ones64", [64, 64], F32, kind="ExternalInput").ap()
    o_d = nc.dram_tensor("o", [64, 2, T], F32, kind="ExternalOutput").ap()

    ident = _sb(nc, st, "ident_sb", [128, 128])
    msk = _sb(nc, st, "msk_sb", [64, 10, 64])
    rmask = _sb(nc, st, "rmask_sb", [64, 2 * SEG])
    ones64 = _sb(nc, st, "ones64_sb", [64, 64])
    gnv = _sb(nc, st, "gnv_sb", [64, 2, 2])
    for dst, src, k in [(ident, id_d, "ident"), (msk, msk_d, "msk"),
                        (rmask, rm_d, "rmask"), (ones64, on_d, "ones64"), (gnv, gnv_d, "gnv")]:
        p.dma("sp", dst[:], src, writes=[k])
    Fin = [_sb(nc, st, f"Fin{i}", [64, 6, 2, SEG]) for i in range(2)]
    gbin = [_sb(nc, st, f"gbin{i}", [64, 2, 2, SEG]) for i in range(1)] * 2
    names = ["logw", "cum", "eg", "einv", "egm", "dte", "Af", "Bf", "Kf", "Rf", "Bh", "Kh"]
    tmpn = ["logw", "cum", "eg", "einv", "egm", "dte"]
    Wtmp = {n: _sb(nc, st, f"wt_{n}", [64, 2, SEG]) for n in tmpn}
    W_ = []
    for i in range(2):
        d_ = dict(Wtmp)
        for n in names:
            if n not in tmpn:
                d_[n] = _sb(nc, st, f"w{i}_{n}", [64, 2, SEG])
        W_.append(d_)
    gC = [_sb(nc, st, f"gC{i}", [64, 2, cps]) for i in range(2)]
    NSL = 4
    TM = [_sb(nc, st, f"TM{i}", [64, 4, 128]) for i in range(NSL)]
    MS = [_sb(nc, st, f"MS{i}", [64, 10, 64]) for i in range(NSL)]
    MQ = [[_sb(nc, st, f"MQ{i}_{j}", [64, 4, 64]) for j in range(2)] for i in range(NSL)]
    XW = [_sb(nc, st, f"XW{i}", [64, 2, 128]) for i in range(NSL)]
    NCH = 6
    CHb = [_sb(nc, st, f"CHb{i}", [64, 4, 128]) for i in range(NCH)]
    Z = _sb(nc, st, "Zst", [64, 2, 64])
    Z2 = _sb(nc, st, "Zst2", [64, 2, 64])
    YT = [_sb(nc, st, f"YT{i}", [64, 2, SEG]) for i in range(2)]
    ps = [_ps(nc, st, f"ps{i}") for i in range(8)]
    psi = [0]

    def nb():
        i = psi[0] % 8
        psi[0] += 1
        return ps[i], f"ps{i}"

    p.op("dve", lambda e: e.memset(Z[:], 0.0), writes=["Z"])

    def load_seg(sg):
        i = sg % 2
        p.dma("sp", Fin[i][:], F_d[:, :, :, sg * SEG:(sg + 1) * SEG], writes=[f"Fin{i}"])

    def prep_seg(sg):
        i = sg % 2
        Fi = Fin[i]
        w = W_[i]
        fk = f"Fin{i}"
        k = lambda n: (f"wt_{n}" if n in tmpn else f"w{i}_{n}")
        fl = lambda ap: ap.rearrange("p h t -> p (h t)")
        p.op("act", lambda e: e.activation(out=fl(w["logw"][:]), in_=fl(Fi[:, 0]), func=AF.Ln), reads=[fk], writes=[k("logw")])
        p.op("dve", lambda e: e.tensor_tensor_scan(out=fl(w["cum"][:]), data0=rmask[:], data1=fl(w["logw"][:]), initial=0.0, op0=ALU.mult, op1=ALU.add),
             reads=["rmask", k("logw")], writes=[k("cum")])
        p.op("act", lambda e: e.activation(out=fl(w["eg"][:]), in_=fl(w["cum"][:]), func=AF.Exp), reads=[k("cum")], writes=[k("eg")])
        p.op("act", lambda e: e.activation(out=fl(w["einv"][:]), in_=fl(w["cum"][:]), func=AF.Exp, scale=-1.0), reads=[k("cum")], writes=[k("einv")])
        p.op("dve", lambda e: e.tensor_tensor(out=fl(w["egm"][:]), in0=fl(w["cum"][:]), in1=fl(w["logw"][:]), op=ALU.subtract), reads=[k("cum"), k("logw")], writes=[k("egm")])
        p.op("act", lambda e: e.activation(out=fl(w["egm"][:]), in_=fl(w["egm"][:]), func=AF.Exp), reads=[k("egm")], writes=[k("egm")])
        cumv = w["cum"][:].rearrange("p h (c t) -> p (h c) t", t=CH_C)
        p.op("dve", lambda e: e.tensor_tensor(out=w["dte"][:].rearrange("p h (c t) -> p (h c) t", t=CH_C), in0=cumv[:, :, CH_C - 1:CH_C].to_broadcast([64, 2 * cps, CH_C]), in1=cumv, op=ALU.subtract),
             reads=[k("cum")], writes=[k("dte")])
        p.op("act", lambda e: e.activation(out=fl(w["dte"][:]), in_=fl(w["dte"][:]), func=AF.Exp), reads=[k("dte")], writes=[k("dte")])
        for out_n, a_idx, b_n, eng in [("Af", 1, "egm", "dve"), ("Bf", 2, "einv", "pool"), ("Kf", 3, "einv", "dve"),
                                       ("Rf", 4, "eg", "pool"), ("Bh", 2, "dte", "dve"), ("Kh", 3, "dte", "pool")]:
            p.op(eng, lambda e, out_n=out_n, a_idx=a_idx, b_n=b_n: e.tensor_tensor(out=fl(w[out_n][:]), in0=fl(Fi[:, a_idx]), in1=fl(w[b_n][:]), op=ALU.mult),
                 reads=[fk, k(b_n)], writes=[k(out_n)])
        egC = w["eg"][:].rearrange("p h (c t) -> p h c t", t=CH_C)[:, :, :, CH_C - 1]
        p.op("act", lambda e: e.copy(out=gC[i][:], in_=egC), reads=[k("eg")], writes=[f"gC{i}"])

    def pre_stages(sg, cl, slot, chslot):
        i = sg % 2
        w = W_[i]
        Fi = Fin[i]
        k = lambda n: f"w{i}_{n}"
        cs = slice(cl * CH_C, (cl + 1) * CH_C)
        tm, ms, mq, xw, chb = TM[slot], MS[slot], MQ[slot], XW[slot], CHb[chslot]
        tmk, msk_, xwk, chk = f"TM{slot}", f"MS{slot}", f"XW{slot}", f"CHb{chslot}"
        stages = []

        def s1():
            b, bkey = nb()
            for q, (src, skey) in enumerate([(w["Af"], k("Af")), (w["Bh"], k("Bh")), (w["Kh"], k("Kh")), (None, f"Fin{i}")]):
                for h in range(2):
                    in_ap = Fi[:, 5, h, cs] if src is None else src[:, h, cs]
                    p.op("pe", lambda e, q=q, h=h, in_ap=in_ap: e.transpose(b[0:64, q * 128 + h * 64:q * 128 + (h + 1) * 64], in_ap, ident[0:64, 0:64]),
                         reads=[skey, "ident"], writes=[bkey])
            p.op("act", lambda e: e.copy(out=tm[:].rearrange("p a b -> p (a b)"), in_=b[0:64, :]), reads=[bkey], writes=[tmk])
        stages.append(s1)

        def s2a():
            b, bkey = nb()
            for h in range(2):
                pb = slice(h * 64, (h + 1) * 64)
                for col, (l, lk, r_, rk) in [(0 + h, (w["Bf"], k("Bf"), w["Af"], k("Af"))), (2 + h, (w["Kf"], k("Kf"), w["Af"], k("Af"))),
                                             (4 + h, (w["Af"], k("Af"), w["Bf"], k("Bf")))]:
                    p.op("pe", lambda e, col=col, l=l, r_=r_, h=h: e.matmul(b[0:64, col * 64:(col + 1) * 64], lhsT=l[:, h, cs], rhs=r_[:, h, cs], start=True, stop=True),
                         reads=[lk, rk], writes=[bkey])
            p.op("dve", lambda e: e.tensor_tensor(out=ms[:, 0:6, :].rearrange("p a b -> p (a b)"), in0=b[0:64, 0:384], in1=msk[:, 0:6, :].rearrange("p a b -> p (a b)"), op=ALU.mult),
                 reads=[bkey, "msk"], writes=[msk_ + "a"])
        stages.append(s2a)

        def s2b():
            b, bkey = nb()
            for h in range(2):
                pb = slice(h * 64, (h + 1) * 64)
                for col, (l, lk) in [(0 + h, (w["Bf"], k("Bf"))), (2 + h, (w["Kf"], k("Kf")))]:
                    p.op("pe", lambda e, col=col, l=l, h=h: e.matmul(b[0:64, col * 64:(col + 1) * 64], lhsT=l[:, h, cs], rhs=w["Rf"][:, h, cs], start=True, stop=True),
                         reads=[lk, k("Rf")], writes=[bkey])
            p.op("dve", lambda e: e.tensor_tensor(out=ms[:, 6:10, :].rearrange("p a b -> p (a b)"), in0=b[0:64, 0:256], in1=msk[:, 6:10, :].rearrange("p a b -> p (a b)"), op=ALU.mult),
                 reads=[bkey, "msk"], writes=[msk_ + "b"])
        stages.append(s2b)

        def s3():
            b, bkey = nb()
            for h in range(2):
                p.op("pe", lambda e, h=h: e.matmul(b[0:64, h * 64:(h + 1) * 64], lhsT=ms[:, 2 + h, :], rhs=tm[:, 3, h * 64:(h + 1) * 64], start=True, stop=True),
                     reads=[msk_ + "a", tmk], writes=[bkey])
            p.op("act", lambda e: e.copy(out=xw[:, :, 64:128], in_=b[0:64, 0:128].rearrange("p (h v) -> p h v", h=2)), reads=[bkey], writes=[xwk + "x"])
            p.op("act", lambda e: e.copy(out=xw[:, :, 0:64], in_=tm[:, 0, :].rearrange("p (h v) -> p h v", h=2)), reads=[tmk], writes=[xwk + "w"])
        stages.append(s3)

        def mk_level(j):
            def lv():
                if j == 0:
                    MT = [ms[:, 0, :], ms[:, 1, :]]
                    M = [ms[:, 4, :], ms[:, 5, :]]
                    mkey = msk_ + "a"
                else:
                    q = mq[j % 2]
                    MT = [q[:, 0, :], q[:, 1, :]]
                    M = [q[:, 2, :], q[:, 3, :]]
                    mkey = f"MQ{slot}_{j % 2}"
                b, bkey = nb()
                for h in range(2):
                    p.op("pe", lambda e, h=h: e.matmul(b[0:64, h * 128:(h + 1) * 128], lhsT=MT[h], rhs=xw[:, h, :], start=True, stop=True),
                         reads=[mkey, xwk + "x", xwk + "w"], writes=[bkey])
                if j < 5:
                    b2, b2key = nb()
                    for h in range(2):
                        p.op("pe", lambda e, h=h: e.matmul(b2[0:64, h * 64:(h + 1) * 64], lhsT=M[h], rhs=MT[h], start=True, stop=True), reads=[mkey], writes=[b2key])
                        p.op("pe", lambda e, h=h: e.matmul(b2[0:64, (2 + h) * 64:(3 + h) * 64], lhsT=MT[h], rhs=M[h], start=True, stop=True), reads=[mkey], writes=[b2key])
                p.op("dve", lambda e: e.tensor_tensor(out=xw[:].rearrange("p a b -> p (a b)"), in0=b[0:64, 0:256], in1=xw[:].rearrange("p a b -> p (a b)"), op=ALU.add),
                     reads=[bkey, xwk + "x", xwk + "w"], writes=[xwk + "x", xwk + "w"])
                if j < 5:
                    nq = mq[(j + 1) % 2]
                    p.op("act", lambda e: e.copy(out=nq[:].rearrange("p a b -> p (a b)"), in_=b2[0:64, 0:256]), reads=[b2key], writes=[f"MQ{slot}_{(j + 1) % 2}"])
            return lv
        for j in range(6):
            stages.append(mk_level(j))

        def s5():
            b, bkey = nb()
            xk = [xwk + "x", xwk + "w"]
            for h in range(2):
                pb = slice(h * 64, (h + 1) * 64)
                hs = slice(h * 64, (h + 1) * 64)
                Wh = xw[:, h, 0:64]
                Xh = xw[:, h, 64:128]
                p.op("pe", lambda e, Wh=Wh, hs=hs, h=h: e.matmul(b[0:64, h * 64:(h + 1) * 64], lhsT=Wh, rhs=tm[:, 1, hs], start=True, stop=True),
                     reads=xk + [tmk], writes=[bkey])
                p.op("pe", lambda e, Xh=Xh, hs=hs, h=h: e.matmul(b[0:64, 128 + h * 64:128 + (h + 1) * 64], lhsT=tm[:, 1, hs], rhs=Xh, start=True, stop=False),
                     reads=xk + [tmk], writes=[bkey])
                p.op("pe", lambda e, hs=hs, h=h: e.matmul(b[0:64, 128 + h * 64:128 + (h + 1) * 64], lhsT=tm[:, 2, hs], rhs=tm[:, 3, hs], start=False, stop=True),
                     reads=[tmk], writes=[bkey])
                p.op("pe", lambda e, Wh=Wh, h=h: e.matmul(b[0:64, 256 + h * 64:256 + (h + 1) * 64], lhsT=Wh, rhs=ms[:, 6 + h, :], start=True, stop=False),
                     reads=xk + [msk_ + "b"], writes=[bkey])
                p.op("pe", lambda e, h=h: e.matmul(b[0:64, 256 + h * 64:256 + (h + 1) * 64], lhsT=ident[0:64, 0:64], rhs=w["Rf"][:, h, cs], start=False, stop=True),
                     reads=["ident", k("Rf")], writes=[bkey])
                p.op("pe", lambda e, Xh=Xh, h=h: e.matmul(b[0:64, 384 + h * 64:384 + (h + 1) * 64], lhsT=Xh, rhs=ms[:, 6 + h, :], start=True, stop=False),
                     reads=xk + [msk_ + "b"], writes=[bkey])
                p.op("pe", lambda e, hs=hs, h=h: e.matmul(b[0:64, 384 + h * 64:384 + (h + 1) * 64], lhsT=tm[:, 3, hs], rhs=ms[:, 8 + h, :], start=False, stop=True),
                     reads=[tmk, msk_ + "b"], writes=[bkey])
            p.op("act", lambda e: e.copy(out=chb[:].rearrange("p a b -> p (a b)"), in_=b[0:64, :]), reads=[bkey], writes=[chk])
        stages.append(s5)
        return stages

    def chain_step(sg, cl, chslot):
        i = sg % 2
        chb = CHb[chslot]
        chk = f"CHb{chslot}"
        yt = YT[i]
        b, bkey = nb()
        for h in range(2):
            hs = slice(h * 64, (h + 1) * 64)
            p.op("pe", lambda e, h=h, hs=hs: e.matmul(b[0:64, hs], lhsT=chb[:, 0, hs], rhs=Z[:, h, :], start=True, stop=True), reads=[chk, "Z"], writes=[bkey])
            p.op("pe", lambda e, h=h, hs=hs: e.matmul(b[0:64, 128 + h * 64:128 + (h + 1) * 64], lhsT=Z[:, h, :], rhs=chb[:, 2, hs], start=True, stop=True), reads=[chk, "Z"], writes=[bkey])
        p.op("dve", lambda e: e.tensor_tensor(out=yt[:, :, cl * CH_C:(cl + 1) * CH_C], in0=b[0:64, 128:256].rearrange("p (h t) -> p h t", h=2),
                                              in1=chb[:, 3, :].rearrange("p (h t) -> p h t", h=2), op=ALU.add),
             reads=[bkey, chk], writes=[f"YT{i}_{cl}"])
        for h in range(2):
            p.op("dve", lambda e, h=h: e.scalar_tensor_tensor(out=Z2[:, h, :], in0=Z[:, h, :], scalar=gC[i][:, h, cl:cl + 1], in1=b[0:64, h * 64:(h + 1) * 64], op0=ALU.mult, op1=ALU.add),
                 reads=["Z", f"gC{i}", bkey], writes=[f"Z2_{h}"])
        p.op("dve", lambda e: e.tensor_tensor(out=Z[:].rearrange("p a b -> p (a b)"), in0=Z2[:].rearrange("p a b -> p (a b)"), in1=chb[:, 1, :], op=ALU.add),
             reads=["Z2_0", "Z2_1", chk], writes=["Z"])

    yc = _sb(nc, st, "yc", [64, 2 * SEG])
    sq = _sb(nc, st, "sq", [64, 2 * SEG])
    rs = _sb(nc, st, "rs", [64, 2 * SEG])
    ot = [_sb(nc, st, f"ot{i}", [64, 2, SEG]) for i in range(1)] * 2

    def epilogue(sg):
        i = sg % 2
        yt = YT[i]
        ykeys = [f"YT{i}_{cl}" for cl in range(cps)]
        ytf = yt[:].rearrange("p h t -> p (h t)")
        p.dma("sp", gbin[0][:], gb_d[:, :, :, sg * SEG:(sg + 1) * SEG], writes=["gbin0"])
        for hh in range(2):
            b, bkey = nb()
            sl_ = slice(hh * SEG, (hh + 1) * SEG)
            p.op("pe", lambda e, sl_=sl_, b=b: e.matmul(b[0:64, :], lhsT=ones64[:], rhs=ytf[:, sl_], start=True, stop=True), reads=["ones64"] + ykeys, writes=[bkey])
            p.op("dve", lambda e, sl_=sl_, b=b: e.scalar_tensor_tensor(out=yc[:, sl_], in0=b[0:64, :], scalar=-1.0 / 64, in1=ytf[:, sl_], op0=ALU.mult, op1=ALU.add),
                 reads=[bkey] + ykeys, writes=[f"yc{hh}"])
            p.op("act", lambda e, sl_=sl_: e.activation(out=sq[:, sl_], in_=yc[:, sl_], func=AF.Square), reads=[f"yc{hh}"], writes=[f"sq{hh}"])
            b2, b2key = nb()
            p.op("pe", lambda e, sl_=sl_, b2=b2: e.matmul(b2[0:64, :], lhsT=ones64[:], rhs=sq[:, sl_], start=True, stop=True), reads=["ones64", f"sq{hh}"], writes=[b2key])
            p.op("dve", lambda e, sl_=sl_, b2=b2: e.tensor_scalar(out=rs[:, sl_], in0=b2[0:64, :], scalar1=1.0 / 64, scalar2=GN_EPS, op0=ALU.mult, op1=ALU.add), reads=[b2key], writes=[f"rs{hh}"])
            p.op("act", lambda e, sl_=sl_: e.activation(out=rs[:, sl_], in_=rs[:, sl_], func=AF.Sqrt), reads=[f"rs{hh}"], writes=[f"rs{hh}"])
            p.op("dve", lambda e, sl_=sl_: e.reciprocal(out=rs[:, sl_], in_=rs[:, sl_]), reads=[f"rs{hh}"], writes=[f"rs{hh}"])
            p.op("dve", lambda e, sl_=sl_: e.tensor_tensor(out=yc[:, sl_], in0=yc[:, sl_], in1=rs[:, sl_], op=ALU.mult), reads=[f"yc{hh}", f"rs{hh}"], writes=[f"yc{hh}"])
            p.op("dve", lambda e, sl_=sl_, hh=hh: e.tensor_scalar(out=yc[:, sl_], in0=yc[:, sl_], scalar1=gnv[:, hh, 0:1], scalar2=gnv[:, hh, 1:2], op0=ALU.mult, op1=ALU.add),
                 reads=[f"yc{hh}", "gnv"], writes=[f"yc{hh}"])
            p.op("pool", lambda e, sl_=sl_, hh=hh: e.tensor_tensor(out=yc[:, sl_], in0=yc[:, sl_], in1=gbin[0][:, 1, hh, :], op=ALU.add), reads=[f"yc{hh}", "gbin0"], writes=[f"yc{hh}"])
            p.op("pool", lambda e, sl_=sl_, hh=hh: e.tensor_tensor(out=ot[0][:, hh, :], in0=yc[:, sl_], in1=gbin[0][:, 0, hh, :], op=ALU.mult), reads=[f"yc{hh}", "gbin0"], writes=[f"ot0_{hh}"])
        p.dma("sp", o_d[:, :, sg * SEG:(sg + 1) * SEG], ot[0][:], reads=["ot0_0", "ot0_1"])

    load_seg(0)
    pending_chain = []
    slot_ctr = 0
    ch_ctr = 0
    for sg in range(nseg):
        if sg + 1 < nseg:
            load_seg(sg + 1)
        prep_seg(sg)
        for c0 in range(0, cps, 2):
            stA = pre_stages(sg, c0, slot_ctr % NSL, ch_ctr % NCH)
            stB = pre_stages(sg, c0 + 1, (slot_ctr + 1) % NSL, (ch_ctr + 1) % NCH)
            new_chain = [(sg, c0, ch_ctr % NCH), (sg, c0 + 1, (ch_ctr + 1) % NCH)]
            slot_ctr += 2
            ch_ctr += 2
            nst = len(stA)
            for si in range(nst):
                stA[si]()
                stB[si]()
                if pending_chain and si in (3, 7):
                    a = pending_chain.pop(0)
                    chain_step(*a)
                    if a[1] == cps - 1:
                        epilogue(a[0])
            pending_chain.extend(new_chain)
    while pending_chain:
        a = pending_chain.pop(0)
        chain_step(*a)
        if a[1] == cps - 1:
            epilogue(a[0])


def launch_rwkv_chunked(lc, I, T=S):
    C = CH_C
    su = np.triu(np.ones((C, C), np.float32), 1)
    sle = np.triu(np.ones((C, C), np.float32), 0)
    msk = np.stack([su, su, su, su, su.T, su.T, sle, sle, sle, sle], 0).transpose(1, 0, 2)
    ident = np.eye(128, dtype=np.float32)
    rmask = np.ones((64, 2 * SEG), np.float32)
    rmask[:, ::C] = 0
    ones64 = np.ones((64, 64), np.float32)
    in_maps = []
    for i in range(8):
        cq = slice(i * 128, (i + 1) * 128)
        F = np.stack([lc[i][n][:, :T].reshape(2, 64, T) for n in ("wdec", "nkk", "kka", "kt", "rT", "vrT")], 0)
        F = F.transpose(2, 0, 1, 3)
        gb = np.stack([lc[i]["g"][:, :T].reshape(2, 64, T), lc[i]["bonus"][:, :T].reshape(2, 64, T)], 0)
        gb = gb.transpose(2, 0, 1, 3)
        gnv = np.stack([I["gn_w"][0][cq].reshape(2, 64), I["gn_b"][0][cq].reshape(2, 64)], -1).transpose(1, 0, 2)
        in_maps.append({"F": np.ascontiguousarray(F), "gb": np.ascontiguousarray(gb), "gnv": np.ascontiguousarray(gnv),
                        "ident": ident, "msk": np.ascontiguousarray(msk), "rmask": rmask, "ones64": ones64})
    res = _run(lambda nc, p, st: build_rwkv_chunked(nc, p, st, T), in_maps)
    return np.concatenate([r["o"].transpose(2, 1, 0).reshape(T, 128) for r in res], axis=1)
```

```python
import numpy as np
import concourse.bass as bass
import concourse.mybir as mybir
from concourse.bass_utils import run_bass_kernel_spmd

F32 = mybir.dt.float32
F32R = mybir.dt.float32r
BF16 = mybir.dt.bfloat16
I32 = mybir.dt.int32
U32 = mybir.dt.uint32
AF = mybir.ActivationFunctionType
ALU = mybir.AluOpType
AX = mybir.AxisListType

NDMA_SLOTS = 6


class Prog:
    def __init__(self, nc):
        self.nc = nc
        self.ops = []
        self.last_w = {}
        self.readers = {}
        self.engs = {"pe": nc.tensor, "act": nc.scalar, "dve": nc.vector,
                     "pool": nc.gpsimd, "sp": nc.sync}

    def op(self, eng, fn, reads=(), writes=(), dma=False, force=False, inc=16):
        deps = set()
        raw = set()
        for k in reads:
            if k in self.last_w:
                deps.add(self.last_w[k])
                raw.add(self.last_w[k])
        for k in writes:
            if k in self.last_w:
                deps.add(self.last_w[k])
                raw.add(self.last_w[k])
            for r in self.readers.get(k, ()):
                deps.add(r)
        idx = len(self.ops)
        self.ops.append(dict(eng=eng, fn=fn, deps=deps, raw=raw, dma=dma, force=force, inc=inc))
        for k in reads:
            self.readers.setdefault(k, []).append(idx)
        for k in writes:
            self.last_w[k] = idx
            self.readers[k] = []
        return idx

    def dma(self, q, out, in_, reads=(), writes=(), **kw):
        return self.op(q, lambda e: e.dma_start(out=out, in_=in_, **kw), reads, writes, dma=True)

    def emit(self, stack):
        nc = self.nc
        ops = self.ops
        need = [False] * len(ops)
        for i, o in enumerate(ops):
            nd = set()
            for d in o["deps"]:
                od = ops[d]
                if od["dma"] or o["dma"] or o["force"] or od["eng"] != o["eng"] or (d in o["raw"] and o["eng"] != "pe"):
                    nd.add(d)
            o["xdeps"] = nd
            for d in nd:
                need[d] = True
        for i, o in enumerate(ops):
            if o["dma"]:
                need[i] = True
        esem = {e: stack.enter_context(nc.semaphore("es_" + e)) for e in self.engs}
        dsem = {e: [stack.enter_context(nc.semaphore(f"ds_{e}_{k}")) for k in range(NDMA_SLOTS)]
                for e in ("sp", "act", "pool")}
        ecount = {e: 0 for e in self.engs}
        dcount = {e: 0 for e in dsem}
        signal = [None] * len(ops)
        waited = {}
        nwaits = 0
        actions = {e: [] for e in self.engs}
        for i, o in enumerate(ops):
            e = o["eng"]
            wl = {}
            for d in o["xdeps"]:
                s_, v = signal[d]
                key = id(s_)
                if waited.get((e, key), 0) >= v:
                    continue
                if key not in wl or wl[key][1] < v:
                    wl[key] = (s_, v)
            if o["dma"]:
                j = dcount[e]
                slot = j % NDMA_SLOTS
                s_ = dsem[e][slot]
                prev = o.get("prev_total", None)
                prev = self._slot_total.get((e, slot), 0) if hasattr(self, "_slot_total") else 0
                if prev > 0 and waited.get((e, id(s_)), 0) < prev:
                    if id(s_) not in wl or wl[id(s_)][1] < prev:
                        wl[id(s_)] = (s_, prev)
            for key, (s_, v) in wl.items():
                waited[(e, key)] = v
                nwaits += 1
            sem = None
            inc = 0
            if o["dma"]:
                if not hasattr(self, "_slot_total"):
                    self._slot_total = {}
                j = dcount[e]
                dcount[e] += 1
                slot = j % NDMA_SLOTS
                sem = dsem[e][slot]
                inc = o.get("inc", 16)
                tot = self._slot_total.get((e, slot), 0) + inc
                self._slot_total[(e, slot)] = tot
                signal[i] = (sem, tot)
            elif need[i]:
                ecount[e] += 1
                sem = esem[e]
                inc = 1
                signal[i] = (sem, ecount[e])
            actions[e].append((list(wl.values()), o["fn"], sem, inc))
        finals = {e: [] for e in self.engs}
        for e in dsem:
            for slot in range(NDMA_SLOTS):
                tot = getattr(self, "_slot_total", {}).get((e, slot), 0)
                if tot > 0:
                    finals[e].append((dsem[e][slot], tot))
        bnames = {"pe": "tensor", "act": "scalar", "dve": "vector", "pool": "gpsimd", "sp": "sync"}
        with nc.Block() as block:
            for e in self.engs:
                if not actions[e] and not finals[e]:
                    continue

                def body(eng, e=e):
                    for waits, fn, sem, inc in actions[e]:
                        for s_, v in waits:
                            eng.wait_ge(s_, v)
                        inst = fn(eng)
                        if sem is not None:
                            inst.then_inc(sem, inc)
                    for s_, v in finals[e]:
                        eng.wait_ge(s_, v)
                getattr(block, bnames[e])(body)
        self.stats = dict(n_ops=len(ops), n_waits=nwaits, ecount=ecount, dcount=dcount)
        return self.stats


from contextlib import ExitStack

S = 8192
D = 2048
EPS = 1e-6
_TRACE = False


def _run(build, in_maps):
    nc = bass.Bass("TRN2", target_bir_lowering=False)
    with ExitStack() as st:
        p = Prog(nc)
        build(nc, p, st)
        p.emit(st)
    if _TRACE:
        r = run_bass_kernel_spmd(nc, in_maps, core_ids=list(range(8)), trace=True)
        print("EXEC_NS", getattr(build, "__name__", "?"), r.exec_time_ns, p.stats, flush=True)
    else:
        r = run_bass_kernel_spmd(nc, in_maps, core_ids=list(range(8)))
    return r.results


def _sb(nc, st, name, shape, dt=F32):
    return st.enter_context(nc.sbuf_tensor(name, shape, dt))


def _ps(nc, st, name, shape=(128, 512), dt=F32):
    return st.enter_context(nc.psum_tensor(name, list(shape), dt))


def build_ada(nc, p, st):
    w = nc.dram_tensor("w", [2048, 1536], F32, kind="ExternalInput").ap()
    c = nc.dram_tensor("c", [128, 16], F32, kind="ExternalInput").ap()
    b = nc.dram_tensor("b", [1, 1536], F32, kind="ExternalInput").ap()
    y = nc.dram_tensor("y", [1, 1536], F32, kind="ExternalOutput").ap()
    wt = [_sb(nc, st, f"wt{i}", [128, 1536]) for i in range(2)]
    ct = _sb(nc, st, "ct", [128, 16])
    bt = _sb(nc, st, "bt", [1, 1536])
    acc = _sb(nc, st, "acc", [128, 1536])
    ones = _sb(nc, st, "ones", [128, 1])
    res = _sb(nc, st, "res", [1, 1536])
    ps = [_ps(nc, st, f"ps{i}", (1, 512)) for i in range(2)]
    p.dma("sp", ct[:], c, writes=["ct"])
    p.dma("sp", bt[:], b, writes=["bt"])
    p.op("dve", lambda e: e.memset(ones[:], 1.0), writes=["ones"])
    for kc in range(16):
        i = kc % 2
        p.dma("sp", wt[i][:], w[kc * 128:(kc + 1) * 128, :], writes=[f"wt{i}"])
        if kc == 0:
            p.op("dve", lambda e, i=i, kc=kc: e.tensor_scalar(out=acc[:], in0=wt[i][:], scalar1=ct[:, kc:kc + 1], scalar2=None, op0=ALU.mult),
                 reads=[f"wt{i}", "ct"], writes=["acc"])
        else:
            p.op("dve", lambda e, i=i, kc=kc: e.scalar_tensor_tensor(out=acc[:], in0=wt[i][:], scalar=ct[:, kc:kc + 1], in1=acc[:], op0=ALU.mult, op1=ALU.add),
                 reads=[f"wt{i}", "ct", "acc"], writes=["acc"])
    for j in range(3):
        pj = ps[j % 2]
        p.op("pe", lambda e, j=j, pj=pj: e.matmul(pj[:], lhsT=ones[:], rhs=acc[:, j * 512:(j + 1) * 512], start=True, stop=True),
             reads=["acc", "ones"], writes=[f"ps{j%2}"])
        p.op("dve", lambda e, j=j, pj=pj: e.tensor_tensor(out=res[:, j * 512:(j + 1) * 512], in0=pj[:], in1=bt[:, j * 512:(j + 1) * 512], op=ALU.add),
             reads=[f"ps{j%2}", "bt"], writes=[f"res{j}"])
    p.dma("sp", y, res[:], reads=["res0", "res1", "res2"])


def launch_ada(c, w_ada, b_ada):
    in_maps = [{"w": np.ascontiguousarray(w_ada[:, i * 1536:(i + 1) * 1536]),
                "c": np.ascontiguousarray(c.reshape(16, 128).T),
                "b": np.ascontiguousarray(b_ada[None, i * 1536:(i + 1) * 1536])} for i in range(8)]
    res = _run(build_ada, in_maps)
    return np.concatenate([r["y"][0] for r in res])


def make_build_norm(add_one, has_b, has_base, ntile=8):
    def build(nc, p, st):
        n = ntile * 128
        y = nc.dram_tensor("y", [n, D], F32, kind="ExternalInput").ap()
        g = nc.dram_tensor("g", [1, D], F32, kind="ExternalInput").ap()
        s = nc.dram_tensor("s", [1, D], F32, kind="ExternalInput").ap()
        bv = nc.dram_tensor("bv", [1, D], F32, kind="ExternalInput").ap() if has_b else None
        base = nc.dram_tensor("base", [n, D], F32, kind="ExternalInput").ap() if has_base else None
        o = nc.dram_tensor("o", [n, D], F32, kind="ExternalOutput").ap()
        gb = _sb(nc, st, "gb", [128, D])
        A = _sb(nc, st, "A", [128, D])
        bb = _sb(nc, st, "bb", [128, D]) if has_b else None
        p.dma("sp", gb[:], g.partition_broadcast(128), writes=["gb"])
        p.dma("sp", A[:], s.partition_broadcast(128), writes=["A"])
        if has_b:
            p.dma("sp", bb[:], bv.partition_broadcast(128), writes=["bb"])
        p.op("dve", lambda e: e.scalar_tensor_tensor(out=A[:], in0=A[:], scalar=float(add_one), in1=gb[:], op0=ALU.add, op1=ALU.mult),
             reads=["gb", "A"], writes=["A"])
        yt = [_sb(nc, st, f"yt{i}", [128, D]) for i in range(2)]
        bt = [_sb(nc, st, f"bt{i}", [128, D]) for i in range(2)] if has_base else None
        junk = _sb(nc, st, "junk", [128, D])
        ot = [_sb(nc, st, f"ot{i}", [128, D]) for i in range(2)]
        ss = [_sb(nc, st, f"ss{i}", [128, 4]) for i in range(2)]
        for t in range(ntile):
            i = t % 2
            rows = slice(t * 128, (t + 1) * 128)
            p.dma("sp", yt[i][:], y[rows, :], writes=[f"yt{i}"])
            if has_base:
                p.dma("sp", bt[i][:], base[rows, :], writes=[f"bt{i}"])
            p.op("act", lambda e, i=i: e.activation(out=junk[:], in_=yt[i][:], func=AF.Square, accum_out=ss[i][:, 0:1]),
                 reads=[f"yt{i}"], writes=["junk", f"ss{i}"])
            p.op("dve", lambda e, i=i: e.tensor_scalar(out=ss[i][:, 1:2], in0=ss[i][:, 0:1], scalar1=1.0 / D, scalar2=EPS, op0=ALU.mult, op1=ALU.add),
                 reads=[f"ss{i}"], writes=[f"ss{i}"])
            p.op("act", lambda e, i=i: e.activation(out=ss[i][:, 2:3], in_=ss[i][:, 1:2], func=AF.Sqrt),
                 reads=[f"ss{i}"], writes=[f"ss{i}"])
            p.op("dve", lambda e, i=i: e.reciprocal(out=ss[i][:, 3:4], in_=ss[i][:, 2:3]),
                 reads=[f"ss{i}"], writes=[f"ss{i}"])
            p.op("dve", lambda e, i=i: e.scalar_tensor_tensor(out=ot[i][:], in0=yt[i][:], scalar=ss[i][:, 3:4], in1=A[:], op0=ALU.mult, op1=ALU.mult),
                 reads=[f"yt{i}", f"ss{i}", "A"], writes=[f"ot{i}"], force=True)
            if has_b:
                p.op("pool", lambda e, i=i: e.tensor_tensor(out=ot[i][:], in0=ot[i][:], in1=bb[:], op=ALU.add),
                     reads=[f"ot{i}", "bb"], writes=[f"ot{i}"])
            if has_base:
                p.op("pool", lambda e, i=i: e.tensor_tensor(out=ot[i][:], in0=ot[i][:], in1=bt[i][:], op=ALU.add),
                     reads=[f"ot{i}", f"bt{i}"], writes=[f"ot{i}"])
            p.dma("pool", o[rows, :], ot[i][:], reads=[f"ot{i}"])
    return build


def launch_norm(y, g, s, add_one, bv=None, base=None):
    n = y.shape[0] // 8
    in_maps = []
    for i in range(8):
        m = {"y": np.ascontiguousarray(y[i * n:(i + 1) * n]), "g": np.ascontiguousarray(g[None, :]),
             "s": np.ascontiguousarray(s[None, :])}
        if bv is not None:
            m["bv"] = np.ascontiguousarray(bv[None, :])
        if base is not None:
            m["base"] = np.ascontiguousarray(base[i * n:(i + 1) * n])
        in_maps.append(m)
    res = _run(make_build_norm(add_one, bv is not None, base is not None, n // 128), in_maps)
    return np.concatenate([r["o"] for r in res], axis=0)


LC_OUTS = ["qT", "kT", "vT", "rT", "krT", "vrT", "wdec", "nkk", "kka", "kt", "g", "bonus"]
WDECAY = 0.6065306597126334


def build_inproj(nc, p, st, ntt=16):
    T = ntt * 512
    hTp = nc.dram_tensor("hTp", [D, T + 1], F32, kind="ExternalInput").ap()
    ws_d = nc.dram_tensor("ws", [D, 448], F32, kind="ExternalInput").ap()
    wd_d = nc.dram_tensor("wd", [D, 384], F32, kind="ExternalInput").ap()
    mucol_d = nc.dram_tensor("mucol", [1, 384], F32, kind="ExternalInput").ap()
    wl_d = nc.dram_tensor("wl", [D, 448], F32, kind="ExternalInput").ap()
    murow_d = nc.dram_tensor("murow", [128, 16, 3], F32, kind="ExternalInput").ap()
    w2w_d = nc.dram_tensor("w2w", [96, 128], F32, kind="ExternalInput").ap()
    w2a_d = nc.dram_tensor("w2a", [96, 128], F32, kind="ExternalInput").ap()
    w2g_d = nc.dram_tensor("w2g", [128, 2, 128], F32, kind="ExternalInput").ap()
    vecs_d = nc.dram_tensor("vecs", [128, 5], F32, kind="ExternalInput").ap()
    cos_d = nc.dram_tensor("cos", [32, T], F32, kind="ExternalInput").ap()
    sin_d = nc.dram_tensor("sin", [32, T], F32, kind="ExternalInput").ap()
    blk_d = nc.dram_tensor("blk", [128, 128], F32, kind="ExternalInput").ap()
    outs = {n: nc.dram_tensor(n, [128, T], F32, kind="ExternalOutput").ap() for n in LC_OUTS}

    w0 = _sb(nc, st, "w0", [128, 16, 448])
    wa = _sb(nc, st, "wa", [128, 16, 448])
    wb = _sb(nc, st, "wb", [128, 16, 448])
    hb = [_sb(nc, st, f"hb{i}", [128, 16, 514]) for i in range(2)]
    mucol = _sb(nc, st, "mucol_sb", [128, 384])
    murow = _sb(nc, st, "murow_sb", [128, 16, 3])
    w2w = _sb(nc, st, "w2w_sb", [96, 128])
    w2a = _sb(nc, st, "w2a_sb", [96, 128])
    w2g = _sb(nc, st, "w2g_sb", [128, 2, 128])
    vecs = _sb(nc, st, "vecs_sb", [128, 5])
    blk = _sb(nc, st, "blk_sb", [128, 128])
    cs = [_sb(nc, st, f"cs{i}", [32, 2, 512]) for i in range(2)]
    ps = [_ps(nc, st, f"ps{i}") for i in range(8)]
    NOB = 6
    ob = [_sb(nc, st, f"ob{i}", [128, 512]) for i in range(NOB)]
    obi = [0]
    psi = [0]

    def nps():
        i = psi[0] % 8
        psi[0] += 1
        return ps[i], f"ps{i}"

    def nob():
        i = obi[0] % NOB
        obi[0] += 1
        return ob[i], f"ob{i}"

    hview = hTp.rearrange("(c p) t -> p c t", p=128)
    r32 = lambda ap: ap.bitcast(F32R)

    for dst, src, k in [(mucol[:], mucol_d.partition_broadcast(128), "mucol"), (murow[:], murow_d, "murow"),
                        (w2w[:], w2w_d, "w2w"), (w2a[:], w2a_d, "w2a"), (w2g[:], w2g_d, "w2g"),
                        (vecs[:], vecs_d, "vecs"), (blk[:], blk_d, "blk")]:
        p.dma("sp", dst, src, writes=[k])

    def load_h(tt):
        i = tt % 2
        p.dma("pool", r32(hb[i][:, :, 0:513]), r32(hview[:, :, tt * 512:tt * 512 + 513]), writes=[f"hb{i}"])

    def gemm(tt, kind, co, M, pst, psk):
        i = tt % 2
        for c in range(16):
            cur = hb[i][:, c, 1:513]
            prev = hb[i][:, c, 0:512]
            if kind == "single":
                p.op("pe", lambda e, c=c, cur=cur: e.matmul(pst[:M, :], lhsT=r32(w0[:, c, co:co + M]), rhs=r32(cur), start=(c == 0), stop=(c == 15)),
                     reads=["w0", f"hb{i}"], writes=[psk])
            else:
                p.op("pe", lambda e, c=c, cur=cur: e.matmul(pst[:M, :], lhsT=r32(wa[:, c, co:co + M]), rhs=r32(cur), start=(c == 0), stop=False),
                     reads=["wa", f"hb{i}"], writes=[psk])
                p.op("pe", lambda e, c=c, prev=prev: e.matmul(pst[:M, :], lhsT=r32(wb[:, c, co:co + M]), rhs=r32(prev), start=False, stop=(c == 15)),
                     reads=["wb", f"hb{i}"], writes=[psk])

    p.dma("pool", r32(w0[:]), r32(ws_d.rearrange("(c p) n -> p c n", p=128)), writes=["w0"])
    p.dma("pool", r32(wa[:, :, 0:384]), r32(wd_d.rearrange("(c p) n -> p c n", p=128)), writes=["wa"])
    for c in range(16):
        p.op("dve", lambda e, c=c: e.tensor_tensor(out=r32(wb[:, c, 0:384]), in0=wa[:, c, 0:384], in1=mucol[:], op=ALU.mult),
             reads=["wa", "mucol"], writes=["wb"])
    p.op("dve", lambda e: e.tensor_tensor(out=r32(wa[:, :, 0:384]), in0=wa[:, :, 0:384], in1=wb[:, :, 0:384], op=ALU.subtract),
         reads=["wa", "wb"], writes=["wa"])
    load_h(0)
    for tt in range(ntt):
        if tt + 1 < ntt:
            load_h(tt + 1)
        tsl = slice(tt * 512, (tt + 1) * 512)
        ci = tt % 2
        p.dma("sp", cs[ci][:, 0, :], cos_d[:, tsl], writes=[f"cs{ci}"])
        p.dma("sp", cs[ci][:, 1, :], sin_d[:, tsl], writes=[f"cs{ci}"])
        for name, co in (("qT", 0), ("kT", 160)):
            pq, pqk = nps()
            gemm(tt, "single", co, 128, pq, pqk)
            psw, pswk = nps()
            gemm(tt, "single", co + 128, 32, psw, pswk)
            o, ok = nob()
            p.op("act", lambda e, o=o, pq=pq: e.copy(out=o[:], in_=pq[:]), reads=[pqk], writes=[ok, ok + "hi"])
            t1, t1k = nob()
            p.op("dve", lambda e, t1=t1, psw=psw, ci=ci: e.tensor_tensor(out=t1[0:32, :], in0=psw[0:32, :], in1=cs[ci][:, 1, :], op=ALU.mult),
                 reads=[pswk, f"cs{ci}"], writes=[t1k])
            p.op("dve", lambda e, o=o, pq=pq, ci=ci: e.tensor_tensor(out=o[0:32, :], in0=pq[0:32, :], in1=cs[ci][:, 0, :], op=ALU.mult),
                 reads=[pqk, f"cs{ci}"], writes=[ok])
            p.op("dve", lambda e, o=o, t1=t1: e.tensor_tensor(out=o[0:32, :], in0=o[0:32, :], in1=t1[0:32, :], op=ALU.add),
                 reads=[ok, t1k], writes=[ok])
            p.dma("pool", outs[name][:, tsl], o[:], reads=[ok, ok + "hi"], writes=[f"d_{name}_{tt}"])
        pv, pvk = nps()
        gemm(tt, "single", 320, 128, pv, pvk)
        o, ok = nob()
        p.op("act", lambda e, o=o, pv=pv: e.copy(out=o[:], in_=pv[:]), reads=[pvk], writes=[ok, ok + "hi"])
        p.dma("pool", outs["vT"][:, tsl], o[:], reads=[ok, ok + "hi"], writes=[f"d_vT_{tt}"])
        for j, name in enumerate(("rT", "krT", "vrT")):
            pr, prk = nps()
            gemm(tt, "dual", j * 128, 128, pr, prk)
            o, ok = nob()
            p.op("act", lambda e, o=o, pr=pr: e.copy(out=o[:], in_=pr[:]), reads=[prk], writes=[ok, ok + "hi"])
            p.dma("pool", outs[name][:, tsl], o[:], reads=[ok, ok + "hi"], writes=[f"d_{name}_{tt}"])

    p.dma("pool", r32(w0[:]), r32(wl_d.rearrange("(c p) n -> p c n", p=128)), writes=["w0"])
    for c in range(16):
        for j, (lo, hi) in enumerate(((0, 96), (96, 192), (192, 448))):
            p.op("dve", lambda e, c=c, j=j, lo=lo, hi=hi: e.tensor_scalar(out=r32(wb[:, c, lo:hi]), in0=w0[:, c, lo:hi], scalar1=murow[:, c, j:j + 1], scalar2=None, op0=ALU.mult),
                 reads=["w0", "murow"], writes=["wb"])
    p.op("dve", lambda e: e.tensor_tensor(out=r32(wa[:]), in0=w0[:], in1=wb[:], op=ALU.subtract),
         reads=["w0", "wb"], writes=["wa"])
    tw = _sb(nc, st, "tw", [96, 512])
    ta = _sb(nc, st, "ta", [96, 512])
    tg = _sb(nc, st, "tg", [128, 2, 512])
    rin = [_sb(nc, st, f"rin{i}", [128, 3, 512]) for i in range(1)]
    tmp = {n: _sb(nc, st, "tmp_" + n, [128, 512]) for n in ["a", "kkr", "sq", "nrm", "rn", "u", "rk"]}
    load_h(0)
    for tt in range(ntt):
        if tt + 1 < ntt:
            load_h(tt + 1)
        tsl = slice(tt * 512, (tt + 1) * 512)
        ri = 0
        for j, name in enumerate(("rT", "krT", "vrT")):
            p.dma("sp", rin[ri][:, j, :], outs[name][:, tsl], reads=[f"d_{name}_{tt}"], writes=[f"rin{ri}_{j}"])
        R_, KR, VR = rin[ri][:, 0, :], rin[ri][:, 1, :], rin[ri][:, 2, :]
        rk_, krk, vrk = f"rin{ri}_0", f"rin{ri}_1", f"rin{ri}_2"
        pw, pwk = nps()
        gemm(tt, "dual", 0, 96, pw, pwk)
        p.op("act", lambda e, pw=pw: e.activation(out=tw[:], in_=pw[:96, :], func=AF.Tanh), reads=[pwk], writes=["tw"])
        pa, pak = nps()
        gemm(tt, "dual", 96, 96, pa, pak)
        p.op("act", lambda e, pa=pa: e.copy(out=ta[:], in_=pa[:96, :]), reads=[pak], writes=["ta"])
        for h in range(2):
            pg, pgk = nps()
            gemm(tt, "dual", 192 + h * 128, 128, pg, pgk)
            p.op("act", lambda e, pg=pg, h=h: e.activation(out=tg[:, h, :], in_=pg[:], func=AF.Sigmoid), reads=[pgk], writes=[f"tg{h}"])
        pd, pdk = nps()
        p.op("pe", lambda e, pd=pd: e.matmul(pd[:], lhsT=w2w[:], rhs=tw[:], start=True, stop=True), reads=["w2w", "tw"], writes=[pdk])
        o_w, o_wk = nob()
        p.op("act", lambda e, pd=pd: e.activation(out=tmp["sq"][:], in_=pd[:], func=AF.Sigmoid, bias=vecs[:, 0:1]), reads=[pdk, "vecs"], writes=["t_sq"])
        p.op("act", lambda e, o_w=o_w: e.activation(out=o_w[:], in_=tmp["sq"][:], func=AF.Exp, scale=-WDECAY), reads=["t_sq"], writes=[o_wk])
        p.dma("pool", outs["wdec"][:, tsl], o_w[:], reads=[o_wk])
        pa2, pa2k = nps()
        p.op("pe", lambda e, pa2=pa2: e.matmul(pa2[:], lhsT=w2a[:], rhs=ta[:], start=True, stop=True), reads=["w2a", "ta"], writes=[pa2k])
        p.op("act", lambda e, pa2=pa2: e.activation(out=tmp["a"][:], in_=pa2[:], func=AF.Sigmoid, bias=vecs[:, 1:2]), reads=[pa2k, "vecs"], writes=["t_a"])
        pg2, pg2k = nps()
        for h in range(2):
            p.op("pe", lambda e, pg2=pg2, h=h: e.matmul(pg2[:], lhsT=w2g[:, h, :], rhs=tg[:, h, :], start=(h == 0), stop=(h == 1)),
                 reads=["w2g", f"tg{h}"], writes=[pg2k])
        o_g, o_gk = nob()
        p.op("act", lambda e, o_g=o_g, pg2=pg2: e.copy(out=o_g[:], in_=pg2[:]), reads=[pg2k], writes=[o_gk])
        p.dma("pool", outs["g"][:, tsl], o_g[:], reads=[o_gk])
        p.op("dve", lambda e, KR=KR: e.tensor_scalar(out=tmp["kkr"][:], in0=KR, scalar1=vecs[:, 2:3], scalar2=None, op0=ALU.mult),
             reads=[krk, "vecs"], writes=["t_kkr"])
        p.op("pool", lambda e: e.tensor_tensor(out=tmp["sq"][:], in0=tmp["kkr"][:], in1=tmp["kkr"][:], op=ALU.mult),
             reads=["t_kkr", "t_sq"], writes=["t_sq"])
        pn, pnk = nps()
        p.op("pe", lambda e, pn=pn: e.matmul(pn[:], lhsT=blk[:], rhs=tmp["sq"][:], start=True, stop=True), reads=["blk", "t_sq"], writes=[pnk])
        p.op("act", lambda e, pn=pn: e.activation(out=tmp["nrm"][:], in_=pn[:], func=AF.Sqrt), reads=[pnk], writes=["t_nrm"])
        p.op("dve", lambda e: e.tensor_scalar(out=tmp["nrm"][:], in0=tmp["nrm"][:], scalar1=1e-12, scalar2=None, op0=ALU.max),
             reads=["t_nrm"], writes=["t_nrm"])
        p.op("dve", lambda e: e.reciprocal(out=tmp["rn"][:], in_=tmp["nrm"][:]), reads=["t_nrm"], writes=["t_rn"])
        o_n, o_nk = nob()
        p.op("dve", lambda e, o_n=o_n: e.scalar_tensor_tensor(out=o_n[:], in0=tmp["kkr"][:], scalar=-1.0, in1=tmp["rn"][:], op0=ALU.mult, op1=ALU.mult),
             reads=["t_kkr", "t_rn"], writes=[o_nk])
        p.dma("pool", outs["nkk"][:, tsl], o_n[:], reads=[o_nk])
        o_ka, o_kak = nob()
        p.op("dve", lambda e, o_n=o_n, o_ka=o_ka: e.scalar_tensor_tensor(out=o_ka[:], in0=o_n[:], scalar=-1.0, in1=tmp["a"][:], op0=ALU.mult, op1=ALU.mult),
             reads=[o_nk, "t_a"], writes=[o_kak])
        p.dma("pool", outs["kka"][:, tsl], o_ka[:], reads=[o_kak])
        p.op("dve", lambda e: e.tensor_scalar(out=tmp["u"][:], in0=tmp["a"][:], scalar1=-1.0, scalar2=vecs[:, 3:4], op0=ALU.add, op1=ALU.mult),
             reads=["t_a", "vecs"], writes=["t_u"])
        o_kt, o_ktk = nob()
        p.op("dve", lambda e, o_kt=o_kt, KR=KR: e.scalar_tensor_tensor(out=o_kt[:], in0=tmp["u"][:], scalar=1.0, in1=KR, op0=ALU.add, op1=ALU.mult),
             reads=["t_u", krk], writes=[o_ktk])
        p.dma("pool", outs["kt"][:, tsl], o_kt[:], reads=[o_ktk])
        p.op("dve", lambda e, o_kt=o_kt, R_=R_: e.scalar_tensor_tensor(out=tmp["rk"][:], in0=R_, scalar=vecs[:, 4:5], in1=o_kt[:], op0=ALU.mult, op1=ALU.mult),
             reads=[rk_, "vecs", o_ktk], writes=["t_rk"])
        pb, pbk = nps()
        p.op("pe", lambda e, pb=pb: e.matmul(pb[:], lhsT=blk[:], rhs=tmp["rk"][:], start=True, stop=True), reads=["blk", "t_rk"], writes=[pbk])
        o_b, o_bk = nob()
        p.op("dve", lambda e, o_b=o_b, pb=pb, VR=VR: e.tensor_tensor(out=o_b[:], in0=pb[:], in1=VR, op=ALU.mult),
             reads=[pbk, vrk], writes=[o_bk])
        p.dma("pool", outs["bonus"][:, tsl], o_b[:], reads=[o_bk])


def _rope_tables(T):
    half = 16
    inv = (500000.0 ** (-np.arange(half, dtype=np.float32) / half)).astype(np.float32)
    ang = np.arange(T, dtype=np.float32)[:, None] * inv[None, :]
    cos = np.cos(ang).astype(np.float32).T
    sin = np.sin(ang).astype(np.float32).T
    COS = np.concatenate([cos, cos], 0)
    SIN = np.concatenate([-sin, sin], 0)
    return np.ascontiguousarray(COS), np.ascontiguousarray(SIN)


def launch_inproj(h, I):
    T = h.shape[0]
    hTp = np.zeros((D, T + 1), np.float32)
    hTp[:, 1:] = h.T
    w_in = I["w_in"][0]
    COS, SIN = _rope_tables(T)
    swp = np.concatenate([np.arange(16, 32), np.arange(0, 16)])
    blk = np.zeros((128, 128), np.float32)
    blk[:64, :64] = 1
    blk[64:, 64:] = 1
    murow = np.stack([I["mu_w"][0], I["mu_a"][0], I["mu_g"][0]], -1).reshape(16, 128, 3).transpose(1, 0, 2)
    wl = np.concatenate([I["w_w1"][0], I["w_a1"][0], I["w_g1"][0]], 1)
    in_maps = []
    for i in range(8):
        cq = slice(i * 128, (i + 1) * 128)
        q = w_in[:, 0:1024][:, cq]
        k = w_in[:, 1024:2048][:, cq]
        v = w_in[:, 2048:3072][:, cq]
        ws = np.concatenate([q, q[:, swp], k, k[:, swp], v], 1)
        r = w_in[:, 3072:4096][:, cq]
        kr = w_in[:, 4096:5120][:, cq]
        vr = w_in[:, 5120:6144][:, cq]
        wd = np.concatenate([r, kr, vr], 1)
        mucol = np.concatenate([I["mu_r"][0][cq], I["mu_k"][0][cq], I["mu_v"][0][cq]])[None, :]
        vecs = np.stack([I["w0"][0][cq], I["a0"][0][cq], I["k_k"][0][cq], I["k_a"][0][cq], I["r_k"][0].reshape(-1)[cq]], -1)
        in_maps.append({
            "hTp": hTp, "ws": np.ascontiguousarray(ws), "wd": np.ascontiguousarray(wd), "mucol": np.ascontiguousarray(mucol),
            "wl": np.ascontiguousarray(wl), "murow": np.ascontiguousarray(murow),
            "w2w": np.ascontiguousarray(I["w_w2"][0][:, cq]), "w2a": np.ascontiguousarray(I["w_a2"][0][:, cq]),
            "w2g": np.ascontiguousarray(I["w_g2"][0][:, cq].reshape(2, 128, 128).transpose(1, 0, 2)),
            "vecs": np.ascontiguousarray(vecs), "cos": COS, "sin": SIN, "blk": blk})
    ntt = T // 512
    res = _run(lambda nc, p, st: build_inproj(nc, p, st, ntt), in_maps)
    return res


GN_EPS = 64e-5
TCH = 32


def build_rwkv(nc, p, st, T=S):
    nch = T // TCH
    bcin_d = nc.dram_tensor("bcin", [2, nch, 5, TCH, 64], F32, kind="ExternalInput").ap()
    vT_d = nc.dram_tensor("vT", [128, T], F32, kind="ExternalInput").ap()
    g_d = nc.dram_tensor("g", [128, T], F32, kind="ExternalInput").ap()
    bonus_d = nc.dram_tensor("bonus", [128, T], F32, kind="ExternalInput").ap()
    gnv_d = nc.dram_tensor("gnv", [128, 2], F32, kind="ExternalInput").ap()
    sel_d = nc.dram_tensor("sel", [128, 128], F32, kind="ExternalInput").ap()
    blk_d = nc.dram_tensor("blk", [128, 128], F32, kind="ExternalInput").ap()
    o_d = nc.dram_tensor("o", [128, T], F32, kind="ExternalOutput").ap()
    r32 = lambda ap: ap.bitcast(F32R)

    vT = _sb(nc, st, "vT_sb", [128, T])
    yT = _sb(nc, st, "yT_sb", [128, T])
    Sst = _sb(nc, st, "S_sb", [128, 64])
    junk = _sb(nc, st, "junk", [128, 64])
    sa = _sb(nc, st, "sa", [128, 1])
    sel = _sb(nc, st, "sel_sb", [128, 128])
    blk = _sb(nc, st, "blk_sb", [128, 128])
    gnv = _sb(nc, st, "gnv_sb", [128, 2])
    bc = [_sb(nc, st, f"bc{i}", [128, 5, TCH, 64]) for i in range(2)]
    ps = [_ps(nc, st, f"ps{i}") for i in range(8)]
    p.dma("pool", r32(sel[:]), r32(sel_d), writes=["sel"])
    p.dma("sp", blk[:], blk_d, writes=["blk"])
    p.dma("sp", gnv[:], gnv_d, writes=["gnv"])
    p.dma("sp", vT[:], vT_d, writes=["vT"])
    p.op("dve", lambda e: e.memset(Sst[:], 0.0), writes=["S"])
    zer_d = nc.dram_tensor("zer", [126, 5, TCH, 64], F32, kind="ExternalInput").ap()
    for i in range(2):
        p.dma("pool", r32(bc[i][2:128]), r32(zer_d), writes=[f"bc{i}"])

    def load_bc(c):
        i = c % 2
        p.dma("pool", r32(bc[i][0:2]), r32(bcin_d[:, c]), writes=[f"bc{i}"])

    load_bc(0)
    grp = 0
    for c in range(nch):
        if c + 1 < nch:
            load_bc(c + 1)
        bi = c % 2
        for g4 in range(TCH // 4):
            base = (grp % 2) * 3
            grp += 1
            views = []
            for j in range(5):
                bank = ps[base + j // 2]
                bk = f"ps{base + j // 2}"
                half = bank[:, (j % 2) * 256:(j % 2) * 256 + 256]
                p.op("pe", lambda e, half=half, j=j, g4=g4, bi=bi: e.matmul(half, lhsT=r32(sel[:]), rhs=r32(bc[bi][:, j, g4 * 4:(g4 + 1) * 4, :]), start=True, stop=True),
                     reads=["sel", f"bc{bi}"], writes=[bk + f"h{j%2}"])
                views.append((half, bk + f"h{j%2}"))
            for tl in range(4):
                t = c * TCH + g4 * 4 + tl
                cs = slice(tl * 64, (tl + 1) * 64)
                wv, nv, kav, ktv, rv = [(v[0][:, cs], v[1]) for v in views]
                p.op("dve", lambda e, nv=nv: e.scalar_tensor_tensor(out=junk[:], in0=Sst[:], scalar=1.0, in1=nv[0], op0=ALU.mult, op1=ALU.mult, accum_out=sa[:]),
                     reads=["S", nv[1]], writes=["junk", "sa"])
                p.op("dve", lambda e, wv=wv: e.tensor_tensor(out=Sst[:], in0=Sst[:], in1=wv[0], op=ALU.mult),
                     reads=["S", wv[1]], writes=["S"])
                p.op("dve", lambda e, kav=kav: e.scalar_tensor_tensor(out=Sst[:], in0=kav[0], scalar=sa[:, 0:1], in1=Sst[:], op0=ALU.mult, op1=ALU.add),
                     reads=["S", "sa", kav[1]], writes=["S"], force=True)
                p.op("dve", lambda e, ktv=ktv, t=t: e.scalar_tensor_tensor(out=Sst[:], in0=ktv[0], scalar=vT[:, t:t + 1], in1=Sst[:], op0=ALU.mult, op1=ALU.add),
                     reads=["S", "vT", ktv[1]], writes=["S"])
                p.op("dve", lambda e, rv=rv, t=t: e.scalar_tensor_tensor(out=junk[:], in0=Sst[:], scalar=1.0, in1=rv[0], op0=ALU.mult, op1=ALU.mult, accum_out=yT[:, t:t + 1]),
                     reads=["S", rv[1]], writes=["junk", f"yT{t // 512}"])
    gt = [_sb(nc, st, f"g_sb{i}", [128, 512]) for i in range(2)]
    bt = [_sb(nc, st, f"b_sb{i}", [128, 512]) for i in range(2)]
    yc = _sb(nc, st, "yc", [128, 512])
    sq = _sb(nc, st, "sq", [128, 512])
    rs = _sb(nc, st, "rs", [128, 512])
    ot = [_sb(nc, st, f"ot{i}", [128, 512]) for i in range(2)]
    for tt in range(T // 512):
        i = tt % 2
        tsl = slice(tt * 512, (tt + 1) * 512)
        p.dma("sp", gt[i][:], g_d[:, tsl], writes=[f"gt{i}"])
        p.dma("sp", bt[i][:], bonus_d[:, tsl], writes=[f"bt{i}"])
        pm, pmk = ps[6], "ps6"
        p.op("pe", lambda e, tsl=tsl: e.matmul(ps[6][:], lhsT=blk[:], rhs=yT[:, tsl], start=True, stop=True), reads=["blk", f"yT{tt}"], writes=["ps6"])
        p.op("dve", lambda e, tsl=tsl: e.scalar_tensor_tensor(out=yc[:], in0=ps[6][:], scalar=-1.0 / 64, in1=yT[:, tsl], op0=ALU.mult, op1=ALU.add),
             reads=["ps6", f"yT{tt}"], writes=["yc"])
        p.op("act", lambda e: e.activation(out=sq[:], in_=yc[:], func=AF.Square), reads=["yc"], writes=["sq"])
        p.op("pe", lambda e: e.matmul(ps[7][:], lhsT=blk[:], rhs=sq[:], start=True, stop=True), reads=["blk", "sq"], writes=["ps7"])
        p.op("dve", lambda e: e.tensor_scalar(out=rs[:], in0=ps[7][:], scalar1=1.0 / 64, scalar2=GN_EPS, op0=ALU.mult, op1=ALU.add), reads=["ps7"], writes=["rs"])
        p.op("act", lambda e: e.activation(out=rs[:], in_=rs[:], func=AF.Sqrt), reads=["rs"], writes=["rs"])
        p.op("dve", lambda e: e.reciprocal(out=rs[:], in_=rs[:]), reads=["rs"], writes=["rs"])
        p.op("dve", lambda e: e.tensor_tensor(out=yc[:], in0=yc[:], in1=rs[:], op=ALU.mult), reads=["yc", "rs"], writes=["yc"])
        p.op("dve", lambda e: e.tensor_scalar(out=yc[:], in0=yc[:], scalar1=gnv[:, 0:1], scalar2=gnv[:, 1:2], op0=ALU.mult, op1=ALU.add), reads=["yc", "gnv"], writes=["yc"])
        p.op("dve", lambda e, i=i: e.tensor_tensor(out=yc[:], in0=yc[:], in1=bt[i][:], op=ALU.add), reads=["yc", f"bt{i}"], writes=["yc"])
        p.op("dve", lambda e, i=i: e.tensor_tensor(out=ot[i][:], in0=yc[:], in1=gt[i][:], op=ALU.mult), reads=["yc", f"gt{i}"], writes=[f"ot{i}"])
        p.dma("sp", o_d[:, tsl], ot[i][:], reads=[f"ot{i}"])


def launch_rwkv(lc, I, T=S):
    sel = np.zeros((128, 128), np.float32)
    sel[0, :64] = 1
    sel[1, 64:] = 1
    blk = np.zeros((128, 128), np.float32)
    blk[:64, :64] = 1
    blk[64:, 64:] = 1
    in_maps = []
    nch = T // TCH
    for i in range(8):
        cq = slice(i * 128, (i + 1) * 128)
        q5 = np.stack([lc[i][n][:, :T] for n in ("wdec", "nkk", "kka", "kt", "rT")], 0)
        q5 = q5.reshape(5, 2, 64, nch, TCH).transpose(1, 3, 0, 4, 2)
        gnv = np.stack([I["gn_w"][0][cq], I["gn_b"][0][cq]], -1)
        in_maps.append({"bcin": np.ascontiguousarray(q5), "vT": np.ascontiguousarray(lc[i]["vrT"][:, :T]),
                        "g": np.ascontiguousarray(lc[i]["g"][:, :T]), "bonus": np.ascontiguousarray(lc[i]["bonus"][:, :T]),
                        "gnv": np.ascontiguousarray(gnv), "sel": sel, "blk": blk, "zer": np.zeros((126, 5, TCH, 64), np.float32)})
    res = _run(lambda nc, p, st: build_rwkv(nc, p, st, T), in_maps)
    return np.concatenate([r["o"].T for r in res], axis=1)


NEGB = 30000.0


def build_moba(nc, p, st, T=S):
    nb = T // 256
    nkt = T // 128
    qT_d = nc.dram_tensor("qT", [128, T], F32, kind="ExternalInput").ap()
    kT_d = nc.dram_tensor("kT", [128, T], F32, kind="ExternalInput").ap()
    v_d = nc.dram_tensor("v", [128, nkt, 128], F32, kind="ExternalInput").ap()
    E_d = nc.dram_tensor("E", [128, T], F32, kind="ExternalInput").ap()
    cm_d = nc.dram_tensor("cm", [128, 256], F32, kind="ExternalInput").ap()
    id_d = nc.dram_tensor("ident", [128, 128], F32, kind="ExternalInput").ap()
    on_d = nc.dram_tensor("ones", [128, 128], F32, kind="ExternalInput").ap()
    o_d = nc.dram_tensor("oT", [128, T], F32, kind="ExternalOutput").ap()
    r32 = lambda ap: ap.bitcast(F32R)
    qT = _sb(nc, st, "qT_sb", [128, T])
    kT = _sb(nc, st, "kT_sb", [128, T])
    va = _sb(nc, st, "va_sb", [128, nkt, 128])
    E = _sb(nc, st, "E_sb", [128, T])
    cm = _sb(nc, st, "cm_sb", [128, 256])
    ident = _sb(nc, st, "id_sb", [128, 128])
    ones = _sb(nc, st, "ones_sb", [128, 128])
    kmean = _sb(nc, st, "kmean", [128, 32])
    gsb = _sb(nc, st, "gsb", [128, 32])
    m8 = _sb(nc, st, "m8", [128, 8])
    bias = _sb(nc, st, "bias", [128, 128])
    biasT = [_sb(nc, st, f"biasT{i}", [128, 256]) for i in range(2)]
    pT = [_sb(nc, st, f"pT{i}", [128, 256]) for i in range(3)]
    osb = [_sb(nc, st, f"osb{i}", [128, 256]) for i in range(2)]
    rden = _sb(nc, st, "rden", [128, 256])
    s_ps = [_ps(nc, st, f"s_ps{i}") for i in range(2)]
    o_ps = [_ps(nc, st, f"o_ps{i}") for i in range(2)]
    d_ps = [_ps(nc, st, f"d_ps{i}") for i in range(2)]
    g_ps = _ps(nc, st, "g_ps")
    t_ps = _ps(nc, st, "t_ps")
    p.dma("pool", r32(qT[:]), r32(qT_d), writes=["qT"])
    p.dma("pool", r32(kT[:]), r32(kT_d), writes=["kT"])
    p.dma("pool", r32(va[:]), r32(v_d), writes=["va"])
    p.dma("pool", r32(E[:]), r32(E_d), writes=["E"])
    p.dma("pool", r32(cm[:]), r32(cm_d), writes=["cm"])
    p.dma("pool", r32(ident[:]), r32(id_d), writes=["ident"])
    p.dma("pool", r32(ones[:]), r32(on_d), writes=["ones"])
    p.op("dve", lambda e: e.tensor_reduce(out=kmean[:, 0:nb], in_=kT[:].bitcast(F32).rearrange("p (n k) -> p n k", k=256), axis=AX.X, op=ALU.add),
         reads=["kT"], writes=["kmean"])
    p.op("dve", lambda e: e.tensor_scalar(out=kmean[:, 0:nb], in0=kmean[:, 0:nb], scalar1=1.0 / 256, scalar2=None, op0=ALU.mult),
         reads=["kmean"], writes=["kmean"])
    p.op("dve", lambda e: e.memset(gsb[:], -1e30), writes=["gsb"])
    p.op("dve", lambda e: e.memset(bias[:], 0.0), writes=["bias"])
    scale = 128 ** -0.5
    pti = 0
    si = 0
    for b in range(nb):
        bT = biasT[b % 2]
        bTk = f"biasT{b % 2}"
        for j in range(2):
            qs = slice(b * 256 + j * 128, b * 256 + (j + 1) * 128)
            if b > 3:
                p.op("pe", lambda e, qs=qs: e.matmul(g_ps[:, 0:nb], lhsT=qT[:, qs], rhs=kmean[:, 0:nb], start=True, stop=True),
                     reads=["qT", "kmean"], writes=["g_ps"])
                p.op("dve", lambda e, b=b: e.tensor_copy(out=gsb[:, 0:b], in_=g_ps[:, 0:b]), reads=["g_ps", "gsb"], writes=["gsb"])
                p.op("dve", lambda e: e.max(out=m8[:], in_=gsb[:]), reads=["gsb"], writes=["m8"])
                p.op("dve", lambda e: e.tensor_scalar(out=bias[:, 0:32], in0=gsb[:], scalar1=m8[:, 2:3], scalar2=None, op0=ALU.is_ge),
                     reads=["gsb", "m8", "bias"], writes=["bias"], force=True)
                p.op("dve", lambda e: e.tensor_scalar(out=bias[:, 0:32], in0=bias[:, 0:32], scalar1=NEGB, scalar2=-NEGB, op0=ALU.mult, op1=ALU.add),
                     reads=["bias"], writes=["bias"])
            else:
                p.op("dve", lambda e: e.memset(bias[:, 0:32], -NEGB), reads=["bias"], writes=["bias"])
                if b > 0:
                    p.op("dve", lambda e, b=b: e.memset(bias[:, 0:b], 0.0), reads=["bias"], writes=["bias"])
            p.op("dve", lambda e, b=b: e.memset(bias[:, b:b + 1], 0.0), reads=["bias"], writes=["bias"])
            p.op("pe", lambda e: e.transpose(t_ps[:, 0:128], bias[:], ident[:].bitcast(F32)), reads=["bias", "ident"], writes=["t_ps"])
            p.op("act", lambda e, bT=bT, j=j: e.copy(out=r32(bT[:, j * 128:(j + 1) * 128]), in_=t_ps[:, 0:128]), reads=["t_ps"], writes=[bTk])
        op_ = o_ps[b % 2]
        opk = f"o_ps{b % 2}"
        dp_ = d_ps[b % 2]
        dpk = f"d_ps{b % 2}"
        nkt_b = 2 * b + 2
        for kt in range(nkt_b):
            sp_ = s_ps[si % 2]
            spk = f"s_ps{si % 2}"
            si += 1
            ks = slice(kt * 128, (kt + 1) * 128)
            own = kt >= 2 * b
            p.op("pe", lambda e, sp_=sp_, ks=ks, b=b: e.matmul(sp_[:, 0:256], lhsT=r32(kT[:, ks]), rhs=r32(qT[:, b * 256:(b + 1) * 256]), start=True, stop=False),
                 reads=["kT", "qT"], writes=[spk])
            p.op("pe", lambda e, sp_=sp_, ks=ks, bT=bT, own=own: e.matmul(sp_[:, 0:256], lhsT=r32(E[:, ks]), rhs=r32(bT[:]), start=False, stop=(not own)),
                 reads=["E", bTk], writes=[spk])
            if own:
                if kt == 2 * b:
                    p.op("pe", lambda e, sp_=sp_: e.matmul(sp_[:, 0:128], lhsT=ident[:].bitcast(F32), rhs=cm[:, 128:256].bitcast(F32), start=False, stop=True),
                         reads=["ident", "cm"], writes=[spk])
                else:
                    p.op("pe", lambda e, sp_=sp_: e.matmul(sp_[:, 0:256], lhsT=r32(ident[:]), rhs=r32(cm[:, 0:256]), start=False, stop=True),
                         reads=["ident", "cm"], writes=[spk])
            pt = pT[pti % 3]
            ptk = f"pT{pti % 3}"
            pti += 1
            p.op("act", lambda e, pt=pt, sp_=sp_: e.activation(out=r32(pt[:]), in_=sp_[:, 0:256], func=AF.Exp, scale=scale), reads=[spk], writes=[ptk])
            p.op("pe", lambda e, pt=pt, kt=kt, op_=op_, nkt_b=nkt_b: e.matmul(op_[:, 0:256], lhsT=r32(va[:, kt, :]), rhs=r32(pt[:]), start=(kt == 0), stop=(kt == nkt_b - 1)),
                 reads=[ptk, "va"], writes=[opk])
            p.op("pe", lambda e, pt=pt, kt=kt, dp_=dp_, nkt_b=nkt_b: e.matmul(dp_[:, 0:256], lhsT=r32(ones[:]), rhs=r32(pt[:]), start=(kt == 0), stop=(kt == nkt_b - 1)),
                 reads=[ptk, "ones"], writes=[dpk])
        ob_ = osb[b % 2]
        p.op("dve", lambda e, dp_=dp_: e.reciprocal(out=rden[:], in_=dp_[:, 0:256]), reads=[dpk], writes=["rden"])
        p.op("dve", lambda e, op_=op_, ob_=ob_: e.tensor_tensor(out=ob_[:], in0=op_[:, 0:256], in1=rden[:], op=ALU.mult), reads=[opk, "rden"], writes=[f"osb{b % 2}"])
        p.dma("sp", o_d[:, b * 256:(b + 1) * 256], ob_[:], reads=[f"osb{b % 2}"])


def launch_moba(lc, T=S):
    nkt = T // 128
    E = np.zeros((128, T), np.float32)
    for n in range(T // 256):
        E[n, n * 256:(n + 1) * 256] = 1
    kk = np.arange(128)
    cmc = np.where(kk[:, None] <= kk[None, :], 0.0, -NEGB).astype(np.float32)
    cm = np.concatenate([np.full((128, 128), -NEGB, np.float32), cmc], 1)
    ident = np.eye(128, dtype=np.float32)
    ones = np.ones((128, 128), np.float32)
    in_maps = []
    for i in range(8):
        v = lc[i]["vT"][:, :T].T.reshape(nkt, 128, 128).transpose(1, 0, 2)
        in_maps.append({"qT": np.ascontiguousarray(lc[i]["qT"][:, :T]), "kT": np.ascontiguousarray(lc[i]["kT"][:, :T]),
                        "v": np.ascontiguousarray(v), "E": E, "cm": cm, "ident": ident, "ones": ones})
    res = _run(lambda nc, p, st: build_moba(nc, p, st, T), in_maps)
    return np.concatenate([r["oT"].T for r in res], axis=1)


def build_merge(nc, p, st, nunit=2):
    TT = 512
    NT = nunit * TT
    hT_d = nc.dram_tensor("hT", [D, NT], F32, kind="ExternalInput").ap()
    oa_d = nc.dram_tensor("oaT", [1024, NT], F32, kind="ExternalInput").ap()
    or_d = nc.dram_tensor("orT", [1024, NT], F32, kind="ExternalInput").ap()
    wg_d = nc.dram_tensor("wg", [D, 4096], F32, kind="ExternalInput").ap()
    wua_d = nc.dram_tensor("wua", [1024, D], F32, kind="ExternalInput").ap()
    wur_d = nc.dram_tensor("wur", [1024, D], F32, kind="ExternalInput").ap()
    wo_d = nc.dram_tensor("wo", [D, D], F32, kind="ExternalInput").ap()
    y_d = nc.dram_tensor("yT", [D, NT], F32, kind="ExternalOutput").ap()
    r32 = lambda ap: ap.bitcast(F32R)
    hT = _sb(nc, st, "hT_sb", [128, 16, TT])
    oa = _sb(nc, st, "oa_sb", [128, 8, TT])
    orr = _sb(nc, st, "or_sb", [128, 8, TT])
    mix = _sb(nc, st, "mix_sb", [128, 16, TT])
    wb = [_sb(nc, st, f"wb{i}", [128, 48, 128]) for i in range(2)]
    wob = [_sb(nc, st, f"wob{i}", [128, 16, 128]) for i in range(2)]
    sg = [_sb(nc, st, f"sg{i}", [128, TT]) for i in range(2)]
    m12 = [_sb(nc, st, f"m12_{i}", [128, TT]) for i in range(2)]
    yo = [_sb(nc, st, f"yo{i}", [128, TT]) for i in range(2)]
    ps = [_ps(nc, st, f"ps{i}") for i in range(8)]
    wgv = wg_d.rearrange("(c p) n -> p c n", p=128)
    wuav = wua_d.rearrange("(c p) n -> p c n", p=128)
    wurv = wur_d.rearrange("(c p) n -> p c n", p=128)
    wov = wo_d.rearrange("(c p) n -> p c n", p=128)
    wcount = 0
    for u in range(nunit):
        ts_ = slice(u * TT, (u + 1) * TT)
        p.dma("pool", r32(hT[:]), r32(hT_d.rearrange("(c p) t -> p c t", p=128)[:, :, ts_]), writes=["hT"])
        p.dma("pool", r32(oa[:]), r32(oa_d.rearrange("(c p) t -> p c t", p=128)[:, :, ts_]), writes=["oa"])
        p.dma("pool", r32(orr[:]), r32(or_d.rearrange("(c p) t -> p c t", p=128)[:, :, ts_]), writes=["or"])
        for n in range(16):
            wi = wcount % 2
            wcount += 1
            W = wb[wi]
            wk = f"wb{wi}"
            ns = slice(n * 128, (n + 1) * 128)
            ns2 = slice(2048 + n * 128, 2048 + (n + 1) * 128)
            p.dma("pool", r32(W[:, 0:8, :]), r32(wgv[:, 0:8, ns]), writes=[wk + "a"])
            p.dma("pool", r32(W[:, 8:16, :]), r32(wgv[:, 8:16, ns]), writes=[wk + "b"])
            p.dma("pool", r32(W[:, 16:24, :]), r32(wgv[:, 0:8, ns2]), writes=[wk + "c"])
            p.dma("pool", r32(W[:, 24:32, :]), r32(wgv[:, 8:16, ns2]), writes=[wk + "d"])
            p.dma("pool", r32(W[:, 32:40, :]), r32(wuav[:, :, ns]), writes=[wk + "e"])
            p.dma("pool", r32(W[:, 40:48, :]), r32(wurv[:, :, ns]), writes=[wk + "f"])
            wkeys = [wk + x for x in "abcdef"]
            b0 = (n % 2) * 4
            pga, pgr, pua, pur = ps[b0], ps[b0 + 1], ps[b0 + 2], ps[b0 + 3]
            for c in range(16):
                p.op("pe", lambda e, c=c, W=W, pga=pga: e.matmul(pga[:], lhsT=r32(W[:, c, :]), rhs=r32(hT[:, c, :]), start=(c == 0), stop=(c == 15)),
                     reads=wkeys + ["hT"], writes=[f"ps{b0}"])
            for c in range(16):
                p.op("pe", lambda e, c=c, W=W, pgr=pgr: e.matmul(pgr[:], lhsT=r32(W[:, 16 + c, :]), rhs=r32(hT[:, c, :]), start=(c == 0), stop=(c == 15)),
                     reads=wkeys + ["hT"], writes=[f"ps{b0 + 1}"])
            for c in range(8):
                p.op("pe", lambda e, c=c, W=W, pua=pua: e.matmul(pua[:], lhsT=r32(W[:, 32 + c, :]), rhs=r32(oa[:, c, :]), start=(c == 0), stop=(c == 7)),
                     reads=wkeys + ["oa"], writes=[f"ps{b0 + 2}"])
            for c in range(8):
                p.op("pe", lambda e, c=c, W=W, pur=pur: e.matmul(pur[:], lhsT=r32(W[:, 40 + c, :]), rhs=r32(orr[:, c, :]), start=(c == 0), stop=(c == 7)),
                     reads=wkeys + ["or"], writes=[f"ps{b0 + 3}"])
            p.op("act", lambda e, pga=pga: e.activation(out=sg[0][:], in_=pga[:], func=AF.Sigmoid), reads=[f"ps{b0}"], writes=["sg0"])
            p.op("act", lambda e, pgr=pgr: e.activation(out=sg[1][:], in_=pgr[:], func=AF.Sigmoid), reads=[f"ps{b0 + 1}"], writes=["sg1"])
            p.op("dve", lambda e, pua=pua: e.tensor_tensor(out=m12[0][:], in0=pua[:], in1=sg[0][:], op=ALU.mult), reads=[f"ps{b0 + 2}", "sg0"], writes=["m0"])
            p.op("dve", lambda e, pur=pur: e.tensor_tensor(out=m12[1][:], in0=pur[:], in1=sg[1][:], op=ALU.mult), reads=[f"ps{b0 + 3}", "sg1"], writes=["m1"])
            p.op("dve", lambda e, n=n: e.tensor_tensor(out=r32(mix[:, n, :]), in0=m12[0][:], in1=m12[1][:], op=ALU.add), reads=["m0", "m1"], writes=[f"mix{n}"])
        for m in range(16):
            wi = m % 2
            ms = slice(m * 128, (m + 1) * 128)
            p.dma("pool", r32(wob[wi][:, 0:8, :]), r32(wov[:, 0:8, ms]), writes=[f"wob{wi}a"])
            p.dma("pool", r32(wob[wi][:, 8:16, :]), r32(wov[:, 8:16, ms]), writes=[f"wob{wi}b"])
            py = ps[m % 2]
            for c in range(16):
                p.op("pe", lambda e, c=c, wi=wi, py=py: e.matmul(py[:], lhsT=r32(wob[wi][:, c, :]), rhs=r32(mix[:, c, :]), start=(c == 0), stop=(c == 15)),
                     reads=[f"wob{wi}a", f"wob{wi}b", f"mix{c}"], writes=[f"ps{m % 2}"])
            p.op("act", lambda e, py=py, wi=wi: e.copy(out=yo[wi][:], in_=py[:]), reads=[f"ps{m % 2}"], writes=[f"yo{wi}"])
            p.dma("pool", y_d[ms, ts_], yo[wi][:], reads=[f"yo{wi}"])


def launch_merge(h1, o_att, o_rwkv, I):
    wg = np.ascontiguousarray(I["w_in"][0][:, 6144:10240])
    in_maps = []
    for i in range(8):
        ts_ = slice(i * 1024, (i + 1) * 1024)
        in_maps.append({"hT": np.ascontiguousarray(h1[ts_].T), "oaT": np.ascontiguousarray(o_att[ts_].T),
                        "orT": np.ascontiguousarray(o_rwkv[ts_].T), "wg": wg,
                        "wua": I["w_up_att"][0], "wur": I["w_up_rwkv"][0], "wo": I["w_o"][0]})
    res = _run(lambda nc, p, st: build_merge(nc, p, st, 2), in_maps)
    return np.concatenate([r["yT"].T for r in res], axis=0)


def build_router(nc, p, st, ntile=8):
    NT = ntile * 128
    hT_d = nc.dram_tensor("hT", [D, NT], F32, kind="ExternalInput").ap()
    wr_d = nc.dram_tensor("wr", [D, 72], F32, kind="ExternalInput").ap()
    br_d = nc.dram_tensor("br", [1, 72], F32, kind="ExternalInput").ap()
    o_d = nc.dram_tensor("o", [NT, 4], F32, kind="ExternalOutput").ap()
    hT = _sb(nc, st, "hT_sb", [128, 16, NT])
    wr = _sb(nc, st, "wr_sb", [128, 16, 72])
    br = _sb(nc, st, "br_sb", [128, 72])
    ps = [_ps(nc, st, f"ps{i}") for i in range(2)]
    p.dma("sp", hT[:], hT_d.rearrange("(c p) t -> p c t", p=128), writes=["hT"])
    p.dma("sp", wr[:], wr_d.rearrange("(c p) n -> p c n", p=128), writes=["wr"])
    p.dma("sp", br[:], br_d.partition_broadcast(128), writes=["br"])
    l_sb = _sb(nc, st, "l_sb", [128, 72])
    lem = _sb(nc, st, "lem", [128, 64])
    m8g = _sb(nc, st, "m8g", [128, 8])
    m8e = _sb(nc, st, "m8e", [128, 8])
    idx = _sb(nc, st, "idx", [128, 8], U32)
    sm = _sb(nc, st, "sm", [128, 8])
    junk = _sb(nc, st, "junk", [128, 8])
    pen = _sb(nc, st, "pen", [128, 8])
    res = [_sb(nc, st, f"res{i}", [128, 4]) for i in range(2)]
    for t in range(ntile):
        pt = ps[t % 2]
        ptk = f"ps{t % 2}"
        rs_ = res[t % 2]
        rk = f"res{t % 2}"
        for c in range(16):
            p.op("pe", lambda e, c=c, t=t, pt=pt: e.matmul(pt[:, 0:72], lhsT=hT[:, c, t * 128:(t + 1) * 128], rhs=wr[:, c, :], start=(c == 0), stop=(c == 15)),
                 reads=["hT", "wr"], writes=[ptk])
        p.op("dve", lambda e, pt=pt: e.tensor_tensor(out=l_sb[:], in0=pt[:, 0:72], in1=br[:], op=ALU.add), reads=[ptk, "br"], writes=["l"])
        p.op("dve", lambda e: e.max(out=m8g[:], in_=l_sb[:, 0:8]), reads=["l"], writes=["m8g"])
        p.op("dve", lambda e: e.tensor_scalar(out=sm[:, 0:1], in0=m8g[:, 0:1], scalar1=-1.0, scalar2=None, op0=ALU.mult), reads=["m8g"], writes=["sm0"])
        p.op("act", lambda e: e.activation(out=junk[:], in_=l_sb[:, 0:8], func=AF.Exp, bias=sm[:, 0:1], accum_out=sm[:, 1:2]), reads=["l", "sm0"], writes=["junk", "sm1"])
        p.op("dve", lambda e: e.reciprocal(out=sm[:, 2:3], in_=sm[:, 1:2]), reads=["sm1"], writes=["sm2"])
        p.op("dve", lambda e: e.tensor_scalar(out=pen[:], in0=l_sb[:, 0:8], scalar1=m8g[:, 0:1], scalar2=None, op0=ALU.is_ge), reads=["l", "m8g"], writes=["pen"], force=True)
        p.op("dve", lambda e: e.tensor_scalar(out=pen[:], in0=pen[:], scalar1=1e30, scalar2=-1e30, op0=ALU.mult, op1=ALU.add), reads=["pen"], writes=["pen"])
        for g in range(8):
            p.op("dve", lambda e, g=g: e.tensor_scalar(out=lem[:, g * 8:(g + 1) * 8], in0=l_sb[:, 8 + g * 8:16 + g * 8], scalar1=pen[:, g:g + 1], scalar2=None, op0=ALU.add),
                 reads=["l", "pen"], writes=["lem"], force=(g == 0))
        p.op("dve", lambda e: e.max(out=m8e[:], in_=lem[:]), reads=["lem"], writes=["m8e"])
        p.op("dve", lambda e: e.max_index(out=idx[:], in_max=m8e[:], in_values=lem[:]), reads=["m8e", "lem"], writes=["idx"], force=True)
        p.op("dve", lambda e, rs_=rs_: e.tensor_copy(out=rs_[:, 0:2], in_=idx[:, 0:2]), reads=["idx"], writes=[rk + "a"], force=True)
        p.op("dve", lambda e: e.tensor_tensor(out=sm[:, 3:4], in0=m8e[:, 0:1], in1=m8e[:, 1:2], op=ALU.subtract), reads=["m8e"], writes=["sm3"], force=True)
        p.op("act", lambda e: e.activation(out=sm[:, 4:5], in_=sm[:, 3:4], func=AF.Sigmoid), reads=["sm3"], writes=["sm4"])
        p.op("dve", lambda e, rs_=rs_: e.tensor_tensor(out=rs_[:, 2:3], in0=sm[:, 4:5], in1=sm[:, 2:3], op=ALU.mult), reads=["sm4", "sm2"], writes=[rk + "b"], force=True)
        p.op("dve", lambda e, rs_=rs_: e.tensor_tensor(out=rs_[:, 3:4], in0=sm[:, 2:3], in1=rs_[:, 2:3], op=ALU.subtract), reads=["sm2", rk + "b"], writes=[rk + "c"], force=True)
        p.dma("sp", o_d[t * 128:(t + 1) * 128, :], rs_[:], reads=[rk + "a", rk + "b", rk + "c"])


def launch_router(h2, I):
    wr = np.ascontiguousarray(np.concatenate([I["w_rg"][0], I["w_re"][0]], 1))
    br = np.ascontiguousarray(np.concatenate([I["b_rg"][0], I["b_re"][0]])[None, :])
    in_maps = [{"hT": np.ascontiguousarray(h2[i * 1024:(i + 1) * 1024].T), "wr": wr, "br": br} for i in range(8)]
    res = _run(lambda nc, p, st: build_router(nc, p, st, 8), in_maps)
    o = np.concatenate([r["o"] for r in res], axis=0)
    return o[:, 0:2].astype(np.int64), o[:, 2:4]


def build_experts(nc, p, st, cap):
    xT_d = nc.dram_tensor("xT", [8, D, cap], F32, kind="ExternalInput").ap()
    wg_d = nc.dram_tensor("wg", [8, D, 512], F32, kind="ExternalInput").ap()
    wu_d = nc.dram_tensor("wu", [8, D, 512], F32, kind="ExternalInput").ap()
    wd_d = nc.dram_tensor("wd", [8, 512, D], F32, kind="ExternalInput").ap()
    y_d = nc.dram_tensor("yT", [8, D, cap], F32, kind="ExternalOutput").ap()
    r32 = lambda ap: ap.bitcast(F32R)
    xT = _sb(nc, st, "xT_sb", [128, 16, cap])
    Wg = _sb(nc, st, "Wg_sb", [128, 16, 512])
    Wu = _sb(nc, st, "Wu_sb", [128, 16, 512])
    Wd = _sb(nc, st, "Wd_sb", [128, 4, D])
    hid = _sb(nc, st, "hid_sb", [128, 4, cap])
    sg = [_sb(nc, st, f"sg{i}", [128, cap]) for i in range(2)]
    yo = [_sb(nc, st, f"yo{i}", [128, cap]) for i in range(3)]
    ps = [_ps(nc, st, f"ps{i}") for i in range(8)]
    for ex in range(8):
        xv = xT_d[ex].rearrange("(c p) t -> p c t", p=128)
        for h in range(4):
            p.dma("pool", r32(xT[:, h * 4:(h + 1) * 4, :]), r32(xv[:, h * 4:(h + 1) * 4, :]), writes=[f"xT{h}"])
        gv = wg_d[ex].rearrange("(c p) n -> p c n", p=128)
        uv = wu_d[ex].rearrange("(c p) n -> p c n", p=128)
        dv = wd_d[ex].rearrange("(c p) n -> p c n", p=128)
        for h in range(8):
            p.dma("pool", r32(Wg[:, h * 2:(h + 1) * 2, :]), r32(gv[:, h * 2:(h + 1) * 2, :]), writes=[f"Wg{h}"])
        for h in range(8):
            p.dma("pool", r32(Wu[:, h * 2:(h + 1) * 2, :]), r32(uv[:, h * 2:(h + 1) * 2, :]), writes=[f"Wu{h}"])
        for h in range(4):
            p.dma("pool", r32(Wd[:, h, 0:1024]), r32(dv[:, h, 0:1024]), writes=[f"Wd{h}a"])
            p.dma("pool", r32(Wd[:, h, 1024:2048]), r32(dv[:, h, 1024:2048]), writes=[f"Wd{h}b"])
        for f in range(4):
            pg = ps[(f % 2) * 2]
            pu = ps[(f % 2) * 2 + 1]
            pgk = f"ps{(f % 2) * 2}"
            puk = f"ps{(f % 2) * 2 + 1}"
            fs = slice(f * 128, (f + 1) * 128)
            for c in range(16):
                p.op("pe", lambda e, c=c, pg=pg, fs=fs: e.matmul(pg[:, 0:cap], lhsT=r32(Wg[:, c, fs]), rhs=r32(xT[:, c, :]), start=(c == 0), stop=(c == 15)),
                     reads=[f"Wg{c // 2}", f"xT{c // 4}"], writes=[pgk])
            for c in range(16):
                p.op("pe", lambda e, c=c, pu=pu, fs=fs: e.matmul(pu[:, 0:cap], lhsT=r32(Wu[:, c, fs]), rhs=r32(xT[:, c, :]), start=(c == 0), stop=(c == 15)),
                     reads=[f"Wu{c // 2}", f"xT{c // 4}"], writes=[puk])
            s_ = sg[f % 2]
            p.op("act", lambda e, s_=s_, pg=pg: e.activation(out=s_[:], in_=pg[:, 0:cap], func=AF.Silu), reads=[pgk], writes=[f"sg{f % 2}"])
            p.op("dve", lambda e, s_=s_, pu=pu, f=f: e.tensor_tensor(out=r32(hid[:, f, :]), in0=pu[:, 0:cap], in1=s_[:], op=ALU.mult),
                 reads=[puk, f"sg{f % 2}"], writes=[f"hid{f}"])
        for d in range(16):
            py = ps[4 + d % 4]
            pyk = f"ps{4 + d % 4}"
            ds_ = slice(d * 128, (d + 1) * 128)
            for f in range(4):
                p.op("pe", lambda e, f=f, py=py, ds_=ds_: e.matmul(py[:, 0:cap], lhsT=r32(Wd[:, f, ds_]), rhs=r32(hid[:, f, :]), start=(f == 0), stop=(f == 3)),
                     reads=[f"Wd{f}a", f"Wd{f}b", f"hid{f}"], writes=[pyk])
            yb = yo[d % 3]
            p.op("act" if d % 2 else "dve", (lambda e, yb=yb, py=py: e.copy(out=yb[:], in_=py[:, 0:cap])) if d % 2 else (lambda e, yb=yb, py=py: e.tensor_copy(out=yb[:], in_=py[:, 0:cap])),
                 reads=[pyk], writes=[f"yo{d % 3}"])
            p.dma("sp", y_d[ex, ds_, :], yb[:], reads=[f"yo{d % 3}"])


def launch_experts(h2, eidx, I):
    N = h2.shape[0]
    flat_e = eidx.reshape(-1)
    flat_t = np.repeat(np.arange(N), 2)
    order = np.argsort(flat_e, kind="stable")
    counts = np.bincount(flat_e, minlength=64)
    cap = int(max(256, -(-counts.max() // 128) * 128))
    starts = np.cumsum(counts) - counts
    xT = np.zeros((64, D, cap), np.float32)
    pos_of = np.zeros(2 * N, np.int64)
    for e in range(64):
        sl = order[starts[e]:starts[e] + counts[e]]
        xT[e, :, :counts[e]] = h2[flat_t[sl]].T
        pos_of[sl] = np.arange(counts[e])
    in_maps = [{"xT": np.ascontiguousarray(xT[g * 8:(g + 1) * 8]), "wg": np.ascontiguousarray(I["w_gate_e"][0][g * 8:(g + 1) * 8]),
                "wu": np.ascontiguousarray(I["w_up_e"][0][g * 8:(g + 1) * 8]), "wd": np.ascontiguousarray(I["w_down_e"][0][g * 8:(g + 1) * 8])} for g in range(8)]
    res = _run(lambda nc, p, st: build_experts(nc, p, st, cap), in_maps)
    yT = np.concatenate([r["yT"] for r in res], axis=0)
    yflat = yT[flat_e, :, pos_of]
    yflat = yflat.reshape(N, 2, D)
    return np.ascontiguousarray(yflat[:, 0]), np.ascontiguousarray(yflat[:, 1])


def build_combine(nc, p, st, ntile=8):
    n = ntile * 128
    ya_d = nc.dram_tensor("ya", [n, D], F32, kind="ExternalInput").ap()
    yb_d = nc.dram_tensor("yb", [n, D], F32, kind="ExternalInput").ap()
    w_d = nc.dram_tensor("w", [n, 2], F32, kind="ExternalInput").ap()
    g = nc.dram_tensor("g", [1, D], F32, kind="ExternalInput").ap()
    s = nc.dram_tensor("s", [1, D], F32, kind="ExternalInput").ap()
    base = nc.dram_tensor("base", [n, D], F32, kind="ExternalInput").ap()
    o = nc.dram_tensor("o", [n, D], F32, kind="ExternalOutput").ap()
    gb = _sb(nc, st, "gb", [128, D])
    A = _sb(nc, st, "A", [128, D])
    p.dma("sp", gb[:], g.partition_broadcast(128), writes=["gb"])
    p.dma("sp", A[:], s.partition_broadcast(128), writes=["A"])
    p.op("dve", lambda e: e.tensor_tensor(out=A[:], in0=A[:], in1=gb[:], op=ALU.mult), reads=["gb", "A"], writes=["A"])
    ya = [_sb(nc, st, f"ya{i}", [128, D]) for i in range(2)]
    yb = [_sb(nc, st, f"yb{i}", [128, D]) for i in range(2)]
    bt = [_sb(nc, st, f"bt{i}", [128, D]) for i in range(2)]
    wt = [_sb(nc, st, f"wt{i}", [128, 2]) for i in range(2)]
    junk = _sb(nc, st, "junk", [128, D])
    ot = [_sb(nc, st, f"ot{i}", [128, D]) for i in range(2)]
    ss = [_sb(nc, st, f"ss{i}", [128, 4]) for i in range(2)]
    for t in range(ntile):
        i = t % 2
        rows = slice(t * 128, (t + 1) * 128)
        p.dma("sp", ya[i][:], ya_d[rows, :], writes=[f"ya{i}"])
        p.dma("sp", yb[i][:], yb_d[rows, :], writes=[f"yb{i}"])
        p.dma("sp", bt[i][:], base[rows, :], writes=[f"bt{i}"])
        p.dma("sp", wt[i][:], w_d[rows, :], writes=[f"wt{i}"])
        p.op("dve", lambda e, i=i: e.tensor_scalar(out=ya[i][:], in0=ya[i][:], scalar1=wt[i][:, 0:1], scalar2=None, op0=ALU.mult),
             reads=[f"ya{i}", f"wt{i}"], writes=[f"ya{i}"])
        p.op("dve", lambda e, i=i: e.scalar_tensor_tensor(out=ya[i][:], in0=yb[i][:], scalar=wt[i][:, 1:2], in1=ya[i][:], op0=ALU.mult, op1=ALU.add),
             reads=[f"ya{i}", f"yb{i}", f"wt{i}"], writes=[f"ya{i}"])
        p.op("act", lambda e, i=i: e.activation(out=junk[:], in_=ya[i][:], func=AF.Square, accum_out=ss[i][:, 0:1]),
             reads=[f"ya{i}"], writes=["junk", f"ss{i}"])
        p.op("dve", lambda e, i=i: e.tensor_scalar(out=ss[i][:, 1:2], in0=ss[i][:, 0:1], scalar1=1.0 / D, scalar2=EPS, op0=ALU.mult, op1=ALU.add),
             reads=[f"ss{i}"], writes=[f"ss{i}"])
        p.op("act", lambda e, i=i: e.activation(out=ss[i][:, 2:3], in_=ss[i][:, 1:2], func=AF.Sqrt), reads=[f"ss{i}"], writes=[f"ss{i}"])
        p.op("dve", lambda e, i=i: e.reciprocal(out=ss[i][:, 3:4], in_=ss[i][:, 2:3]), reads=[f"ss{i}"], writes=[f"ss{i}"])
        p.op("dve", lambda e, i=i: e.scalar_tensor_tensor(out=ot[i][:], in0=ya[i][:], scalar=ss[i][:, 3:4], in1=A[:], op0=ALU.mult, op1=ALU.mult),
             reads=[f"ya{i}", f"ss{i}", "A"], writes=[f"ot{i}"], force=True)
        p.op("pool", lambda e, i=i: e.tensor_tensor(out=ot[i][:], in0=ot[i][:], in1=bt[i][:], op=ALU.add),
             reads=[f"ot{i}", f"bt{i}"], writes=[f"ot{i}"])
        p.dma("pool", o[rows, :], ot[i][:], reads=[f"ot{i}"])


def launch_combine(ya, yb, w, g, s, base):
    n = ya.shape[0] // 8
    in_maps = [{"ya": np.ascontiguousarray(ya[i * n:(i + 1) * n]), "yb": np.ascontiguousarray(yb[i * n:(i + 1) * n]),
                "w": np.ascontiguousarray(w[i * n:(i + 1) * n]), "g": np.ascontiguousarray(g[None, :]),
                "s": np.ascontiguousarray(s[None, :]), "base": np.ascontiguousarray(base[i * n:(i + 1) * n])} for i in range(8)]
    res = _run(lambda nc, p, st: build_combine(nc, p, st, n // 128), in_maps)
    return np.concatenate([r["o"] for r in res], axis=0)


def kernel(**inputs):
    I = {k: np.asarray(v) for k, v in inputs.items()}
    x = I["x"][0]
    ada = launch_ada(I["c"][0], I["w_ada"][0], I["b_ada"][0])
    sh1, sc1, gt1, sh2, sc2, gt2 = np.split(ada, 6)
    h1 = launch_norm(x, I["g_pre_mix"][0], sc1, 1.0, bv=sh1)
    lc = launch_inproj(h1, I)
    o_att = launch_moba(lc)
    o_rwkv = launch_rwkv_chunked(lc, I)
    y1 = launch_merge(h1, o_att, o_rwkv, I)
    x1 = launch_norm(y1, I["g_post_mix"][0], gt1, 0.0, base=x)
    h2 = launch_norm(x1, I["g_pre_ffn"][0], sc2, 1.0, bv=sh2)
    eidx, ew = launch_router(h2, I)
    ya, yb = launch_experts(h2, eidx, I)
    out = launch_combine(ya, yb, ew, I["g_post_ffn"][0], gt2, x1)
    return out[None].astype(np.float32)


CH_C = 64
SEG = 512


def build_rwkv_chunked(nc, p, st, T=S):
    nseg = T // SEG
    cps = SEG // CH_C
    F_d = nc.dram_tensor("F", [64, 6, 2, T], F32, kind="ExternalInput").ap()
    gb_d = nc.dram_tensor("gb", [64, 2, 2, T], F32, kind="ExternalInput").ap()
    gnv_d = nc.dram_tensor("gnv", [64, 2, 2], F32, kind="ExternalInput").ap()
    id_d = nc.dram_tensor("ident", [128, 128], F32, kind="ExternalInput").ap()
    msk_d = nc.dram_tensor("msk", [64, 10, 64], F32, kind="ExternalInput").ap()
    rm_d = nc.dram_tensor("rmask", [64, 2 * SEG], F32, kind="ExternalInput").ap()
    on_d = nc.dram_tensor("ones64", [64, 64], F32, kind="ExternalInput").ap()
    o_d = nc.dram_tensor("o", [64, 2, T], F32, kind="ExternalOutput").ap()

    ident = _sb(nc, st, "ident_sb", [128, 128])
    msk = _sb(nc, st, "msk_sb", [64, 10, 64])
    rmask = _sb(nc, st, "rmask_sb", [64, 2 * SEG])
    ones64 = _sb(nc, st, "ones64_sb", [64, 64])
    gnv = _sb(nc, st, "gnv_sb", [64, 2, 2])
    for dst, src, k in [(ident, id_d, "ident"), (msk, msk_d, "msk"),
                        (rmask, rm_d, "rmask"), (ones64, on_d, "ones64"), (gnv, gnv_d, "gnv")]:
        p.dma("sp", dst[:], src, writes=[k])
    Fin = [_sb(nc, st, f"Fin{i}", [64, 6, 2, SEG]) for i in range(2)]
    gbin = [_sb(nc, st, f"gbin{i}", [64, 2, 2, SEG]) for i in range(1)] * 2
    names = ["logw", "cum", "eg", "einv", "egm", "dte", "Af", "Bf", "Kf", "Rf", "Bh", "Kh"]
    tmpn = ["logw", "cum", "eg", "einv", "egm", "dte"]
    Wtmp = {n: _sb(nc, st, f"wt_{n}", [64, 2, SEG]) for n in tmpn}
    W_ = []
    for i in range(2):
        d_ = dict(Wtmp)
        for n in names:
            if n not in tmpn:
                d_[n] = _sb(nc, st, f"w{i}_{n}", [64, 2, SEG])
        W_.append(d_)
    gC = [_sb(nc, st, f"gC{i}", [64, 2, cps]) for i in range(2)]
    NSL = 4
    TM = [_sb(nc, st, f"TM{i}", [64, 4, 128]) for i in range(NSL)]
    MS = [_sb(nc, st, f"MS{i}", [64, 10, 64]) for i in range(NSL)]
    MQ = [[_sb(nc, st, f"MQ{i}_{j}", [64, 4, 64]) for j in range(2)] for i in range(NSL)]
    XW = [_sb(nc, st, f"XW{i}", [64, 2, 128]) for i in range(NSL)]
    NCH = 6
    CHb = [_sb(nc, st, f"CHb{i}", [64, 4, 128]) for i in range(NCH)]
    Z = _sb(nc, st, "Zst", [64, 2, 64])
    Z2 = _sb(nc, st, "Zst2", [64, 2, 64])
    YT = [_sb(nc, st, f"YT{i}", [64, 2, SEG]) for i in range(2)]
    ps = [_ps(nc, st, f"ps{i}") for i in range(8)]
    psi = [0]

    def nb():
        i = psi[0] % 8
        psi[0] += 1
        return ps[i], f"ps{i}"

    p.op("dve", lambda e: e.memset(Z[:], 0.0), writes=["Z"])

    def load_seg(sg):
        i = sg % 2
        p.dma("sp", Fin[i][:], F_d[:, :, :, sg * SEG:(sg + 1) * SEG], writes=[f"Fin{i}"])

    def prep_seg(sg):
        i = sg % 2
        Fi = Fin[i]
        w = W_[i]
        fk = f"Fin{i}"
        k = lambda n: (f"wt_{n}" if n in tmpn else f"w{i}_{n}")
        fl = lambda ap: ap.rearrange("p h t -> p (h t)")
        p.op("act", lambda e: e.activation(out=fl(w["logw"][:]), in_=fl(Fi[:, 0]), func=AF.Ln), reads=[fk], writes=[k("logw")])
        p.op("dve", lambda e: e.tensor_tensor_scan(out=fl(w["cum"][:]), data0=rmask[:], data1=fl(w["logw"][:]), initial=0.0, op0=ALU.mult, op1=ALU.add),
             reads=["rmask", k("logw")], writes=[k("cum")])
        p.op("act", lambda e: e.activation(out=fl(w["eg"][:]), in_=fl(w["cum"][:]), func=AF.Exp), reads=[k("cum")], writes=[k("eg")])
        p.op("act", lambda e: e.activation(out=fl(w["einv"][:]), in_=fl(w["cum"][:]), func=AF.Exp, scale=-1.0), reads=[k("cum")], writes=[k("einv")])
        p.op("dve", lambda e: e.tensor_tensor(out=fl(w["egm"][:]), in0=fl(w["cum"][:]), in1=fl(w["logw"][:]), op=ALU.subtract), reads=[k("cum"), k("logw")], writes=[k("egm")])
        p.op("act", lambda e: e.activation(out=fl(w["egm"][:]), in_=fl(w["egm"][:]), func=AF.Exp), reads=[k("egm")], writes=[k("egm")])
        cumv = w["cum"][:].rearrange("p h (c t) -> p (h c) t", t=CH_C)
        p.op("dve", lambda e: e.tensor_tensor(out=w["dte"][:].rearrange("p h (c t) -> p (h c) t", t=CH_C), in0=cumv[:, :, CH_C - 1:CH_C].to_broadcast([64, 2 * cps, CH_C]), in1=cumv, op=ALU.subtract),
             reads=[k("cum")], writes=[k("dte")])
        p.op("act", lambda e: e.activation(out=fl(w["dte"][:]), in_=fl(w["dte"][:]), func=AF.Exp), reads=[k("dte")], writes=[k("dte")])
        for out_n, a_idx, b_n, eng in [("Af", 1, "egm", "dve"), ("Bf", 2, "einv", "pool"), ("Kf", 3, "einv", "dve"),
                                       ("Rf", 4, "eg", "pool"), ("Bh", 2, "dte", "dve"), ("Kh", 3, "dte", "pool")]:
            p.op(eng, lambda e, out_n=out_n, a_idx=a_idx, b_n=b_n: e.tensor_tensor(out=fl(w[out_n][:]), in0=fl(Fi[:, a_idx]), in1=fl(w[b_n][:]), op=ALU.mult),
                 reads=[fk, k(b_n)], writes=[k(out_n)])
        egC = w["eg"][:].rearrange("p h (c t) -> p h c t", t=CH_C)[:, :, :, CH_C - 1]
        p.op("act", lambda e: e.copy(out=gC[i][:], in_=egC), reads=[k("eg")], writes=[f"gC{i}"])

    def pre_stages(sg, cl, slot, chslot):
        i = sg % 2
        w = W_[i]
        Fi = Fin[i]
        k = lambda n: f"w{i}_{n}"
        cs = slice(cl * CH_C, (cl + 1) * CH_C)
        tm, ms, mq, xw, chb = TM[slot], MS[slot], MQ[slot], XW[slot], CHb[chslot]
        tmk, msk_, xwk, chk = f"TM{slot}", f"MS{slot}", f"XW{slot}", f"CHb{chslot}"
        stages = []

        def s1():
            b, bkey = nb()
            for q, (src, skey) in enumerate([(w["Af"], k("Af")), (w["Bh"], k("Bh")), (w["Kh"], k("Kh")), (None, f"Fin{i}")]):
                for h in range(2):
                    in_ap = Fi[:, 5, h, cs] if src is None else src[:, h, cs]
                    p.op("pe", lambda e, q=q, h=h, in_ap=in_ap: e.transpose(b[0:64, q * 128 + h * 64:q * 128 + (h + 1) * 64], in_ap, ident[0:64, 0:64]),
                         reads=[skey, "ident"], writes=[bkey])
            p.op("act", lambda e: e.copy(out=tm[:].rearrange("p a b -> p (a b)"), in_=b[0:64, :]), reads=[bkey], writes=[tmk])
        stages.append(s1)

        def s2a():
            b, bkey = nb()
            for h in range(2):
                pb = slice(h * 64, (h + 1) * 64)
                for col, (l, lk, r_, rk) in [(0 + h, (w["Bf"], k("Bf"), w["Af"], k("Af"))), (2 + h, (w["Kf"], k("Kf"), w["Af"], k("Af"))),
                                             (4 + h, (w["Af"], k("Af"), w["Bf"], k("Bf")))]:
                    p.op("pe", lambda e, col=col, l=l, r_=r_, h=h: e.matmul(b[0:64, col * 64:(col + 1) * 64], lhsT=l[:, h, cs], rhs=r_[:, h, cs], start=True, stop=True),
                         reads=[lk, rk], writes=[bkey])
            p.op("dve", lambda e: e.tensor_tensor(out=ms[:, 0:6, :].rearrange("p a b -> p (a b)"), in0=b[0:64, 0:384], in1=msk[:, 0:6, :].rearrange("p a b -> p (a b)"), op=ALU.mult),
                 reads=[bkey, "msk"], writes=[msk_ + "a"])
        stages.append(s2a)

        def s2b():
            b, bkey = nb()
            for h in range(2):
                pb = slice(h * 64, (h + 1) * 64)
                for col, (l, lk) in [(0 + h, (w["Bf"], k("Bf"))), (2 + h, (w["Kf"], k("Kf")))]:
                    p.op("pe", lambda e, col=col, l=l, h=h: e.matmul(b[0:64, col * 64:(col + 1) * 64], lhsT=l[:, h, cs], rhs=w["Rf"][:, h, cs], start=True, stop=True),
                         reads=[lk, k("Rf")], writes=[bkey])
            p.op("dve", lambda e: e.tensor_tensor(out=ms[:, 6:10, :].rearrange("p a b -> p (a b)"), in0=b[0:64, 0:256], in1=msk[:, 6:10, :].rearrange("p a b -> p (a b)"), op=ALU.mult),
                 reads=[bkey, "msk"], writes=[msk_ + "b"])
        stages.append(s2b)

        def s3():
            b, bkey = nb()
            for h in range(2):
                p.op("pe", lambda e, h=h: e.matmul(b[0:64, h * 64:(h + 1) * 64], lhsT=ms[:, 2 + h, :], rhs=tm[:, 3, h * 64:(h + 1) * 64], start=True, stop=True),
                     reads=[msk_ + "a", tmk], writes=[bkey])
            p.op("act", lambda e: e.copy(out=xw[:, :, 64:128], in_=b[0:64, 0:128].rearrange("p (h v) -> p h v", h=2)), reads=[bkey], writes=[xwk + "x"])
            p.op("act", lambda e: e.copy(out=xw[:, :, 0:64], in_=tm[:, 0, :].rearrange("p (h v) -> p h v", h=2)), reads=[tmk], writes=[xwk + "w"])
        stages.append(s3)

        def mk_level(j):
            def lv():
                if j == 0:
                    MT = [ms[:, 0, :], ms[:, 1, :]]
                    M = [ms[:, 4, :], ms[:, 5, :]]
                    mkey = msk_ + "a"
                else:
                    q = mq[j % 2]
                    MT = [q[:, 0, :], q[:, 1, :]]
                    M = [q[:, 2, :], q[:, 3, :]]
                    mkey = f"MQ{slot}_{j % 2}"
                b, bkey = nb()
                for h in range(2):
                    p.op("pe", lambda e, h=h: e.matmul(b[0:64, h * 128:(h + 1) * 128], lhsT=MT[h], rhs=xw[:, h, :], start=True, stop=True),
                         reads=[mkey, xwk + "x", xwk + "w"], writes=[bkey])
                if j < 5:
                    b2, b2key = nb()
                    for h in range(2):
                        p.op("pe", lambda e, h=h: e.matmul(b2[0:64, h * 64:(h + 1) * 64], lhsT=M[h], rhs=MT[h], start=True, stop=True), reads=[mkey], writes=[b2key])
                        p.op("pe", lambda e, h=h: e.matmul(b2[0:64, (2 + h) * 64:(3 + h) * 64], lhsT=MT[h], rhs=M[h], start=True, stop=True), reads=[mkey], writes=[b2key])
                p.op("dve", lambda e: e.tensor_tensor(out=xw[:].rearrange("p a b -> p (a b)"), in0=b[0:64, 0:256], in1=xw[:].rearrange("p a b -> p (a b)"), op=ALU.add),
                     reads=[bkey, xwk + "x", xwk + "w"], writes=[xwk + "x", xwk + "w"])
                if j < 5:
                    nq = mq[(j + 1) % 2]
                    p.op("act", lambda e: e.copy(out=nq[:].rearrange("p a b -> p (a b)"), in_=b2[0:64, 0:256]), reads=[b2key], writes=[f"MQ{slot}_{(j + 1) % 2}"])
            return lv
        for j in range(6):
            stages.append(mk_level(j))

        def s5():
            b, bkey = nb()
            xk = [xwk + "x", xwk + "w"]
            for h in range(2):
                pb = slice(h * 64, (h + 1) * 64)
                hs = slice(h * 64, (h + 1) * 64)
                Wh = xw[:, h, 0:64]
                Xh = xw[:, h, 64:128]
                p.op("pe", lambda e, Wh=Wh, hs=hs, h=h: e.matmul(b[0:64, h * 64:(h + 1) * 64], lhsT=Wh, rhs=tm[:, 1, hs], start=True, stop=True),
                     reads=xk + [tmk], writes=[bkey])
                p.op("pe", lambda e, Xh=Xh, hs=hs, h=h: e.matmul(b[0:64, 128 + h * 64:128 + (h + 1) * 64], lhsT=tm[:, 1, hs], rhs=Xh, start=True, stop=False),
                     reads=xk + [tmk], writes=[bkey])
                p.op("pe", lambda e, hs=hs, h=h: e.matmul(b[0:64, 128 + h * 64:128 + (h + 1) * 64], lhsT=tm[:, 2, hs], rhs=tm[:, 3, hs], start=False, stop=True),
                     reads=[tmk], writes=[bkey])
                p.op("pe", lambda e, Wh=Wh, h=h: e.matmul(b[0:64, 256 + h * 64:256 + (h + 1) * 64], lhsT=Wh, rhs=ms[:, 6 + h, :], start=True, stop=False),
                     reads=xk + [msk_ + "b"], writes=[bkey])
                p.op("pe", lambda e, h=h: e.matmul(b[0:64, 256 + h * 64:256 + (h + 1) * 64], lhsT=ident[0:64, 0:64], rhs=w["Rf"][:, h, cs], start=False, stop=True),
                     reads=["ident", k("Rf")], writes=[bkey])
                p.op("pe", lambda e, Xh=Xh, h=h: e.matmul(b[0:64, 384 + h * 64:384 + (h + 1) * 64], lhsT=Xh, rhs=ms[:, 6 + h, :], start=True, stop=False),
                     reads=xk + [msk_ + "b"], writes=[bkey])
                p.op("pe", lambda e, hs=hs, h=h: e.matmul(b[0:64, 384 + h * 64:384 + (h + 1) * 64], lhsT=tm[:, 3, hs], rhs=ms[:, 8 + h, :], start=False, stop=True),
                     reads=[tmk, msk_ + "b"], writes=[bkey])
            p.op("act", lambda e: e.copy(out=chb[:].rearrange("p a b -> p (a b)"), in_=b[0:64, :]), reads=[bkey], writes=[chk])
        stages.append(s5)
        return stages

    def chain_step(sg, cl, chslot):
        i = sg % 2
        chb = CHb[chslot]
        chk = f"CHb{chslot}"
        yt = YT[i]
        b, bkey = nb()
        for h in range(2):
            hs = slice(h * 64, (h + 1) * 64)
            p.op("pe", lambda e, h=h, hs=hs: e.matmul(b[0:64, hs], lhsT=chb[:, 0, hs], rhs=Z[:, h, :], start=True, stop=True), reads=[chk, "Z"], writes=[bkey])
            p.op("pe", lambda e, h=h, hs=hs: e.matmul(b[0:64, 128 + h * 64:128 + (h + 1) * 64], lhsT=Z[:, h, :], rhs=chb[:, 2, hs], start=True, stop=True), reads=[chk, "Z"], writes=[bkey])
        p.op("dve", lambda e: e.tensor_tensor(out=yt[:, :, cl * CH_C:(cl + 1) * CH_C], in0=b[0:64, 128:256].rearrange("p (h t) -> p h t", h=2),
                                              in1=chb[:, 3, :].rearrange("p (h t) -> p h t", h=2), op=ALU.add),
             reads=[bkey, chk], writes=[f"YT{i}_{cl}"])
        for h in range(2):
            p.op("dve", lambda e, h=h: e.scalar_tensor_tensor(out=Z2[:, h, :], in0=Z[:, h, :], scalar=gC[i][:, h, cl:cl + 1], in1=b[0:64, h * 64:(h + 1) * 64], op0=ALU.mult, op1=ALU.add),
                 reads=["Z", f"gC{i}", bkey], writes=[f"Z2_{h}"])
        p.op("dve", lambda e: e.tensor_tensor(out=Z[:].rearrange("p a b -> p (a b)"), in0=Z2[:].rearrange("p a b -> p (a b)"), in1=chb[:, 1, :], op=ALU.add),
             reads=["Z2_0", "Z2_1", chk], writes=["Z"])

    yc = _sb(nc, st, "yc", [64, 2 * SEG])
    sq = _sb(nc, st, "sq", [64, 2 * SEG])
    rs = _sb(nc, st, "rs", [64, 2 * SEG])
    ot = [_sb(nc, st, f"ot{i}", [64, 2, SEG]) for i in range(1)] * 2

    def epilogue(sg):
        i = sg % 2
        yt = YT[i]
        ykeys = [f"YT{i}_{cl}" for cl in range(cps)]
        ytf = yt[:].rearrange("p h t -> p (h t)")
        p.dma("sp", gbin[0][:], gb_d[:, :, :, sg * SEG:(sg + 1) * SEG], writes=["gbin0"])
        for hh in range(2):
            b, bkey = nb()
            sl_ = slice(hh * SEG, (hh + 1) * SEG)
            p.op("pe", lambda e, sl_=sl_, b=b: e.matmul(b[0:64, :], lhsT=ones64[:], rhs=ytf[:, sl_], start=True, stop=True), reads=["ones64"] + ykeys, writes=[bkey])
            p.op("dve", lambda e, sl_=sl_, b=b: e.scalar_tensor_tensor(out=yc[:, sl_], in0=b[0:64, :], scalar=-1.0 / 64, in1=ytf[:, sl_], op0=ALU.mult, op1=ALU.add),
                 reads=[bkey] + ykeys, writes=[f"yc{hh}"])
            p.op("act", lambda e, sl_=sl_: e.activation(out=sq[:, sl_], in_=yc[:, sl_], func=AF.Square), reads=[f"yc{hh}"], writes=[f"sq{hh}"])
            b2, b2key = nb()
            p.op("pe", lambda e, sl_=sl_, b2=b2: e.matmul(b2[0:64, :], lhsT=ones64[:], rhs=sq[:, sl_], start=True, stop=True), reads=["ones64", f"sq{hh}"], writes=[b2key])
            p.op("dve", lambda e, sl_=sl_, b2=b2: e.tensor_scalar(out=rs[:, sl_], in0=b2[0:64, :], scalar1=1.0 / 64, scalar2=GN_EPS, op0=ALU.mult, op1=ALU.add), reads=[b2key], writes=[f"rs{hh}"])
            p.op("act", lambda e, sl_=sl_: e.activation(out=rs[:, sl_], in_=rs[:, sl_], func=AF.Sqrt), reads=[f"rs{hh}"], writes=[f"rs{hh}"])
            p.op("dve", lambda e, sl_=sl_: e.reciprocal(out=rs[:, sl_], in_=rs[:, sl_]), reads=[f"rs{hh}"], writes=[f"rs{hh}"])
            p.op("dve", lambda e, sl_=sl_: e.tensor_tensor(out=yc[:, sl_], in0=yc[:, sl_], in1=rs[:, sl_], op=ALU.mult), reads=[f"yc{hh}", f"rs{hh}"], writes=[f"yc{hh}"])
            p.op("dve", lambda e, sl_=sl_, hh=hh: e.tensor_scalar(out=yc[:, sl_], in0=yc[:, sl_], scalar1=gnv[:, hh, 0:1], scalar2=gnv[:, hh, 1:2], op0=ALU.mult, op1=ALU.add),
                 reads=[f"yc{hh}", "gnv"], writes=[f"yc{hh}"])
            p.op("pool", lambda e, sl_=sl_, hh=hh: e.tensor_tensor(out=yc[:, sl_], in0=yc[:, sl_], in1=gbin[0][:, 1, hh, :], op=ALU.add), reads=[f"yc{hh}", "gbin0"], writes=[f"yc{hh}"])
            p.op("pool", lambda e, sl_=sl_, hh=hh: e.tensor_tensor(out=ot[0][:, hh, :], in0=yc[:, sl_], in1=gbin[0][:, 0, hh, :], op=ALU.mult), reads=[f"yc{hh}", "gbin0"], writes=[f"ot0_{hh}"])
        p.dma("sp", o_d[:, :, sg * SEG:(sg + 1) * SEG], ot[0][:], reads=["ot0_0", "ot0_1"])

    load_seg(0)
    pending_chain = []
    slot_ctr = 0
    ch_ctr = 0
    for sg in range(nseg):
        if sg + 1 < nseg:
            load_seg(sg + 1)
        prep_seg(sg)
        for c0 in range(0, cps, 2):
            stA = pre_stages(sg, c0, slot_ctr % NSL, ch_ctr % NCH)
            stB = pre_stages(sg, c0 + 1, (slot_ctr + 1) % NSL, (ch_ctr + 1) % NCH)
            new_chain = [(sg, c0, ch_ctr % NCH), (sg, c0 + 1, (ch_ctr + 1) % NCH)]
            slot_ctr += 2
            ch_ctr += 2
            nst = len(stA)
            for si in range(nst):
                stA[si]()
                stB[si]()
                if pending_chain and si in (3, 7):
                    a = pending_chain.pop(0)
                    chain_step(*a)
                    if a[1] == cps - 1:
                        epilogue(a[0])
            pending_chain.extend(new_chain)
    while pending_chain:
        a = pending_chain.pop(0)
        chain_step(*a)
        if a[1] == cps - 1:
            epilogue(a[0])


def launch_rwkv_chunked(lc, I, T=S):
    C = CH_C
    su = np.triu(np.ones((C, C), np.float32), 1)
    sle = np.triu(np.ones((C, C), np.float32), 0)
    msk = np.stack([su, su, su, su, su.T, su.T, sle, sle, sle, sle], 0).transpose(1, 0, 2)
    ident = np.eye(128, dtype=np.float32)
    rmask = np.ones((64, 2 * SEG), np.float32)
    rmask[:, ::C] = 0
    ones64 = np.ones((64, 64), np.float32)
    in_maps = []
    for i in range(8):
        cq = slice(i * 128, (i + 1) * 128)
        F = np.stack([lc[i][n][:, :T].reshape(2, 64, T) for n in ("wdec", "nkk", "kka", "kt", "rT", "vrT")], 0)
        F = F.transpose(2, 0, 1, 3)
        gb = np.stack([lc[i]["g"][:, :T].reshape(2, 64, T), lc[i]["bonus"][:, :T].reshape(2, 64, T)], 0)
        gb = gb.transpose(2, 0, 1, 3)
        gnv = np.stack([I["gn_w"][0][cq].reshape(2, 64), I["gn_b"][0][cq].reshape(2, 64)], -1).transpose(1, 0, 2)
        in_maps.append({"F": np.ascontiguousarray(F), "gb": np.ascontiguousarray(gb), "gnv": np.ascontiguousarray(gnv),
                        "ident": ident, "msk": np.ascontiguousarray(msk), "rmask": rmask, "ones64": ones64})
    res = _run(lambda nc, p, st: build_rwkv_chunked(nc, p, st, T), in_maps)
    return np.concatenate([r["o"].transpose(2, 1, 0).reshape(T, 128) for r in res], axis=1)
```

```python
import numpy as np
import concourse.bass as bass
import concourse.mybir as mybir
from concourse.bass_utils import run_bass_kernel_spmd

F32 = mybir.dt.float32
F32R = mybir.dt.float32r
BF16 = mybir.dt.bfloat16
I32 = mybir.dt.int32
U32 = mybir.dt.uint32
AF = mybir.ActivationFunctionType
ALU = mybir.AluOpType
AX = mybir.AxisListType

NDMA_SLOTS = 6


class Prog:
    def __init__(self, nc):
        self.nc = nc
        self.ops = []
        self.last_w = {}
        self.readers = {}
        self.engs = {"pe": nc.tensor, "act": nc.scalar, "dve": nc.vector,
                     "pool": nc.gpsimd, "sp": nc.sync}

    def op(self, eng, fn, reads=(), writes=(), dma=False, force=False, inc=16):
        deps = set()
        raw = set()
        for k in reads:
            if k in self.last_w:
                deps.add(self.last_w[k])
                raw.add(self.last_w[k])
        for k in writes:
            if k in self.last_w:
                deps.add(self.last_w[k])
                raw.add(self.last_w[k])
            for r in self.readers.get(k, ()):
                deps.add(r)
        idx = len(self.ops)
        self.ops.append(dict(eng=eng, fn=fn, deps=deps, raw=raw, dma=dma, force=force, inc=inc))
        for k in reads:
            self.readers.setdefault(k, []).append(idx)
        for k in writes:
            self.last_w[k] = idx
            self.readers[k] = []
        return idx

    def dma(self, q, out, in_, reads=(), writes=(), **kw):
        return self.op(q, lambda e: e.dma_start(out=out, in_=in_, **kw), reads, writes, dma=True)

    def emit(self, stack):
        nc = self.nc
        ops = self.ops
        need = [False] * len(ops)
        for i, o in enumerate(ops):
            nd = set()
            for d in o["deps"]:
                od = ops[d]
                if od["dma"] or o["dma"] or o["force"] or od["eng"] != o["eng"] or (d in o["raw"] and o["eng"] != "pe"):
                    nd.add(d)
            o["xdeps"] = nd
            for d in nd:
                need[d] = True
        for i, o in enumerate(ops):
            if o["dma"]:
                need[i] = True
        esem = {e: stack.enter_context(nc.semaphore("es_" + e)) for e in self.engs}
        dsem = {e: [stack.enter_context(nc.semaphore(f"ds_{e}_{k}")) for k in range(NDMA_SLOTS)]
                for e in ("sp", "act", "pool")}
        ecount = {e: 0 for e in self.engs}
        dcount = {e: 0 for e in dsem}
        signal = [None] * len(ops)
        waited = {}
        nwaits = 0
        actions = {e: [] for e in self.engs}
        for i, o in enumerate(ops):
            e = o["eng"]
            wl = {}
            for d in o["xdeps"]:
                s_, v = signal[d]
                key = id(s_)
                if waited.get((e, key), 0) >= v:
                    continue
                if key not in wl or wl[key][1] < v:
                    wl[key] = (s_, v)
            if o["dma"]:
                j = dcount[e]
                slot = j % NDMA_SLOTS
                s_ = dsem[e][slot]
                prev = o.get("prev_total", None)
                prev = self._slot_total.get((e, slot), 0) if hasattr(self, "_slot_total") else 0
                if prev > 0 and waited.get((e, id(s_)), 0) < prev:
                    if id(s_) not in wl or wl[id(s_)][1] < prev:
                        wl[id(s_)] = (s_, prev)
            for key, (s_, v) in wl.items():
                waited[(e, key)] = v
                nwaits += 1
            sem = None
            inc = 0
            if o["dma"]:
                if not hasattr(self, "_slot_total"):
                    self._slot_total = {}
                j = dcount[e]
                dcount[e] += 1
                slot = j % NDMA_SLOTS
                sem = dsem[e][slot]
                inc = o.get("inc", 16)
                tot = self._slot_total.get((e, slot), 0) + inc
                self._slot_total[(e, slot)] = tot
                signal[i] = (sem, tot)
            elif need[i]:
                ecount[e] += 1
                sem = esem[e]
                inc = 1
                signal[i] = (sem, ecount[e])
            actions[e].append((list(wl.values()), o["fn"], sem, inc))
        finals = {e: [] for e in self.engs}
        for e in dsem:
            for slot in range(NDMA_SLOTS):
                tot = getattr(self, "_slot_total", {}).get((e, slot), 0)
                if tot > 0:
                    finals[e].append((dsem[e][slot], tot))
        bnames = {"pe": "tensor", "act": "scalar", "dve": "vector", "pool": "gpsimd", "sp": "sync"}
        with nc.Block() as block:
            for e in self.engs:
                if not actions[e] and not finals[e]:
                    continue

                def body(eng, e=e):
                    for waits, fn, sem, inc in actions[e]:
                        for s_, v in waits:
                            eng.wait_ge(s_, v)
                        inst = fn(eng)
                        if sem is not None:
                            inst.then_inc(sem, inc)
                    for s_, v in finals[e]:
                        eng.wait_ge(s_, v)
                getattr(block, bnames[e])(body)
        self.stats = dict(n_ops=len(ops), n_waits=nwaits, ecount=ecount, dcount=dcount)
        return self.stats


from contextlib import ExitStack

S = 8192
D = 2048
EPS = 1e-6
_TRACE = False


def _run(build, in_maps):
    nc = bass.Bass("TRN2", target_bir_lowering=False)
    with ExitStack() as st:
        p = Prog(nc)
        build(nc, p, st)
        p.emit(st)
    if _TRACE:
        r = run_bass_kernel_spmd(nc, in_maps, core_ids=list(range(8)), trace=True)
        print("EXEC_NS", getattr(build, "__name__", "?"), r.exec_time_ns, p.stats, flush=True)
    else:
        r = run_bass_kernel_spmd(nc, in_maps, core_ids=list(range(8)))
    return r.results


def _sb(nc, st, name, shape, dt=F32):
    return st.enter_context(nc.sbuf_tensor(name, shape, dt))


def _ps(nc, st, name, shape=(128, 512), dt=F32):
    return st.enter_context(nc.psum_tensor(name, list(shape), dt))


def build_ada(nc, p, st):
    w = nc.dram_tensor("w", [2048, 1536], F32, kind="ExternalInput").ap()
    c = nc.dram_tensor("c", [128, 16], F32, kind="ExternalInput").ap()
    b = nc.dram_tensor("b", [1, 1536], F32, kind="ExternalInput").ap()
    y = nc.dram_tensor("y", [1, 1536], F32, kind="ExternalOutput").ap()
    wt = [_sb(nc, st, f"wt{i}", [128, 1536]) for i in range(2)]
    ct = _sb(nc, st, "ct", [128, 16])
    bt = _sb(nc, st, "bt", [1, 1536])
    acc = _sb(nc, st, "acc", [128, 1536])
    ones = _sb(nc, st, "ones", [128, 1])
    res = _sb(nc, st, "res", [1, 1536])
    ps = [_ps(nc, st, f"ps{i}", (1, 512)) for i in range(2)]
    p.dma("sp", ct[:], c, writes=["ct"])
    p.dma("sp", bt[:], b, writes=["bt"])
    p.op("dve", lambda e: e.memset(ones[:], 1.0), writes=["ones"])
    for kc in range(16):
        i = kc % 2
        p.dma("sp", wt[i][:], w[kc * 128:(kc + 1) * 128, :], writes=[f"wt{i}"])
        if kc == 0:
            p.op("dve", lambda e, i=i, kc=kc: e.tensor_scalar(out=acc[:], in0=wt[i][:], scalar1=ct[:, kc:kc + 1], scalar2=None, op0=ALU.mult),
                 reads=[f"wt{i}", "ct"], writes=["acc"])
        else:
            p.op("dve", lambda e, i=i, kc=kc: e.scalar_tensor_tensor(out=acc[:], in0=wt[i][:], scalar=ct[:, kc:kc + 1], in1=acc[:], op0=ALU.mult, op1=ALU.add),
                 reads=[f"wt{i}", "ct", "acc"], writes=["acc"])
    for j in range(3):
        pj = ps[j % 2]
        p.op("pe", lambda e, j=j, pj=pj: e.matmul(pj[:], lhsT=ones[:], rhs=acc[:, j * 512:(j + 1) * 512], start=True, stop=True),
             reads=["acc", "ones"], writes=[f"ps{j%2}"])
        p.op("dve", lambda e, j=j, pj=pj: e.tensor_tensor(out=res[:, j * 512:(j + 1) * 512], in0=pj[:], in1=bt[:, j * 512:(j + 1) * 512], op=ALU.add),
             reads=[f"ps{j%2}", "bt"], writes=[f"res{j}"])
    p.dma("sp", y, res[:], reads=["res0", "res1", "res2"])


def launch_ada(c, w_ada, b_ada):
    in_maps = [{"w": np.ascontiguousarray(w_ada[:, i * 1536:(i + 1) * 1536]),
                "c": np.ascontiguousarray(c.reshape(16, 128).T),
                "b": np.ascontiguousarray(b_ada[None, i * 1536:(i + 1) * 1536])} for i in range(8)]
    res = _run(build_ada, in_maps)
    return np.concatenate([r["y"][0] for r in res])


def make_build_norm(add_one, has_b, has_base, ntile=8):
    def build(nc, p, st):
        n = ntile * 128
        y = nc.dram_tensor("y", [n, D], F32, kind="ExternalInput").ap()
        g = nc.dram_tensor("g", [1, D], F32, kind="ExternalInput").ap()
        s = nc.dram_tensor("s", [1, D], F32, kind="ExternalInput").ap()
        bv = nc.dram_tensor("bv", [1, D], F32, kind="ExternalInput").ap() if has_b else None
        base = nc.dram_tensor("base", [n, D], F32, kind="ExternalInput").ap() if has_base else None
        o = nc.dram_tensor("o", [n, D], F32, kind="ExternalOutput").ap()
        gb = _sb(nc, st, "gb", [128, D])
        A = _sb(nc, st, "A", [128, D])
        bb = _sb(nc, st, "bb", [128, D]) if has_b else None
        p.dma("sp", gb[:], g.partition_broadcast(128), writes=["gb"])
        p.dma("sp", A[:], s.partition_broadcast(128), writes=["A"])
        if has_b:
            p.dma("sp", bb[:], bv.partition_broadcast(128), writes=["bb"])
        p.op("dve", lambda e: e.scalar_tensor_tensor(out=A[:], in0=A[:], scalar=float(add_one), in1=gb[:], op0=ALU.add, op1=ALU.mult),
             reads=["gb", "A"], writes=["A"])
        yt = [_sb(nc, st, f"yt{i}", [128, D]) for i in range(2)]
        bt = [_sb(nc, st, f"bt{i}", [128, D]) for i in range(2)] if has_base else None
        junk = _sb(nc, st, "junk", [128, D])
        ot = [_sb(nc, st, f"ot{i}", [128, D]) for i in range(2)]
        ss = [_sb(nc, st, f"ss{i}", [128, 4]) for i in range(2)]
        for t in range(ntile):
            i = t % 2
            rows = slice(t * 128, (t + 1) * 128)
            p.dma("sp", yt[i][:], y[rows, :], writes=[f"yt{i}"])
            if has_base:
                p.dma("sp", bt[i][:], base[rows, :], writes=[f"bt{i}"])
            p.op("act", lambda e, i=i: e.activation(out=junk[:], in_=yt[i][:], func=AF.Square, accum_out=ss[i][:, 0:1]),
                 reads=[f"yt{i}"], writes=["junk", f"ss{i}"])
            p.op("dve", lambda e, i=i: e.tensor_scalar(out=ss[i][:, 1:2], in0=ss[i][:, 0:1], scalar1=1.0 / D, scalar2=EPS, op0=ALU.mult, op1=ALU.add),
                 reads=[f"ss{i}"], writes=[f"ss{i}"])
            p.op("act", lambda e, i=i: e.activation(out=ss[i][:, 2:3], in_=ss[i][:, 1:2], func=AF.Sqrt),
                 reads=[f"ss{i}"], writes=[f"ss{i}"])
            p.op("dve", lambda e, i=i: e.reciprocal(out=ss[i][:, 3:4], in_=ss[i][:, 2:3]),
                 reads=[f"ss{i}"], writes=[f"ss{i}"])
            p.op("dve", lambda e, i=i: e.scalar_tensor_tensor(out=ot[i][:], in0=yt[i][:], scalar=ss[i][:, 3:4], in1=A[:], op0=ALU.mult, op1=ALU.mult),
                 reads=[f"yt{i}", f"ss{i}", "A"], writes=[f"ot{i}"], force=True)
            if has_b:
                p.op("pool", lambda e, i=i: e.tensor_tensor(out=ot[i][:], in0=ot[i][:], in1=bb[:], op=ALU.add),
                     reads=[f"ot{i}", "bb"], writes=[f"ot{i}"])
            if has_base:
                p.op("pool", lambda e, i=i: e.tensor_tensor(out=ot[i][:], in0=ot[i][:], in1=bt[i][:], op=ALU.add),
                     reads=[f"ot{i}", f"bt{i}"], writes=[f"ot{i}"])
            p.dma("pool", o[rows, :], ot[i][:], reads=[f"ot{i}"])
    return build


def launch_norm(y, g, s, add_one, bv=None, base=None):
    n = y.shape[0] // 8
    in_maps = []
    for i in range(8):
        m = {"y": np.ascontiguousarray(y[i * n:(i + 1) * n]), "g": np.ascontiguousarray(g[None, :]),
             "s": np.ascontiguousarray(s[None, :])}
        if bv is not None:
            m["bv"] = np.ascontiguousarray(bv[None, :])
        if base is not None:
            m["base"] = np.ascontiguousarray(base[i * n:(i + 1) * n])
        in_maps.append(m)
    res = _run(make_build_norm(add_one, bv is not None, base is not None, n // 128), in_maps)
    return np.concatenate([r["o"] for r in res], axis=0)


LC_OUTS = ["qT", "kT", "vT", "rT", "krT", "vrT", "wdec", "nkk", "kka", "kt", "g", "bonus"]
WDECAY = 0.6065306597126334


def build_inproj(nc, p, st, ntt=16):
    T = ntt * 512
    hTp = nc.dram_tensor("hTp", [D, T + 1], F32, kind="ExternalInput").ap()
    ws_d = nc.dram_tensor("ws", [D, 448], F32, kind="ExternalInput").ap()
    wd_d = nc.dram_tensor("wd", [D, 384], F32, kind="ExternalInput").ap()
    mucol_d = nc.dram_tensor("mucol", [1, 384], F32, kind="ExternalInput").ap()
    wl_d = nc.dram_tensor("wl", [D, 448], F32, kind="ExternalInput").ap()
    murow_d = nc.dram_tensor("murow", [128, 16, 3], F32, kind="ExternalInput").ap()
    w2w_d = nc.dram_tensor("w2w", [96, 128], F32, kind="ExternalInput").ap()
    w2a_d = nc.dram_tensor("w2a", [96, 128], F32, kind="ExternalInput").ap()
    w2g_d = nc.dram_tensor("w2g", [128, 2, 128], F32, kind="ExternalInput").ap()
    vecs_d = nc.dram_tensor("vecs", [128, 5], F32, kind="ExternalInput").ap()
    cos_d = nc.dram_tensor("cos", [32, T], F32, kind="ExternalInput").ap()
    sin_d = nc.dram_tensor("sin", [32, T], F32, kind="ExternalInput").ap()
    blk_d = nc.dram_tensor("blk", [128, 128], F32, kind="ExternalInput").ap()
    outs = {n: nc.dram_tensor(n, [128, T], F32, kind="ExternalOutput").ap() for n in LC_OUTS}

    w0 = _sb(nc, st, "w0", [128, 16, 448])
    wa = _sb(nc, st, "wa", [128, 16, 448])
    wb = _sb(nc, st, "wb", [128, 16, 448])
    hb = [_sb(nc, st, f"hb{i}", [128, 16, 514]) for i in range(2)]
    mucol = _sb(nc, st, "mucol_sb", [128, 384])
    murow = _sb(nc, st, "murow_sb", [128, 16, 3])
    w2w = _sb(nc, st, "w2w_sb", [96, 128])
    w2a = _sb(nc, st, "w2a_sb", [96, 128])
    w2g = _sb(nc, st, "w2g_sb", [128, 2, 128])
    vecs = _sb(nc, st, "vecs_sb", [128, 5])
    blk = _sb(nc, st, "blk_sb", [128, 128])
    cs = [_sb(nc, st, f"cs{i}", [32, 2, 512]) for i in range(2)]
    ps = [_ps(nc, st, f"ps{i}") for i in range(8)]
    NOB = 6
    ob = [_sb(nc, st, f"ob{i}", [128, 512]) for i in range(NOB)]
    obi = [0]
    psi = [0]

    def nps():
        i = psi[0] % 8
        psi[0] += 1
        return ps[i], f"ps{i}"

    def nob():
        i = obi[0] % NOB
        obi[0] += 1
        return ob[i], f"ob{i}"

    hview = hTp.rearrange("(c p) t -> p c t", p=128)
    r32 = lambda ap: ap.bitcast(F32R)

    for dst, src, k in [(mucol[:], mucol_d.partition_broadcast(128), "mucol"), (murow[:], murow_d, "murow"),
                        (w2w[:], w2w_d, "w2w"), (w2a[:], w2a_d, "w2a"), (w2g[:], w2g_d, "w2g"),
                        (vecs[:], vecs_d, "vecs"), (blk[:], blk_d, "blk")]:
        p.dma("sp", dst, src, writes=[k])

    def load_h(tt):
        i = tt % 2
        p.dma("pool", r32(hb[i][:, :, 0:513]), r32(hview[:, :, tt * 512:tt * 512 + 513]), writes=[f"hb{i}"])

    def gemm(tt, kind, co, M, pst, psk):
        i = tt % 2
        for c in range(16):
            cur = hb[i][:, c, 1:513]
            prev = hb[i][:, c, 0:512]
            if kind == "single":
                p.op("pe", lambda e, c=c, cur=cur: e.matmul(pst[:M, :], lhsT=r32(w0[:, c, co:co + M]), rhs=r32(cur), start=(c == 0), stop=(c == 15)),
                     reads=["w0", f"hb{i}"], writes=[psk])
            else:
                p.op("pe", lambda e, c=c, cur=cur: e.matmul(pst[:M, :], lhsT=r32(wa[:, c, co:co + M]), rhs=r32(cur), start=(c == 0), stop=False),
                     reads=["wa", f"hb{i}"], writes=[psk])
                p.op("pe", lambda e, c=c, prev=prev: e.matmul(pst[:M, :], lhsT=r32(wb[:, c, co:co + M]), rhs=r32(prev), start=False, stop=(c == 15)),
                     reads=["wb", f"hb{i}"], writes=[psk])

    p.dma("pool", r32(w0[:]), r32(ws_d.rearrange("(c p) n -> p c n", p=128)), writes=["w0"])
    p.dma("pool", r32(wa[:, :, 0:384]), r32(wd_d.rearrange("(c p) n -> p c n", p=128)), writes=["wa"])
    for c in range(16):
        p.op("dve", lambda e, c=c: e.tensor_tensor(out=r32(wb[:, c, 0:384]), in0=wa[:, c, 0:384], in1=mucol[:], op=ALU.mult),
             reads=["wa", "mucol"], writes=["wb"])
    p.op("dve", lambda e: e.tensor_tensor(out=r32(wa[:, :, 0:384]), in0=wa[:, :, 0:384], in1=wb[:, :, 0:384], op=ALU.subtract),
         reads=["wa", "wb"], writes=["wa"])
    load_h(0)
    for tt in range(ntt):
        if tt + 1 < ntt:
            load_h(tt + 1)
        tsl = slice(tt * 512, (tt + 1) * 512)
        ci = tt % 2
        p.dma("sp", cs[ci][:, 0, :], cos_d[:, tsl], writes=[f"cs{ci}"])
        p.dma("sp", cs[ci][:, 1, :], sin_d[:, tsl], writes=[f"cs{ci}"])
        for name, co in (("qT", 0), ("kT", 160)):
            pq, pqk = nps()
            gemm(tt, "single", co, 128, pq, pqk)
            psw, pswk = nps()
            gemm(tt, "single", co + 128, 32, psw, pswk)
            o, ok = nob()
            p.op("act", lambda e, o=o, pq=pq: e.copy(out=o[:], in_=pq[:]), reads=[pqk], writes=[ok, ok + "hi"])
            t1, t1k = nob()
            p.op("dve", lambda e, t1=t1, psw=psw, ci=ci: e.tensor_tensor(out=t1[0:32, :], in0=psw[0:32, :], in1=cs[ci][:, 1, :], op=ALU.mult),
                 reads=[pswk, f"cs{ci}"], writes=[t1k])
            p.op("dve", lambda e, o=o, pq=pq, ci=ci: e.tensor_tensor(out=o[0:32, :], in0=pq[0:32, :], in1=cs[ci][:, 0, :], op=ALU.mult),
                 reads=[pqk, f"cs{ci}"], writes=[ok])
            p.op("dve", lambda e, o=o, t1=t1: e.tensor_tensor(out=o[0:32, :], in0=o[0:32, :], in1=t1[0:32, :], op=ALU.add),
                 reads=[ok, t1k], writes=[ok])
            p.dma("pool", outs[name][:, tsl], o[:], reads=[ok, ok + "hi"], writes=[f"d_{name}_{tt}"])
        pv, pvk = nps()
        gemm(tt, "single", 320, 128, pv, pvk)
        o, ok = nob()
        p.op("act", lambda e, o=o, pv=pv: e.copy(out=o[:], in_=pv[:]), reads=[pvk], writes=[ok, ok + "hi"])
        p.dma("pool", outs["vT"][:, tsl], o[:], reads=[ok, ok + "hi"], writes=[f"d_vT_{tt}"])
        for j, name in enumerate(("rT", "krT", "vrT")):
            pr, prk = nps()
            gemm(tt, "dual", j * 128, 128, pr, prk)
            o, ok = nob()
            p.op("act", lambda e, o=o, pr=pr: e.copy(out=o[:], in_=pr[:]), reads=[prk], writes=[ok, ok + "hi"])
            p.dma("pool", outs[name][:, tsl], o[:], reads=[ok, ok + "hi"], writes=[f"d_{name}_{tt}"])

    p.dma("pool", r32(w0[:]), r32(wl_d.rearrange("(c p) n -> p c n", p=128)), writes=["w0"])
    for c in range(16):
        for j, (lo, hi) in enumerate(((0, 96), (96, 192), (192, 448))):
            p.op("dve", lambda e, c=c, j=j, lo=lo, hi=hi: e.tensor_scalar(out=r32(wb[:, c, lo:hi]), in0=w0[:, c, lo:hi], scalar1=murow[:, c, j:j + 1], scalar2=None, op0=ALU.mult),
                 reads=["w0", "murow"], writes=["wb"])
    p.op("dve", lambda e: e.tensor_tensor(out=r32(wa[:]), in0=w0[:], in1=wb[:], op=ALU.subtract),
         reads=["w0", "wb"], writes=["wa"])
    tw = _sb(nc, st, "tw", [96, 512])
    ta = _sb(nc, st, "ta", [96, 512])
    tg = _sb(nc, st, "tg", [128, 2, 512])
    rin = [_sb(nc, st, f"rin{i}", [128, 3, 512]) for i in range(1)]
    tmp = {n: _sb(nc, st, "tmp_" + n, [128, 512]) for n in ["a", "kkr", "sq", "nrm", "rn", "u", "rk"]}
    load_h(0)
    for tt in range(ntt):
        if tt + 1 < ntt:
            load_h(tt + 1)
        tsl = slice(tt * 512, (tt + 1) * 512)
        ri = 0
        for j, name in enumerate(("rT", "krT", "vrT")):
            p.dma("sp", rin[ri][:, j, :], outs[name][:, tsl], reads=[f"d_{name}_{tt}"], writes=[f"rin{ri}_{j}"])
        R_, KR, VR = rin[ri][:, 0, :], rin[ri][:, 1, :], rin[ri][:, 2, :]
        rk_, krk, vrk = f"rin{ri}_0", f"rin{ri}_1", f"rin{ri}_2"
        pw, pwk = nps()
        gemm(tt, "dual", 0, 96, pw, pwk)
        p.op("act", lambda e, pw=pw: e.activation(out=tw[:], in_=pw[:96, :], func=AF.Tanh), reads=[pwk], writes=["tw"])
        pa, pak = nps()
        gemm(tt, "dual", 96, 96, pa, pak)
        p.op("act", lambda e, pa=pa: e.copy(out=ta[:], in_=pa[:96, :]), reads=[pak], writes=["ta"])
        for h in range(2):
            pg, pgk = nps()
            gemm(tt, "dual", 192 + h * 128, 128, pg, pgk)
            p.op("act", lambda e, pg=pg, h=h: e.activation(out=tg[:, h, :], in_=pg[:], func=AF.Sigmoid), reads=[pgk], writes=[f"tg{h}"])
        pd, pdk = nps()
        p.op("pe", lambda e, pd=pd: e.matmul(pd[:], lhsT=w2w[:], rhs=tw[:], start=True, stop=True), reads=["w2w", "tw"], writes=[pdk])
        o_w, o_wk = nob()
        p.op("act", lambda e, pd=pd: e.activation(out=tmp["sq"][:], in_=pd[:], func=AF.Sigmoid, bias=vecs[:, 0:1]), reads=[pdk, "vecs"], writes=["t_sq"])
        p.op("act", lambda e, o_w=o_w: e.activation(out=o_w[:], in_=tmp["sq"][:], func=AF.Exp, scale=-WDECAY), reads=["t_sq"], writes=[o_wk])
        p.dma("pool", outs["wdec"][:, tsl], o_w[:], reads=[o_wk])
        pa2, pa2k = nps()
        p.op("pe", lambda e, pa2=pa2: e.matmul(pa2[:], lhsT=w2a[:], rhs=ta[:], start=True, stop=True), reads=["w2a", "ta"], writes=[pa2k])
        p.op("act", lambda e, pa2=pa2: e.activation(out=tmp["a"][:], in_=pa2[:], func=AF.Sigmoid, bias=vecs[:, 1:2]), reads=[pa2k, "vecs"], writes=["t_a"])
        pg2, pg2k = nps()
        for h in range(2):
            p.op("pe", lambda e, pg2=pg2, h=h: e.matmul(pg2[:], lhsT=w2g[:, h, :], rhs=tg[:, h, :], start=(h == 0), stop=(h == 1)),
                 reads=["w2g", f"tg{h}"], writes=[pg2k])
        o_g, o_gk = nob()
        p.op("act", lambda e, o_g=o_g, pg2=pg2: e.copy(out=o_g[:], in_=pg2[:]), reads=[pg2k], writes=[o_gk])
        p.dma("pool", outs["g"][:, tsl], o_g[:], reads=[o_gk])
        p.op("dve", lambda e, KR=KR: e.tensor_scalar(out=tmp["kkr"][:], in0=KR, scalar1=vecs[:, 2:3], scalar2=None, op0=ALU.mult),
             reads=[krk, "vecs"], writes=["t_kkr"])
        p.op("pool", lambda e: e.tensor_tensor(out=tmp["sq"][:], in0=tmp["kkr"][:], in1=tmp["kkr"][:], op=ALU.mult),
             reads=["t_kkr", "t_sq"], writes=["t_sq"])
        pn, pnk = nps()
        p.op("pe", lambda e, pn=pn: e.matmul(pn[:], lhsT=blk[:], rhs=tmp["sq"][:], start=True, stop=True), reads=["blk", "t_sq"], writes=[pnk])
        p.op("act", lambda e, pn=pn: e.activation(out=tmp["nrm"][:], in_=pn[:], func=AF.Sqrt), reads=[pnk], writes=["t_nrm"])
        p.op("dve", lambda e: e.tensor_scalar(out=tmp["nrm"][:], in0=tmp["nrm"][:], scalar1=1e-12, scalar2=None, op0=ALU.max),
             reads=["t_nrm"], writes=["t_nrm"])
        p.op("dve", lambda e: e.reciprocal(out=tmp["rn"][:], in_=tmp["nrm"][:]), reads=["t_nrm"], writes=["t_rn"])
        o_n, o_nk = nob()
        p.op("dve", lambda e, o_n=o_n: e.scalar_tensor_tensor(out=o_n[:], in0=tmp["kkr"][:], scalar=-1.0, in1=tmp["rn"][:], op0=ALU.mult, op1=ALU.mult),
             reads=["t_kkr", "t_rn"], writes=[o_nk])
        p.dma("pool", outs["nkk"][:, tsl], o_n[:], reads=[o_nk])
        o_ka, o_kak = nob()
        p.op("dve", lambda e, o_n=o_n, o_ka=o_ka: e.scalar_tensor_tensor(out=o_ka[:], in0=o_n[:], scalar=-1.0, in1=tmp["a"][:], op0=ALU.mult, op1=ALU.mult),
             reads=[o_nk, "t_a"], writes=[o_kak])
        p.dma("pool", outs["kka"][:, tsl], o_ka[:], reads=[o_kak])
        p.op("dve", lambda e: e.tensor_scalar(out=tmp["u"][:], in0=tmp["a"][:], scalar1=-1.0, scalar2=vecs[:, 3:4], op0=ALU.add, op1=ALU.mult),
             reads=["t_a", "vecs"], writes=["t_u"])
        o_kt, o_ktk = nob()
        p.op("dve", lambda e, o_kt=o_kt, KR=KR: e.scalar_tensor_tensor(out=o_kt[:], in0=tmp["u"][:], scalar=1.0, in1=KR, op0=ALU.add, op1=ALU.mult),
             reads=["t_u", krk], writes=[o_ktk])
        p.dma("pool", outs["kt"][:, tsl], o_kt[:], reads=[o_ktk])
        p.op("dve", lambda e, o_kt=o_kt, R_=R_: e.scalar_tensor_tensor(out=tmp["rk"][:], in0=R_, scalar=vecs[:, 4:5], in1=o_kt[:], op0=ALU.mult, op1=ALU.mult),
             reads=[rk_, "vecs", o_ktk], writes=["t_rk"])
        pb, pbk = nps()
        p.op("pe", lambda e, pb=pb: e.matmul(pb[:], lhsT=blk[:], rhs=tmp["rk"][:], start=True, stop=True), reads=["blk", "t_rk"], writes=[pbk])
        o_b, o_bk = nob()
        p.op("dve", lambda e, o_b=o_b, pb=pb, VR=VR: e.tensor_tensor(out=o_b[:], in0=pb[:], in1=VR, op=ALU.mult),
             reads=[pbk, vrk], writes=[o_bk])
        p.dma("pool", outs["bonus"][:, tsl], o_b[:], reads=[o_bk])


def _rope_tables(T):
    half = 16
    inv = (500000.0 ** (-np.arange(half, dtype=np.float32) / half)).astype(np.float32)
    ang = np.arange(T, dtype=np.float32)[:, None] * inv[None, :]
    cos = np.cos(ang).astype(np.float32).T
    sin = np.sin(ang).astype(np.float32).T
    COS = np.concatenate([cos, cos], 0)
    SIN = np.concatenate([-sin, sin], 0)
    return np.ascontiguousarray(COS), np.ascontiguousarray(SIN)


def launch_inproj(h, I):
    T = h.shape[0]
    hTp = np.zeros((D, T + 1), np.float32)
    hTp[:, 1:] = h.T
    w_in = I["w_in"][0]
    COS, SIN = _rope_tables(T)
    swp = np.concatenate([np.arange(16, 32), np.arange(0, 16)])
    blk = np.zeros((128, 128), np.float32)
    blk[:64, :64] = 1
    blk[64:, 64:] = 1
    murow = np.stack([I["mu_w"][0], I["mu_a"][0], I["mu_g"][0]], -1).reshape(16, 128, 3).transpose(1, 0, 2)
    wl = np.concatenate([I["w_w1"][0], I["w_a1"][0], I["w_g1"][0]], 1)
    in_maps = []
    for i in range(8):
        cq = slice(i * 128, (i + 1) * 128)
        q = w_in[:, 0:1024][:, cq]
        k = w_in[:, 1024:2048][:, cq]
        v = w_in[:, 2048:3072][:, cq]
        ws = np.concatenate([q, q[:, swp], k, k[:, swp], v], 1)
        r = w_in[:, 3072:4096][:, cq]
        kr = w_in[:, 4096:5120][:, cq]
        vr = w_in[:, 5120:6144][:, cq]
        wd = np.concatenate([r, kr, vr], 1)
        mucol = np.concatenate([I["mu_r"][0][cq], I["mu_k"][0][cq], I["mu_v"][0][cq]])[None, :]
        vecs = np.stack([I["w0"][0][cq], I["a0"][0][cq], I["k_k"][0][cq], I["k_a"][0][cq], I["r_k"][0].reshape(-1)[cq]], -1)
        in_maps.append({
            "hTp": hTp, "ws": np.ascontiguousarray(ws), "wd": np.ascontiguousarray(wd), "mucol": np.ascontiguousarray(mucol),
            "wl": np.ascontiguousarray(wl), "murow": np.ascontiguousarray(murow),
            "w2w": np.ascontiguousarray(I["w_w2"][0][:, cq]), "w2a": np.ascontiguousarray(I["w_a2"][0][:, cq]),
            "w2g": np.ascontiguousarray(I["w_g2"][0][:, cq].reshape(2, 128, 128).transpose(1, 0, 2)),
            "vecs": np.ascontiguousarray(vecs), "cos": COS, "sin": SIN, "blk": blk})
    ntt = T // 512
    res = _run(lambda nc, p, st: build_inproj(nc, p, st, ntt), in_maps)
    return res


GN_EPS = 64e-5
TCH = 32


def build_rwkv(nc, p, st, T=S):
    nch = T // TCH
    bcin_d = nc.dram_tensor("bcin", [2, nch, 5, TCH, 64], F32, kind="ExternalInput").ap()
    vT_d = nc.dram_tensor("vT", [128, T], F32, kind="ExternalInput").ap()
    g_d = nc.dram_tensor("g", [128, T], F32, kind="ExternalInput").ap()
    bonus_d = nc.dram_tensor("bonus", [128, T], F32, kind="ExternalInput").ap()
    gnv_d = nc.dram_tensor("gnv", [128, 2], F32, kind="ExternalInput").ap()
    sel_d = nc.dram_tensor("sel", [128, 128], F32, kind="ExternalInput").ap()
    blk_d = nc.dram_tensor("blk", [128, 128], F32, kind="ExternalInput").ap()
    o_d = nc.dram_tensor("o", [128, T], F32, kind="ExternalOutput").ap()
    r32 = lambda ap: ap.bitcast(F32R)

    vT = _sb(nc, st, "vT_sb", [128, T])
    yT = _sb(nc, st, "yT_sb", [128, T])
    Sst = _sb(nc, st, "S_sb", [128, 64])
    junk = _sb(nc, st, "junk", [128, 64])
    sa = _sb(nc, st, "sa", [128, 1])
    sel = _sb(nc, st, "sel_sb", [128, 128])
    blk = _sb(nc, st, "blk_sb", [128, 128])
    gnv = _sb(nc, st, "gnv_sb", [128, 2])
    bc = [_sb(nc, st, f"bc{i}", [128, 5, TCH, 64]) for i in range(2)]
    ps = [_ps(nc, st, f"ps{i}") for i in range(8)]
    p.dma("pool", r32(sel[:]), r32(sel_d), writes=["sel"])
    p.dma("sp", blk[:], blk_d, writes=["blk"])
    p.dma("sp", gnv[:], gnv_d, writes=["gnv"])
    p.dma("sp", vT[:], vT_d, writes=["vT"])
    p.op("dve", lambda e: e.memset(Sst[:], 0.0), writes=["S"])
    zer_d = nc.dram_tensor("zer", [126, 5, TCH, 64], F32, kind="ExternalInput").ap()
    for i in range(2):
        p.dma("pool", r32(bc[i][2:128]), r32(zer_d), writes=[f"bc{i}"])

    def load_bc(c):
        i = c % 2
        p.dma("pool", r32(bc[i][0:2]), r32(bcin_d[:, c]), writes=[f"bc{i}"])

    load_bc(0)
    grp = 0
    for c in range(nch):
        if c + 1 < nch:
            load_bc(c + 1)
        bi = c % 2
        for g4 in range(TCH // 4):
            base = (grp % 2) * 3
            grp += 1
            views = []
            for j in range(5):
                bank = ps[base + j // 2]
                bk = f"ps{base + j // 2}"
                half = bank[:, (j % 2) * 256:(j % 2) * 256 + 256]
                p.op("pe", lambda e, half=half, j=j, g4=g4, bi=bi: e.matmul(half, lhsT=r32(sel[:]), rhs=r32(bc[bi][:, j, g4 * 4:(g4 + 1) * 4, :]), start=True, stop=True),
                     reads=["sel", f"bc{bi}"], writes=[bk + f"h{j%2}"])
                views.append((half, bk + f"h{j%2}"))
            for tl in range(4):
                t = c * TCH + g4 * 4 + tl
                cs = slice(tl * 64, (tl + 1) * 64)
                wv, nv, kav, ktv, rv = [(v[0][:, cs], v[1]) for v in views]
                p.op("dve", lambda e, nv=nv: e.scalar_tensor_tensor(out=junk[:], in0=Sst[:], scalar=1.0, in1=nv[0], op0=ALU.mult, op1=ALU.mult, accum_out=sa[:]),
                     reads=["S", nv[1]], writes=["junk", "sa"])
                p.op("dve", lambda e, wv=wv: e.tensor_tensor(out=Sst[:], in0=Sst[:], in1=wv[0], op=ALU.mult),
                     reads=["S", wv[1]], writes=["S"])
                p.op("dve", lambda e, kav=kav: e.scalar_tensor_tensor(out=Sst[:], in0=kav[0], scalar=sa[:, 0:1], in1=Sst[:], op0=ALU.mult, op1=ALU.add),
                     reads=["S", "sa", kav[1]], writes=["S"], force=True)
                p.op("dve", lambda e, ktv=ktv, t=t: e.scalar_tensor_tensor(out=Sst[:], in0=ktv[0], scalar=vT[:, t:t + 1], in1=Sst[:], op0=ALU.mult, op1=ALU.add),
                     reads=["S", "vT", ktv[1]], writes=["S"])
                p.op("dve", lambda e, rv=rv, t=t: e.scalar_tensor_tensor(out=junk[:], in0=Sst[:], scalar=1.0, in1=rv[0], op0=ALU.mult, op1=ALU.mult, accum_out=yT[:, t:t + 1]),
                     reads=["S", rv[1]], writes=["junk", f"yT{t // 512}"])
    gt = [_sb(nc, st, f"g_sb{i}", [128, 512]) for i in range(2)]
    bt = [_sb(nc, st, f"b_sb{i}", [128, 512]) for i in range(2)]
    yc = _sb(nc, st, "yc", [128, 512])
    sq = _sb(nc, st, "sq", [128, 512])
    rs = _sb(nc, st, "rs", [128, 512])
    ot = [_sb(nc, st, f"ot{i}", [128, 512]) for i in range(2)]
    for tt in range(T // 512):
        i = tt % 2
        tsl = slice(tt * 512, (tt + 1) * 512)
        p.dma("sp", gt[i][:], g_d[:, tsl], writes=[f"gt{i}"])
        p.dma("sp", bt[i][:], bonus_d[:, tsl], writes=[f"bt{i}"])
        pm, pmk = ps[6], "ps6"
        p.op("pe", lambda e, tsl=tsl: e.matmul(ps[6][:], lhsT=blk[:], rhs=yT[:, tsl], start=True, stop=True), reads=["blk", f"yT{tt}"], writes=["ps6"])
        p.op("dve", lambda e, tsl=tsl: e.scalar_tensor_tensor(out=yc[:], in0=ps[6][:], scalar=-1.0 / 64, in1=yT[:, tsl], op0=ALU.mult, op1=ALU.add),
             reads=["ps6", f"yT{tt}"], writes=["yc"])
        p.op("act", lambda e: e.activation(out=sq[:], in_=yc[:], func=AF.Square), reads=["yc"], writes=["sq"])
        p.op("pe", lambda e: e.matmul(ps[7][:], lhsT=blk[:], rhs=sq[:], start=True, stop=True), reads=["blk", "sq"], writes=["ps7"])
        p.op("dve", lambda e: e.tensor_scalar(out=rs[:], in0=ps[7][:], scalar1=1.0 / 64, scalar2=GN_EPS, op0=ALU.mult, op1=ALU.add), reads=["ps7"], writes=["rs"])
        p.op("act", lambda e: e.activation(out=rs[:], in_=rs[:], func=AF.Sqrt), reads=["rs"], writes=["rs"])
        p.op("dve", lambda e: e.reciprocal(out=rs[:], in_=rs[:]), reads=["rs"], writes=["rs"])
        p.op("dve", lambda e: e.tensor_tensor(out=yc[:], in0=yc[:], in1=rs[:], op=ALU.mult), reads=["yc", "rs"], writes=["yc"])
        p.op("dve", lambda e: e.tensor_scalar(out=yc[:], in0=yc[:], scalar1=gnv[:, 0:1], scalar2=gnv[:, 1:2], op0=ALU.mult, op1=ALU.add), reads=["yc", "gnv"], writes=["yc"])
        p.op("dve", lambda e, i=i: e.tensor_tensor(out=yc[:], in0=yc[:], in1=bt[i][:], op=ALU.add), reads=["yc", f"bt{i}"], writes=["yc"])
        p.op("dve", lambda e, i=i: e.tensor_tensor(out=ot[i][:], in0=yc[:], in1=gt[i][:], op=ALU.mult), reads=["yc", f"gt{i}"], writes=[f"ot{i}"])
        p.dma("sp", o_d[:, tsl], ot[i][:], reads=[f"ot{i}"])


def launch_rwkv(lc, I, T=S):
    sel = np.zeros((128, 128), np.float32)
    sel[0, :64] = 1
    sel[1, 64:] = 1
    blk = np.zeros((128, 128), np.float32)
    blk[:64, :64] = 1
    blk[64:, 64:] = 1
    in_maps = []
    nch = T // TCH
    for i in range(8):
        cq = slice(i * 128, (i + 1) * 128)
        q5 = np.stack([lc[i][n][:, :T] for n in ("wdec", "nkk", "kka", "kt", "rT")], 0)
        q5 = q5.reshape(5, 2, 64, nch, TCH).transpose(1, 3, 0, 4, 2)
        gnv = np.stack([I["gn_w"][0][cq], I["gn_b"][0][cq]], -1)
        in_maps.append({"bcin": np.ascontiguousarray(q5), "vT": np.ascontiguousarray(lc[i]["vrT"][:, :T]),
                        "g": np.ascontiguousarray(lc[i]["g"][:, :T]), "bonus": np.ascontiguousarray(lc[i]["bonus"][:, :T]),
                        "gnv": np.ascontiguousarray(gnv), "sel": sel, "blk": blk, "zer": np.zeros((126, 5, TCH, 64), np.float32)})
    res = _run(lambda nc, p, st: build_rwkv(nc, p, st, T), in_maps)
    return np.concatenate([r["o"].T for r in res], axis=1)


NEGB = 30000.0


def build_moba(nc, p, st, T=S):
    nb = T // 256
    nkt = T // 128
    qT_d = nc.dram_tensor("qT", [128, T], F32, kind="ExternalInput").ap()
    kT_d = nc.dram_tensor("kT", [128, T], F32, kind="ExternalInput").ap()
    v_d = nc.dram_tensor("v", [128, nkt, 128], F32, kind="ExternalInput").ap()
    E_d = nc.dram_tensor("E", [128, T], F32, kind="ExternalInput").ap()
    cm_d = nc.dram_tensor("cm", [128, 256], F32, kind="ExternalInput").ap()
    id_d = nc.dram_tensor("ident", [128, 128], F32, kind="ExternalInput").ap()
    on_d = nc.dram_tensor("ones", [128, 128], F32, kind="ExternalInput").ap()
    o_d = nc.dram_tensor("oT", [128, T], F32, kind="ExternalOutput").ap()
    r32 = lambda ap: ap.bitcast(F32R)
    qT = _sb(nc, st, "qT_sb", [128, T])
    kT = _sb(nc, st, "kT_sb", [128, T])
    va = _sb(nc, st, "va_sb", [128, nkt, 128])
    E = _sb(nc, st, "E_sb", [128, T])
    cm = _sb(nc, st, "cm_sb", [128, 256])
    ident = _sb(nc, st, "id_sb", [128, 128])
    ones = _sb(nc, st, "ones_sb", [128, 128])
    kmean = _sb(nc, st, "kmean", [128, 32])
    gsb = _sb(nc, st, "gsb", [128, 32])
    m8 = _sb(nc, st, "m8", [128, 8])
    bias = _sb(nc, st, "bias", [128, 128])
    biasT = [_sb(nc, st, f"biasT{i}", [128, 256]) for i in range(2)]
    pT = [_sb(nc, st, f"pT{i}", [128, 256]) for i in range(3)]
    osb = [_sb(nc, st, f"osb{i}", [128, 256]) for i in range(2)]
    rden = _sb(nc, st, "rden", [128, 256])
    s_ps = [_ps(nc, st, f"s_ps{i}") for i in range(2)]
    o_ps = [_ps(nc, st, f"o_ps{i}") for i in range(2)]
    d_ps = [_ps(nc, st, f"d_ps{i}") for i in range(2)]
    g_ps = _ps(nc, st, "g_ps")
    t_ps = _ps(nc, st, "t_ps")
    p.dma("pool", r32(qT[:]), r32(qT_d), writes=["qT"])
    p.dma("pool", r32(kT[:]), r32(kT_d), writes=["kT"])
    p.dma("pool", r32(va[:]), r32(v_d), writes=["va"])
    p.dma("pool", r32(E[:]), r32(E_d), writes=["E"])
    p.dma("pool", r32(cm[:]), r32(cm_d), writes=["cm"])
    p.dma("pool", r32(ident[:]), r32(id_d), writes=["ident"])
    p.dma("pool", r32(ones[:]), r32(on_d), writes=["ones"])
    p.op("dve", lambda e: e.tensor_reduce(out=kmean[:, 0:nb], in_=kT[:].bitcast(F32).rearrange("p (n k) -> p n k", k=256), axis=AX.X, op=ALU.add),
         reads=["kT"], writes=["kmean"])
    p.op("dve", lambda e: e.tensor_scalar(out=kmean[:, 0:nb], in0=kmean[:, 0:nb], scalar1=1.0 / 256, scalar2=None, op0=ALU.mult),
         reads=["kmean"], writes=["kmean"])
    p.op("dve", lambda e: e.memset(gsb[:], -1e30), writes=["gsb"])
    p.op("dve", lambda e: e.memset(bias[:], 0.0), writes=["bias"])
    scale = 128 ** -0.5
    pti = 0
    si = 0
    for b in range(nb):
        bT = biasT[b % 2]
        bTk = f"biasT{b % 2}"
        for j in range(2):
            qs = slice(b * 256 + j * 128, b * 256 + (j + 1) * 128)
            if b > 3:
                p.op("pe", lambda e, qs=qs: e.matmul(g_ps[:, 0:nb], lhsT=qT[:, qs], rhs=kmean[:, 0:nb], start=True, stop=True),
                     reads=["qT", "kmean"], writes=["g_ps"])
                p.op("dve", lambda e, b=b: e.tensor_copy(out=gsb[:, 0:b], in_=g_ps[:, 0:b]), reads=["g_ps", "gsb"], writes=["gsb"])
                p.op("dve", lambda e: e.max(out=m8[:], in_=gsb[:]), reads=["gsb"], writes=["m8"])
                p.op("dve", lambda e: e.tensor_scalar(out=bias[:, 0:32], in0=gsb[:], scalar1=m8[:, 2:3], scalar2=None, op0=ALU.is_ge),
                     reads=["gsb", "m8", "bias"], writes=["bias"], force=True)
                p.op("dve", lambda e: e.tensor_scalar(out=bias[:, 0:32], in0=bias[:, 0:32], scalar1=NEGB, scalar2=-NEGB, op0=ALU.mult, op1=ALU.add),
                     reads=["bias"], writes=["bias"])
            else:
                p.op("dve", lambda e: e.memset(bias[:, 0:32], -NEGB), reads=["bias"], writes=["bias"])
                if b > 0:
                    p.op("dve", lambda e, b=b: e.memset(bias[:, 0:b], 0.0), reads=["bias"], writes=["bias"])
            p.op("dve", lambda e, b=b: e.memset(bias[:, b:b + 1], 0.0), reads=["bias"], writes=["bias"])
            p.op("pe", lambda e: e.transpose(t_ps[:, 0:128], bias[:], ident[:].bitcast(F32)), reads=["bias", "ident"], writes=["t_ps"])
            p.op("act", lambda e, bT=bT, j=j: e.copy(out=r32(bT[:, j * 128:(j + 1) * 128]), in_=t_ps[:, 0:128]), reads=["t_ps"], writes=[bTk])
        op_ = o_ps[b % 2]
        opk = f"o_ps{b % 2}"
        dp_ = d_ps[b % 2]
        dpk = f"d_ps{b % 2}"
        nkt_b = 2 * b + 2

        def qkm(kt, sp_, spk, b=b, bT=bT, bTk=bTk):
            ks = slice(kt * 128, (kt + 1) * 128)
            own = kt >= 2 * b
            p.op("pe", lambda e: e.matmul(sp_[:, 0:256], lhsT=r32(kT[:, ks]), rhs=r32(qT[:, b * 256:(b + 1) * 256]), start=True, stop=False),
                 reads=["kT", "qT"], writes=[spk])
            p.op("pe", lambda e: e.matmul(sp_[:, 0:256], lhsT=r32(E[:, ks]), rhs=r32(bT[:]), start=False, stop=(not own)),
                 reads=["E", bTk], writes=[spk])
            if own:
                if kt == 2 * b:
                    p.op("pe", lambda e: e.matmul(sp_[:, 0:128], lhsT=ident[:].bitcast(F32), rhs=cm[:, 128:256].bitcast(F32), start=False, stop=True),
                         reads=["ident", "cm"], writes=[spk])
                else:
                    p.op("pe", lambda e: e.matmul(sp_[:, 0:256], lhsT=r32(ident[:]), rhs=r32(cm[:, 0:256]), start=False, stop=True),
                         reads=["ident", "cm"], writes=[spk])

        cur = (s_ps[si % 2], f"s_ps{si % 2}")
        si += 1
        qkm(0, *cur)
        for kt in range(nkt_b):
            nxt = None
            if kt + 1 < nkt_b:
                nxt = (s_ps[si % 2], f"s_ps{si % 2}")
                si += 1
                qkm(kt + 1, *nxt)
            sp_, spk = cur
            pt = pT[pti % 3]
            ptk = f"pT{pti % 3}"
            pti += 1
            p.op("act", lambda e, pt=pt, sp_=sp_: e.activation(out=r32(pt[:]), in_=sp_[:, 0:256], func=AF.Exp, scale=scale), reads=[spk], writes=[ptk])
            p.op("pe", lambda e, pt=pt, kt=kt, op_=op_, nkt_b=nkt_b: e.matmul(op_[:, 0:256], lhsT=r32(va[:, kt, :]), rhs=r32(pt[:]), start=(kt == 0), stop=(kt == nkt_b - 1)),
                 reads=[ptk, "va"], writes=[opk])
            p.op("pe", lambda e, pt=pt, kt=kt, dp_=dp_, nkt_b=nkt_b: e.matmul(dp_[:, 0:256], lhsT=r32(ones[:]), rhs=r32(pt[:]), start=(kt == 0), stop=(kt == nkt_b - 1)),
                 reads=[ptk, "ones"], writes=[dpk])
            cur = nxt
        ob_ = osb[b % 2]
        p.op("dve", lambda e, dp_=dp_: e.reciprocal(out=rden[:], in_=dp_[:, 0:256]), reads=[dpk], writes=["rden"])
        p.op("dve", lambda e, op_=op_, ob_=ob_: e.tensor_tensor(out=ob_[:], in0=op_[:, 0:256], in1=rden[:], op=ALU.mult), reads=[opk, "rden"], writes=[f"osb{b % 2}"])
        p.dma("sp", o_d[:, b * 256:(b + 1) * 256], ob_[:], reads=[f"osb{b % 2}"])


def launch_moba(lc, T=S):
    nkt = T // 128
    E = np.zeros((128, T), np.float32)
    for n in range(T // 256):
        E[n, n * 256:(n + 1) * 256] = 1
    kk = np.arange(128)
    cmc = np.where(kk[:, None] <= kk[None, :], 0.0, -NEGB).astype(np.float32)
    cm = np.concatenate([np.full((128, 128), -NEGB, np.float32), cmc], 1)
    ident = np.eye(128, dtype=np.float32)
    ones = np.ones((128, 128), np.float32)
    in_maps = []
    for i in range(8):
        v = lc[i]["vT"][:, :T].T.reshape(nkt, 128, 128).transpose(1, 0, 2)
        in_maps.append({"qT": np.ascontiguousarray(lc[i]["qT"][:, :T]), "kT": np.ascontiguousarray(lc[i]["kT"][:, :T]),
                        "v": np.ascontiguousarray(v), "E": E, "cm": cm, "ident": ident, "ones": ones})
    res = _run(lambda nc, p, st: build_moba(nc, p, st, T), in_maps)
    return np.concatenate([r["oT"].T for r in res], axis=1)


def build_merge(nc, p, st, nunit=2):
    TT = 512
    NT = nunit * TT
    hT_d = nc.dram_tensor("hT", [D, NT], F32, kind="ExternalInput").ap()
    oa_d = nc.dram_tensor("oaT", [1024, NT], F32, kind="ExternalInput").ap()
    or_d = nc.dram_tensor("orT", [1024, NT], F32, kind="ExternalInput").ap()
    wg_d = nc.dram_tensor("wg", [D, 4096], F32, kind="ExternalInput").ap()
    wua_d = nc.dram_tensor("wua", [1024, D], F32, kind="ExternalInput").ap()
    wur_d = nc.dram_tensor("wur", [1024, D], F32, kind="ExternalInput").ap()
    wo_d = nc.dram_tensor("wo", [D, D], F32, kind="ExternalInput").ap()
    y_d = nc.dram_tensor("yT", [D, NT], F32, kind="ExternalOutput").ap()
    r32 = lambda ap: ap.bitcast(F32R)
    hT = _sb(nc, st, "hT_sb", [128, 16, TT])
    oa = _sb(nc, st, "oa_sb", [128, 8, TT])
    orr = _sb(nc, st, "or_sb", [128, 8, TT])
    mix = _sb(nc, st, "mix_sb", [128, 16, TT])
    wb = [_sb(nc, st, f"wb{i}", [128, 48, 128]) for i in range(2)]
    wob = [_sb(nc, st, f"wob{i}", [128, 16, 128]) for i in range(2)]
    sg = [_sb(nc, st, f"sg{i}", [128, TT]) for i in range(2)]
    m12 = [_sb(nc, st, f"m12_{i}", [128, TT]) for i in range(2)]
    yo = [_sb(nc, st, f"yo{i}", [128, TT]) for i in range(2)]
    ps = [_ps(nc, st, f"ps{i}") for i in range(8)]
    wgv = wg_d.rearrange("(c p) n -> p c n", p=128)
    wuav = wua_d.rearrange("(c p) n -> p c n", p=128)
    wurv = wur_d.rearrange("(c p) n -> p c n", p=128)
    wov = wo_d.rearrange("(c p) n -> p c n", p=128)
    wcount = 0
    for u in range(nunit):
        ts_ = slice(u * TT, (u + 1) * TT)
        p.dma("pool", r32(hT[:]), r32(hT_d.rearrange("(c p) t -> p c t", p=128)[:, :, ts_]), writes=["hT"])
        p.dma("pool", r32(oa[:]), r32(oa_d.rearrange("(c p) t -> p c t", p=128)[:, :, ts_]), writes=["oa"])
        p.dma("pool", r32(orr[:]), r32(or_d.rearrange("(c p) t -> p c t", p=128)[:, :, ts_]), writes=["or"])
        for n in range(16):
            wi = wcount % 2
            wcount += 1
            W = wb[wi]
            wk = f"wb{wi}"
            ns = slice(n * 128, (n + 1) * 128)
            ns2 = slice(2048 + n * 128, 2048 + (n + 1) * 128)
            p.dma("pool", r32(W[:, 0:8, :]), r32(wgv[:, 0:8, ns]), writes=[wk + "a"])
            p.dma("pool", r32(W[:, 8:16, :]), r32(wgv[:, 8:16, ns]), writes=[wk + "b"])
            p.dma("pool", r32(W[:, 16:24, :]), r32(wgv[:, 0:8, ns2]), writes=[wk + "c"])
            p.dma("pool", r32(W[:, 24:32, :]), r32(wgv[:, 8:16, ns2]), writes=[wk + "d"])
            p.dma("pool", r32(W[:, 32:40, :]), r32(wuav[:, :, ns]), writes=[wk + "e"])
            p.dma("pool", r32(W[:, 40:48, :]), r32(wurv[:, :, ns]), writes=[wk + "f"])
            wkeys = [wk + x for x in "abcdef"]
            b0 = (n % 2) * 4
            pga, pgr, pua, pur = ps[b0], ps[b0 + 1], ps[b0 + 2], ps[b0 + 3]
            for c in range(16):
                p.op("pe", lambda e, c=c, W=W, pga=pga: e.matmul(pga[:], lhsT=r32(W[:, c, :]), rhs=r32(hT[:, c, :]), start=(c == 0), stop=(c == 15)),
                     reads=wkeys + ["hT"], writes=[f"ps{b0}"])
            for c in range(16):
                p.op("pe", lambda e, c=c, W=W, pgr=pgr: e.matmul(pgr[:], lhsT=r32(W[:, 16 + c, :]), rhs=r32(hT[:, c, :]), start=(c == 0), stop=(c == 15)),
                     reads=wkeys + ["hT"], writes=[f"ps{b0 + 1}"])
            for c in range(8):
                p.op("pe", lambda e, c=c, W=W, pua=pua: e.matmul(pua[:], lhsT=r32(W[:, 32 + c, :]), rhs=r32(oa[:, c, :]), start=(c == 0), stop=(c == 7)),
                     reads=wkeys + ["oa"], writes=[f"ps{b0 + 2}"])
            for c in range(8):
                p.op("pe", lambda e, c=c, W=W, pur=pur: e.matmul(pur[:], lhsT=r32(W[:, 40 + c, :]), rhs=r32(orr[:, c, :]), start=(c == 0), stop=(c == 7)),
                     reads=wkeys + ["or"], writes=[f"ps{b0 + 3}"])
            p.op("act", lambda e, pga=pga: e.activation(out=sg[0][:], in_=pga[:], func=AF.Sigmoid), reads=[f"ps{b0}"], writes=["sg0"])
            p.op("act", lambda e, pgr=pgr: e.activation(out=sg[1][:], in_=pgr[:], func=AF.Sigmoid), reads=[f"ps{b0 + 1}"], writes=["sg1"])
            p.op("dve", lambda e, pua=pua: e.tensor_tensor(out=m12[0][:], in0=pua[:], in1=sg[0][:], op=ALU.mult), reads=[f"ps{b0 + 2}", "sg0"], writes=["m0"])
            p.op("dve", lambda e, pur=pur: e.tensor_tensor(out=m12[1][:], in0=pur[:], in1=sg[1][:], op=ALU.mult), reads=[f"ps{b0 + 3}", "sg1"], writes=["m1"])
            p.op("dve", lambda e, n=n: e.tensor_tensor(out=r32(mix[:, n, :]), in0=m12[0][:], in1=m12[1][:], op=ALU.add), reads=["m0", "m1"], writes=[f"mix{n}"])
        for m in range(16):
            wi = m % 2
            ms = slice(m * 128, (m + 1) * 128)
            p.dma("pool", r32(wob[wi][:, 0:8, :]), r32(wov[:, 0:8, ms]), writes=[f"wob{wi}a"])
            p.dma("pool", r32(wob[wi][:, 8:16, :]), r32(wov[:, 8:16, ms]), writes=[f"wob{wi}b"])
            py = ps[m % 2]
            for c in range(16):
                p.op("pe", lambda e, c=c, wi=wi, py=py: e.matmul(py[:], lhsT=r32(wob[wi][:, c, :]), rhs=r32(mix[:, c, :]), start=(c == 0), stop=(c == 15)),
                     reads=[f"wob{wi}a", f"wob{wi}b", f"mix{c}"], writes=[f"ps{m % 2}"])
            p.op("act", lambda e, py=py, wi=wi: e.copy(out=yo[wi][:], in_=py[:]), reads=[f"ps{m % 2}"], writes=[f"yo{wi}"])
            p.dma("pool", y_d[ms, ts_], yo[wi][:], reads=[f"yo{wi}"])


def launch_merge(h1, o_att, o_rwkv, I):
    wg = np.ascontiguousarray(I["w_in"][0][:, 6144:10240])
    in_maps = []
    for i in range(8):
        ts_ = slice(i * 1024, (i + 1) * 1024)
        in_maps.append({"hT": np.ascontiguousarray(h1[ts_].T), "oaT": np.ascontiguousarray(o_att[ts_].T),
                        "orT": np.ascontiguousarray(o_rwkv[ts_].T), "wg": wg,
                        "wua": I["w_up_att"][0], "wur": I["w_up_rwkv"][0], "wo": I["w_o"][0]})
    res = _run(lambda nc, p, st: build_merge(nc, p, st, 2), in_maps)
    return np.concatenate([r["yT"].T for r in res], axis=0)


def build_router(nc, p, st, ntile=8):
    NT = ntile * 128
    hT_d = nc.dram_tensor("hT", [D, NT], F32, kind="ExternalInput").ap()
    wr_d = nc.dram_tensor("wr", [D, 72], F32, kind="ExternalInput").ap()
    br_d = nc.dram_tensor("br", [1, 72], F32, kind="ExternalInput").ap()
    o_d = nc.dram_tensor("o", [NT, 4], F32, kind="ExternalOutput").ap()
    hT = _sb(nc, st, "hT_sb", [128, 16, NT])
    wr = _sb(nc, st, "wr_sb", [128, 16, 72])
    br = _sb(nc, st, "br_sb", [128, 72])
    ps = [_ps(nc, st, f"ps{i}") for i in range(2)]
    p.dma("sp", hT[:], hT_d.rearrange("(c p) t -> p c t", p=128), writes=["hT"])
    p.dma("sp", wr[:], wr_d.rearrange("(c p) n -> p c n", p=128), writes=["wr"])
    p.dma("sp", br[:], br_d.partition_broadcast(128), writes=["br"])
    l_sb = _sb(nc, st, "l_sb", [128, 72])
    lem = _sb(nc, st, "lem", [128, 64])
    m8g = _sb(nc, st, "m8g", [128, 8])
    m8e = _sb(nc, st, "m8e", [128, 8])
    idx = _sb(nc, st, "idx", [128, 8], U32)
    sm = _sb(nc, st, "sm", [128, 8])
    junk = _sb(nc, st, "junk", [128, 8])
    pen = _sb(nc, st, "pen", [128, 8])
    res = [_sb(nc, st, f"res{i}", [128, 4]) for i in range(2)]
    for t in range(ntile):
        pt = ps[t % 2]
        ptk = f"ps{t % 2}"
        rs_ = res[t % 2]
        rk = f"res{t % 2}"
        for c in range(16):
            p.op("pe", lambda e, c=c, t=t, pt=pt: e.matmul(pt[:, 0:72], lhsT=hT[:, c, t * 128:(t + 1) * 128], rhs=wr[:, c, :], start=(c == 0), stop=(c == 15)),
                 reads=["hT", "wr"], writes=[ptk])
        p.op("dve", lambda e, pt=pt: e.tensor_tensor(out=l_sb[:], in0=pt[:, 0:72], in1=br[:], op=ALU.add), reads=[ptk, "br"], writes=["l"])
        p.op("dve", lambda e: e.max(out=m8g[:], in_=l_sb[:, 0:8]), reads=["l"], writes=["m8g"])
        p.op("dve", lambda e: e.tensor_scalar(out=sm[:, 0:1], in0=m8g[:, 0:1], scalar1=-1.0, scalar2=None, op0=ALU.mult), reads=["m8g"], writes=["sm0"])
        p.op("act", lambda e: e.activation(out=junk[:], in_=l_sb[:, 0:8], func=AF.Exp, bias=sm[:, 0:1], accum_out=sm[:, 1:2]), reads=["l", "sm0"], writes=["junk", "sm1"])
        p.op("dve", lambda e: e.reciprocal(out=sm[:, 2:3], in_=sm[:, 1:2]), reads=["sm1"], writes=["sm2"])
        p.op("dve", lambda e: e.tensor_scalar(out=pen[:], in0=l_sb[:, 0:8], scalar1=m8g[:, 0:1], scalar2=None, op0=ALU.is_ge), reads=["l", "m8g"], writes=["pen"], force=True)
        p.op("dve", lambda e: e.tensor_scalar(out=pen[:], in0=pen[:], scalar1=1e30, scalar2=-1e30, op0=ALU.mult, op1=ALU.add), reads=["pen"], writes=["pen"])
        for g in range(8):
            p.op("dve", lambda e, g=g: e.tensor_scalar(out=lem[:, g * 8:(g + 1) * 8], in0=l_sb[:, 8 + g * 8:16 + g * 8], scalar1=pen[:, g:g + 1], scalar2=None, op0=ALU.add),
                 reads=["l", "pen"], writes=["lem"], force=(g == 0))
        p.op("dve", lambda e: e.max(out=m8e[:], in_=lem[:]), reads=["lem"], writes=["m8e"])
        p.op("dve", lambda e: e.max_index(out=idx[:], in_max=m8e[:], in_values=lem[:]), reads=["m8e", "lem"], writes=["idx"], force=True)
        p.op("dve", lambda e, rs_=rs_: e.tensor_copy(out=rs_[:, 0:2], in_=idx[:, 0:2]), reads=["idx"], writes=[rk + "a"], force=True)
        p.op("dve", lambda e: e.tensor_tensor(out=sm[:, 3:4], in0=m8e[:, 0:1], in1=m8e[:, 1:2], op=ALU.subtract), reads=["m8e"], writes=["sm3"], force=True)
        p.op("act", lambda e: e.activation(out=sm[:, 4:5], in_=sm[:, 3:4], func=AF.Sigmoid), reads=["sm3"], writes=["sm4"])
        p.op("dve", lambda e, rs_=rs_: e.tensor_tensor(out=rs_[:, 2:3], in0=sm[:, 4:5], in1=sm[:, 2:3], op=ALU.mult), reads=["sm4", "sm2"], writes=[rk + "b"], force=True)
        p.op("dve", lambda e, rs_=rs_: e.tensor_tensor(out=rs_[:, 3:4], in0=sm[:, 2:3], in1=rs_[:, 2:3], op=ALU.subtract), reads=["sm2", rk + "b"], writes=[rk + "c"], force=True)
        p.dma("sp", o_d[t * 128:(t + 1) * 128, :], rs_[:], reads=[rk + "a", rk + "b", rk + "c"])


def launch_router(h2, I):
    wr = np.ascontiguousarray(np.concatenate([I["w_rg"][0], I["w_re"][0]], 1))
    br = np.ascontiguousarray(np.concatenate([I["b_rg"][0], I["b_re"][0]])[None, :])
    in_maps = [{"hT": np.ascontiguousarray(h2[i * 1024:(i + 1) * 1024].T), "wr": wr, "br": br} for i in range(8)]
    res = _run(lambda nc, p, st: build_router(nc, p, st, 8), in_maps)
    o = np.concatenate([r["o"] for r in res], axis=0)
    return o[:, 0:2].astype(np.int64), o[:, 2:4]


def build_experts(nc, p, st, cap):
    xT_d = nc.dram_tensor("xT", [8, D, cap], F32, kind="ExternalInput").ap()
    wg_d = nc.dram_tensor("wg", [8, D, 512], F32, kind="ExternalInput").ap()
    wu_d = nc.dram_tensor("wu", [8, D, 512], F32, kind="ExternalInput").ap()
    wd_d = nc.dram_tensor("wd", [8, 512, D], F32, kind="ExternalInput").ap()
    y_d = nc.dram_tensor("yT", [8, D, cap], F32, kind="ExternalOutput").ap()
    r32 = lambda ap: ap.bitcast(F32R)
    xT = _sb(nc, st, "xT_sb", [128, 16, cap])
    Wg = _sb(nc, st, "Wg_sb", [128, 16, 512])
    Wu = _sb(nc, st, "Wu_sb", [128, 16, 512])
    Wd = _sb(nc, st, "Wd_sb", [128, 4, D])
    hid = _sb(nc, st, "hid_sb", [128, 4, cap])
    sg = [_sb(nc, st, f"sg{i}", [128, cap]) for i in range(2)]
    yo = [_sb(nc, st, f"yo{i}", [128, cap]) for i in range(3)]
    ps = [_ps(nc, st, f"ps{i}") for i in range(8)]
    for ex in range(8):
        xv = xT_d[ex].rearrange("(c p) t -> p c t", p=128)
        for h in range(4):
            p.dma("pool", r32(xT[:, h * 4:(h + 1) * 4, :]), r32(xv[:, h * 4:(h + 1) * 4, :]), writes=[f"xT{h}"])
        gv = wg_d[ex].rearrange("(c p) n -> p c n", p=128)
        uv = wu_d[ex].rearrange("(c p) n -> p c n", p=128)
        dv = wd_d[ex].rearrange("(c p) n -> p c n", p=128)
        for h in range(8):
            p.dma("pool", r32(Wg[:, h * 2:(h + 1) * 2, :]), r32(gv[:, h * 2:(h + 1) * 2, :]), writes=[f"Wg{h}"])
        for h in range(8):
            p.dma("pool", r32(Wu[:, h * 2:(h + 1) * 2, :]), r32(uv[:, h * 2:(h + 1) * 2, :]), writes=[f"Wu{h}"])
        for h in range(4):
            p.dma("pool", r32(Wd[:, h, 0:1024]), r32(dv[:, h, 0:1024]), writes=[f"Wd{h}a"])
            p.dma("pool", r32(Wd[:, h, 1024:2048]), r32(dv[:, h, 1024:2048]), writes=[f"Wd{h}b"])
        for f in range(4):
            pg = ps[(f % 2) * 2]
            pu = ps[(f % 2) * 2 + 1]
            pgk = f"ps{(f % 2) * 2}"
            puk = f"ps{(f % 2) * 2 + 1}"
            fs = slice(f * 128, (f + 1) * 128)
            for c in range(16):
                p.op("pe", lambda e, c=c, pg=pg, fs=fs: e.matmul(pg[:, 0:cap], lhsT=r32(Wg[:, c, fs]), rhs=r32(xT[:, c, :]), start=(c == 0), stop=(c == 15)),
                     reads=[f"Wg{c // 2}", f"xT{c // 4}"], writes=[pgk])
            for c in range(16):
                p.op("pe", lambda e, c=c, pu=pu, fs=fs: e.matmul(pu[:, 0:cap], lhsT=r32(Wu[:, c, fs]), rhs=r32(xT[:, c, :]), start=(c == 0), stop=(c == 15)),
                     reads=[f"Wu{c // 2}", f"xT{c // 4}"], writes=[puk])
            s_ = sg[f % 2]
            p.op("act", lambda e, s_=s_, pg=pg: e.activation(out=s_[:], in_=pg[:, 0:cap], func=AF.Silu), reads=[pgk], writes=[f"sg{f % 2}"])
            p.op("dve", lambda e, s_=s_, pu=pu, f=f: e.tensor_tensor(out=r32(hid[:, f, :]), in0=pu[:, 0:cap], in1=s_[:], op=ALU.mult),
                 reads=[puk, f"sg{f % 2}"], writes=[f"hid{f}"])
        for d in range(16):
            py = ps[4 + d % 4]
            pyk = f"ps{4 + d % 4}"
            ds_ = slice(d * 128, (d + 1) * 128)
            for f in range(4):
                p.op("pe", lambda e, f=f, py=py, ds_=ds_: e.matmul(py[:, 0:cap], lhsT=r32(Wd[:, f, ds_]), rhs=r32(hid[:, f, :]), start=(f == 0), stop=(f == 3)),
                     reads=[f"Wd{f}a", f"Wd{f}b", f"hid{f}"], writes=[pyk])
            yb = yo[d % 3]
            p.op("act" if d % 2 else "dve", (lambda e, yb=yb, py=py: e.copy(out=yb[:], in_=py[:, 0:cap])) if d % 2 else (lambda e, yb=yb, py=py: e.tensor_copy(out=yb[:], in_=py[:, 0:cap])),
                 reads=[pyk], writes=[f"yo{d % 3}"])
            p.dma("sp", y_d[ex, ds_, :], yb[:], reads=[f"yo{d % 3}"])


def launch_experts(h2, eidx, I):
    N = h2.shape[0]
    flat_e = eidx.reshape(-1)
    flat_t = np.repeat(np.arange(N), 2)
    order = np.argsort(flat_e, kind="stable")
    counts = np.bincount(flat_e, minlength=64)
    cap = int(max(256, -(-counts.max() // 128) * 128))
    starts = np.cumsum(counts) - counts
    xT = np.zeros((64, D, cap), np.float32)
    pos_of = np.zeros(2 * N, np.int64)
    for e in range(64):
        sl = order[starts[e]:starts[e] + counts[e]]
        xT[e, :, :counts[e]] = h2[flat_t[sl]].T
        pos_of[sl] = np.arange(counts[e])
    in_maps = [{"xT": np.ascontiguousarray(xT[g * 8:(g + 1) * 8]), "wg": np.ascontiguousarray(I["w_gate_e"][0][g * 8:(g + 1) * 8]),
                "wu": np.ascontiguousarray(I["w_up_e"][0][g * 8:(g + 1) * 8]), "wd": np.ascontiguousarray(I["w_down_e"][0][g * 8:(g + 1) * 8])} for g in range(8)]
    res = _run(lambda nc, p, st: build_experts(nc, p, st, cap), in_maps)
    yT = np.concatenate([r["yT"] for r in res], axis=0)
    yflat = yT[flat_e, :, pos_of]
    yflat = yflat.reshape(N, 2, D)
    return np.ascontiguousarray(yflat[:, 0]), np.ascontiguousarray(yflat[:, 1])


def build_combine(nc, p, st, ntile=8):
    n = ntile * 128
    ya_d = nc.dram_tensor("ya", [n, D], F32, kind="ExternalInput").ap()
    yb_d = nc.dram_tensor("yb", [n, D], F32, kind="ExternalInput").ap()
    w_d = nc.dram_tensor("w", [n, 2], F32, kind="ExternalInput").ap()
    g = nc.dram_tensor("g", [1, D], F32, kind="ExternalInput").ap()
    s = nc.dram_tensor("s", [1, D], F32, kind="ExternalInput").ap()
    base = nc.dram_tensor("base", [n, D], F32, kind="ExternalInput").ap()
    o = nc.dram_tensor("o", [n, D], F32, kind="ExternalOutput").ap()
    gb = _sb(nc, st, "gb", [128, D])
    A = _sb(nc, st, "A", [128, D])
    p.dma("sp", gb[:], g.partition_broadcast(128), writes=["gb"])
    p.dma("sp", A[:], s.partition_broadcast(128), writes=["A"])
    p.op("dve", lambda e: e.tensor_tensor(out=A[:], in0=A[:], in1=gb[:], op=ALU.mult), reads=["gb", "A"], writes=["A"])
    ya = [_sb(nc, st, f"ya{i}", [128, D]) for i in range(2)]
    yb = [_sb(nc, st, f"yb{i}", [128, D]) for i in range(2)]
    bt = [_sb(nc, st, f"bt{i}", [128, D]) for i in range(2)]
    wt = [_sb(nc, st, f"wt{i}", [128, 2]) for i in range(2)]
    junk = _sb(nc, st, "junk", [128, D])
    ot = [_sb(nc, st, f"ot{i}", [128, D]) for i in range(2)]
    ss = [_sb(nc, st, f"ss{i}", [128, 4]) for i in range(2)]
    for t in range(ntile):
        i = t % 2
        rows = slice(t * 128, (t + 1) * 128)
        p.dma("sp", ya[i][:], ya_d[rows, :], writes=[f"ya{i}"])
        p.dma("sp", yb[i][:], yb_d[rows, :], writes=[f"yb{i}"])
        p.dma("sp", bt[i][:], base[rows, :], writes=[f"bt{i}"])
        p.dma("sp", wt[i][:], w_d[rows, :], writes=[f"wt{i}"])
        p.op("dve", lambda e, i=i: e.tensor_scalar(out=ya[i][:], in0=ya[i][:], scalar1=wt[i][:, 0:1], scalar2=None, op0=ALU.mult),
             reads=[f"ya{i}", f"wt{i}"], writes=[f"ya{i}"])
        p.op("dve", lambda e, i=i: e.scalar_tensor_tensor(out=ya[i][:], in0=yb[i][:], scalar=wt[i][:, 1:2], in1=ya[i][:], op0=ALU.mult, op1=ALU.add),
             reads=[f"ya{i}", f"yb{i}", f"wt{i}"], writes=[f"ya{i}"])
        p.op("act", lambda e, i=i: e.activation(out=junk[:], in_=ya[i][:], func=AF.Square, accum_out=ss[i][:, 0:1]),
             reads=[f"ya{i}"], writes=["junk", f"ss{i}"])
        p.op("dve", lambda e, i=i: e.tensor_scalar(out=ss[i][:, 1:2], in0=ss[i][:, 0:1], scalar1=1.0 / D, scalar2=EPS, op0=ALU.mult, op1=ALU.add),
             reads=[f"ss{i}"], writes=[f"ss{i}"])
        p.op("act", lambda e, i=i: e.activation(out=ss[i][:, 2:3], in_=ss[i][:, 1:2], func=AF.Sqrt), reads=[f"ss{i}"], writes=[f"ss{i}"])
        p.op("dve", lambda e, i=i: e.reciprocal(out=ss[i][:, 3:4], in_=ss[i][:, 2:3]), reads=[f"ss{i}"], writes=[f"ss{i}"])
        p.op("dve", lambda e, i=i: e.scalar_tensor_tensor(out=ot[i][:], in0=ya[i][:], scalar=ss[i][:, 3:4], in1=A[:], op0=ALU.mult, op1=ALU.mult),
             reads=[f"ya{i}", f"ss{i}", "A"], writes=[f"ot{i}"], force=True)
        p.op("pool", lambda e, i=i: e.tensor_tensor(out=ot[i][:], in0=ot[i][:], in1=bt[i][:], op=ALU.add),
             reads=[f"ot{i}", f"bt{i}"], writes=[f"ot{i}"])
        p.dma("pool", o[rows, :], ot[i][:], reads=[f"ot{i}"])


def launch_combine(ya, yb, w, g, s, base):
    n = ya.shape[0] // 8
    in_maps = [{"ya": np.ascontiguousarray(ya[i * n:(i + 1) * n]), "yb": np.ascontiguousarray(yb[i * n:(i + 1) * n]),
                "w": np.ascontiguousarray(w[i * n:(i + 1) * n]), "g": np.ascontiguousarray(g[None, :]),
                "s": np.ascontiguousarray(s[None, :]), "base": np.ascontiguousarray(base[i * n:(i + 1) * n])} for i in range(8)]
    res = _run(lambda nc, p, st: build_combine(nc, p, st, n // 128), in_maps)
    return np.concatenate([r["o"] for r in res], axis=0)


def kernel(**inputs):
    I = {k: np.asarray(v) for k, v in inputs.items()}
    x = I["x"][0]
    ada = launch_ada(I["c"][0], I["w_ada"][0], I["b_ada"][0])
    sh1, sc1, gt1, sh2, sc2, gt2 = np.split(ada, 6)
    h1 = launch_norm(x, I["g_pre_mix"][0], sc1, 1.0, bv=sh1)
    lc = launch_inproj(h1, I)
    o_att = launch_moba(lc)
    o_rwkv = launch_rwkv_chunked(lc, I)
    y1 = launch_merge(h1, o_att, o_rwkv, I)
    x1 = launch_norm(y1, I["g_post_mix"][0], gt1, 0.0, base=x)
    h2 = launch_norm(x1, I["g_pre_ffn"][0], sc2, 1.0, bv=sh2)
    eidx, ew = launch_router(h2, I)
    ya, yb = launch_experts(h2, eidx, I)
    out = launch_combine(ya, yb, ew, I["g_post_ffn"][0], gt2, x1)
    return out[None].astype(np.float32)


CH_C = 64
SEG = 256
LOCK = 4


def build_rwkv_chunked(nc, p, st, T=S):
    nseg = T // SEG
    cps = SEG // CH_C
    F_d = nc.dram_tensor("F", [64, 6, 2, T], F32, kind="ExternalInput").ap()
    gb_d = nc.dram_tensor("gb", [64, 2, 2, T], F32, kind="ExternalInput").ap()
    gnv_d = nc.dram_tensor("gnv", [64, 2, 2], F32, kind="ExternalInput").ap()
    id_d = nc.dram_tensor("ident", [128, 128], F32, kind="ExternalInput").ap()
    msk_d = nc.dram_tensor("msk", [64, 10, 64], F32, kind="ExternalInput").ap()
    rm_d = nc.dram_tensor("rmask", [64, 2 * SEG], F32, kind="ExternalInput").ap()
    on_d = nc.dram_tensor("ones64", [64, 64], F32, kind="ExternalInput").ap()
    o_d = nc.dram_tensor("o", [64, 2, T], F32, kind="ExternalOutput").ap()

    ident = _sb(nc, st, "ident_sb", [128, 128])
    msk = _sb(nc, st, "msk_sb", [64, 10, 64])
    rmask = _sb(nc, st, "rmask_sb", [64, 2 * SEG])
    ones64 = _sb(nc, st, "ones64_sb", [64, 64])
    gnv = _sb(nc, st, "gnv_sb", [64, 2, 2])
    for dst, src, k in [(ident, id_d, "ident"), (msk, msk_d, "msk"),
                        (rmask, rm_d, "rmask"), (ones64, on_d, "ones64"), (gnv, gnv_d, "gnv")]:
        p.dma("sp", dst[:], src, writes=[k])
    Fin = [_sb(nc, st, f"Fin{i}", [64, 6, 2, SEG]) for i in range(2)]
    gbin = [_sb(nc, st, f"gbin{i}", [64, 2, 2, SEG]) for i in range(1)] * 2
    names = ["logw", "cum", "eg", "einv", "egm", "dte", "Af", "Bf", "Kf", "Rf", "Bh", "Kh", "Af32"]
    b16n = ("Af", "Bf", "Kf", "Rf")
    tmpn = ["logw", "cum", "eg", "einv", "egm", "dte"]
    Wtmp = {n: _sb(nc, st, f"wt_{n}", [64, 2, SEG]) for n in tmpn}
    W_ = []
    for i in range(2):
        d_ = dict(Wtmp)
        for n in names:
            if n not in tmpn:
                d_[n] = _sb(nc, st, f"w{i}_{n}", [64, 2, SEG], BF16 if n in b16n else F32)
        W_.append(d_)
    gC = [_sb(nc, st, f"gC{i}", [64, 2, cps]) for i in range(2)]
    NSL = 2 * LOCK
    TM = [_sb(nc, st, f"TM{i}", [64, 4, 128], BF16) for i in range(NSL)]
    MS = [_sb(nc, st, f"MS{i}", [64, 10, 64], BF16) for i in range(NSL)]
    MQ = [[_sb(nc, st, f"MQ{i}_{j}", [64, 4, 64], BF16) for j in range(2)] for i in range(NSL)]
    XW = [_sb(nc, st, f"XW{i}", [64, 2, 128], BF16) for i in range(NSL)]
    ident16 = _sb(nc, st, "ident16", [64, 64], BF16)
    p.op("dve", lambda e: e.tensor_copy(out=ident16[:], in_=ident[0:64, 0:64]), reads=["ident"], writes=["ident16"])
    NCH = 2 * LOCK + 2
    CHb = [_sb(nc, st, f"CHb{i}", [64, 4, 128]) for i in range(NCH)]
    Z = _sb(nc, st, "Zst", [64, 2, 64])
    Z2 = _sb(nc, st, "Zst2", [64, 2, 64])
    YT = [_sb(nc, st, f"YT{i}", [64, 2, SEG]) for i in range(2)]
    ps = [_ps(nc, st, f"ps{i}") for i in range(8)]
    psi = [0]

    def nb():
        i = psi[0] % 8
        psi[0] += 1
        return ps[i], f"ps{i}"

    p.op("dve", lambda e: e.memset(Z[:], 0.0), writes=["Z"])

    def load_seg(sg):
        i = sg % 2
        p.dma("sp", Fin[i][:], F_d[:, :, :, sg * SEG:(sg + 1) * SEG], writes=[f"Fin{i}"])

    def prep_seg(sg):
        i = sg % 2
        Fi = Fin[i]
        w = W_[i]
        fk = f"Fin{i}"
        k = lambda n: (f"wt_{n}" if n in tmpn else f"w{i}_{n}")
        fl = lambda ap: ap.rearrange("p h t -> p (h t)")
        p.op("act", lambda e: e.activation(out=fl(w["logw"][:]), in_=fl(Fi[:, 0]), func=AF.Ln), reads=[fk], writes=[k("logw")])
        p.op("dve", lambda e: e.tensor_tensor_scan(out=fl(w["cum"][:]), data0=rmask[:], data1=fl(w["logw"][:]), initial=0.0, op0=ALU.mult, op1=ALU.add),
             reads=["rmask", k("logw")], writes=[k("cum")])
        p.op("act", lambda e: e.activation(out=fl(w["eg"][:]), in_=fl(w["cum"][:]), func=AF.Exp), reads=[k("cum")], writes=[k("eg")])
        p.op("act", lambda e: e.activation(out=fl(w["einv"][:]), in_=fl(w["cum"][:]), func=AF.Exp, scale=-1.0), reads=[k("cum")], writes=[k("einv")])
        p.op("dve", lambda e: e.tensor_tensor(out=fl(w["egm"][:]), in0=fl(w["cum"][:]), in1=fl(w["logw"][:]), op=ALU.subtract), reads=[k("cum"), k("logw")], writes=[k("egm")])
        p.op("act", lambda e: e.activation(out=fl(w["egm"][:]), in_=fl(w["egm"][:]), func=AF.Exp), reads=[k("egm")], writes=[k("egm")])
        cumv = w["cum"][:].rearrange("p h (c t) -> p (h c) t", t=CH_C)
        p.op("dve", lambda e: e.tensor_tensor(out=w["dte"][:].rearrange("p h (c t) -> p (h c) t", t=CH_C), in0=cumv[:, :, CH_C - 1:CH_C].to_broadcast([64, 2 * cps, CH_C]), in1=cumv, op=ALU.subtract),
             reads=[k("cum")], writes=[k("dte")])
        p.op("act", lambda e: e.activation(out=fl(w["dte"][:]), in_=fl(w["dte"][:]), func=AF.Exp), reads=[k("dte")], writes=[k("dte")])
        for out_n, a_idx, b_n, eng in [("Af", 1, "egm", "dve"), ("Bf", 2, "einv", "pool"), ("Kf", 3, "einv", "dve"),
                                       ("Rf", 4, "eg", "pool"), ("Bh", 2, "dte", "dve"), ("Kh", 3, "dte", "pool"), ("Af32", 1, "egm", "pool")]:
            p.op(eng, lambda e, out_n=out_n, a_idx=a_idx, b_n=b_n: e.tensor_tensor(out=fl(w[out_n][:]), in0=fl(Fi[:, a_idx]), in1=fl(w[b_n][:]), op=ALU.mult),
                 reads=[fk, k(b_n)], writes=[k(out_n)])
        egC = w["eg"][:].rearrange("p h (c t) -> p h c t", t=CH_C)[:, :, :, CH_C - 1]
        p.op("act", lambda e: e.copy(out=gC[i][:], in_=egC), reads=[k("eg")], writes=[f"gC{i}"])

    def pre_stages(sg, cl, slot, chslot):
        i = sg % 2
        w = W_[i]
        Fi = Fin[i]
        k = lambda n: f"w{i}_{n}"
        cs = slice(cl * CH_C, (cl + 1) * CH_C)
        tm, ms, mq, xw, chb = TM[slot], MS[slot], MQ[slot], XW[slot], CHb[chslot]
        tmk, msk_, xwk, chk = f"TM{slot}", f"MS{slot}", f"XW{slot}", f"CHb{chslot}"
        stages = []

        def s1():
            b, bkey = nb()
            for q, (src, skey) in enumerate([(w["Af32"], k("Af32")), (w["Bh"], k("Bh")), (w["Kh"], k("Kh")), (None, f"Fin{i}")]):
                for h in range(2):
                    in_ap = Fi[:, 5, h, cs] if src is None else src[:, h, cs]
                    p.op("pe", lambda e, q=q, h=h, in_ap=in_ap: e.transpose(b[0:64, q * 128 + h * 64:q * 128 + (h + 1) * 64], in_ap, ident[0:64, 0:64]),
                         reads=[skey, "ident"], writes=[bkey])
            p.op("act", lambda e: e.copy(out=tm[:].rearrange("p a b -> p (a b)"), in_=b[0:64, :]), reads=[bkey], writes=[tmk])
        stages.append(s1)

        def s2a():
            b, bkey = nb()
            for h in range(2):
                pb = slice(h * 64, (h + 1) * 64)
                for col, (l, lk, r_, rk) in [(0 + h, (w["Bf"], k("Bf"), w["Af"], k("Af"))), (2 + h, (w["Kf"], k("Kf"), w["Af"], k("Af"))),
                                             (4 + h, (w["Af"], k("Af"), w["Bf"], k("Bf")))]:
                    p.op("pe", lambda e, col=col, l=l, r_=r_, h=h: e.matmul(b[0:64, col * 64:(col + 1) * 64], lhsT=l[:, h, cs], rhs=r_[:, h, cs], start=True, stop=True),
                         reads=[lk, rk], writes=[bkey])
            p.op("dve", lambda e: e.tensor_tensor(out=ms[:, 0:6, :].rearrange("p a b -> p (a b)"), in0=b[0:64, 0:384], in1=msk[:, 0:6, :].rearrange("p a b -> p (a b)"), op=ALU.mult),
                 reads=[bkey, "msk"], writes=[msk_ + "a"])
        stages.append(s2a)

        def s2b():
            b, bkey = nb()
            for h in range(2):
                pb = slice(h * 64, (h + 1) * 64)
                for col, (l, lk) in [(0 + h, (w["Bf"], k("Bf"))), (2 + h, (w["Kf"], k("Kf")))]:
                    p.op("pe", lambda e, col=col, l=l, h=h: e.matmul(b[0:64, col * 64:(col + 1) * 64], lhsT=l[:, h, cs], rhs=w["Rf"][:, h, cs], start=True, stop=True),
                         reads=[lk, k("Rf")], writes=[bkey])
            p.op("dve", lambda e: e.tensor_tensor(out=ms[:, 6:10, :].rearrange("p a b -> p (a b)"), in0=b[0:64, 0:256], in1=msk[:, 6:10, :].rearrange("p a b -> p (a b)"), op=ALU.mult),
                 reads=[bkey, "msk"], writes=[msk_ + "b"])
        stages.append(s2b)

        def s3():
            b, bkey = nb()
            for h in range(2):
                p.op("pe", lambda e, h=h: e.matmul(b[0:64, h * 64:(h + 1) * 64], lhsT=ms[:, 2 + h, :], rhs=tm[:, 3, h * 64:(h + 1) * 64], start=True, stop=True),
                     reads=[msk_ + "a", tmk], writes=[bkey])
            p.op("act", lambda e: e.copy(out=xw[:, :, 64:128], in_=b[0:64, 0:128].rearrange("p (h v) -> p h v", h=2)), reads=[bkey], writes=[xwk + "x"])
            p.op("act", lambda e: e.copy(out=xw[:, :, 0:64], in_=tm[:, 0, :].rearrange("p (h v) -> p h v", h=2)), reads=[tmk], writes=[xwk + "w"])
        stages.append(s3)

        def mk_level(j):
            def lv():
                if j == 0:
                    MT = [ms[:, 0, :], ms[:, 1, :]]
                    M = [ms[:, 4, :], ms[:, 5, :]]
                    mkey = msk_ + "a"
                else:
                    q = mq[j % 2]
                    MT = [q[:, 0, :], q[:, 1, :]]
                    M = [q[:, 2, :], q[:, 3, :]]
                    mkey = f"MQ{slot}_{j % 2}"
                b, bkey = nb()
                for h in range(2):
                    p.op("pe", lambda e, h=h: e.matmul(b[0:64, h * 128:(h + 1) * 128], lhsT=MT[h], rhs=xw[:, h, :], start=True, stop=True),
                         reads=[mkey, xwk + "x", xwk + "w"], writes=[bkey])
                if j < 5:
                    b2, b2key = nb()
                    for h in range(2):
                        p.op("pe", lambda e, h=h: e.matmul(b2[0:64, h * 64:(h + 1) * 64], lhsT=M[h], rhs=MT[h], start=True, stop=True), reads=[mkey], writes=[b2key])
                        p.op("pe", lambda e, h=h: e.matmul(b2[0:64, (2 + h) * 64:(3 + h) * 64], lhsT=MT[h], rhs=M[h], start=True, stop=True), reads=[mkey], writes=[b2key])
                p.op("dve", lambda e: e.tensor_tensor(out=xw[:].rearrange("p a b -> p (a b)"), in0=b[0:64, 0:256], in1=xw[:].rearrange("p a b -> p (a b)"), op=ALU.add),
                     reads=[bkey, xwk + "x", xwk + "w"], writes=[xwk + "x", xwk + "w"])
                if j < 5:
                    nq = mq[(j + 1) % 2]
                    p.op("act", lambda e: e.copy(out=nq[:].rearrange("p a b -> p (a b)"), in_=b2[0:64, 0:256]), reads=[b2key], writes=[f"MQ{slot}_{(j + 1) % 2}"])
            return lv
        for j in range(6):
            stages.append(mk_level(j))

        def s5():
            b, bkey = nb()
            xk = [xwk + "x", xwk + "w"]
            for h in range(2):
                pb = slice(h * 64, (h + 1) * 64)
                hs = slice(h * 64, (h + 1) * 64)
                Wh = xw[:, h, 0:64]
                Xh = xw[:, h, 64:128]
                p.op("pe", lambda e, Wh=Wh, hs=hs, h=h: e.matmul(b[0:64, h * 64:(h + 1) * 64], lhsT=Wh, rhs=tm[:, 1, hs], start=True, stop=True),
                     reads=xk + [tmk], writes=[bkey])
                p.op("pe", lambda e, Xh=Xh, hs=hs, h=h: e.matmul(b[0:64, 128 + h * 64:128 + (h + 1) * 64], lhsT=tm[:, 1, hs], rhs=Xh, start=True, stop=False),
                     reads=xk + [tmk], writes=[bkey])
                p.op("pe", lambda e, hs=hs, h=h: e.matmul(b[0:64, 128 + h * 64:128 + (h + 1) * 64], lhsT=tm[:, 2, hs], rhs=tm[:, 3, hs], start=False, stop=True),
                     reads=[tmk], writes=[bkey])
                p.op("pe", lambda e, Wh=Wh, h=h: e.matmul(b[0:64, 256 + h * 64:256 + (h + 1) * 64], lhsT=Wh, rhs=ms[:, 6 + h, :], start=True, stop=False),
                     reads=xk + [msk_ + "b"], writes=[bkey])
                p.op("pe", lambda e, h=h: e.matmul(b[0:64, 256 + h * 64:256 + (h + 1) * 64], lhsT=ident16[:], rhs=w["Rf"][:, h, cs], start=False, stop=True),
                     reads=["ident16", k("Rf")], writes=[bkey])
                p.op("pe", lambda e, Xh=Xh, h=h: e.matmul(b[0:64, 384 + h * 64:384 + (h + 1) * 64], lhsT=Xh, rhs=ms[:, 6 + h, :], start=True, stop=False),
                     reads=xk + [msk_ + "b"], writes=[bkey])
                p.op("pe", lambda e, hs=hs, h=h: e.matmul(b[0:64, 384 + h * 64:384 + (h + 1) * 64], lhsT=tm[:, 3, hs], rhs=ms[:, 8 + h, :], start=False, stop=True),
                     reads=[tmk, msk_ + "b"], writes=[bkey])
            p.op("act", lambda e: e.copy(out=chb[:].rearrange("p a b -> p (a b)"), in_=b[0:64, :]), reads=[bkey], writes=[chk])
        stages.append(s5)
        return stages

    def chain_step(sg, cl, chslot):
        i = sg % 2
        chb = CHb[chslot]
        chk = f"CHb{chslot}"
        yt = YT[i]
        b, bkey = nb()
        for h in range(2):
            hs = slice(h * 64, (h + 1) * 64)
            p.op("pe", lambda e, h=h, hs=hs: e.matmul(b[0:64, hs], lhsT=chb[:, 0, hs], rhs=Z[:, h, :], start=True, stop=True), reads=[chk, "Z"], writes=[bkey])
            p.op("pe", lambda e, h=h, hs=hs: e.matmul(b[0:64, 128 + h * 64:128 + (h + 1) * 64], lhsT=Z[:, h, :], rhs=chb[:, 2, hs], start=True, stop=True), reads=[chk, "Z"], writes=[bkey])
        p.op("dve", lambda e: e.tensor_tensor(out=yt[:, :, cl * CH_C:(cl + 1) * CH_C], in0=b[0:64, 128:256].rearrange("p (h t) -> p h t", h=2),
                                              in1=chb[:, 3, :].rearrange("p (h t) -> p h t", h=2), op=ALU.add),
             reads=[bkey, chk], writes=[f"YT{i}_{cl}"])
        for h in range(2):
            p.op("dve", lambda e, h=h: e.scalar_tensor_tensor(out=Z2[:, h, :], in0=Z[:, h, :], scalar=gC[i][:, h, cl:cl + 1], in1=b[0:64, h * 64:(h + 1) * 64], op0=ALU.mult, op1=ALU.add),
                 reads=["Z", f"gC{i}", bkey], writes=[f"Z2_{h}"])
        p.op("dve", lambda e: e.tensor_tensor(out=Z[:].rearrange("p a b -> p (a b)"), in0=Z2[:].rearrange("p a b -> p (a b)"), in1=chb[:, 1, :], op=ALU.add),
             reads=["Z2_0", "Z2_1", chk], writes=["Z"])

    yc = _sb(nc, st, "yc", [64, 2 * SEG])
    sq = _sb(nc, st, "sq", [64, 2 * SEG])
    rs = _sb(nc, st, "rs", [64, 2 * SEG])
    ot = [_sb(nc, st, f"ot{i}", [64, 2, SEG]) for i in range(1)] * 2

    def epilogue(sg):
        i = sg % 2
        yt = YT[i]
        ykeys = [f"YT{i}_{cl}" for cl in range(cps)]
        ytf = yt[:].rearrange("p h t -> p (h t)")
        p.dma("sp", gbin[0][:], gb_d[:, :, :, sg * SEG:(sg + 1) * SEG], writes=["gbin0"])
        for hh in range(2):
            b, bkey = nb()
            sl_ = slice(hh * SEG, (hh + 1) * SEG)
            p.op("pe", lambda e, sl_=sl_, b=b: e.matmul(b[0:64, 0:SEG], lhsT=ones64[:], rhs=ytf[:, sl_], start=True, stop=True), reads=["ones64"] + ykeys, writes=[bkey])
            p.op("dve", lambda e, sl_=sl_, b=b: e.scalar_tensor_tensor(out=yc[:, sl_], in0=b[0:64, 0:SEG], scalar=-1.0 / 64, in1=ytf[:, sl_], op0=ALU.mult, op1=ALU.add),
                 reads=[bkey] + ykeys, writes=[f"yc{hh}"])
            p.op("act", lambda e, sl_=sl_: e.activation(out=sq[:, sl_], in_=yc[:, sl_], func=AF.Square), reads=[f"yc{hh}"], writes=[f"sq{hh}"])
            b2, b2key = nb()
            p.op("pe", lambda e, sl_=sl_, b2=b2: e.matmul(b2[0:64, 0:SEG], lhsT=ones64[:], rhs=sq[:, sl_], start=True, stop=True), reads=["ones64", f"sq{hh}"], writes=[b2key])
            p.op("dve", lambda e, sl_=sl_, b2=b2: e.tensor_scalar(out=rs[:, sl_], in0=b2[0:64, 0:SEG], scalar1=1.0 / 64, scalar2=GN_EPS, op0=ALU.mult, op1=ALU.add), reads=[b2key], writes=[f"rs{hh}"])
            p.op("act", lambda e, sl_=sl_: e.activation(out=rs[:, sl_], in_=rs[:, sl_], func=AF.Sqrt), reads=[f"rs{hh}"], writes=[f"rs{hh}"])
            p.op("dve", lambda e, sl_=sl_: e.reciprocal(out=rs[:, sl_], in_=rs[:, sl_]), reads=[f"rs{hh}"], writes=[f"rs{hh}"])
            p.op("dve", lambda e, sl_=sl_: e.tensor_tensor(out=yc[:, sl_], in0=yc[:, sl_], in1=rs[:, sl_], op=ALU.mult), reads=[f"yc{hh}", f"rs{hh}"], writes=[f"yc{hh}"])
            p.op("dve", lambda e, sl_=sl_, hh=hh: e.tensor_scalar(out=yc[:, sl_], in0=yc[:, sl_], scalar1=gnv[:, hh, 0:1], scalar2=gnv[:, hh, 1:2], op0=ALU.mult, op1=ALU.add),
                 reads=[f"yc{hh}", "gnv"], writes=[f"yc{hh}"])
            p.op("pool", lambda e, sl_=sl_, hh=hh: e.tensor_tensor(out=yc[:, sl_], in0=yc[:, sl_], in1=gbin[0][:, 1, hh, :], op=ALU.add), reads=[f"yc{hh}", "gbin0"], writes=[f"yc{hh}"])
            p.op("pool", lambda e, sl_=sl_, hh=hh: e.tensor_tensor(out=ot[0][:, hh, :], in0=yc[:, sl_], in1=gbin[0][:, 0, hh, :], op=ALU.mult), reads=[f"yc{hh}", "gbin0"], writes=[f"ot0_{hh}"])
        p.dma("sp", o_d[:, :, sg * SEG:(sg + 1) * SEG], ot[0][:], reads=["ot0_0", "ot0_1"])

    load_seg(0)
    pending_chain = []
    slot_ctr = 0
    ch_ctr = 0
    for sg in range(nseg):
        if sg + 1 < nseg:
            load_seg(sg + 1)
        prep_seg(sg)
        for c0 in range(0, cps, LOCK):
            sts = []
            new_chain = []
            for gi in range(LOCK):
                sts.append(pre_stages(sg, c0 + gi, slot_ctr % NSL, ch_ctr % NCH))
                new_chain.append((sg, c0 + gi, ch_ctr % NCH))
                slot_ctr += 1
                ch_ctr += 1
            nst = len(sts[0])
            pop_at = set(range(1, nst, max(1, (nst - 1) // LOCK)))
            for si in range(nst):
                for gi in range(LOCK):
                    sts[gi][si]()
                if pending_chain and si in pop_at:
                    a = pending_chain.pop(0)
                    chain_step(*a)
                    if a[1] == cps - 1:
                        epilogue(a[0])
            pending_chain.extend(new_chain)
    while pending_chain:
        a = pending_chain.pop(0)
        chain_step(*a)
        if a[1] == cps - 1:
            epilogue(a[0])


def launch_rwkv_chunked(lc, I, T=S):
    C = CH_C
    su = np.triu(np.ones((C, C), np.float32), 1)
    sle = np.triu(np.ones((C, C), np.float32), 0)
    msk = np.stack([su, su, su, su, su.T, su.T, sle, sle, sle, sle], 0).transpose(1, 0, 2)
    ident = np.eye(128, dtype=np.float32)
    rmask = np.ones((64, 2 * SEG), np.float32)
    rmask[:, ::C] = 0
    ones64 = np.ones((64, 64), np.float32)
    in_maps = []
    for i in range(8):
        cq = slice(i * 128, (i + 1) * 128)
        F = np.stack([lc[i][n][:, :T].reshape(2, 64, T) for n in ("wdec", "nkk", "kka", "kt", "rT", "vrT")], 0)
        F = F.transpose(2, 0, 1, 3)
        gb = np.stack([lc[i]["g"][:, :T].reshape(2, 64, T), lc[i]["bonus"][:, :T].reshape(2, 64, T)], 0)
        gb = gb.transpose(2, 0, 1, 3)
        gnv = np.stack([I["gn_w"][0][cq].reshape(2, 64), I["gn_b"][0][cq].reshape(2, 64)], -1).transpose(1, 0, 2)
        in_maps.append({"F": np.ascontiguousarray(F), "gb": np.ascontiguousarray(gb), "gnv": np.ascontiguousarray(gnv),
                        "ident": ident, "msk": np.ascontiguousarray(msk), "rmask": rmask, "ones64": ones64})
    res = _run(lambda nc, p, st: build_rwkv_chunked(nc, p, st, T), in_maps)
    return np.concatenate([r["o"].transpose(2, 1, 0).reshape(T, 128) for r in res], axis=1)
```

```python
import numpy as np
import concourse.bass as bass
import concourse.mybir as mybir
from concourse.bass_utils import run_bass_kernel_spmd

F32 = mybir.dt.float32
F32R = mybir.dt.float32r
BF16 = mybir.dt.bfloat16
I32 = mybir.dt.int32
U32 = mybir.dt.uint32
AF = mybir.ActivationFunctionType
ALU = mybir.AluOpType
AX = mybir.AxisListType

NDMA_SLOTS = 6


class Prog:
    def __init__(self, nc):
        self.nc = nc
        self.ops = []
        self.last_w = {}
        self.readers = {}
        self.engs = {"pe": nc.tensor, "act": nc.scalar, "dve": nc.vector,
                     "pool": nc.gpsimd, "sp": nc.sync}

    def op(self, eng, fn, reads=(), writes=(), dma=False, force=False, inc=16):
        deps = set()
        raw = set()
        for k in reads:
            if k in self.last_w:
                deps.add(self.last_w[k])
                raw.add(self.last_w[k])
        for k in writes:
            if k in self.last_w:
                deps.add(self.last_w[k])
                raw.add(self.last_w[k])
            for r in self.readers.get(k, ()):
                deps.add(r)
        idx = len(self.ops)
        self.ops.append(dict(eng=eng, fn=fn, deps=deps, raw=raw, dma=dma, force=force, inc=inc))
        for k in reads:
            self.readers.setdefault(k, []).append(idx)
        for k in writes:
            self.last_w[k] = idx
            self.readers[k] = []
        return idx

    def dma(self, q, out, in_, reads=(), writes=(), **kw):
        return self.op(q, lambda e: e.dma_start(out=out, in_=in_, **kw), reads, writes, dma=True)

    def emit(self, stack):
        nc = self.nc
        ops = self.ops
        need = [False] * len(ops)
        for i, o in enumerate(ops):
            nd = set()
            for d in o["deps"]:
                od = ops[d]
                if od["dma"] or o["dma"] or o["force"] or od["eng"] != o["eng"] or (d in o["raw"] and o["eng"] != "pe"):
                    nd.add(d)
            o["xdeps"] = nd
            for d in nd:
                need[d] = True
        for i, o in enumerate(ops):
            if o["dma"]:
                need[i] = True
        esem = {e: stack.enter_context(nc.semaphore("es_" + e)) for e in self.engs}
        dsem = {e: [stack.enter_context(nc.semaphore(f"ds_{e}_{k}")) for k in range(NDMA_SLOTS)]
                for e in ("sp", "act", "pool")}
        ecount = {e: 0 for e in self.engs}
        dcount = {e: 0 for e in dsem}
        signal = [None] * len(ops)
        waited = {}
        nwaits = 0
        actions = {e: [] for e in self.engs}
        for i, o in enumerate(ops):
            e = o["eng"]
            wl = {}
            for d in o["xdeps"]:
                s_, v = signal[d]
                key = id(s_)
                if waited.get((e, key), 0) >= v:
                    continue
                if key not in wl or wl[key][1] < v:
                    wl[key] = (s_, v)
            if o["dma"]:
                j = dcount[e]
                slot = j % NDMA_SLOTS
                s_ = dsem[e][slot]
                prev = o.get("prev_total", None)
                prev = self._slot_total.get((e, slot), 0) if hasattr(self, "_slot_total") else 0
                if prev > 0 and waited.get((e, id(s_)), 0) < prev:
                    if id(s_) not in wl or wl[id(s_)][1] < prev:
                        wl[id(s_)] = (s_, prev)
            for key, (s_, v) in wl.items():
                waited[(e, key)] = v
                nwaits += 1
            sem = None
            inc = 0
            if o["dma"]:
                if not hasattr(self, "_slot_total"):
                    self._slot_total = {}
                j = dcount[e]
                dcount[e] += 1
                slot = j % NDMA_SLOTS
                sem = dsem[e][slot]
                inc = o.get("inc", 16)
                tot = self._slot_total.get((e, slot), 0) + inc
                self._slot_total[(e, slot)] = tot
                signal[i] = (sem, tot)
            elif need[i]:
                ecount[e] += 1
                sem = esem[e]
                inc = 1
                signal[i] = (sem, ecount[e])
            actions[e].append((list(wl.values()), o["fn"], sem, inc))
        finals = {e: [] for e in self.engs}
        for e in dsem:
            for slot in range(NDMA_SLOTS):
                tot = getattr(self, "_slot_total", {}).get((e, slot), 0)
                if tot > 0:
                    finals[e].append((dsem[e][slot], tot))
        bnames = {"pe": "tensor", "act": "scalar", "dve": "vector", "pool": "gpsimd", "sp": "sync"}
        with nc.Block() as block:
            for e in self.engs:
                if not actions[e] and not finals[e]:
                    continue

                def body(eng, e=e):
                    for waits, fn, sem, inc in actions[e]:
                        for s_, v in waits:
                            eng.wait_ge(s_, v)
                        inst = fn(eng)
                        if sem is not None:
                            inst.then_inc(sem, inc)
                    for s_, v in finals[e]:
                        eng.wait_ge(s_, v)
                getattr(block, bnames[e])(body)
        self.stats = dict(n_ops=len(ops), n_waits=nwaits, ecount=ecount, dcount=dcount)
        return self.stats


from contextlib import ExitStack

S = 8192
D = 2048
EPS = 1e-6
_TRACE = False


def _run(build, in_maps):
    nc = bass.Bass("TRN2", target_bir_lowering=False)
    with ExitStack() as st:
        p = Prog(nc)
        build(nc, p, st)
        p.emit(st)
    if _TRACE:
        r = run_bass_kernel_spmd(nc, in_maps, core_ids=list(range(8)), trace=True)
        print("EXEC_NS", getattr(build, "__name__", "?"), r.exec_time_ns, p.stats, flush=True)
    else:
        r = run_bass_kernel_spmd(nc, in_maps, core_ids=list(range(8)))
    return r.results


def _sb(nc, st, name, shape, dt=F32):
    return st.enter_context(nc.sbuf_tensor(name, shape, dt))


def _ps(nc, st, name, shape=(128, 512), dt=F32):
    return st.enter_context(nc.psum_tensor(name, list(shape), dt))


def build_ada(nc, p, st):
    w = nc.dram_tensor("w", [2048, 1536], F32, kind="ExternalInput").ap()
    c = nc.dram_tensor("c", [128, 16], F32, kind="ExternalInput").ap()
    b = nc.dram_tensor("b", [1, 1536], F32, kind="ExternalInput").ap()
    y = nc.dram_tensor("y", [1, 1536], F32, kind="ExternalOutput").ap()
    wt = [_sb(nc, st, f"wt{i}", [128, 1536]) for i in range(2)]
    ct = _sb(nc, st, "ct", [128, 16])
    bt = _sb(nc, st, "bt", [1, 1536])
    acc = _sb(nc, st, "acc", [128, 1536])
    ones = _sb(nc, st, "ones", [128, 1])
    res = _sb(nc, st, "res", [1, 1536])
    ps = [_ps(nc, st, f"ps{i}", (1, 512)) for i in range(2)]
    p.dma("sp", ct[:], c, writes=["ct"])
    p.dma("sp", bt[:], b, writes=["bt"])
    p.op("dve", lambda e: e.memset(ones[:], 1.0), writes=["ones"])
    for kc in range(16):
        i = kc % 2
        p.dma("sp", wt[i][:], w[kc * 128:(kc + 1) * 128, :], writes=[f"wt{i}"])
        if kc == 0:
            p.op("dve", lambda e, i=i, kc=kc: e.tensor_scalar(out=acc[:], in0=wt[i][:], scalar1=ct[:, kc:kc + 1], scalar2=None, op0=ALU.mult),
                 reads=[f"wt{i}", "ct"], writes=["acc"])
        else:
            p.op("dve", lambda e, i=i, kc=kc: e.scalar_tensor_tensor(out=acc[:], in0=wt[i][:], scalar=ct[:, kc:kc + 1], in1=acc[:], op0=ALU.mult, op1=ALU.add),
                 reads=[f"wt{i}", "ct", "acc"], writes=["acc"])
    for j in range(3):
        pj = ps[j % 2]
        p.op("pe", lambda e, j=j, pj=pj: e.matmul(pj[:], lhsT=ones[:], rhs=acc[:, j * 512:(j + 1) * 512], start=True, stop=True),
             reads=["acc", "ones"], writes=[f"ps{j%2}"])
        p.op("dve", lambda e, j=j, pj=pj: e.tensor_tensor(out=res[:, j * 512:(j + 1) * 512], in0=pj[:], in1=bt[:, j * 512:(j + 1) * 512], op=ALU.add),
             reads=[f"ps{j%2}", "bt"], writes=[f"res{j}"])
    p.dma("sp", y, res[:], reads=["res0", "res1", "res2"])


def launch_ada(c, w_ada, b_ada):
    in_maps = [{"w": np.ascontiguousarray(w_ada[:, i * 1536:(i + 1) * 1536]),
                "c": np.ascontiguousarray(c.reshape(16, 128).T),
                "b": np.ascontiguousarray(b_ada[None, i * 1536:(i + 1) * 1536])} for i in range(8)]
    res = _run(build_ada, in_maps)
    return np.concatenate([r["y"][0] for r in res])


def make_build_norm(add_one, has_b, has_base, ntile=8):
    def build(nc, p, st):
        n = ntile * 128
        y = nc.dram_tensor("y", [n, D], F32, kind="ExternalInput").ap()
        g = nc.dram_tensor("g", [1, D], F32, kind="ExternalInput").ap()
        s = nc.dram_tensor("s", [1, D], F32, kind="ExternalInput").ap()
        bv = nc.dram_tensor("bv", [1, D], F32, kind="ExternalInput").ap() if has_b else None
        base = nc.dram_tensor("base", [n, D], F32, kind="ExternalInput").ap() if has_base else None
        o = nc.dram_tensor("o", [n, D], F32, kind="ExternalOutput").ap()
        gb = _sb(nc, st, "gb", [128, D])
        A = _sb(nc, st, "A", [128, D])
        bb = _sb(nc, st, "bb", [128, D]) if has_b else None
        p.dma("sp", gb[:], g.partition_broadcast(128), writes=["gb"])
        p.dma("sp", A[:], s.partition_broadcast(128), writes=["A"])
        if has_b:
            p.dma("sp", bb[:], bv.partition_broadcast(128), writes=["bb"])
        p.op("dve", lambda e: e.scalar_tensor_tensor(out=A[:], in0=A[:], scalar=float(add_one), in1=gb[:], op0=ALU.add, op1=ALU.mult),
             reads=["gb", "A"], writes=["A"])
        yt = [_sb(nc, st, f"yt{i}", [128, D]) for i in range(2)]
        bt = [_sb(nc, st, f"bt{i}", [128, D]) for i in range(2)] if has_base else None
        junk = _sb(nc, st, "junk", [128, D])
        ot = [_sb(nc, st, f"ot{i}", [128, D]) for i in range(2)]
        ss = [_sb(nc, st, f"ss{i}", [128, 4]) for i in range(2)]
        for t in range(ntile):
            i = t % 2
            rows = slice(t * 128, (t + 1) * 128)
            p.dma("sp", yt[i][:], y[rows, :], writes=[f"yt{i}"])
            if has_base:
                p.dma("sp", bt[i][:], base[rows, :], writes=[f"bt{i}"])
            p.op("act", lambda e, i=i: e.activation(out=junk[:], in_=yt[i][:], func=AF.Square, accum_out=ss[i][:, 0:1]),
                 reads=[f"yt{i}"], writes=["junk", f"ss{i}"])
            p.op("dve", lambda e, i=i: e.tensor_scalar(out=ss[i][:, 1:2], in0=ss[i][:, 0:1], scalar1=1.0 / D, scalar2=EPS, op0=ALU.mult, op1=ALU.add),
                 reads=[f"ss{i}"], writes=[f"ss{i}"])
            p.op("act", lambda e, i=i: e.activation(out=ss[i][:, 2:3], in_=ss[i][:, 1:2], func=AF.Sqrt),
                 reads=[f"ss{i}"], writes=[f"ss{i}"])
            p.op("dve", lambda e, i=i: e.reciprocal(out=ss[i][:, 3:4], in_=ss[i][:, 2:3]),
                 reads=[f"ss{i}"], writes=[f"ss{i}"])
            p.op("dve", lambda e, i=i: e.scalar_tensor_tensor(out=ot[i][:], in0=yt[i][:], scalar=ss[i][:, 3:4], in1=A[:], op0=ALU.mult, op1=ALU.mult),
                 reads=[f"yt{i}", f"ss{i}", "A"], writes=[f"ot{i}"], force=True)
            if has_b:
                p.op("pool", lambda e, i=i: e.tensor_tensor(out=ot[i][:], in0=ot[i][:], in1=bb[:], op=ALU.add),
                     reads=[f"ot{i}", "bb"], writes=[f"ot{i}"])
            if has_base:
                p.op("pool", lambda e, i=i: e.tensor_tensor(out=ot[i][:], in0=ot[i][:], in1=bt[i][:], op=ALU.add),
                     reads=[f"ot{i}", f"bt{i}"], writes=[f"ot{i}"])
            p.dma("pool", o[rows, :], ot[i][:], reads=[f"ot{i}"])
    return build


def launch_norm(y, g, s, add_one, bv=None, base=None):
    n = y.shape[0] // 8
    in_maps = []
    for i in range(8):
        m = {"y": np.ascontiguousarray(y[i * n:(i + 1) * n]), "g": np.ascontiguousarray(g[None, :]),
             "s": np.ascontiguousarray(s[None, :])}
        if bv is not None:
            m["bv"] = np.ascontiguousarray(bv[None, :])
        if base is not None:
            m["base"] = np.ascontiguousarray(base[i * n:(i + 1) * n])
        in_maps.append(m)
    res = _run(make_build_norm(add_one, bv is not None, base is not None, n // 128), in_maps)
    return np.concatenate([r["o"] for r in res], axis=0)


LC_OUTS = ["qT", "kT", "vT", "rT", "krT", "vrT", "wdec", "nkk", "kka", "kt", "g", "bonus"]
WDECAY = 0.6065306597126334


def build_inproj(nc, p, st, ntt=16):
    T = ntt * 512
    hTp = nc.dram_tensor("hTp", [D, T + 1], F32, kind="ExternalInput").ap()
    ws_d = nc.dram_tensor("ws", [D, 448], F32, kind="ExternalInput").ap()
    wd_d = nc.dram_tensor("wd", [D, 384], F32, kind="ExternalInput").ap()
    mucol_d = nc.dram_tensor("mucol", [1, 384], F32, kind="ExternalInput").ap()
    mupp_d = nc.dram_tensor("mupp", [128, 3], F32, kind="ExternalInput").ap()
    wl_d = nc.dram_tensor("wl", [D, 448], F32, kind="ExternalInput").ap()
    murow_d = nc.dram_tensor("murow", [128, 16, 3], F32, kind="ExternalInput").ap()
    w2w_d = nc.dram_tensor("w2w", [96, 128], F32, kind="ExternalInput").ap()
    w2a_d = nc.dram_tensor("w2a", [96, 128], F32, kind="ExternalInput").ap()
    w2g_d = nc.dram_tensor("w2g", [128, 2, 128], F32, kind="ExternalInput").ap()
    vecs_d = nc.dram_tensor("vecs", [128, 5], F32, kind="ExternalInput").ap()
    cos_d = nc.dram_tensor("cos", [32, T], F32, kind="ExternalInput").ap()
    sin_d = nc.dram_tensor("sin", [32, T], F32, kind="ExternalInput").ap()
    blk_d = nc.dram_tensor("blk", [128, 128], F32, kind="ExternalInput").ap()
    outs = {n: nc.dram_tensor(n, [128, T], F32, kind="ExternalOutput").ap() for n in LC_OUTS}

    w0 = _sb(nc, st, "w0", [128, 16, 448])
    wa = _sb(nc, st, "wa", [128, 16, 448])
    wb = _sb(nc, st, "wb", [128, 16, 448])
    hb = [_sb(nc, st, f"hb{i}", [128, 16, 514]) for i in range(2)]
    mucol = _sb(nc, st, "mucol_sb", [128, 384])
    murow = _sb(nc, st, "murow_sb", [128, 16, 3])
    w2w = _sb(nc, st, "w2w_sb", [96, 128])
    w2a = _sb(nc, st, "w2a_sb", [96, 128])
    w2g = _sb(nc, st, "w2g_sb", [128, 2, 128])
    vecs = _sb(nc, st, "vecs_sb", [128, 5])
    blk = _sb(nc, st, "blk_sb", [128, 128])
    cs = [_sb(nc, st, f"cs{i}", [32, 2, 512]) for i in range(2)]
    mupp = _sb(nc, st, "mupp_sb", [128, 3])
    carry = _sb(nc, st, "carry_sb", [128, 3])
    Tb = [_sb(nc, st, f"Tb{i}", [128, 514]) for i in range(2)]
    ps = [_ps(nc, st, f"ps{i}") for i in range(8)]
    NOB = 6
    ob = [_sb(nc, st, f"ob{i}", [128, 512]) for i in range(NOB)]
    obi = [0]
    psi = [0]

    def nps():
        i = psi[0] % 8
        psi[0] += 1
        return ps[i], f"ps{i}"

    def nob():
        i = obi[0] % NOB
        obi[0] += 1
        return ob[i], f"ob{i}"

    hview = hTp.rearrange("(c p) t -> p c t", p=128)
    r32 = lambda ap: ap.bitcast(F32R)

    for dst, src, k in [(mucol[:], mucol_d.partition_broadcast(128), "mucol"), (murow[:], murow_d, "murow"),
                        (w2w[:], w2w_d, "w2w"), (w2a[:], w2a_d, "w2a"), (w2g[:], w2g_d, "w2g"),
                        (vecs[:], vecs_d, "vecs"), (blk[:], blk_d, "blk"), (mupp[:], mupp_d, "mupp")]:
        p.dma("sp", dst, src, writes=[k])

    def load_h(tt):
        i = tt % 2
        p.dma("pool", r32(hb[i][:, :, 0:513]), r32(hview[:, :, tt * 512:tt * 512 + 513]), writes=[f"hb{i}"])

    def gemm(tt, kind, co, M, pst, psk):
        i = tt % 2
        for c in range(16):
            cur = hb[i][:, c, 1:513]
            prev = hb[i][:, c, 0:512]
            if kind == "singleA":
                p.op("pe", lambda e, c=c, cur=cur: e.matmul(pst[:M, :], lhsT=r32(wa[:, c, co:co + M]), rhs=r32(cur), start=(c == 0), stop=(c == 15)),
                     reads=["wa", f"hb{i}"], writes=[psk])
            elif kind == "single":
                p.op("pe", lambda e, c=c, cur=cur: e.matmul(pst[:M, :], lhsT=r32(w0[:, c, co:co + M]), rhs=r32(cur), start=(c == 0), stop=(c == 15)),
                     reads=["w0", f"hb{i}"], writes=[psk])
            else:
                p.op("pe", lambda e, c=c, cur=cur: e.matmul(pst[:M, :], lhsT=r32(wa[:, c, co:co + M]), rhs=r32(cur), start=(c == 0), stop=False),
                     reads=["wa", f"hb{i}"], writes=[psk])
                p.op("pe", lambda e, c=c, prev=prev: e.matmul(pst[:M, :], lhsT=r32(wb[:, c, co:co + M]), rhs=r32(prev), start=False, stop=(c == 15)),
                     reads=["wb", f"hb{i}"], writes=[psk])

    p.dma("pool", r32(w0[:]), r32(ws_d.rearrange("(c p) n -> p c n", p=128)), writes=["w0"])
    p.dma("pool", r32(wa[:, :, 0:384]), r32(wd_d.rearrange("(c p) n -> p c n", p=128)), writes=["wa"])
    p.op("dve", lambda e: e.memset(carry[:], 0.0), writes=["carry0", "carry1", "carry2"])
    load_h(0)
    for tt in range(ntt):
        if tt + 1 < ntt:
            load_h(tt + 1)
        tsl = slice(tt * 512, (tt + 1) * 512)
        ci = tt % 2
        p.dma("sp", cs[ci][:, 0, :], cos_d[:, tsl], writes=[f"cs{ci}"])
        p.dma("sp", cs[ci][:, 1, :], sin_d[:, tsl], writes=[f"cs{ci}"])
        for name, co in (("qT", 0), ("kT", 160)):
            pq, pqk = nps()
            gemm(tt, "single", co, 128, pq, pqk)
            psw, pswk = nps()
            gemm(tt, "single", co + 128, 32, psw, pswk)
            o, ok = nob()
            p.op("act", lambda e, o=o, pq=pq: e.copy(out=o[:], in_=pq[:]), reads=[pqk], writes=[ok, ok + "hi"])
            t1, t1k = nob()
            p.op("dve", lambda e, t1=t1, psw=psw, ci=ci: e.tensor_tensor(out=t1[0:32, :], in0=psw[0:32, :], in1=cs[ci][:, 1, :], op=ALU.mult),
                 reads=[pswk, f"cs{ci}"], writes=[t1k])
            p.op("dve", lambda e, o=o, pq=pq, ci=ci: e.tensor_tensor(out=o[0:32, :], in0=pq[0:32, :], in1=cs[ci][:, 0, :], op=ALU.mult),
                 reads=[pqk, f"cs{ci}"], writes=[ok])
            p.op("dve", lambda e, o=o, t1=t1: e.tensor_tensor(out=o[0:32, :], in0=o[0:32, :], in1=t1[0:32, :], op=ALU.add),
                 reads=[ok, t1k], writes=[ok])
            p.dma("pool", outs[name][:, tsl], o[:], reads=[ok, ok + "hi"], writes=[f"d_{name}_{tt}"])
        pv, pvk = nps()
        gemm(tt, "single", 320, 128, pv, pvk)
        o, ok = nob()
        p.op("act", lambda e, o=o, pv=pv: e.copy(out=o[:], in_=pv[:]), reads=[pvk], writes=[ok, ok + "hi"])
        p.dma("pool", outs["vT"][:, tsl], o[:], reads=[ok, ok + "hi"], writes=[f"d_vT_{tt}"])
        for j, name in enumerate(("rT", "krT", "vrT")):
            pr, prk = nps()
            gemm(tt, "singleA", j * 128, 128, pr, prk)
            tb = Tb[(tt * 3 + j) % 2]
            tbk = f"Tb{(tt * 3 + j) % 2}"
            p.op("act", lambda e, tb=tb, j=j: e.copy(out=tb[:, 0:1], in_=carry[:, j:j + 1]), reads=[f"carry{j}"], writes=[tbk + "c"])
            p.op("act", lambda e, tb=tb, pr=pr: e.copy(out=tb[:, 1:513], in_=pr[:]), reads=[prk], writes=[tbk])
            p.op("act", lambda e, tb=tb, j=j: e.copy(out=carry[:, j:j + 1], in_=tb[:, 512:513]), reads=[tbk], writes=[f"carry{j}"])
            d_, dk = nob()
            p.op("dve", lambda e, tb=tb, d_=d_: e.tensor_tensor(out=d_[:], in0=tb[:, 0:512], in1=tb[:, 1:513], op=ALU.subtract), reads=[tbk, tbk + "c"], writes=[dk, dk + "hi"])
            o, ok = nob()
            p.op("dve", lambda e, tb=tb, d_=d_, o=o, j=j: e.scalar_tensor_tensor(out=o[:], in0=d_[:], scalar=mupp[:, j:j + 1], in1=tb[:, 1:513], op0=ALU.mult, op1=ALU.add),
                 reads=[dk, dk + "hi", tbk, "mupp"], writes=[ok, ok + "hi"])
            p.dma("pool", outs[name][:, tsl], o[:], reads=[ok, ok + "hi"], writes=[f"d_{name}_{tt}"])

    p.dma("pool", r32(w0[:]), r32(wl_d.rearrange("(c p) n -> p c n", p=128)), writes=["w0"])
    for c in range(16):
        for j, (lo, hi) in enumerate(((0, 96), (96, 192), (192, 448))):
            p.op("dve", lambda e, c=c, j=j, lo=lo, hi=hi: e.tensor_scalar(out=r32(wb[:, c, lo:hi]), in0=w0[:, c, lo:hi], scalar1=murow[:, c, j:j + 1], scalar2=None, op0=ALU.mult),
                 reads=["w0", "murow"], writes=["wb"])
    p.op("dve", lambda e: e.tensor_tensor(out=r32(wa[:]), in0=w0[:], in1=wb[:], op=ALU.subtract),
         reads=["w0", "wb"], writes=["wa"])
    tw = _sb(nc, st, "tw", [96, 512])
    ta = _sb(nc, st, "ta", [96, 512])
    tg = _sb(nc, st, "tg", [128, 2, 512])
    rin = [_sb(nc, st, f"rin{i}", [128, 3, 512]) for i in range(1)]
    tmp = {n: _sb(nc, st, "tmp_" + n, [128, 512]) for n in ["a", "kkr", "sq", "nrm", "rn", "u", "rk"]}
    load_h(0)
    for tt in range(ntt):
        if tt + 1 < ntt:
            load_h(tt + 1)
        tsl = slice(tt * 512, (tt + 1) * 512)
        ri = 0
        for j, name in enumerate(("rT", "krT", "vrT")):
            p.dma("sp", rin[ri][:, j, :], outs[name][:, tsl], reads=[f"d_{name}_{tt}"], writes=[f"rin{ri}_{j}"])
        R_, KR, VR = rin[ri][:, 0, :], rin[ri][:, 1, :], rin[ri][:, 2, :]
        rk_, krk, vrk = f"rin{ri}_0", f"rin{ri}_1", f"rin{ri}_2"
        pw, pwk = nps()
        gemm(tt, "dual", 0, 96, pw, pwk)
        p.op("act", lambda e, pw=pw: e.activation(out=tw[:], in_=pw[:96, :], func=AF.Tanh), reads=[pwk], writes=["tw"])
        pa, pak = nps()
        gemm(tt, "dual", 96, 96, pa, pak)
        p.op("act", lambda e, pa=pa: e.copy(out=ta[:], in_=pa[:96, :]), reads=[pak], writes=["ta"])
        for h in range(2):
            pg, pgk = nps()
            gemm(tt, "dual", 192 + h * 128, 128, pg, pgk)
            p.op("act", lambda e, pg=pg, h=h: e.activation(out=tg[:, h, :], in_=pg[:], func=AF.Sigmoid), reads=[pgk], writes=[f"tg{h}"])
        pd, pdk = nps()
        p.op("pe", lambda e, pd=pd: e.matmul(pd[:], lhsT=w2w[:], rhs=tw[:], start=True, stop=True), reads=["w2w", "tw"], writes=[pdk])
        o_w, o_wk = nob()
        p.op("act", lambda e, pd=pd: e.activation(out=tmp["sq"][:], in_=pd[:], func=AF.Sigmoid, bias=vecs[:, 0:1]), reads=[pdk, "vecs"], writes=["t_sq"])
        p.op("act", lambda e, o_w=o_w: e.activation(out=o_w[:], in_=tmp["sq"][:], func=AF.Exp, scale=-WDECAY), reads=["t_sq"], writes=[o_wk])
        p.dma("pool", outs["wdec"][:, tsl], o_w[:], reads=[o_wk])
        pa2, pa2k = nps()
        p.op("pe", lambda e, pa2=pa2: e.matmul(pa2[:], lhsT=w2a[:], rhs=ta[:], start=True, stop=True), reads=["w2a", "ta"], writes=[pa2k])
        p.op("act", lambda e, pa2=pa2: e.activation(out=tmp["a"][:], in_=pa2[:], func=AF.Sigmoid, bias=vecs[:, 1:2]), reads=[pa2k, "vecs"], writes=["t_a"])
        pg2, pg2k = nps()
        for h in range(2):
            p.op("pe", lambda e, pg2=pg2, h=h: e.matmul(pg2[:], lhsT=w2g[:, h, :], rhs=tg[:, h, :], start=(h == 0), stop=(h == 1)),
                 reads=["w2g", f"tg{h}"], writes=[pg2k])
        o_g, o_gk = nob()
        p.op("act", lambda e, o_g=o_g, pg2=pg2: e.copy(out=o_g[:], in_=pg2[:]), reads=[pg2k], writes=[o_gk])
        p.dma("pool", outs["g"][:, tsl], o_g[:], reads=[o_gk])
        p.op("dve", lambda e, KR=KR: e.tensor_scalar(out=tmp["kkr"][:], in0=KR, scalar1=vecs[:, 2:3], scalar2=None, op0=ALU.mult),
             reads=[krk, "vecs"], writes=["t_kkr"])
        p.op("pool", lambda e: e.tensor_tensor(out=tmp["sq"][:], in0=tmp["kkr"][:], in1=tmp["kkr"][:], op=ALU.mult),
             reads=["t_kkr", "t_sq"], writes=["t_sq"])
        pn, pnk = nps()
        p.op("pe", lambda e, pn=pn: e.matmul(pn[:], lhsT=blk[:], rhs=tmp["sq"][:], start=True, stop=True), reads=["blk", "t_sq"], writes=[pnk])
        p.op("act", lambda e, pn=pn: e.activation(out=tmp["nrm"][:], in_=pn[:], func=AF.Sqrt), reads=[pnk], writes=["t_nrm"])
        p.op("dve", lambda e: e.tensor_scalar(out=tmp["nrm"][:], in0=tmp["nrm"][:], scalar1=1e-12, scalar2=None, op0=ALU.max),
             reads=["t_nrm"], writes=["t_nrm"])
        p.op("dve", lambda e: e.reciprocal(out=tmp["rn"][:], in_=tmp["nrm"][:]), reads=["t_nrm"], writes=["t_rn"])
        o_n, o_nk = nob()
        p.op("dve", lambda e, o_n=o_n: e.scalar_tensor_tensor(out=o_n[:], in0=tmp["kkr"][:], scalar=-1.0, in1=tmp["rn"][:], op0=ALU.mult, op1=ALU.mult),
             reads=["t_kkr", "t_rn"], writes=[o_nk])
        p.dma("pool", outs["nkk"][:, tsl], o_n[:], reads=[o_nk])
        o_ka, o_kak = nob()
        p.op("dve", lambda e, o_n=o_n, o_ka=o_ka: e.scalar_tensor_tensor(out=o_ka[:], in0=o_n[:], scalar=-1.0, in1=tmp["a"][:], op0=ALU.mult, op1=ALU.mult),
             reads=[o_nk, "t_a"], writes=[o_kak])
        p.dma("pool", outs["kka"][:, tsl], o_ka[:], reads=[o_kak])
        p.op("dve", lambda e: e.tensor_scalar(out=tmp["u"][:], in0=tmp["a"][:], scalar1=-1.0, scalar2=vecs[:, 3:4], op0=ALU.add, op1=ALU.mult),
             reads=["t_a", "vecs"], writes=["t_u"])
        o_kt, o_ktk = nob()
        p.op("dve", lambda e, o_kt=o_kt, KR=KR: e.scalar_tensor_tensor(out=o_kt[:], in0=tmp["u"][:], scalar=1.0, in1=KR, op0=ALU.add, op1=ALU.mult),
             reads=["t_u", krk], writes=[o_ktk])
        p.dma("pool", outs["kt"][:, tsl], o_kt[:], reads=[o_ktk])
        p.op("dve", lambda e, o_kt=o_kt, R_=R_: e.scalar_tensor_tensor(out=tmp["rk"][:], in0=R_, scalar=vecs[:, 4:5], in1=o_kt[:], op0=ALU.mult, op1=ALU.mult),
             reads=[rk_, "vecs", o_ktk], writes=["t_rk"])
        pb, pbk = nps()
        p.op("pe", lambda e, pb=pb: e.matmul(pb[:], lhsT=blk[:], rhs=tmp["rk"][:], start=True, stop=True), reads=["blk", "t_rk"], writes=[pbk])
        o_b, o_bk = nob()
        p.op("dve", lambda e, o_b=o_b, pb=pb, VR=VR: e.tensor_tensor(out=o_b[:], in0=pb[:], in1=VR, op=ALU.mult),
             reads=[pbk, vrk], writes=[o_bk])
        p.dma("pool", outs["bonus"][:, tsl], o_b[:], reads=[o_bk])


def _rope_tables(T):
    half = 16
    inv = (500000.0 ** (-np.arange(half, dtype=np.float32) / half)).astype(np.float32)
    ang = np.arange(T, dtype=np.float32)[:, None] * inv[None, :]
    cos = np.cos(ang).astype(np.float32).T
    sin = np.sin(ang).astype(np.float32).T
    COS = np.concatenate([cos, cos], 0)
    SIN = np.concatenate([-sin, sin], 0)
    return np.ascontiguousarray(COS), np.ascontiguousarray(SIN)


def launch_inproj(h, I):
    T = h.shape[0]
    hTp = np.zeros((D, T + 1), np.float32)
    hTp[:, 1:] = h.T
    w_in = I["w_in"][0]
    COS, SIN = _rope_tables(T)
    swp = np.concatenate([np.arange(16, 32), np.arange(0, 16)])
    blk = np.zeros((128, 128), np.float32)
    blk[:64, :64] = 1
    blk[64:, 64:] = 1
    murow = np.stack([I["mu_w"][0], I["mu_a"][0], I["mu_g"][0]], -1).reshape(16, 128, 3).transpose(1, 0, 2)
    wl = np.concatenate([I["w_w1"][0], I["w_a1"][0], I["w_g1"][0]], 1)
    in_maps = []
    for i in range(8):
        cq = slice(i * 128, (i + 1) * 128)
        q = w_in[:, 0:1024][:, cq]
        k = w_in[:, 1024:2048][:, cq]
        v = w_in[:, 2048:3072][:, cq]
        ws = np.concatenate([q, q[:, swp], k, k[:, swp], v], 1)
        r = w_in[:, 3072:4096][:, cq]
        kr = w_in[:, 4096:5120][:, cq]
        vr = w_in[:, 5120:6144][:, cq]
        wd = np.concatenate([r, kr, vr], 1)
        mucol = np.concatenate([I["mu_r"][0][cq], I["mu_k"][0][cq], I["mu_v"][0][cq]])[None, :]
        vecs = np.stack([I["w0"][0][cq], I["a0"][0][cq], I["k_k"][0][cq], I["k_a"][0][cq], I["r_k"][0].reshape(-1)[cq]], -1)
        mupp = np.stack([I["mu_r"][0][cq], I["mu_k"][0][cq], I["mu_v"][0][cq]], -1)
        in_maps.append({
            "hTp": hTp, "ws": np.ascontiguousarray(ws), "wd": np.ascontiguousarray(wd), "mucol": np.ascontiguousarray(mucol),
            "wl": np.ascontiguousarray(wl), "murow": np.ascontiguousarray(murow),
            "w2w": np.ascontiguousarray(I["w_w2"][0][:, cq]), "w2a": np.ascontiguousarray(I["w_a2"][0][:, cq]),
            "w2g": np.ascontiguousarray(I["w_g2"][0][:, cq].reshape(2, 128, 128).transpose(1, 0, 2)),
            "vecs": np.ascontiguousarray(vecs), "cos": COS, "sin": SIN, "blk": blk, "mupp": np.ascontiguousarray(mupp)})
    ntt = T // 512
    res = _run(lambda nc, p, st: build_inproj(nc, p, st, ntt), in_maps)
    return res


GN_EPS = 64e-5
TCH = 32


def build_rwkv(nc, p, st, T=S):
    nch = T // TCH
    bcin_d = nc.dram_tensor("bcin", [2, nch, 5, TCH, 64], F32, kind="ExternalInput").ap()
    vT_d = nc.dram_tensor("vT", [128, T], F32, kind="ExternalInput").ap()
    g_d = nc.dram_tensor("g", [128, T], F32, kind="ExternalInput").ap()
    bonus_d = nc.dram_tensor("bonus", [128, T], F32, kind="ExternalInput").ap()
    gnv_d = nc.dram_tensor("gnv", [128, 2], F32, kind="ExternalInput").ap()
    sel_d = nc.dram_tensor("sel", [128, 128], F32, kind="ExternalInput").ap()
    blk_d = nc.dram_tensor("blk", [128, 128], F32, kind="ExternalInput").ap()
    o_d = nc.dram_tensor("o", [128, T], F32, kind="ExternalOutput").ap()
    r32 = lambda ap: ap.bitcast(F32R)

    vT = _sb(nc, st, "vT_sb", [128, T])
    yT = _sb(nc, st, "yT_sb", [128, T])
    Sst = _sb(nc, st, "S_sb", [128, 64])
    junk = _sb(nc, st, "junk", [128, 64])
    sa = _sb(nc, st, "sa", [128, 1])
    sel = _sb(nc, st, "sel_sb", [128, 128])
    blk = _sb(nc, st, "blk_sb", [128, 128])
    gnv = _sb(nc, st, "gnv_sb", [128, 2])
    bc = [_sb(nc, st, f"bc{i}", [128, 5, TCH, 64]) for i in range(2)]
    ps = [_ps(nc, st, f"ps{i}") for i in range(8)]
    p.dma("pool", r32(sel[:]), r32(sel_d), writes=["sel"])
    p.dma("sp", blk[:], blk_d, writes=["blk"])
    p.dma("sp", gnv[:], gnv_d, writes=["gnv"])
    p.dma("sp", vT[:], vT_d, writes=["vT"])
    p.op("dve", lambda e: e.memset(Sst[:], 0.0), writes=["S"])
    zer_d = nc.dram_tensor("zer", [126, 5, TCH, 64], F32, kind="ExternalInput").ap()
    for i in range(2):
        p.dma("pool", r32(bc[i][2:128]), r32(zer_d), writes=[f"bc{i}"])

    def load_bc(c):
        i = c % 2
        p.dma("pool", r32(bc[i][0:2]), r32(bcin_d[:, c]), writes=[f"bc{i}"])

    load_bc(0)
    grp = 0
    for c in range(nch):
        if c + 1 < nch:
            load_bc(c + 1)
        bi = c % 2
        for g4 in range(TCH // 4):
            base = (grp % 2) * 3
            grp += 1
            views = []
            for j in range(5):
                bank = ps[base + j // 2]
                bk = f"ps{base + j // 2}"
                half = bank[:, (j % 2) * 256:(j % 2) * 256 + 256]
                p.op("pe", lambda e, half=half, j=j, g4=g4, bi=bi: e.matmul(half, lhsT=r32(sel[:]), rhs=r32(bc[bi][:, j, g4 * 4:(g4 + 1) * 4, :]), start=True, stop=True),
                     reads=["sel", f"bc{bi}"], writes=[bk + f"h{j%2}"])
                views.append((half, bk + f"h{j%2}"))
            for tl in range(4):
                t = c * TCH + g4 * 4 + tl
                cs = slice(tl * 64, (tl + 1) * 64)
                wv, nv, kav, ktv, rv = [(v[0][:, cs], v[1]) for v in views]
                p.op("dve", lambda e, nv=nv: e.scalar_tensor_tensor(out=junk[:], in0=Sst[:], scalar=1.0, in1=nv[0], op0=ALU.mult, op1=ALU.mult, accum_out=sa[:]),
                     reads=["S", nv[1]], writes=["junk", "sa"])
                p.op("dve", lambda e, wv=wv: e.tensor_tensor(out=Sst[:], in0=Sst[:], in1=wv[0], op=ALU.mult),
                     reads=["S", wv[1]], writes=["S"])
                p.op("dve", lambda e, kav=kav: e.scalar_tensor_tensor(out=Sst[:], in0=kav[0], scalar=sa[:, 0:1], in1=Sst[:], op0=ALU.mult, op1=ALU.add),
                     reads=["S", "sa", kav[1]], writes=["S"], force=True)
                p.op("dve", lambda e, ktv=ktv, t=t: e.scalar_tensor_tensor(out=Sst[:], in0=ktv[0], scalar=vT[:, t:t + 1], in1=Sst[:], op0=ALU.mult, op1=ALU.add),
                     reads=["S", "vT", ktv[1]], writes=["S"])
                p.op("dve", lambda e, rv=rv, t=t: e.scalar_tensor_tensor(out=junk[:], in0=Sst[:], scalar=1.0, in1=rv[0], op0=ALU.mult, op1=ALU.mult, accum_out=yT[:, t:t + 1]),
                     reads=["S", rv[1]], writes=["junk", f"yT{t // 512}"])
    gt = [_sb(nc, st, f"g_sb{i}", [128, 512]) for i in range(2)]
    bt = [_sb(nc, st, f"b_sb{i}", [128, 512]) for i in range(2)]
    yc = _sb(nc, st, "yc", [128, 512])
    sq = _sb(nc, st, "sq", [128, 512])
    rs = _sb(nc, st, "rs", [128, 512])
    ot = [_sb(nc, st, f"ot{i}", [128, 512]) for i in range(2)]
    for tt in range(T // 512):
        i = tt % 2
        tsl = slice(tt * 512, (tt + 1) * 512)
        p.dma("sp", gt[i][:], g_d[:, tsl], writes=[f"gt{i}"])
        p.dma("sp", bt[i][:], bonus_d[:, tsl], writes=[f"bt{i}"])
        pm, pmk = ps[6], "ps6"
        p.op("pe", lambda e, tsl=tsl: e.matmul(ps[6][:], lhsT=blk[:], rhs=yT[:, tsl], start=True, stop=True), reads=["blk", f"yT{tt}"], writes=["ps6"])
        p.op("dve", lambda e, tsl=tsl: e.scalar_tensor_tensor(out=yc[:], in0=ps[6][:], scalar=-1.0 / 64, in1=yT[:, tsl], op0=ALU.mult, op1=ALU.add),
             reads=["ps6", f"yT{tt}"], writes=["yc"])
        p.op("act", lambda e: e.activation(out=sq[:], in_=yc[:], func=AF.Square), reads=["yc"], writes=["sq"])
        p.op("pe", lambda e: e.matmul(ps[7][:], lhsT=blk[:], rhs=sq[:], start=True, stop=True), reads=["blk", "sq"], writes=["ps7"])
        p.op("dve", lambda e: e.tensor_scalar(out=rs[:], in0=ps[7][:], scalar1=1.0 / 64, scalar2=GN_EPS, op0=ALU.mult, op1=ALU.add), reads=["ps7"], writes=["rs"])
        p.op("act", lambda e: e.activation(out=rs[:], in_=rs[:], func=AF.Sqrt), reads=["rs"], writes=["rs"])
        p.op("dve", lambda e: e.reciprocal(out=rs[:], in_=rs[:]), reads=["rs"], writes=["rs"])
        p.op("dve", lambda e: e.tensor_tensor(out=yc[:], in0=yc[:], in1=rs[:], op=ALU.mult), reads=["yc", "rs"], writes=["yc"])
        p.op("dve", lambda e: e.tensor_scalar(out=yc[:], in0=yc[:], scalar1=gnv[:, 0:1], scalar2=gnv[:, 1:2], op0=ALU.mult, op1=ALU.add), reads=["yc", "gnv"], writes=["yc"])
        p.op("dve", lambda e, i=i: e.tensor_tensor(out=yc[:], in0=yc[:], in1=bt[i][:], op=ALU.add), reads=["yc", f"bt{i}"], writes=["yc"])
        p.op("dve", lambda e, i=i: e.tensor_tensor(out=ot[i][:], in0=yc[:], in1=gt[i][:], op=ALU.mult), reads=["yc", f"gt{i}"], writes=[f"ot{i}"])
        p.dma("sp", o_d[:, tsl], ot[i][:], reads=[f"ot{i}"])


def launch_rwkv(lc, I, T=S):
    sel = np.zeros((128, 128), np.float32)
    sel[0, :64] = 1
    sel[1, 64:] = 1
    blk = np.zeros((128, 128), np.float32)
    blk[:64, :64] = 1
    blk[64:, 64:] = 1
    in_maps = []
    nch = T // TCH
    for i in range(8):
        cq = slice(i * 128, (i + 1) * 128)
        q5 = np.stack([lc[i][n][:, :T] for n in ("wdec", "nkk", "kka", "kt", "rT")], 0)
        q5 = q5.reshape(5, 2, 64, nch, TCH).transpose(1, 3, 0, 4, 2)
        gnv = np.stack([I["gn_w"][0][cq], I["gn_b"][0][cq]], -1)
        in_maps.append({"bcin": np.ascontiguousarray(q5), "vT": np.ascontiguousarray(lc[i]["vrT"][:, :T]),
                        "g": np.ascontiguousarray(lc[i]["g"][:, :T]), "bonus": np.ascontiguousarray(lc[i]["bonus"][:, :T]),
                        "gnv": np.ascontiguousarray(gnv), "sel": sel, "blk": blk, "zer": np.zeros((126, 5, TCH, 64), np.float32)})
    res = _run(lambda nc, p, st: build_rwkv(nc, p, st, T), in_maps)
    return np.concatenate([r["o"].T for r in res], axis=1)


NEGB = 30000.0


def build_moba(nc, p, st, T=S):
    nb = T // 256
    nkt = T // 128
    qT_d = nc.dram_tensor("qT", [128, T], F32, kind="ExternalInput").ap()
    kT_d = nc.dram_tensor("kT", [128, T], F32, kind="ExternalInput").ap()
    v_d = nc.dram_tensor("v", [128, nkt, 128], F32, kind="ExternalInput").ap()
    E_d = nc.dram_tensor("E", [128, T], F32, kind="ExternalInput").ap()
    cm_d = nc.dram_tensor("cm", [128, 256], F32, kind="ExternalInput").ap()
    id_d = nc.dram_tensor("ident", [128, 128], F32, kind="ExternalInput").ap()
    on_d = nc.dram_tensor("ones", [128, 128], F32, kind="ExternalInput").ap()
    o_d = nc.dram_tensor("oT", [128, T], F32, kind="ExternalOutput").ap()
    r32 = lambda ap: ap.bitcast(F32R)
    qT = _sb(nc, st, "qT_sb", [128, T])
    kT = _sb(nc, st, "kT_sb", [128, T])
    va = _sb(nc, st, "va_sb", [128, nkt, 128])
    E = _sb(nc, st, "E_sb", [128, T])
    cm = _sb(nc, st, "cm_sb", [128, 256])
    ident = _sb(nc, st, "id_sb", [128, 128])
    ones = _sb(nc, st, "ones_sb", [128, 128])
    kmean = _sb(nc, st, "kmean", [128, 32])
    gsb = _sb(nc, st, "gsb", [128, 32])
    m8 = _sb(nc, st, "m8", [128, 8])
    bias = _sb(nc, st, "bias", [128, 128])
    biasT = [_sb(nc, st, f"biasT{i}", [128, 256]) for i in range(2)]
    pT = [_sb(nc, st, f"pT{i}", [128, 256]) for i in range(3)]
    osb = [_sb(nc, st, f"osb{i}", [128, 256]) for i in range(2)]
    rden = _sb(nc, st, "rden", [128, 256])
    s_ps = [_ps(nc, st, f"s_ps{i}") for i in range(2)]
    o_ps = [_ps(nc, st, f"o_ps{i}") for i in range(2)]
    d_ps = [_ps(nc, st, f"d_ps{i}") for i in range(2)]
    g_ps = _ps(nc, st, "g_ps")
    t_ps = _ps(nc, st, "t_ps")
    p.dma("pool", r32(cm[:]), r32(cm_d), writes=["cm"])
    p.dma("pool", r32(ident[:]), r32(id_d), writes=["ident"])
    p.dma("pool", r32(ones[:]), r32(on_d), writes=["ones"])
    PCS = 1024
    npc = max(1, T // PCS)
    pc = lambda tok: min(tok // PCS, npc - 1)
    for i_ in range(npc):
        tsl_ = slice(i_ * PCS, min(T, (i_ + 1) * PCS))
        ktl_ = slice(i_ * PCS // 128, min(T, (i_ + 1) * PCS) // 128)
        p.dma("pool", r32(qT[:, tsl_]), r32(qT_d[:, tsl_]), writes=[f"qT{i_}"])
        p.dma("pool", r32(kT[:, tsl_]), r32(kT_d[:, tsl_]), writes=[f"kT{i_}"])
        p.dma("pool", r32(va[:, ktl_, :]), r32(v_d[:, ktl_, :]), writes=[f"va{i_}"])
        p.dma("pool", r32(E[:, tsl_]), r32(E_d[:, tsl_]), writes=[f"E{i_}"])
        nb0 = i_ * PCS // 256
        nb1 = min(T, (i_ + 1) * PCS) // 256
        p.op("dve", lambda e, tsl_=tsl_, nb0=nb0, nb1=nb1: e.tensor_reduce(out=kmean[:, nb0:nb1], in_=kT[:, tsl_].bitcast(F32).rearrange("p (n k) -> p n k", k=256), axis=AX.X, op=ALU.add),
             reads=[f"kT{i_}"], writes=[f"kmean{i_}"])
        p.op("dve", lambda e, nb0=nb0, nb1=nb1: e.tensor_scalar(out=kmean[:, nb0:nb1], in0=kmean[:, nb0:nb1], scalar1=1.0 / 256, scalar2=None, op0=ALU.mult),
             reads=[f"kmean{i_}"], writes=[f"kmean{i_}"])
    kmkeys = lambda b_: [f"kmean{i_}" for i_ in range(pc(b_ * 256) + 1)]
    p.op("dve", lambda e: e.memset(gsb[:], -1e30), writes=["gsb"])
    p.op("dve", lambda e: e.memset(bias[:], 0.0), writes=["bias"])
    scale = 128 ** -0.5
    pti = 0
    si = 0
    for b in range(nb):
        bT = biasT[b % 2]
        bTk = f"biasT{b % 2}"
        for j in range(2):
            qs = slice(b * 256 + j * 128, b * 256 + (j + 1) * 128)
            if b > 3:
                p.op("pe", lambda e, qs=qs: e.matmul(g_ps[:, 0:nb], lhsT=qT[:, qs], rhs=kmean[:, 0:nb], start=True, stop=True),
                     reads=[f"qT{pc(b * 256)}"] + kmkeys(b), writes=["g_ps"])
                p.op("dve", lambda e, b=b: e.tensor_copy(out=gsb[:, 0:b], in_=g_ps[:, 0:b]), reads=["g_ps", "gsb"], writes=["gsb"])
                p.op("dve", lambda e: e.max(out=m8[:], in_=gsb[:]), reads=["gsb"], writes=["m8"])
                p.op("dve", lambda e: e.tensor_scalar(out=bias[:, 0:32], in0=gsb[:], scalar1=m8[:, 2:3], scalar2=None, op0=ALU.is_ge),
                     reads=["gsb", "m8", "bias"], writes=["bias"], force=True)
                p.op("dve", lambda e: e.tensor_scalar(out=bias[:, 0:32], in0=bias[:, 0:32], scalar1=NEGB, scalar2=-NEGB, op0=ALU.mult, op1=ALU.add),
                     reads=["bias"], writes=["bias"])
            else:
                p.op("dve", lambda e: e.memset(bias[:, 0:32], -NEGB), reads=["bias"], writes=["bias"])
                if b > 0:
                    p.op("dve", lambda e, b=b: e.memset(bias[:, 0:b], 0.0), reads=["bias"], writes=["bias"])
            p.op("dve", lambda e, b=b: e.memset(bias[:, b:b + 1], 0.0), reads=["bias"], writes=["bias"])
            p.op("pe", lambda e: e.transpose(t_ps[:, 0:128], bias[:], ident[:].bitcast(F32)), reads=["bias", "ident"], writes=["t_ps"])
            p.op("act", lambda e, bT=bT, j=j: e.copy(out=r32(bT[:, j * 128:(j + 1) * 128]), in_=t_ps[:, 0:128]), reads=["t_ps"], writes=[bTk])
        op_ = o_ps[b % 2]
        opk = f"o_ps{b % 2}"
        dp_ = d_ps[b % 2]
        dpk = f"d_ps{b % 2}"
        nkt_b = 2 * b + 2

        def qkm(kt, sp_, spk, b=b, bT=bT, bTk=bTk):
            ks = slice(kt * 128, (kt + 1) * 128)
            own = kt >= 2 * b
            p.op("pe", lambda e: e.matmul(sp_[:, 0:256], lhsT=r32(kT[:, ks]), rhs=r32(qT[:, b * 256:(b + 1) * 256]), start=True, stop=False),
                 reads=[f"kT{pc(kt * 128)}", f"qT{pc(b * 256)}"], writes=[spk])
            p.op("pe", lambda e: e.matmul(sp_[:, 0:256], lhsT=r32(E[:, ks]), rhs=r32(bT[:]), start=False, stop=(not own)),
                 reads=[f"E{pc(kt * 128)}", bTk], writes=[spk])
            if own:
                if kt == 2 * b:
                    p.op("pe", lambda e: e.matmul(sp_[:, 0:128], lhsT=ident[:].bitcast(F32), rhs=cm[:, 128:256].bitcast(F32), start=False, stop=True),
                         reads=["ident", "cm"], writes=[spk])
                else:
                    p.op("pe", lambda e: e.matmul(sp_[:, 0:256], lhsT=r32(ident[:]), rhs=r32(cm[:, 0:256]), start=False, stop=True),
                         reads=["ident", "cm"], writes=[spk])

        cur = (s_ps[si % 2], f"s_ps{si % 2}")
        si += 1
        qkm(0, *cur)
        for kt in range(nkt_b):
            nxt = None
            if kt + 1 < nkt_b:
                nxt = (s_ps[si % 2], f"s_ps{si % 2}")
                si += 1
                qkm(kt + 1, *nxt)
            sp_, spk = cur
            pt = pT[pti % 3]
            ptk = f"pT{pti % 3}"
            pti += 1
            p.op("act", lambda e, pt=pt, sp_=sp_: e.activation(out=r32(pt[:]), in_=sp_[:, 0:256], func=AF.Exp, scale=scale), reads=[spk], writes=[ptk])
            p.op("pe", lambda e, pt=pt, kt=kt, op_=op_, nkt_b=nkt_b: e.matmul(op_[:, 0:256], lhsT=r32(va[:, kt, :]), rhs=r32(pt[:]), start=(kt == 0), stop=(kt == nkt_b - 1)),
                 reads=[ptk, f"va{pc(kt * 128)}"], writes=[opk])
            p.op("pe", lambda e, pt=pt, kt=kt, dp_=dp_, nkt_b=nkt_b: e.matmul(dp_[:, 0:256], lhsT=r32(ones[:]), rhs=r32(pt[:]), start=(kt == 0), stop=(kt == nkt_b - 1)),
                 reads=[ptk, "ones"], writes=[dpk])
            cur = nxt
        ob_ = osb[b % 2]
        p.op("dve", lambda e, dp_=dp_: e.reciprocal(out=rden[:], in_=dp_[:, 0:256]), reads=[dpk], writes=["rden"])
        p.op("dve", lambda e, op_=op_, ob_=ob_: e.tensor_tensor(out=ob_[:], in0=op_[:, 0:256], in1=rden[:], op=ALU.mult), reads=[opk, "rden"], writes=[f"osb{b % 2}"])
        p.dma("sp", o_d[:, b * 256:(b + 1) * 256], ob_[:], reads=[f"osb{b % 2}"])


def launch_moba(lc, T=S):
    nkt = T // 128
    E = np.zeros((128, T), np.float32)
    for n in range(T // 256):
        E[n, n * 256:(n + 1) * 256] = 1
    kk = np.arange(128)
    cmc = np.where(kk[:, None] <= kk[None, :], 0.0, -NEGB).astype(np.float32)
    cm = np.concatenate([np.full((128, 128), -NEGB, np.float32), cmc], 1)
    ident = np.eye(128, dtype=np.float32)
    ones = np.ones((128, 128), np.float32)
    in_maps = []
    for i in range(8):
        v = lc[i]["vT"][:, :T].T.reshape(nkt, 128, 128).transpose(1, 0, 2)
        in_maps.append({"qT": np.ascontiguousarray(lc[i]["qT"][:, :T]), "kT": np.ascontiguousarray(lc[i]["kT"][:, :T]),
                        "v": np.ascontiguousarray(v), "E": E, "cm": cm, "ident": ident, "ones": ones})
    res = _run(lambda nc, p, st: build_moba(nc, p, st, T), in_maps)
    return np.concatenate([r["oT"].T for r in res], axis=1)


def build_merge(nc, p, st, nunit=2):
    TT = 512
    NT = nunit * TT
    hT_d = nc.dram_tensor("hT", [D, NT], F32, kind="ExternalInput").ap()
    oa_d = nc.dram_tensor("oaT", [1024, NT], F32, kind="ExternalInput").ap()
    or_d = nc.dram_tensor("orT", [1024, NT], F32, kind="ExternalInput").ap()
    wg_d = nc.dram_tensor("wg", [D, 4096], F32, kind="ExternalInput").ap()
    wua_d = nc.dram_tensor("wua", [1024, D], F32, kind="ExternalInput").ap()
    wur_d = nc.dram_tensor("wur", [1024, D], F32, kind="ExternalInput").ap()
    wo_d = nc.dram_tensor("wo", [D, D], F32, kind="ExternalInput").ap()
    y_d = nc.dram_tensor("yT", [D, NT], F32, kind="ExternalOutput").ap()
    r32 = lambda ap: ap.bitcast(F32R)
    hT = _sb(nc, st, "hT_sb", [128, 16, TT])
    oa = _sb(nc, st, "oa_sb", [128, 8, TT])
    orr = _sb(nc, st, "or_sb", [128, 8, TT])
    mix = _sb(nc, st, "mix_sb", [128, 16, TT])
    wb = [_sb(nc, st, f"wb{i}", [128, 48, 128]) for i in range(2)]
    wob = [_sb(nc, st, f"wob{i}", [128, 16, 128]) for i in range(2)]
    sg = [_sb(nc, st, f"sg{i}", [128, TT]) for i in range(2)]
    m12 = [_sb(nc, st, f"m12_{i}", [128, TT]) for i in range(2)]
    yo = [_sb(nc, st, f"yo{i}", [128, TT]) for i in range(2)]
    ps = [_ps(nc, st, f"ps{i}") for i in range(8)]
    wgv = wg_d.rearrange("(c p) n -> p c n", p=128)
    wuav = wua_d.rearrange("(c p) n -> p c n", p=128)
    wurv = wur_d.rearrange("(c p) n -> p c n", p=128)
    wov = wo_d.rearrange("(c p) n -> p c n", p=128)
    wcount = 0
    for u in range(nunit):
        ts_ = slice(u * TT, (u + 1) * TT)
        p.dma("pool", r32(hT[:]), r32(hT_d.rearrange("(c p) t -> p c t", p=128)[:, :, ts_]), writes=["hT"])
        p.dma("pool", r32(oa[:]), r32(oa_d.rearrange("(c p) t -> p c t", p=128)[:, :, ts_]), writes=["oa"])
        p.dma("pool", r32(orr[:]), r32(or_d.rearrange("(c p) t -> p c t", p=128)[:, :, ts_]), writes=["or"])
        for n in range(16):
            wi = wcount % 2
            wcount += 1
            W = wb[wi]
            wk = f"wb{wi}"
            ns = slice(n * 128, (n + 1) * 128)
            ns2 = slice(2048 + n * 128, 2048 + (n + 1) * 128)
            p.dma("pool", r32(W[:, 0:8, :]), r32(wgv[:, 0:8, ns]), writes=[wk + "a"])
            p.dma("pool", r32(W[:, 8:16, :]), r32(wgv[:, 8:16, ns]), writes=[wk + "b"])
            p.dma("pool", r32(W[:, 16:24, :]), r32(wgv[:, 0:8, ns2]), writes=[wk + "c"])
            p.dma("pool", r32(W[:, 24:32, :]), r32(wgv[:, 8:16, ns2]), writes=[wk + "d"])
            p.dma("pool", r32(W[:, 32:40, :]), r32(wuav[:, :, ns]), writes=[wk + "e"])
            p.dma("pool", r32(W[:, 40:48, :]), r32(wurv[:, :, ns]), writes=[wk + "f"])
            wkeys = [wk + x for x in "abcdef"]
            b0 = (n % 2) * 4
            pga, pgr, pua, pur = ps[b0], ps[b0 + 1], ps[b0 + 2], ps[b0 + 3]
            for c in range(16):
                p.op("pe", lambda e, c=c, W=W, pga=pga: e.matmul(pga[:], lhsT=r32(W[:, c, :]), rhs=r32(hT[:, c, :]), start=(c == 0), stop=(c == 15)),
                     reads=wkeys + ["hT"], writes=[f"ps{b0}"])
            for c in range(16):
                p.op("pe", lambda e, c=c, W=W, pgr=pgr: e.matmul(pgr[:], lhsT=r32(W[:, 16 + c, :]), rhs=r32(hT[:, c, :]), start=(c == 0), stop=(c == 15)),
                     reads=wkeys + ["hT"], writes=[f"ps{b0 + 1}"])
            for c in range(8):
                p.op("pe", lambda e, c=c, W=W, pua=pua: e.matmul(pua[:], lhsT=r32(W[:, 32 + c, :]), rhs=r32(oa[:, c, :]), start=(c == 0), stop=(c == 7)),
                     reads=wkeys + ["oa"], writes=[f"ps{b0 + 2}"])
            for c in range(8):
                p.op("pe", lambda e, c=c, W=W, pur=pur: e.matmul(pur[:], lhsT=r32(W[:, 40 + c, :]), rhs=r32(orr[:, c, :]), start=(c == 0), stop=(c == 7)),
                     reads=wkeys + ["or"], writes=[f"ps{b0 + 3}"])
            p.op("act", lambda e, pga=pga: e.activation(out=sg[0][:], in_=pga[:], func=AF.Sigmoid), reads=[f"ps{b0}"], writes=["sg0"])
            p.op("act", lambda e, pgr=pgr: e.activation(out=sg[1][:], in_=pgr[:], func=AF.Sigmoid), reads=[f"ps{b0 + 1}"], writes=["sg1"])
            p.op("dve", lambda e, pua=pua: e.tensor_tensor(out=m12[0][:], in0=pua[:], in1=sg[0][:], op=ALU.mult), reads=[f"ps{b0 + 2}", "sg0"], writes=["m0"])
            p.op("dve", lambda e, pur=pur: e.tensor_tensor(out=m12[1][:], in0=pur[:], in1=sg[1][:], op=ALU.mult), reads=[f"ps{b0 + 3}", "sg1"], writes=["m1"])
            p.op("dve", lambda e, n=n: e.tensor_tensor(out=r32(mix[:, n, :]), in0=m12[0][:], in1=m12[1][:], op=ALU.add), reads=["m0", "m1"], writes=[f"mix{n}"])
        for m in range(16):
            wi = m % 2
            ms = slice(m * 128, (m + 1) * 128)
            p.dma("pool", r32(wob[wi][:, 0:8, :]), r32(wov[:, 0:8, ms]), writes=[f"wob{wi}a"])
            p.dma("pool", r32(wob[wi][:, 8:16, :]), r32(wov[:, 8:16, ms]), writes=[f"wob{wi}b"])
            py = ps[m % 2]
            for c in range(16):
                p.op("pe", lambda e, c=c, wi=wi, py=py: e.matmul(py[:], lhsT=r32(wob[wi][:, c, :]), rhs=r32(mix[:, c, :]), start=(c == 0), stop=(c == 15)),
                     reads=[f"wob{wi}a", f"wob{wi}b", f"mix{c}"], writes=[f"ps{m % 2}"])
            p.op("act", lambda e, py=py, wi=wi: e.copy(out=yo[wi][:], in_=py[:]), reads=[f"ps{m % 2}"], writes=[f"yo{wi}"])
            p.dma("pool", y_d[ms, ts_], yo[wi][:], reads=[f"yo{wi}"])


def launch_merge(h1, o_att, o_rwkv, I):
    wg = np.ascontiguousarray(I["w_in"][0][:, 6144:10240])
    in_maps = []
    for i in range(8):
        ts_ = slice(i * 1024, (i + 1) * 1024)
        in_maps.append({"hT": np.ascontiguousarray(h1[ts_].T), "oaT": np.ascontiguousarray(o_att[ts_].T),
                        "orT": np.ascontiguousarray(o_rwkv[ts_].T), "wg": wg,
                        "wua": I["w_up_att"][0], "wur": I["w_up_rwkv"][0], "wo": I["w_o"][0]})
    res = _run(lambda nc, p, st: build_merge(nc, p, st, 2), in_maps)
    return np.concatenate([r["yT"].T for r in res], axis=0)


def build_router(nc, p, st, ntile=8):
    NT = ntile * 128
    hT_d = nc.dram_tensor("hT", [D, NT], F32, kind="ExternalInput").ap()
    wr_d = nc.dram_tensor("wr", [D, 72], F32, kind="ExternalInput").ap()
    br_d = nc.dram_tensor("br", [1, 72], F32, kind="ExternalInput").ap()
    o_d = nc.dram_tensor("o", [NT, 4], F32, kind="ExternalOutput").ap()
    hT = _sb(nc, st, "hT_sb", [128, 16, NT])
    wr = _sb(nc, st, "wr_sb", [128, 16, 72])
    br = _sb(nc, st, "br_sb", [128, 72])
    ps = [_ps(nc, st, f"ps{i}") for i in range(2)]
    p.dma("sp", hT[:], hT_d.rearrange("(c p) t -> p c t", p=128), writes=["hT"])
    p.dma("sp", wr[:], wr_d.rearrange("(c p) n -> p c n", p=128), writes=["wr"])
    p.dma("sp", br[:], br_d.partition_broadcast(128), writes=["br"])
    l_sb = _sb(nc, st, "l_sb", [128, 72])
    lem = _sb(nc, st, "lem", [128, 64])
    m8g = _sb(nc, st, "m8g", [128, 8])
    m8e = _sb(nc, st, "m8e", [128, 8])
    idx = _sb(nc, st, "idx", [128, 8], U32)
    sm = _sb(nc, st, "sm", [128, 8])
    junk = _sb(nc, st, "junk", [128, 8])
    pen = _sb(nc, st, "pen", [128, 8])
    res = [_sb(nc, st, f"res{i}", [128, 4]) for i in range(2)]
    for t in range(ntile):
        pt = ps[t % 2]
        ptk = f"ps{t % 2}"
        rs_ = res[t % 2]
        rk = f"res{t % 2}"
        for c in range(16):
            p.op("pe", lambda e, c=c, t=t, pt=pt: e.matmul(pt[:, 0:72], lhsT=hT[:, c, t * 128:(t + 1) * 128], rhs=wr[:, c, :], start=(c == 0), stop=(c == 15)),
                 reads=["hT", "wr"], writes=[ptk])
        p.op("dve", lambda e, pt=pt: e.tensor_tensor(out=l_sb[:], in0=pt[:, 0:72], in1=br[:], op=ALU.add), reads=[ptk, "br"], writes=["l"])
        p.op("dve", lambda e: e.max(out=m8g[:], in_=l_sb[:, 0:8]), reads=["l"], writes=["m8g"])
        p.op("dve", lambda e: e.tensor_scalar(out=sm[:, 0:1], in0=m8g[:, 0:1], scalar1=-1.0, scalar2=None, op0=ALU.mult), reads=["m8g"], writes=["sm0"])
        p.op("act", lambda e: e.activation(out=junk[:], in_=l_sb[:, 0:8], func=AF.Exp, bias=sm[:, 0:1], accum_out=sm[:, 1:2]), reads=["l", "sm0"], writes=["junk", "sm1"])
        p.op("dve", lambda e: e.reciprocal(out=sm[:, 2:3], in_=sm[:, 1:2]), reads=["sm1"], writes=["sm2"])
        p.op("dve", lambda e: e.tensor_scalar(out=pen[:], in0=l_sb[:, 0:8], scalar1=m8g[:, 0:1], scalar2=None, op0=ALU.is_ge), reads=["l", "m8g"], writes=["pen"], force=True)
        p.op("dve", lambda e: e.tensor_scalar(out=pen[:], in0=pen[:], scalar1=1e30, scalar2=-1e30, op0=ALU.mult, op1=ALU.add), reads=["pen"], writes=["pen"])
        for g in range(8):
            p.op("dve", lambda e, g=g: e.tensor_scalar(out=lem[:, g * 8:(g + 1) * 8], in0=l_sb[:, 8 + g * 8:16 + g * 8], scalar1=pen[:, g:g + 1], scalar2=None, op0=ALU.add),
                 reads=["l", "pen"], writes=["lem"], force=(g == 0))
        p.op("dve", lambda e: e.max(out=m8e[:], in_=lem[:]), reads=["lem"], writes=["m8e"])
        p.op("dve", lambda e: e.max_index(out=idx[:], in_max=m8e[:], in_values=lem[:]), reads=["m8e", "lem"], writes=["idx"], force=True)
        p.op("dve", lambda e, rs_=rs_: e.tensor_copy(out=rs_[:, 0:2], in_=idx[:, 0:2]), reads=["idx"], writes=[rk + "a"], force=True)
        p.op("dve", lambda e: e.tensor_tensor(out=sm[:, 3:4], in0=m8e[:, 0:1], in1=m8e[:, 1:2], op=ALU.subtract), reads=["m8e"], writes=["sm3"], force=True)
        p.op("act", lambda e: e.activation(out=sm[:, 4:5], in_=sm[:, 3:4], func=AF.Sigmoid), reads=["sm3"], writes=["sm4"])
        p.op("dve", lambda e, rs_=rs_: e.tensor_tensor(out=rs_[:, 2:3], in0=sm[:, 4:5], in1=sm[:, 2:3], op=ALU.mult), reads=["sm4", "sm2"], writes=[rk + "b"], force=True)
        p.op("dve", lambda e, rs_=rs_: e.tensor_tensor(out=rs_[:, 3:4], in0=sm[:, 2:3], in1=rs_[:, 2:3], op=ALU.subtract), reads=["sm2", rk + "b"], writes=[rk + "c"], force=True)
        p.dma("sp", o_d[t * 128:(t + 1) * 128, :], rs_[:], reads=[rk + "a", rk + "b", rk + "c"])


def launch_router(h2, I):
    wr = np.ascontiguousarray(np.concatenate([I["w_rg"][0], I["w_re"][0]], 1))
    br = np.ascontiguousarray(np.concatenate([I["b_rg"][0], I["b_re"][0]])[None, :])
    in_maps = [{"hT": np.ascontiguousarray(h2[i * 1024:(i + 1) * 1024].T), "wr": wr, "br": br} for i in range(8)]
    res = _run(lambda nc, p, st: build_router(nc, p, st, 8), in_maps)
    o = np.concatenate([r["o"] for r in res], axis=0)
    return o[:, 0:2].astype(np.int64), o[:, 2:4]


def build_experts(nc, p, st, cap):
    xT_d = nc.dram_tensor("xT", [8, D, cap], F32, kind="ExternalInput").ap()
    wg_d = nc.dram_tensor("wg", [8, D, 512], F32, kind="ExternalInput").ap()
    wu_d = nc.dram_tensor("wu", [8, D, 512], F32, kind="ExternalInput").ap()
    wd_d = nc.dram_tensor("wd", [8, 512, D], F32, kind="ExternalInput").ap()
    y_d = nc.dram_tensor("yT", [8, D, cap], F32, kind="ExternalOutput").ap()
    r32 = lambda ap: ap.bitcast(F32R)
    xT = _sb(nc, st, "xT_sb", [128, 16, cap])
    Wg = _sb(nc, st, "Wg_sb", [128, 16, 512])
    Wu = _sb(nc, st, "Wu_sb", [128, 16, 512])
    Wd = _sb(nc, st, "Wd_sb", [128, 4, D])
    hid = _sb(nc, st, "hid_sb", [128, 4, cap])
    sg = [_sb(nc, st, f"sg{i}", [128, cap]) for i in range(2)]
    yo = [_sb(nc, st, f"yo{i}", [128, cap]) for i in range(3)]
    ps = [_ps(nc, st, f"ps{i}") for i in range(8)]
    for ex in range(8):
        xv = xT_d[ex].rearrange("(c p) t -> p c t", p=128)
        for h in range(4):
            p.dma("pool", r32(xT[:, h * 4:(h + 1) * 4, :]), r32(xv[:, h * 4:(h + 1) * 4, :]), writes=[f"xT{h}"])
        gv = wg_d[ex].rearrange("(c p) n -> p c n", p=128)
        uv = wu_d[ex].rearrange("(c p) n -> p c n", p=128)
        dv = wd_d[ex].rearrange("(c p) n -> p c n", p=128)
        for h in range(8):
            p.dma("pool", r32(Wg[:, h * 2:(h + 1) * 2, :]), r32(gv[:, h * 2:(h + 1) * 2, :]), writes=[f"Wg{h}"])
        for h in range(8):
            p.dma("pool", r32(Wu[:, h * 2:(h + 1) * 2, :]), r32(uv[:, h * 2:(h + 1) * 2, :]), writes=[f"Wu{h}"])
        for h in range(4):
            p.dma("pool", r32(Wd[:, h, 0:1024]), r32(dv[:, h, 0:1024]), writes=[f"Wd{h}a"])
            p.dma("pool", r32(Wd[:, h, 1024:2048]), r32(dv[:, h, 1024:2048]), writes=[f"Wd{h}b"])
        for f in range(4):
            pg = ps[(f % 2) * 2]
            pu = ps[(f % 2) * 2 + 1]
            pgk = f"ps{(f % 2) * 2}"
            puk = f"ps{(f % 2) * 2 + 1}"
            fs = slice(f * 128, (f + 1) * 128)
            for c in range(16):
                p.op("pe", lambda e, c=c, pg=pg, fs=fs: e.matmul(pg[:, 0:cap], lhsT=r32(Wg[:, c, fs]), rhs=r32(xT[:, c, :]), start=(c == 0), stop=(c == 15)),
                     reads=[f"Wg{c // 2}", f"xT{c // 4}"], writes=[pgk])
            for c in range(16):
                p.op("pe", lambda e, c=c, pu=pu, fs=fs: e.matmul(pu[:, 0:cap], lhsT=r32(Wu[:, c, fs]), rhs=r32(xT[:, c, :]), start=(c == 0), stop=(c == 15)),
                     reads=[f"Wu{c // 2}", f"xT{c // 4}"], writes=[puk])
            s_ = sg[f % 2]
            p.op("act", lambda e, s_=s_, pg=pg: e.activation(out=s_[:], in_=pg[:, 0:cap], func=AF.Silu), reads=[pgk], writes=[f"sg{f % 2}"])
            p.op("dve", lambda e, s_=s_, pu=pu, f=f: e.tensor_tensor(out=r32(hid[:, f, :]), in0=pu[:, 0:cap], in1=s_[:], op=ALU.mult),
                 reads=[puk, f"sg{f % 2}"], writes=[f"hid{f}"])
        for d in range(16):
            py = ps[4 + d % 4]
            pyk = f"ps{4 + d % 4}"
            ds_ = slice(d * 128, (d + 1) * 128)
            for f in range(4):
                p.op("pe", lambda e, f=f, py=py, ds_=ds_: e.matmul(py[:, 0:cap], lhsT=r32(Wd[:, f, ds_]), rhs=r32(hid[:, f, :]), start=(f == 0), stop=(f == 3)),
                     reads=[f"Wd{f}a", f"Wd{f}b", f"hid{f}"], writes=[pyk])
            yb = yo[d % 3]
            p.op("act" if d % 2 else "dve", (lambda e, yb=yb, py=py: e.copy(out=yb[:], in_=py[:, 0:cap])) if d % 2 else (lambda e, yb=yb, py=py: e.tensor_copy(out=yb[:], in_=py[:, 0:cap])),
                 reads=[pyk], writes=[f"yo{d % 3}"])
            p.dma("sp", y_d[ex, ds_, :], yb[:], reads=[f"yo{d % 3}"])


def launch_experts(h2, eidx, I):
    N = h2.shape[0]
    flat_e = eidx.reshape(-1)
    flat_t = np.repeat(np.arange(N), 2)
    order = np.argsort(flat_e, kind="stable")
    counts = np.bincount(flat_e, minlength=64)
    cap = int(max(256, -(-counts.max() // 128) * 128))
    starts = np.cumsum(counts) - counts
    xT = np.zeros((64, D, cap), np.float32)
    pos_of = np.zeros(2 * N, np.int64)
    for e in range(64):
        sl = order[starts[e]:starts[e] + counts[e]]
        xT[e, :, :counts[e]] = h2[flat_t[sl]].T
        pos_of[sl] = np.arange(counts[e])
    in_maps = [{"xT": np.ascontiguousarray(xT[g * 8:(g + 1) * 8]), "wg": np.ascontiguousarray(I["w_gate_e"][0][g * 8:(g + 1) * 8]),
                "wu": np.ascontiguousarray(I["w_up_e"][0][g * 8:(g + 1) * 8]), "wd": np.ascontiguousarray(I["w_down_e"][0][g * 8:(g + 1) * 8])} for g in range(8)]
    res = _run(lambda nc, p, st: build_experts(nc, p, st, cap), in_maps)
    yT = np.concatenate([r["yT"] for r in res], axis=0)
    yflat = yT[flat_e, :, pos_of]
    yflat = yflat.reshape(N, 2, D)
    return np.ascontiguousarray(yflat[:, 0]), np.ascontiguousarray(yflat[:, 1])


def build_combine(nc, p, st, ntile=8):
    n = ntile * 128
    ya_d = nc.dram_tensor("ya", [n, D], F32, kind="ExternalInput").ap()
    yb_d = nc.dram_tensor("yb", [n, D], F32, kind="ExternalInput").ap()
    w_d = nc.dram_tensor("w", [n, 2], F32, kind="ExternalInput").ap()
    g = nc.dram_tensor("g", [1, D], F32, kind="ExternalInput").ap()
    s = nc.dram_tensor("s", [1, D], F32, kind="ExternalInput").ap()
    base = nc.dram_tensor("base", [n, D], F32, kind="ExternalInput").ap()
    o = nc.dram_tensor("o", [n, D], F32, kind="ExternalOutput").ap()
    gb = _sb(nc, st, "gb", [128, D])
    A = _sb(nc, st, "A", [128, D])
    p.dma("sp", gb[:], g.partition_broadcast(128), writes=["gb"])
    p.dma("sp", A[:], s.partition_broadcast(128), writes=["A"])
    p.op("dve", lambda e: e.tensor_tensor(out=A[:], in0=A[:], in1=gb[:], op=ALU.mult), reads=["gb", "A"], writes=["A"])
    ya = [_sb(nc, st, f"ya{i}", [128, D]) for i in range(2)]
    yb = [_sb(nc, st, f"yb{i}", [128, D]) for i in range(2)]
    bt = [_sb(nc, st, f"bt{i}", [128, D]) for i in range(2)]
    wt = [_sb(nc, st, f"wt{i}", [128, 2]) for i in range(2)]
    junk = _sb(nc, st, "junk", [128, D])
    ot = [_sb(nc, st, f"ot{i}", [128, D]) for i in range(2)]
    ss = [_sb(nc, st, f"ss{i}", [128, 4]) for i in range(2)]
    for t in range(ntile):
        i = t % 2
        rows = slice(t * 128, (t + 1) * 128)
        p.dma("sp", ya[i][:], ya_d[rows, :], writes=[f"ya{i}"])
        p.dma("sp", yb[i][:], yb_d[rows, :], writes=[f"yb{i}"])
        p.dma("sp", bt[i][:], base[rows, :], writes=[f"bt{i}"])
        p.dma("sp", wt[i][:], w_d[rows, :], writes=[f"wt{i}"])
        p.op("dve", lambda e, i=i: e.tensor_scalar(out=ya[i][:], in0=ya[i][:], scalar1=wt[i][:, 0:1], scalar2=None, op0=ALU.mult),
             reads=[f"ya{i}", f"wt{i}"], writes=[f"ya{i}"])
        p.op("dve", lambda e, i=i: e.scalar_tensor_tensor(out=ya[i][:], in0=yb[i][:], scalar=wt[i][:, 1:2], in1=ya[i][:], op0=ALU.mult, op1=ALU.add),
             reads=[f"ya{i}", f"yb{i}", f"wt{i}"], writes=[f"ya{i}"])
        p.op("act", lambda e, i=i: e.activation(out=junk[:], in_=ya[i][:], func=AF.Square, accum_out=ss[i][:, 0:1]),
             reads=[f"ya{i}"], writes=["junk", f"ss{i}"])
        p.op("dve", lambda e, i=i: e.tensor_scalar(out=ss[i][:, 1:2], in0=ss[i][:, 0:1], scalar1=1.0 / D, scalar2=EPS, op0=ALU.mult, op1=ALU.add),
             reads=[f"ss{i}"], writes=[f"ss{i}"])
        p.op("act", lambda e, i=i: e.activation(out=ss[i][:, 2:3], in_=ss[i][:, 1:2], func=AF.Sqrt), reads=[f"ss{i}"], writes=[f"ss{i}"])
        p.op("dve", lambda e, i=i: e.reciprocal(out=ss[i][:, 3:4], in_=ss[i][:, 2:3]), reads=[f"ss{i}"], writes=[f"ss{i}"])
        p.op("dve", lambda e, i=i: e.scalar_tensor_tensor(out=ot[i][:], in0=ya[i][:], scalar=ss[i][:, 3:4], in1=A[:], op0=ALU.mult, op1=ALU.mult),
             reads=[f"ya{i}", f"ss{i}", "A"], writes=[f"ot{i}"], force=True)
        p.op("pool", lambda e, i=i: e.tensor_tensor(out=ot[i][:], in0=ot[i][:], in1=bt[i][:], op=ALU.add),
             reads=[f"ot{i}", f"bt{i}"], writes=[f"ot{i}"])
        p.dma("pool", o[rows, :], ot[i][:], reads=[f"ot{i}"])


def launch_combine(ya, yb, w, g, s, base):
    n = ya.shape[0] // 8
    in_maps = [{"ya": np.ascontiguousarray(ya[i * n:(i + 1) * n]), "yb": np.ascontiguousarray(yb[i * n:(i + 1) * n]),
                "w": np.ascontiguousarray(w[i * n:(i + 1) * n]), "g": np.ascontiguousarray(g[None, :]),
                "s": np.ascontiguousarray(s[None, :]), "base": np.ascontiguousarray(base[i * n:(i + 1) * n])} for i in range(8)]
    res = _run(lambda nc, p, st: build_combine(nc, p, st, n // 128), in_maps)
    return np.concatenate([r["o"] for r in res], axis=0)


def kernel(**inputs):
    I = {k: np.asarray(v) for k, v in inputs.items()}
    x = I["x"][0]
    ada = launch_ada(I["c"][0], I["w_ada"][0], I["b_ada"][0])
    sh1, sc1, gt1, sh2, sc2, gt2 = np.split(ada, 6)
    h1 = launch_norm(x, I["g_pre_mix"][0], sc1, 1.0, bv=sh1)
    lc = launch_inproj(h1, I)
    o_att = launch_moba(lc)
    o_rwkv = launch_rwkv_chunked(lc, I)
    y1 = launch_merge(h1, o_att, o_rwkv, I)
    x1 = launch_norm(y1, I["g_post_mix"][0], gt1, 0.0, base=x)
    h2 = launch_norm(x1, I["g_pre_ffn"][0], sc2, 1.0, bv=sh2)
    eidx, ew = launch_router(h2, I)
    ya, yb = launch_experts(h2, eidx, I)
    out = launch_combine(ya, yb, ew, I["g_post_ffn"][0], gt2, x1)
    return out[None].astype(np.float32)


CH_C = 64
SEG = 256
LOCK = 4


def build_rwkv_chunked(nc, p, st, T=S):
    nseg = T // SEG
    cps = SEG // CH_C
    F_d = nc.dram_tensor("F", [64, 6, 2, T], F32, kind="ExternalInput").ap()
    gb_d = nc.dram_tensor("gb", [64, 2, 2, T], F32, kind="ExternalInput").ap()
    gnv_d = nc.dram_tensor("gnv", [64, 2, 2], F32, kind="ExternalInput").ap()
    id_d = nc.dram_tensor("ident", [128, 128], F32, kind="ExternalInput").ap()
    msk_d = nc.dram_tensor("msk", [64, 10, 64], F32, kind="ExternalInput").ap()
    rm_d = nc.dram_tensor("rmask", [64, 2 * SEG], F32, kind="ExternalInput").ap()
    on_d = nc.dram_tensor("ones64", [64, 64], F32, kind="ExternalInput").ap()
    o_d = nc.dram_tensor("o", [64, 2, T], F32, kind="ExternalOutput").ap()

    ident = _sb(nc, st, "ident_sb", [128, 128])
    msk = _sb(nc, st, "msk_sb", [64, 10, 64])
    rmask = _sb(nc, st, "rmask_sb", [64, 2 * SEG])
    ones64 = _sb(nc, st, "ones64_sb", [64, 64])
    gnv = _sb(nc, st, "gnv_sb", [64, 2, 2])
    for dst, src, k in [(ident, id_d, "ident"), (msk, msk_d, "msk"),
                        (rmask, rm_d, "rmask"), (ones64, on_d, "ones64"), (gnv, gnv_d, "gnv")]:
        p.dma("sp", dst[:], src, writes=[k])
    Fin = [_sb(nc, st, f"Fin{i}", [64, 6, 2, SEG]) for i in range(2)]
    gbin = [_sb(nc, st, f"gbin{i}", [64, 2, 2, SEG]) for i in range(1)] * 2
    names = ["logw", "cum", "eg", "einv", "egm", "dte", "Af", "Bf", "Kf", "Rf", "Bh", "Kh", "Af32"]
    b16n = ("Af", "Bf", "Kf", "Rf")
    tmpn = ["logw", "cum", "eg", "einv", "egm", "dte"]
    Wtmp = {n: _sb(nc, st, f"wt_{n}", [64, 2, SEG]) for n in tmpn}
    W_ = []
    for i in range(2):
        d_ = dict(Wtmp)
        for n in names:
            if n not in tmpn:
                d_[n] = _sb(nc, st, f"w{i}_{n}", [64, 2, SEG], BF16 if n in b16n else F32)
        W_.append(d_)
    gC = [_sb(nc, st, f"gC{i}", [64, 2, cps]) for i in range(2)]
    NSL = 2 * LOCK
    TM = [_sb(nc, st, f"TM{i}", [64, 4, 128], BF16) for i in range(NSL)]
    MS = [_sb(nc, st, f"MS{i}", [64, 10, 64], BF16) for i in range(NSL)]
    MQ = [[_sb(nc, st, f"MQ{i}_{j}", [64, 4, 64], BF16) for j in range(2)] for i in range(NSL)]
    XW = [_sb(nc, st, f"XW{i}", [64, 2, 128], BF16) for i in range(NSL)]
    ident16 = _sb(nc, st, "ident16", [64, 64], BF16)
    p.op("dve", lambda e: e.tensor_copy(out=ident16[:], in_=ident[0:64, 0:64]), reads=["ident"], writes=["ident16"])
    NCH = 2 * LOCK + 2
    CHb = [_sb(nc, st, f"CHb{i}", [64, 4, 128]) for i in range(NCH)]
    Z = _sb(nc, st, "Zst", [64, 2, 64])
    Z2 = _sb(nc, st, "Zst2", [64, 2, 64])
    YT = [_sb(nc, st, f"YT{i}", [64, 2, SEG]) for i in range(2)]
    ps = [_ps(nc, st, f"ps{i}") for i in range(8)]
    psi = [0]

    def nb():
        i = psi[0] % 8
        psi[0] += 1
        return ps[i], f"ps{i}"

    p.op("dve", lambda e: e.memset(Z[:], 0.0), writes=["Z"])

    def load_seg(sg):
        i = sg % 2
        p.dma("sp", Fin[i][:], F_d[:, :, :, sg * SEG:(sg + 1) * SEG], writes=[f"Fin{i}"])

    def prep_seg(sg):
        i = sg % 2
        Fi = Fin[i]
        w = W_[i]
        fk = f"Fin{i}"
        k = lambda n: (f"wt_{n}" if n in tmpn else f"w{i}_{n}")
        fl = lambda ap: ap.rearrange("p h t -> p (h t)")
        p.op("act", lambda e: e.activation(out=fl(w["logw"][:]), in_=fl(Fi[:, 0]), func=AF.Ln), reads=[fk], writes=[k("logw")])
        p.op("dve", lambda e: e.tensor_tensor_scan(out=fl(w["cum"][:]), data0=rmask[:], data1=fl(w["logw"][:]), initial=0.0, op0=ALU.mult, op1=ALU.add),
             reads=["rmask", k("logw")], writes=[k("cum")])
        p.op("act", lambda e: e.activation(out=fl(w["eg"][:]), in_=fl(w["cum"][:]), func=AF.Exp), reads=[k("cum")], writes=[k("eg")])
        p.op("act", lambda e: e.activation(out=fl(w["einv"][:]), in_=fl(w["cum"][:]), func=AF.Exp, scale=-1.0), reads=[k("cum")], writes=[k("einv")])
        p.op("dve", lambda e: e.tensor_tensor(out=fl(w["egm"][:]), in0=fl(w["cum"][:]), in1=fl(w["logw"][:]), op=ALU.subtract), reads=[k("cum"), k("logw")], writes=[k("egm")])
        p.op("act", lambda e: e.activation(out=fl(w["egm"][:]), in_=fl(w["egm"][:]), func=AF.Exp), reads=[k("egm")], writes=[k("egm")])
        cumv = w["cum"][:].rearrange("p h (c t) -> p (h c) t", t=CH_C)
        p.op("dve", lambda e: e.tensor_tensor(out=w["dte"][:].rearrange("p h (c t) -> p (h c) t", t=CH_C), in0=cumv[:, :, CH_C - 1:CH_C].to_broadcast([64, 2 * cps, CH_C]), in1=cumv, op=ALU.subtract),
             reads=[k("cum")], writes=[k("dte")])
        p.op("act", lambda e: e.activation(out=fl(w["dte"][:]), in_=fl(w["dte"][:]), func=AF.Exp), reads=[k("dte")], writes=[k("dte")])
        for out_n, a_idx, b_n, eng in [("Af", 1, "egm", "dve"), ("Bf", 2, "einv", "pool"), ("Kf", 3, "einv", "dve"),
                                       ("Rf", 4, "eg", "pool"), ("Bh", 2, "dte", "dve"), ("Kh", 3, "dte", "pool"), ("Af32", 1, "egm", "pool")]:
            p.op(eng, lambda e, out_n=out_n, a_idx=a_idx, b_n=b_n: e.tensor_tensor(out=fl(w[out_n][:]), in0=fl(Fi[:, a_idx]), in1=fl(w[b_n][:]), op=ALU.mult),
                 reads=[fk, k(b_n)], writes=[k(out_n)])
        egC = w["eg"][:].rearrange("p h (c t) -> p h c t", t=CH_C)[:, :, :, CH_C - 1]
        p.op("act", lambda e: e.copy(out=gC[i][:], in_=egC), reads=[k("eg")], writes=[f"gC{i}"])

    def pre_stages(sg, cl, slot, chslot):
        i = sg % 2
        w = W_[i]
        Fi = Fin[i]
        k = lambda n: f"w{i}_{n}"
        cs = slice(cl * CH_C, (cl + 1) * CH_C)
        tm, ms, mq, xw, chb = TM[slot], MS[slot], MQ[slot], XW[slot], CHb[chslot]
        tmk, msk_, xwk, chk = f"TM{slot}", f"MS{slot}", f"XW{slot}", f"CHb{chslot}"
        stages = []

        def s1():
            b, bkey = nb()
            for q, (src, skey) in enumerate([(w["Af32"], k("Af32")), (w["Bh"], k("Bh")), (w["Kh"], k("Kh")), (None, f"Fin{i}")]):
                for h in range(2):
                    in_ap = Fi[:, 5, h, cs] if src is None else src[:, h, cs]
                    p.op("pe", lambda e, q=q, h=h, in_ap=in_ap: e.transpose(b[0:64, q * 128 + h * 64:q * 128 + (h + 1) * 64], in_ap, ident[0:64, 0:64]),
                         reads=[skey, "ident"], writes=[bkey])
            p.op("act", lambda e: e.copy(out=tm[:].rearrange("p a b -> p (a b)"), in_=b[0:64, :]), reads=[bkey], writes=[tmk])
        stages.append(s1)

        def s2a():
            b, bkey = nb()
            for h in range(2):
                pb = slice(h * 64, (h + 1) * 64)
                for col, (l, lk, r_, rk) in [(0 + h, (w["Bf"], k("Bf"), w["Af"], k("Af"))), (2 + h, (w["Kf"], k("Kf"), w["Af"], k("Af"))),
                                             (4 + h, (w["Af"], k("Af"), w["Bf"], k("Bf")))]:
                    p.op("pe", lambda e, col=col, l=l, r_=r_, h=h: e.matmul(b[0:64, col * 64:(col + 1) * 64], lhsT=l[:, h, cs], rhs=r_[:, h, cs], start=True, stop=True),
                         reads=[lk, rk], writes=[bkey])
            p.op("dve", lambda e: e.tensor_tensor(out=ms[:, 0:6, :].rearrange("p a b -> p (a b)"), in0=b[0:64, 0:384], in1=msk[:, 0:6, :].rearrange("p a b -> p (a b)"), op=ALU.mult),
                 reads=[bkey, "msk"], writes=[msk_ + "a"])
        stages.append(s2a)

        def s2b():
            b, bkey = nb()
            for h in range(2):
                pb = slice(h * 64, (h + 1) * 64)
                for col, (l, lk) in [(0 + h, (w["Bf"], k("Bf"))), (2 + h, (w["Kf"], k("Kf")))]:
                    p.op("pe", lambda e, col=col, l=l, h=h: e.matmul(b[0:64, col * 64:(col + 1) * 64], lhsT=l[:, h, cs], rhs=w["Rf"][:, h, cs], start=True, stop=True),
                         reads=[lk, k("Rf")], writes=[bkey])
            p.op("dve", lambda e: e.tensor_tensor(out=ms[:, 6:10, :].rearrange("p a b -> p (a b)"), in0=b[0:64, 0:256], in1=msk[:, 6:10, :].rearrange("p a b -> p (a b)"), op=ALU.mult),
                 reads=[bkey, "msk"], writes=[msk_ + "b"])
        stages.append(s2b)

        def s3():
            b, bkey = nb()
            for h in range(2):
                p.op("pe", lambda e, h=h: e.matmul(b[0:64, h * 64:(h + 1) * 64], lhsT=ms[:, 2 + h, :], rhs=tm[:, 3, h * 64:(h + 1) * 64], start=True, stop=True),
                     reads=[msk_ + "a", tmk], writes=[bkey])
            p.op("act", lambda e: e.copy(out=xw[:, :, 64:128], in_=b[0:64, 0:128].rearrange("p (h v) -> p h v", h=2)), reads=[bkey], writes=[xwk + "x"])
            p.op("act", lambda e: e.copy(out=xw[:, :, 0:64], in_=tm[:, 0, :].rearrange("p (h v) -> p h v", h=2)), reads=[tmk], writes=[xwk + "w"])
        stages.append(s3)

        def mk_level(j):
            def lv():
                if j == 0:
                    MT = [ms[:, 0, :], ms[:, 1, :]]
                    M = [ms[:, 4, :], ms[:, 5, :]]
                    mkey = msk_ + "a"
                else:
                    q = mq[j % 2]
                    MT = [q[:, 0, :], q[:, 1, :]]
                    M = [q[:, 2, :], q[:, 3, :]]
                    mkey = f"MQ{slot}_{j % 2}"
                b, bkey = nb()
                for h in range(2):
                    p.op("pe", lambda e, h=h: e.matmul(b[0:64, h * 128:(h + 1) * 128], lhsT=MT[h], rhs=xw[:, h, :], start=True, stop=True),
                         reads=[mkey, xwk + "x", xwk + "w"], writes=[bkey])
                if j < 5:
                    b2, b2key = nb()
                    for h in range(2):
                        p.op("pe", lambda e, h=h: e.matmul(b2[0:64, h * 64:(h + 1) * 64], lhsT=M[h], rhs=MT[h], start=True, stop=True), reads=[mkey], writes=[b2key])
                        p.op("pe", lambda e, h=h: e.matmul(b2[0:64, (2 + h) * 64:(3 + h) * 64], lhsT=MT[h], rhs=M[h], start=True, stop=True), reads=[mkey], writes=[b2key])
                p.op("dve", lambda e: e.tensor_tensor(out=xw[:].rearrange("p a b -> p (a b)"), in0=b[0:64, 0:256], in1=xw[:].rearrange("p a b -> p (a b)"), op=ALU.add),
                     reads=[bkey, xwk + "x", xwk + "w"], writes=[xwk + "x", xwk + "w"])
                if j < 5:
                    nq = mq[(j + 1) % 2]
                    p.op("act", lambda e: e.copy(out=nq[:].rearrange("p a b -> p (a b)"), in_=b2[0:64, 0:256]), reads=[b2key], writes=[f"MQ{slot}_{(j + 1) % 2}"])
            return lv
        for j in range(6):
            stages.append(mk_level(j))

        def s5():
            b, bkey = nb()
            xk = [xwk + "x", xwk + "w"]
            for h in range(2):
                pb = slice(h * 64, (h + 1) * 64)
                hs = slice(h * 64, (h + 1) * 64)
                Wh = xw[:, h, 0:64]
                Xh = xw[:, h, 64:128]
                p.op("pe", lambda e, Wh=Wh, hs=hs, h=h: e.matmul(b[0:64, h * 64:(h + 1) * 64], lhsT=Wh, rhs=tm[:, 1, hs], start=True, stop=True),
                     reads=xk + [tmk], writes=[bkey])
                p.op("pe", lambda e, Xh=Xh, hs=hs, h=h: e.matmul(b[0:64, 128 + h * 64:128 + (h + 1) * 64], lhsT=tm[:, 1, hs], rhs=Xh, start=True, stop=False),
                     reads=xk + [tmk], writes=[bkey])
                p.op("pe", lambda e, hs=hs, h=h: e.matmul(b[0:64, 128 + h * 64:128 + (h + 1) * 64], lhsT=tm[:, 2, hs], rhs=tm[:, 3, hs], start=False, stop=True),
                     reads=[tmk], writes=[bkey])
                p.op("pe", lambda e, Wh=Wh, h=h: e.matmul(b[0:64, 256 + h * 64:256 + (h + 1) * 64], lhsT=Wh, rhs=ms[:, 6 + h, :], start=True, stop=False),
                     reads=xk + [msk_ + "b"], writes=[bkey])
                p.op("pe", lambda e, h=h: e.matmul(b[0:64, 256 + h * 64:256 + (h + 1) * 64], lhsT=ident16[:], rhs=w["Rf"][:, h, cs], start=False, stop=True),
                     reads=["ident16", k("Rf")], writes=[bkey])
                p.op("pe", lambda e, Xh=Xh, h=h: e.matmul(b[0:64, 384 + h * 64:384 + (h + 1) * 64], lhsT=Xh, rhs=ms[:, 6 + h, :], start=True, stop=False),
                     reads=xk + [msk_ + "b"], writes=[bkey])
                p.op("pe", lambda e, hs=hs, h=h: e.matmul(b[0:64, 384 + h * 64:384 + (h + 1) * 64], lhsT=tm[:, 3, hs], rhs=ms[:, 8 + h, :], start=False, stop=True),
                     reads=[tmk, msk_ + "b"], writes=[bkey])
            p.op("act", lambda e: e.copy(out=chb[:].rearrange("p a b -> p (a b)"), in_=b[0:64, :]), reads=[bkey], writes=[chk])
        stages.append(s5)
        return stages

    def chain_step(sg, cl, chslot):
        i = sg % 2
        chb = CHb[chslot]
        chk = f"CHb{chslot}"
        yt = YT[i]
        b, bkey = nb()
        for h in range(2):
            hs = slice(h * 64, (h + 1) * 64)
            p.op("pe", lambda e, h=h, hs=hs: e.matmul(b[0:64, hs], lhsT=chb[:, 0, hs], rhs=Z[:, h, :], start=True, stop=True), reads=[chk, "Z"], writes=[bkey])
            p.op("pe", lambda e, h=h, hs=hs: e.matmul(b[0:64, 128 + h * 64:128 + (h + 1) * 64], lhsT=Z[:, h, :], rhs=chb[:, 2, hs], start=True, stop=True), reads=[chk, "Z"], writes=[bkey])
        p.op("dve", lambda e: e.tensor_tensor(out=yt[:, :, cl * CH_C:(cl + 1) * CH_C], in0=b[0:64, 128:256].rearrange("p (h t) -> p h t", h=2),
                                              in1=chb[:, 3, :].rearrange("p (h t) -> p h t", h=2), op=ALU.add),
             reads=[bkey, chk], writes=[f"YT{i}_{cl}"])
        for h in range(2):
            p.op("dve", lambda e, h=h: e.scalar_tensor_tensor(out=Z2[:, h, :], in0=Z[:, h, :], scalar=gC[i][:, h, cl:cl + 1], in1=b[0:64, h * 64:(h + 1) * 64], op0=ALU.mult, op1=ALU.add),
                 reads=["Z", f"gC{i}", bkey], writes=[f"Z2_{h}"])
        p.op("dve", lambda e: e.tensor_tensor(out=Z[:].rearrange("p a b -> p (a b)"), in0=Z2[:].rearrange("p a b -> p (a b)"), in1=chb[:, 1, :], op=ALU.add),
             reads=["Z2_0", "Z2_1", chk], writes=["Z"])

    yc = _sb(nc, st, "yc", [64, 2 * SEG])
    sq = _sb(nc, st, "sq", [64, 2 * SEG])
    rs = _sb(nc, st, "rs", [64, 2 * SEG])
    ot = [_sb(nc, st, f"ot{i}", [64, 2, SEG]) for i in range(1)] * 2

    def epilogue(sg):
        i = sg % 2
        yt = YT[i]
        ykeys = [f"YT{i}_{cl}" for cl in range(cps)]
        ytf = yt[:].rearrange("p h t -> p (h t)")
        p.dma("sp", gbin[0][:], gb_d[:, :, :, sg * SEG:(sg + 1) * SEG], writes=["gbin0"])
        for hh in range(2):
            b, bkey = nb()
            sl_ = slice(hh * SEG, (hh + 1) * SEG)
            p.op("pe", lambda e, sl_=sl_, b=b: e.matmul(b[0:64, 0:SEG], lhsT=ones64[:], rhs=ytf[:, sl_], start=True, stop=True), reads=["ones64"] + ykeys, writes=[bkey])
            p.op("dve", lambda e, sl_=sl_, b=b: e.scalar_tensor_tensor(out=yc[:, sl_], in0=b[0:64, 0:SEG], scalar=-1.0 / 64, in1=ytf[:, sl_], op0=ALU.mult, op1=ALU.add),
                 reads=[bkey] + ykeys, writes=[f"yc{hh}"])
            p.op("act", lambda e, sl_=sl_: e.activation(out=sq[:, sl_], in_=yc[:, sl_], func=AF.Square), reads=[f"yc{hh}"], writes=[f"sq{hh}"])
            b2, b2key = nb()
            p.op("pe", lambda e, sl_=sl_, b2=b2: e.matmul(b2[0:64, 0:SEG], lhsT=ones64[:], rhs=sq[:, sl_], start=True, stop=True), reads=["ones64", f"sq{hh}"], writes=[b2key])
            p.op("dve", lambda e, sl_=sl_, b2=b2: e.tensor_scalar(out=rs[:, sl_], in0=b2[0:64, 0:SEG], scalar1=1.0 / 64, scalar2=GN_EPS, op0=ALU.mult, op1=ALU.add), reads=[b2key], writes=[f"rs{hh}"])
            p.op("act", lambda e, sl_=sl_: e.activation(out=rs[:, sl_], in_=rs[:, sl_], func=AF.Sqrt), reads=[f"rs{hh}"], writes=[f"rs{hh}"])
            p.op("dve", lambda e, sl_=sl_: e.reciprocal(out=rs[:, sl_], in_=rs[:, sl_]), reads=[f"rs{hh}"], writes=[f"rs{hh}"])
            p.op("dve", lambda e, sl_=sl_: e.tensor_tensor(out=yc[:, sl_], in0=yc[:, sl_], in1=rs[:, sl_], op=ALU.mult), reads=[f"yc{hh}", f"rs{hh}"], writes=[f"yc{hh}"])
            p.op("dve", lambda e, sl_=sl_, hh=hh: e.tensor_scalar(out=yc[:, sl_], in0=yc[:, sl_], scalar1=gnv[:, hh, 0:1], scalar2=gnv[:, hh, 1:2], op0=ALU.mult, op1=ALU.add),
                 reads=[f"yc{hh}", "gnv"], writes=[f"yc{hh}"])
            p.op("pool", lambda e, sl_=sl_, hh=hh: e.tensor_tensor(out=yc[:, sl_], in0=yc[:, sl_], in1=gbin[0][:, 1, hh, :], op=ALU.add), reads=[f"yc{hh}", "gbin0"], writes=[f"yc{hh}"])
            p.op("pool", lambda e, sl_=sl_, hh=hh: e.tensor_tensor(out=ot[0][:, hh, :], in0=yc[:, sl_], in1=gbin[0][:, 0, hh, :], op=ALU.mult), reads=[f"yc{hh}", "gbin0"], writes=[f"ot0_{hh}"])
        p.dma("sp", o_d[:, :, sg * SEG:(sg + 1) * SEG], ot[0][:], reads=["ot0_0", "ot0_1"])

    load_seg(0)
    pending_chain = []
    slot_ctr = 0
    ch_ctr = 0
    for sg in range(nseg):
        if sg + 1 < nseg:
            load_seg(sg + 1)
        prep_seg(sg)
        for c0 in range(0, cps, LOCK):
            sts = []
            new_chain = []
            for gi in range(LOCK):
                sts.append(pre_stages(sg, c0 + gi, slot_ctr % NSL, ch_ctr % NCH))
                new_chain.append((sg, c0 + gi, ch_ctr % NCH))
                slot_ctr += 1
                ch_ctr += 1
            nst = len(sts[0])
            pop_at = set(range(1, nst, max(1, (nst - 1) // LOCK)))
            for si in range(nst):
                for gi in range(LOCK):
                    sts[gi][si]()
                if pending_chain and si in pop_at:
                    a = pending_chain.pop(0)
                    chain_step(*a)
                    if a[1] == cps - 1:
                        epilogue(a[0])
            pending_chain.extend(new_chain)
    while pending_chain:
        a = pending_chain.pop(0)
        chain_step(*a)
        if a[1] == cps - 1:
            epilogue(a[0])


def launch_rwkv_chunked(lc, I, T=S):
    C = CH_C
    su = np.triu(np.ones((C, C), np.float32), 1)
    sle = np.triu(np.ones((C, C), np.float32), 0)
    msk = np.stack([su, su, su, su, su.T, su.T, sle, sle, sle, sle], 0).transpose(1, 0, 2)
    ident = np.eye(128, dtype=np.float32)
    rmask = np.ones((64, 2 * SEG), np.float32)
    rmask[:, ::C] = 0
    ones64 = np.ones((64, 64), np.float32)
    in_maps = []
    for i in range(8):
        cq = slice(i * 128, (i + 1) * 128)
        F = np.stack([lc[i][n][:, :T].reshape(2, 64, T) for n in ("wdec", "nkk", "kka", "kt", "rT", "vrT")], 0)
        F = F.transpose(2, 0, 1, 3)
        gb = np.stack([lc[i]["g"][:, :T].reshape(2, 64, T), lc[i]["bonus"][:, :T].reshape(2, 64, T)], 0)
        gb = gb.transpose(2, 0, 1, 3)
        gnv = np.stack([I["gn_w"][0][cq].reshape(2, 64), I["gn_b"][0][cq].reshape(2, 64)], -1).transpose(1, 0, 2)
        in_maps.append({"F": np.ascontiguousarray(F), "gb": np.ascontiguousarray(gb), "gnv": np.ascontiguousarray(gnv),
                        "ident": ident, "msk": np.ascontiguousarray(msk), "rmask": rmask, "ones64": ones64})
    res = _run(lambda nc, p, st: build_rwkv_chunked(nc, p, st, T), in_maps)
    return np.concatenate([r["o"].transpose(2, 1, 0).reshape(T, 128) for r in res], axis=1)
```

```python
import numpy as np
import concourse.bass as bass
import concourse.mybir as mybir
from concourse.bass_utils import run_bass_kernel_spmd

F32 = mybir.dt.float32
F32R = mybir.dt.float32r
BF16 = mybir.dt.bfloat16
I32 = mybir.dt.int32
U32 = mybir.dt.uint32
AF = mybir.ActivationFunctionType
ALU = mybir.AluOpType
AX = mybir.AxisListType

NDMA_SLOTS = 6


class Prog:
    def __init__(self, nc):
        self.nc = nc
        self.ops = []
        self.last_w = {}
        self.readers = {}
        self.engs = {"pe": nc.tensor, "act": nc.scalar, "dve": nc.vector,
                     "pool": nc.gpsimd, "sp": nc.sync}

    def op(self, eng, fn, reads=(), writes=(), dma=False, force=False, inc=16):
        deps = set()
        raw = set()
        for k in reads:
            if k in self.last_w:
                deps.add(self.last_w[k])
                raw.add(self.last_w[k])
        for k in writes:
            if k in self.last_w:
                deps.add(self.last_w[k])
                raw.add(self.last_w[k])
            for r in self.readers.get(k, ()):
                deps.add(r)
        idx = len(self.ops)
        self.ops.append(dict(eng=eng, fn=fn, deps=deps, raw=raw, dma=dma, force=force, inc=inc))
        for k in reads:
            self.readers.setdefault(k, []).append(idx)
        for k in writes:
            self.last_w[k] = idx
            self.readers[k] = []
        return idx

    def dma(self, q, out, in_, reads=(), writes=(), **kw):
        return self.op(q, lambda e: e.dma_start(out=out, in_=in_, **kw), reads, writes, dma=True)

    def emit(self, stack):
        nc = self.nc
        ops = self.ops
        need = [False] * len(ops)
        for i, o in enumerate(ops):
            nd = set()
            for d in o["deps"]:
                od = ops[d]
                if od["dma"] or o["dma"] or o["force"] or od["eng"] != o["eng"] or (d in o["raw"] and o["eng"] != "pe"):
                    nd.add(d)
            o["xdeps"] = nd
            for d in nd:
                need[d] = True
        for i, o in enumerate(ops):
            if o["dma"]:
                need[i] = True
        esem = {e: stack.enter_context(nc.semaphore("es_" + e)) for e in self.engs}
        dsem = {e: [stack.enter_context(nc.semaphore(f"ds_{e}_{k}")) for k in range(NDMA_SLOTS)]
                for e in ("sp", "act", "pool")}
        ecount = {e: 0 for e in self.engs}
        dcount = {e: 0 for e in dsem}
        signal = [None] * len(ops)
        waited = {}
        nwaits = 0
        actions = {e: [] for e in self.engs}
        for i, o in enumerate(ops):
            e = o["eng"]
            wl = {}
            for d in o["xdeps"]:
                s_, v = signal[d]
                key = id(s_)
                if waited.get((e, key), 0) >= v:
                    continue
                if key not in wl or wl[key][1] < v:
                    wl[key] = (s_, v)
            if o["dma"]:
                j = dcount[e]
                slot = j % NDMA_SLOTS
                s_ = dsem[e][slot]
                prev = o.get("prev_total", None)
                prev = self._slot_total.get((e, slot), 0) if hasattr(self, "_slot_total") else 0
                if prev > 0 and waited.get((e, id(s_)), 0) < prev:
                    if id(s_) not in wl or wl[id(s_)][1] < prev:
                        wl[id(s_)] = (s_, prev)
            for key, (s_, v) in wl.items():
                waited[(e, key)] = v
                nwaits += 1
            sem = None
            inc = 0
            if o["dma"]:
                if not hasattr(self, "_slot_total"):
                    self._slot_total = {}
                j = dcount[e]
                dcount[e] += 1
                slot = j % NDMA_SLOTS
                sem = dsem[e][slot]
                inc = o.get("inc", 16)
                tot = self._slot_total.get((e, slot), 0) + inc
                self._slot_total[(e, slot)] = tot
                signal[i] = (sem, tot)
            elif need[i]:
                ecount[e] += 1
                sem = esem[e]
                inc = 1
                signal[i] = (sem, ecount[e])
            actions[e].append((list(wl.values()), o["fn"], sem, inc))
        finals = {e: [] for e in self.engs}
        for e in dsem:
            for slot in range(NDMA_SLOTS):
                tot = getattr(self, "_slot_total", {}).get((e, slot), 0)
                if tot > 0:
                    finals[e].append((dsem[e][slot], tot))
        bnames = {"pe": "tensor", "act": "scalar", "dve": "vector", "pool": "gpsimd", "sp": "sync"}
        with nc.Block() as block:
            for e in self.engs:
                if not actions[e] and not finals[e]:
                    continue

                def body(eng, e=e):
                    for waits, fn, sem, inc in actions[e]:
                        for s_, v in waits:
                            eng.wait_ge(s_, v)
                        inst = fn(eng)
                        if sem is not None:
                            inst.then_inc(sem, inc)
                    for s_, v in finals[e]:
                        eng.wait_ge(s_, v)
                getattr(block, bnames[e])(body)
        self.stats = dict(n_ops=len(ops), n_waits=nwaits, ecount=ecount, dcount=dcount)
        return self.stats


from contextlib import ExitStack

S = 8192
D = 2048
EPS = 1e-6
_TRACE = False


def _run(build, in_maps):
    nc = bass.Bass("TRN2", target_bir_lowering=False)
    with ExitStack() as st:
        p = Prog(nc)
        build(nc, p, st)
        p.emit(st)
    if _TRACE:
        r = run_bass_kernel_spmd(nc, in_maps, core_ids=list(range(8)), trace=True)
        print("EXEC_NS", getattr(build, "__name__", "?"), r.exec_time_ns, p.stats, flush=True)
    else:
        r = run_bass_kernel_spmd(nc, in_maps, core_ids=list(range(8)))
    return r.results


def _sb(nc, st, name, shape, dt=F32):
    return st.enter_context(nc.sbuf_tensor(name, shape, dt))


def _ps(nc, st, name, shape=(128, 512), dt=F32):
    return st.enter_context(nc.psum_tensor(name, list(shape), dt))


def build_ada(nc, p, st):
    w = nc.dram_tensor("w", [2048, 1536], F32, kind="ExternalInput").ap()
    c = nc.dram_tensor("c", [128, 16], F32, kind="ExternalInput").ap()
    b = nc.dram_tensor("b", [1, 1536], F32, kind="ExternalInput").ap()
    y = nc.dram_tensor("y", [1, 1536], F32, kind="ExternalOutput").ap()
    wt = [_sb(nc, st, f"wt{i}", [128, 1536]) for i in range(2)]
    ct = _sb(nc, st, "ct", [128, 16])
    bt = _sb(nc, st, "bt", [1, 1536])
    acc = _sb(nc, st, "acc", [128, 1536])
    ones = _sb(nc, st, "ones", [128, 1])
    res = _sb(nc, st, "res", [1, 1536])
    ps = [_ps(nc, st, f"ps{i}", (1, 512)) for i in range(2)]
    p.dma("sp", ct[:], c, writes=["ct"])
    p.dma("sp", bt[:], b, writes=["bt"])
    p.op("dve", lambda e: e.memset(ones[:], 1.0), writes=["ones"])
    for kc in range(16):
        i = kc % 2
        p.dma("sp", wt[i][:], w[kc * 128:(kc + 1) * 128, :], writes=[f"wt{i}"])
        if kc == 0:
            p.op("dve", lambda e, i=i, kc=kc: e.tensor_scalar(out=acc[:], in0=wt[i][:], scalar1=ct[:, kc:kc + 1], scalar2=None, op0=ALU.mult),
                 reads=[f"wt{i}", "ct"], writes=["acc"])
        else:
            p.op("dve", lambda e, i=i, kc=kc: e.scalar_tensor_tensor(out=acc[:], in0=wt[i][:], scalar=ct[:, kc:kc + 1], in1=acc[:], op0=ALU.mult, op1=ALU.add),
                 reads=[f"wt{i}", "ct", "acc"], writes=["acc"])
    for j in range(3):
        pj = ps[j % 2]
        p.op("pe", lambda e, j=j, pj=pj: e.matmul(pj[:], lhsT=ones[:], rhs=acc[:, j * 512:(j + 1) * 512], start=True, stop=True),
             reads=["acc", "ones"], writes=[f"ps{j%2}"])
        p.op("dve", lambda e, j=j, pj=pj: e.tensor_tensor(out=res[:, j * 512:(j + 1) * 512], in0=pj[:], in1=bt[:, j * 512:(j + 1) * 512], op=ALU.add),
             reads=[f"ps{j%2}", "bt"], writes=[f"res{j}"])
    p.dma("sp", y, res[:], reads=["res0", "res1", "res2"])


def launch_ada(c, w_ada, b_ada):
    in_maps = [{"w": np.ascontiguousarray(w_ada[:, i * 1536:(i + 1) * 1536]),
                "c": np.ascontiguousarray(c.reshape(16, 128).T),
                "b": np.ascontiguousarray(b_ada[None, i * 1536:(i + 1) * 1536])} for i in range(8)]
    res = _run(build_ada, in_maps)
    return np.concatenate([r["y"][0] for r in res])


def make_build_norm(add_one, has_b, has_base, ntile=8):
    def build(nc, p, st):
        n = ntile * 128
        y = nc.dram_tensor("y", [n, D], F32, kind="ExternalInput").ap()
        g = nc.dram_tensor("g", [1, D], F32, kind="ExternalInput").ap()
        s = nc.dram_tensor("s", [1, D], F32, kind="ExternalInput").ap()
        bv = nc.dram_tensor("bv", [1, D], F32, kind="ExternalInput").ap() if has_b else None
        base = nc.dram_tensor("base", [n, D], F32, kind="ExternalInput").ap() if has_base else None
        o = nc.dram_tensor("o", [n, D], F32, kind="ExternalOutput").ap()
        gb = _sb(nc, st, "gb", [128, D])
        A = _sb(nc, st, "A", [128, D])
        bb = _sb(nc, st, "bb", [128, D]) if has_b else None
        p.dma("sp", gb[:], g.partition_broadcast(128), writes=["gb"])
        p.dma("sp", A[:], s.partition_broadcast(128), writes=["A"])
        if has_b:
            p.dma("sp", bb[:], bv.partition_broadcast(128), writes=["bb"])
        p.op("dve", lambda e: e.scalar_tensor_tensor(out=A[:], in0=A[:], scalar=float(add_one), in1=gb[:], op0=ALU.add, op1=ALU.mult),
             reads=["gb", "A"], writes=["A"])
        yt = [_sb(nc, st, f"yt{i}", [128, D]) for i in range(2)]
        bt = [_sb(nc, st, f"bt{i}", [128, D]) for i in range(2)] if has_base else None
        junk = _sb(nc, st, "junk", [128, D])
        ot = [_sb(nc, st, f"ot{i}", [128, D]) for i in range(2)]
        ss = [_sb(nc, st, f"ss{i}", [128, 4]) for i in range(2)]
        for t in range(ntile):
            i = t % 2
            rows = slice(t * 128, (t + 1) * 128)
            p.dma("sp", yt[i][:], y[rows, :], writes=[f"yt{i}"])
            if has_base:
                p.dma("sp", bt[i][:], base[rows, :], writes=[f"bt{i}"])
            p.op("act", lambda e, i=i: e.activation(out=junk[:], in_=yt[i][:], func=AF.Square, accum_out=ss[i][:, 0:1]),
                 reads=[f"yt{i}"], writes=["junk", f"ss{i}"])
            p.op("dve", lambda e, i=i: e.tensor_scalar(out=ss[i][:, 1:2], in0=ss[i][:, 0:1], scalar1=1.0 / D, scalar2=EPS, op0=ALU.mult, op1=ALU.add),
                 reads=[f"ss{i}"], writes=[f"ss{i}"])
            p.op("act", lambda e, i=i: e.activation(out=ss[i][:, 2:3], in_=ss[i][:, 1:2], func=AF.Sqrt),
                 reads=[f"ss{i}"], writes=[f"ss{i}"])
            p.op("dve", lambda e, i=i: e.reciprocal(out=ss[i][:, 3:4], in_=ss[i][:, 2:3]),
                 reads=[f"ss{i}"], writes=[f"ss{i}"])
            p.op("dve", lambda e, i=i: e.scalar_tensor_tensor(out=ot[i][:], in0=yt[i][:], scalar=ss[i][:, 3:4], in1=A[:], op0=ALU.mult, op1=ALU.mult),
                 reads=[f"yt{i}", f"ss{i}", "A"], writes=[f"ot{i}"], force=True)
            if has_b:
                p.op("pool", lambda e, i=i: e.tensor_tensor(out=ot[i][:], in0=ot[i][:], in1=bb[:], op=ALU.add),
                     reads=[f"ot{i}", "bb"], writes=[f"ot{i}"])
            if has_base:
                p.op("pool", lambda e, i=i: e.tensor_tensor(out=ot[i][:], in0=ot[i][:], in1=bt[i][:], op=ALU.add),
                     reads=[f"ot{i}", f"bt{i}"], writes=[f"ot{i}"])
            p.dma("pool", o[rows, :], ot[i][:], reads=[f"ot{i}"])
    return build


def launch_norm(y, g, s, add_one, bv=None, base=None):
    n = y.shape[0] // 8
    in_maps = []
    for i in range(8):
        m = {"y": np.ascontiguousarray(y[i * n:(i + 1) * n]), "g": np.ascontiguousarray(g[None, :]),
             "s": np.ascontiguousarray(s[None, :])}
        if bv is not None:
            m["bv"] = np.ascontiguousarray(bv[None, :])
        if base is not None:
            m["base"] = np.ascontiguousarray(base[i * n:(i + 1) * n])
        in_maps.append(m)
    res = _run(make_build_norm(add_one, bv is not None, base is not None, n // 128), in_maps)
    return np.concatenate([r["o"] for r in res], axis=0)


LC_OUTS = ["qT", "kT", "vT", "rT", "krT", "vrT", "wdec", "nkk", "kka", "kt", "g", "bonus"]
WDECAY = 0.6065306597126334


def build_inproj(nc, p, st, ntt=16):
    T = ntt * 512
    hTp = nc.dram_tensor("hTp", [D, T + 1], F32, kind="ExternalInput").ap()
    ws_d = nc.dram_tensor("ws", [D, 448], F32, kind="ExternalInput").ap()
    wd_d = nc.dram_tensor("wd", [D, 384], F32, kind="ExternalInput").ap()
    mucol_d = nc.dram_tensor("mucol", [1, 384], F32, kind="ExternalInput").ap()
    mupp_d = nc.dram_tensor("mupp", [128, 3], F32, kind="ExternalInput").ap()
    wl_d = nc.dram_tensor("wl", [D, 448], F32, kind="ExternalInput").ap()
    murow_d = nc.dram_tensor("murow", [128, 16, 3], F32, kind="ExternalInput").ap()
    w2w_d = nc.dram_tensor("w2w", [96, 128], F32, kind="ExternalInput").ap()
    w2a_d = nc.dram_tensor("w2a", [96, 128], F32, kind="ExternalInput").ap()
    w2g_d = nc.dram_tensor("w2g", [128, 2, 128], F32, kind="ExternalInput").ap()
    vecs_d = nc.dram_tensor("vecs", [128, 5], F32, kind="ExternalInput").ap()
    cos_d = nc.dram_tensor("cos", [32, T], F32, kind="ExternalInput").ap()
    sin_d = nc.dram_tensor("sin", [32, T], F32, kind="ExternalInput").ap()
    blk_d = nc.dram_tensor("blk", [128, 128], F32, kind="ExternalInput").ap()
    outs = {n: nc.dram_tensor(n, [128, T], F32, kind="ExternalOutput").ap() for n in LC_OUTS}

    w0 = _sb(nc, st, "w0", [128, 16, 448])
    wa = _sb(nc, st, "wa", [128, 16, 448])
    wb = _sb(nc, st, "wb", [128, 16, 448])
    hb = [_sb(nc, st, f"hb{i}", [128, 16, 514]) for i in range(2)]
    mucol = _sb(nc, st, "mucol_sb", [128, 384])
    murow = _sb(nc, st, "murow_sb", [128, 16, 3])
    w2w = _sb(nc, st, "w2w_sb", [96, 128])
    w2a = _sb(nc, st, "w2a_sb", [96, 128])
    w2g = _sb(nc, st, "w2g_sb", [128, 2, 128])
    vecs = _sb(nc, st, "vecs_sb", [128, 5])
    blk = _sb(nc, st, "blk_sb", [128, 128])
    cs = [_sb(nc, st, f"cs{i}", [32, 2, 512]) for i in range(2)]
    mupp = _sb(nc, st, "mupp_sb", [128, 3])
    carry = _sb(nc, st, "carry_sb", [128, 3])
    Tb = [_sb(nc, st, f"Tb{i}", [128, 514]) for i in range(2)]
    ps = [_ps(nc, st, f"ps{i}") for i in range(8)]
    NOB = 6
    ob = [_sb(nc, st, f"ob{i}", [128, 512]) for i in range(NOB)]
    obi = [0]
    psi = [0]

    def nps():
        i = psi[0] % 8
        psi[0] += 1
        return ps[i], f"ps{i}"

    def nob():
        i = obi[0] % NOB
        obi[0] += 1
        return ob[i], f"ob{i}"

    hview = hTp.rearrange("(c p) t -> p c t", p=128)
    r32 = lambda ap: ap.bitcast(F32R)

    for dst, src, k in [(mucol[:], mucol_d.partition_broadcast(128), "mucol"), (murow[:], murow_d, "murow"),
                        (w2w[:], w2w_d, "w2w"), (w2a[:], w2a_d, "w2a"), (w2g[:], w2g_d, "w2g"),
                        (vecs[:], vecs_d, "vecs"), (blk[:], blk_d, "blk"), (mupp[:], mupp_d, "mupp")]:
        p.dma("sp", dst, src, writes=[k])

    def load_h(tt):
        i = tt % 2
        p.dma("pool", r32(hb[i][:, :, 0:513]), r32(hview[:, :, tt * 512:tt * 512 + 513]), writes=[f"hb{i}"])

    def gemm(tt, kind, co, M, pst, psk):
        i = tt % 2
        for c in range(16):
            cur = hb[i][:, c, 1:513]
            prev = hb[i][:, c, 0:512]
            if kind == "singleA":
                p.op("pe", lambda e, c=c, cur=cur: e.matmul(pst[:M, :], lhsT=r32(wa[:, c, co:co + M]), rhs=r32(cur), start=(c == 0), stop=(c == 15)),
                     reads=["wa", f"hb{i}"], writes=[psk])
            elif kind == "single":
                p.op("pe", lambda e, c=c, cur=cur: e.matmul(pst[:M, :], lhsT=r32(w0[:, c, co:co + M]), rhs=r32(cur), start=(c == 0), stop=(c == 15)),
                     reads=["w0", f"hb{i}"], writes=[psk])
            else:
                p.op("pe", lambda e, c=c, cur=cur: e.matmul(pst[:M, :], lhsT=r32(wa[:, c, co:co + M]), rhs=r32(cur), start=(c == 0), stop=False),
                     reads=["wa", f"hb{i}"], writes=[psk])
                p.op("pe", lambda e, c=c, prev=prev: e.matmul(pst[:M, :], lhsT=r32(wb[:, c, co:co + M]), rhs=r32(prev), start=False, stop=(c == 15)),
                     reads=["wb", f"hb{i}"], writes=[psk])

    p.dma("pool", r32(w0[:]), r32(ws_d.rearrange("(c p) n -> p c n", p=128)), writes=["w0"])
    p.dma("pool", r32(wa[:, :, 0:384]), r32(wd_d.rearrange("(c p) n -> p c n", p=128)), writes=["wa"])
    p.op("dve", lambda e: e.memset(carry[:], 0.0), writes=["carry0", "carry1", "carry2"])
    load_h(0)
    for tt in range(ntt):
        if tt + 1 < ntt:
            load_h(tt + 1)
        tsl = slice(tt * 512, (tt + 1) * 512)
        ci = tt % 2
        p.dma("sp", cs[ci][:, 0, :], cos_d[:, tsl], writes=[f"cs{ci}"])
        p.dma("sp", cs[ci][:, 1, :], sin_d[:, tsl], writes=[f"cs{ci}"])
        for name, co in (("qT", 0), ("kT", 160)):
            pq, pqk = nps()
            gemm(tt, "single", co, 128, pq, pqk)
            psw, pswk = nps()
            gemm(tt, "single", co + 128, 32, psw, pswk)
            o, ok = nob()
            p.op("act", lambda e, o=o, pq=pq: e.copy(out=o[:], in_=pq[:]), reads=[pqk], writes=[ok, ok + "hi"])
            t1, t1k = nob()
            p.op("dve", lambda e, t1=t1, psw=psw, ci=ci: e.tensor_tensor(out=t1[0:32, :], in0=psw[0:32, :], in1=cs[ci][:, 1, :], op=ALU.mult),
                 reads=[pswk, f"cs{ci}"], writes=[t1k])
            p.op("dve", lambda e, o=o, pq=pq, ci=ci: e.tensor_tensor(out=o[0:32, :], in0=pq[0:32, :], in1=cs[ci][:, 0, :], op=ALU.mult),
                 reads=[pqk, f"cs{ci}"], writes=[ok])
            p.op("dve", lambda e, o=o, t1=t1: e.tensor_tensor(out=o[0:32, :], in0=o[0:32, :], in1=t1[0:32, :], op=ALU.add),
                 reads=[ok, t1k], writes=[ok])
            p.dma("pool", outs[name][:, tsl], o[:], reads=[ok, ok + "hi"], writes=[f"d_{name}_{tt}"])
        pv, pvk = nps()
        gemm(tt, "single", 320, 128, pv, pvk)
        o, ok = nob()
        p.op("act", lambda e, o=o, pv=pv: e.copy(out=o[:], in_=pv[:]), reads=[pvk], writes=[ok, ok + "hi"])
        p.dma("pool", outs["vT"][:, tsl], o[:], reads=[ok, ok + "hi"], writes=[f"d_vT_{tt}"])
        for j, name in enumerate(("rT", "krT", "vrT")):
            pr, prk = nps()
            gemm(tt, "singleA", j * 128, 128, pr, prk)
            tb = Tb[(tt * 3 + j) % 2]
            tbk = f"Tb{(tt * 3 + j) % 2}"
            p.op("act", lambda e, tb=tb, j=j: e.copy(out=tb[:, 0:1], in_=carry[:, j:j + 1]), reads=[f"carry{j}"], writes=[tbk + "c"])
            p.op("act", lambda e, tb=tb, pr=pr: e.copy(out=tb[:, 1:513], in_=pr[:]), reads=[prk], writes=[tbk])
            p.op("act", lambda e, tb=tb, j=j: e.copy(out=carry[:, j:j + 1], in_=tb[:, 512:513]), reads=[tbk], writes=[f"carry{j}"])
            d_, dk = nob()
            p.op("dve", lambda e, tb=tb, d_=d_: e.tensor_tensor(out=d_[:], in0=tb[:, 0:512], in1=tb[:, 1:513], op=ALU.subtract), reads=[tbk, tbk + "c"], writes=[dk, dk + "hi"])
            o, ok = nob()
            p.op("dve", lambda e, tb=tb, d_=d_, o=o, j=j: e.scalar_tensor_tensor(out=o[:], in0=d_[:], scalar=mupp[:, j:j + 1], in1=tb[:, 1:513], op0=ALU.mult, op1=ALU.add),
                 reads=[dk, dk + "hi", tbk, "mupp"], writes=[ok, ok + "hi"])
            p.dma("pool", outs[name][:, tsl], o[:], reads=[ok, ok + "hi"], writes=[f"d_{name}_{tt}"])

    p.dma("pool", r32(w0[:]), r32(wl_d.rearrange("(c p) n -> p c n", p=128)), writes=["w0"])
    for c in range(16):
        for j, (lo, hi) in enumerate(((0, 96), (96, 192), (192, 448))):
            p.op("dve", lambda e, c=c, j=j, lo=lo, hi=hi: e.tensor_scalar(out=r32(wb[:, c, lo:hi]), in0=w0[:, c, lo:hi], scalar1=murow[:, c, j:j + 1], scalar2=None, op0=ALU.mult),
                 reads=["w0", "murow"], writes=["wb"])
    p.op("dve", lambda e: e.tensor_tensor(out=r32(wa[:]), in0=w0[:], in1=wb[:], op=ALU.subtract),
         reads=["w0", "wb"], writes=["wa"])
    tw = _sb(nc, st, "tw", [96, 512])
    ta = _sb(nc, st, "ta", [96, 512])
    tg = _sb(nc, st, "tg", [128, 2, 512])
    rin = [_sb(nc, st, f"rin{i}", [128, 3, 512]) for i in range(1)]
    tmp = {n: _sb(nc, st, "tmp_" + n, [128, 512]) for n in ["a", "kkr", "sq", "nrm", "rn", "u", "rk"]}
    load_h(0)
    for tt in range(ntt):
        if tt + 1 < ntt:
            load_h(tt + 1)
        tsl = slice(tt * 512, (tt + 1) * 512)
        ri = 0
        for j, name in enumerate(("rT", "krT", "vrT")):
            p.dma("sp", rin[ri][:, j, :], outs[name][:, tsl], reads=[f"d_{name}_{tt}"], writes=[f"rin{ri}_{j}"])
        R_, KR, VR = rin[ri][:, 0, :], rin[ri][:, 1, :], rin[ri][:, 2, :]
        rk_, krk, vrk = f"rin{ri}_0", f"rin{ri}_1", f"rin{ri}_2"
        pw, pwk = nps()
        gemm(tt, "dual", 0, 96, pw, pwk)
        p.op("act", lambda e, pw=pw: e.activation(out=tw[:], in_=pw[:96, :], func=AF.Tanh), reads=[pwk], writes=["tw"])
        pa, pak = nps()
        gemm(tt, "dual", 96, 96, pa, pak)
        p.op("act", lambda e, pa=pa: e.copy(out=ta[:], in_=pa[:96, :]), reads=[pak], writes=["ta"])
        for h in range(2):
            pg, pgk = nps()
            gemm(tt, "dual", 192 + h * 128, 128, pg, pgk)
            p.op("act", lambda e, pg=pg, h=h: e.activation(out=tg[:, h, :], in_=pg[:], func=AF.Sigmoid), reads=[pgk], writes=[f"tg{h}"])
        pd, pdk = nps()
        p.op("pe", lambda e, pd=pd: e.matmul(pd[:], lhsT=w2w[:], rhs=tw[:], start=True, stop=True), reads=["w2w", "tw"], writes=[pdk])
        o_w, o_wk = nob()
        p.op("act", lambda e, pd=pd: e.activation(out=tmp["sq"][:], in_=pd[:], func=AF.Sigmoid, bias=vecs[:, 0:1]), reads=[pdk, "vecs"], writes=["t_sq"])
        p.op("act", lambda e, o_w=o_w: e.activation(out=o_w[:], in_=tmp["sq"][:], func=AF.Exp, scale=-WDECAY), reads=["t_sq"], writes=[o_wk])
        p.dma("pool", outs["wdec"][:, tsl], o_w[:], reads=[o_wk])
        pa2, pa2k = nps()
        p.op("pe", lambda e, pa2=pa2: e.matmul(pa2[:], lhsT=w2a[:], rhs=ta[:], start=True, stop=True), reads=["w2a", "ta"], writes=[pa2k])
        p.op("act", lambda e, pa2=pa2: e.activation(out=tmp["a"][:], in_=pa2[:], func=AF.Sigmoid, bias=vecs[:, 1:2]), reads=[pa2k, "vecs"], writes=["t_a"])
        pg2, pg2k = nps()
        for h in range(2):
            p.op("pe", lambda e, pg2=pg2, h=h: e.matmul(pg2[:], lhsT=w2g[:, h, :], rhs=tg[:, h, :], start=(h == 0), stop=(h == 1)),
                 reads=["w2g", f"tg{h}"], writes=[pg2k])
        o_g, o_gk = nob()
        p.op("act", lambda e, o_g=o_g, pg2=pg2: e.copy(out=o_g[:], in_=pg2[:]), reads=[pg2k], writes=[o_gk])
        p.dma("pool", outs["g"][:, tsl], o_g[:], reads=[o_gk])
        p.op("dve", lambda e, KR=KR: e.tensor_scalar(out=tmp["kkr"][:], in0=KR, scalar1=vecs[:, 2:3], scalar2=None, op0=ALU.mult),
             reads=[krk, "vecs"], writes=["t_kkr"])
        p.op("pool", lambda e: e.tensor_tensor(out=tmp["sq"][:], in0=tmp["kkr"][:], in1=tmp["kkr"][:], op=ALU.mult),
             reads=["t_kkr", "t_sq"], writes=["t_sq"])
        pn, pnk = nps()
        p.op("pe", lambda e, pn=pn: e.matmul(pn[:], lhsT=blk[:], rhs=tmp["sq"][:], start=True, stop=True), reads=["blk", "t_sq"], writes=[pnk])
        p.op("act", lambda e, pn=pn: e.activation(out=tmp["nrm"][:], in_=pn[:], func=AF.Sqrt), reads=[pnk], writes=["t_nrm"])
        p.op("dve", lambda e: e.tensor_scalar(out=tmp["nrm"][:], in0=tmp["nrm"][:], scalar1=1e-12, scalar2=None, op0=ALU.max),
             reads=["t_nrm"], writes=["t_nrm"])
        p.op("dve", lambda e: e.reciprocal(out=tmp["rn"][:], in_=tmp["nrm"][:]), reads=["t_nrm"], writes=["t_rn"])
        o_n, o_nk = nob()
        p.op("dve", lambda e, o_n=o_n: e.scalar_tensor_tensor(out=o_n[:], in0=tmp["kkr"][:], scalar=-1.0, in1=tmp["rn"][:], op0=ALU.mult, op1=ALU.mult),
             reads=["t_kkr", "t_rn"], writes=[o_nk])
        p.dma("pool", outs["nkk"][:, tsl], o_n[:], reads=[o_nk])
        o_ka, o_kak = nob()
        p.op("dve", lambda e, o_n=o_n, o_ka=o_ka: e.scalar_tensor_tensor(out=o_ka[:], in0=o_n[:], scalar=-1.0, in1=tmp["a"][:], op0=ALU.mult, op1=ALU.mult),
             reads=[o_nk, "t_a"], writes=[o_kak])
        p.dma("pool", outs["kka"][:, tsl], o_ka[:], reads=[o_kak])
        p.op("dve", lambda e: e.tensor_scalar(out=tmp["u"][:], in0=tmp["a"][:], scalar1=-1.0, scalar2=vecs[:, 3:4], op0=ALU.add, op1=ALU.mult),
             reads=["t_a", "vecs"], writes=["t_u"])
        o_kt, o_ktk = nob()
        p.op("dve", lambda e, o_kt=o_kt, KR=KR: e.scalar_tensor_tensor(out=o_kt[:], in0=tmp["u"][:], scalar=1.0, in1=KR, op0=ALU.add, op1=ALU.mult),
             reads=["t_u", krk], writes=[o_ktk])
        p.dma("pool", outs["kt"][:, tsl], o_kt[:], reads=[o_ktk])
        p.op("dve", lambda e, o_kt=o_kt, R_=R_: e.scalar_tensor_tensor(out=tmp["rk"][:], in0=R_, scalar=vecs[:, 4:5], in1=o_kt[:], op0=ALU.mult, op1=ALU.mult),
             reads=[rk_, "vecs", o_ktk], writes=["t_rk"])
        pb, pbk = nps()
        p.op("pe", lambda e, pb=pb: e.matmul(pb[:], lhsT=blk[:], rhs=tmp["rk"][:], start=True, stop=True), reads=["blk", "t_rk"], writes=[pbk])
        o_b, o_bk = nob()
        p.op("dve", lambda e, o_b=o_b, pb=pb, VR=VR: e.tensor_tensor(out=o_b[:], in0=pb[:], in1=VR, op=ALU.mult),
             reads=[pbk, vrk], writes=[o_bk])
        p.dma("pool", outs["bonus"][:, tsl], o_b[:], reads=[o_bk])


def _rope_tables(T):
    half = 16
    inv = (500000.0 ** (-np.arange(half, dtype=np.float32) / half)).astype(np.float32)
    ang = np.arange(T, dtype=np.float32)[:, None] * inv[None, :]
    cos = np.cos(ang).astype(np.float32).T
    sin = np.sin(ang).astype(np.float32).T
    COS = np.concatenate([cos, cos], 0)
    SIN = np.concatenate([-sin, sin], 0)
    return np.ascontiguousarray(COS), np.ascontiguousarray(SIN)


def launch_inproj(h, I):
    T = h.shape[0]
    hTp = np.zeros((D, T + 1), np.float32)
    hTp[:, 1:] = h.T
    w_in = I["w_in"][0]
    COS, SIN = _rope_tables(T)
    swp = np.concatenate([np.arange(16, 32), np.arange(0, 16)])
    blk = np.zeros((128, 128), np.float32)
    blk[:64, :64] = 1
    blk[64:, 64:] = 1
    murow = np.stack([I["mu_w"][0], I["mu_a"][0], I["mu_g"][0]], -1).reshape(16, 128, 3).transpose(1, 0, 2)
    wl = np.concatenate([I["w_w1"][0], I["w_a1"][0], I["w_g1"][0]], 1)
    in_maps = []
    for i in range(8):
        cq = slice(i * 128, (i + 1) * 128)
        q = w_in[:, 0:1024][:, cq]
        k = w_in[:, 1024:2048][:, cq]
        v = w_in[:, 2048:3072][:, cq]
        ws = np.concatenate([q, q[:, swp], k, k[:, swp], v], 1)
        r = w_in[:, 3072:4096][:, cq]
        kr = w_in[:, 4096:5120][:, cq]
        vr = w_in[:, 5120:6144][:, cq]
        wd = np.concatenate([r, kr, vr], 1)
        mucol = np.concatenate([I["mu_r"][0][cq], I["mu_k"][0][cq], I["mu_v"][0][cq]])[None, :]
        vecs = np.stack([I["w0"][0][cq], I["a0"][0][cq], I["k_k"][0][cq], I["k_a"][0][cq], I["r_k"][0].reshape(-1)[cq]], -1)
        mupp = np.stack([I["mu_r"][0][cq], I["mu_k"][0][cq], I["mu_v"][0][cq]], -1)
        in_maps.append({
            "hTp": hTp, "ws": np.ascontiguousarray(ws), "wd": np.ascontiguousarray(wd), "mucol": np.ascontiguousarray(mucol),
            "wl": np.ascontiguousarray(wl), "murow": np.ascontiguousarray(murow),
            "w2w": np.ascontiguousarray(I["w_w2"][0][:, cq]), "w2a": np.ascontiguousarray(I["w_a2"][0][:, cq]),
            "w2g": np.ascontiguousarray(I["w_g2"][0][:, cq].reshape(2, 128, 128).transpose(1, 0, 2)),
            "vecs": np.ascontiguousarray(vecs), "cos": COS, "sin": SIN, "blk": blk, "mupp": np.ascontiguousarray(mupp)})
    ntt = T // 512
    res = _run(lambda nc, p, st: build_inproj(nc, p, st, ntt), in_maps)
    return res


GN_EPS = 64e-5
TCH = 32


def build_rwkv(nc, p, st, T=S):
    nch = T // TCH
    bcin_d = nc.dram_tensor("bcin", [2, nch, 5, TCH, 64], F32, kind="ExternalInput").ap()
    vT_d = nc.dram_tensor("vT", [128, T], F32, kind="ExternalInput").ap()
    g_d = nc.dram_tensor("g", [128, T], F32, kind="ExternalInput").ap()
    bonus_d = nc.dram_tensor("bonus", [128, T], F32, kind="ExternalInput").ap()
    gnv_d = nc.dram_tensor("gnv", [128, 2], F32, kind="ExternalInput").ap()
    sel_d = nc.dram_tensor("sel", [128, 128], F32, kind="ExternalInput").ap()
    blk_d = nc.dram_tensor("blk", [128, 128], F32, kind="ExternalInput").ap()
    o_d = nc.dram_tensor("o", [128, T], F32, kind="ExternalOutput").ap()
    r32 = lambda ap: ap.bitcast(F32R)

    vT = _sb(nc, st, "vT_sb", [128, T])
    yT = _sb(nc, st, "yT_sb", [128, T])
    Sst = _sb(nc, st, "S_sb", [128, 64])
    junk = _sb(nc, st, "junk", [128, 64])
    sa = _sb(nc, st, "sa", [128, 1])
    sel = _sb(nc, st, "sel_sb", [128, 128])
    blk = _sb(nc, st, "blk_sb", [128, 128])
    gnv = _sb(nc, st, "gnv_sb", [128, 2])
    bc = [_sb(nc, st, f"bc{i}", [128, 5, TCH, 64]) for i in range(2)]
    ps = [_ps(nc, st, f"ps{i}") for i in range(8)]
    p.dma("pool", r32(sel[:]), r32(sel_d), writes=["sel"])
    p.dma("sp", blk[:], blk_d, writes=["blk"])
    p.dma("sp", gnv[:], gnv_d, writes=["gnv"])
    p.dma("sp", vT[:], vT_d, writes=["vT"])
    p.op("dve", lambda e: e.memset(Sst[:], 0.0), writes=["S"])
    zer_d = nc.dram_tensor("zer", [126, 5, TCH, 64], F32, kind="ExternalInput").ap()
    for i in range(2):
        p.dma("pool", r32(bc[i][2:128]), r32(zer_d), writes=[f"bc{i}"])

    def load_bc(c):
        i = c % 2
        p.dma("pool", r32(bc[i][0:2]), r32(bcin_d[:, c]), writes=[f"bc{i}"])

    load_bc(0)
    grp = 0
    for c in range(nch):
        if c + 1 < nch:
            load_bc(c + 1)
        bi = c % 2
        for g4 in range(TCH // 4):
            base = (grp % 2) * 3
            grp += 1
            views = []
            for j in range(5):
                bank = ps[base + j // 2]
                bk = f"ps{base + j // 2}"
                half = bank[:, (j % 2) * 256:(j % 2) * 256 + 256]
                p.op("pe", lambda e, half=half, j=j, g4=g4, bi=bi: e.matmul(half, lhsT=r32(sel[:]), rhs=r32(bc[bi][:, j, g4 * 4:(g4 + 1) * 4, :]), start=True, stop=True),
                     reads=["sel", f"bc{bi}"], writes=[bk + f"h{j%2}"])
                views.append((half, bk + f"h{j%2}"))
            for tl in range(4):
                t = c * TCH + g4 * 4 + tl
                cs = slice(tl * 64, (tl + 1) * 64)
                wv, nv, kav, ktv, rv = [(v[0][:, cs], v[1]) for v in views]
                p.op("dve", lambda e, nv=nv: e.scalar_tensor_tensor(out=junk[:], in0=Sst[:], scalar=1.0, in1=nv[0], op0=ALU.mult, op1=ALU.mult, accum_out=sa[:]),
                     reads=["S", nv[1]], writes=["junk", "sa"])
                p.op("dve", lambda e, wv=wv: e.tensor_tensor(out=Sst[:], in0=Sst[:], in1=wv[0], op=ALU.mult),
                     reads=["S", wv[1]], writes=["S"])
                p.op("dve", lambda e, kav=kav: e.scalar_tensor_tensor(out=Sst[:], in0=kav[0], scalar=sa[:, 0:1], in1=Sst[:], op0=ALU.mult, op1=ALU.add),
                     reads=["S", "sa", kav[1]], writes=["S"], force=True)
                p.op("dve", lambda e, ktv=ktv, t=t: e.scalar_tensor_tensor(out=Sst[:], in0=ktv[0], scalar=vT[:, t:t + 1], in1=Sst[:], op0=ALU.mult, op1=ALU.add),
                     reads=["S", "vT", ktv[1]], writes=["S"])
                p.op("dve", lambda e, rv=rv, t=t: e.scalar_tensor_tensor(out=junk[:], in0=Sst[:], scalar=1.0, in1=rv[0], op0=ALU.mult, op1=ALU.mult, accum_out=yT[:, t:t + 1]),
                     reads=["S", rv[1]], writes=["junk", f"yT{t // 512}"])
    gt = [_sb(nc, st, f"g_sb{i}", [128, 512]) for i in range(2)]
    bt = [_sb(nc, st, f"b_sb{i}", [128, 512]) for i in range(2)]
    yc = _sb(nc, st, "yc", [128, 512])
    sq = _sb(nc, st, "sq", [128, 512])
    rs = _sb(nc, st, "rs", [128, 512])
    ot = [_sb(nc, st, f"ot{i}", [128, 512]) for i in range(2)]
    for tt in range(T // 512):
        i = tt % 2
        tsl = slice(tt * 512, (tt + 1) * 512)
        p.dma("sp", gt[i][:], g_d[:, tsl], writes=[f"gt{i}"])
        p.dma("sp", bt[i][:], bonus_d[:, tsl], writes=[f"bt{i}"])
        pm, pmk = ps[6], "ps6"
        p.op("pe", lambda e, tsl=tsl: e.matmul(ps[6][:], lhsT=blk[:], rhs=yT[:, tsl], start=True, stop=True), reads=["blk", f"yT{tt}"], writes=["ps6"])
        p.op("dve", lambda e, tsl=tsl: e.scalar_tensor_tensor(out=yc[:], in0=ps[6][:], scalar=-1.0 / 64, in1=yT[:, tsl], op0=ALU.mult, op1=ALU.add),
             reads=["ps6", f"yT{tt}"], writes=["yc"])
        p.op("act", lambda e: e.activation(out=sq[:], in_=yc[:], func=AF.Square), reads=["yc"], writes=["sq"])
        p.op("pe", lambda e: e.matmul(ps[7][:], lhsT=blk[:], rhs=sq[:], start=True, stop=True), reads=["blk", "sq"], writes=["ps7"])
        p.op("dve", lambda e: e.tensor_scalar(out=rs[:], in0=ps[7][:], scalar1=1.0 / 64, scalar2=GN_EPS, op0=ALU.mult, op1=ALU.add), reads=["ps7"], writes=["rs"])
        p.op("act", lambda e: e.activation(out=rs[:], in_=rs[:], func=AF.Sqrt), reads=["rs"], writes=["rs"])
        p.op("dve", lambda e: e.reciprocal(out=rs[:], in_=rs[:]), reads=["rs"], writes=["rs"])
        p.op("dve", lambda e: e.tensor_tensor(out=yc[:], in0=yc[:], in1=rs[:], op=ALU.mult), reads=["yc", "rs"], writes=["yc"])
        p.op("dve", lambda e: e.tensor_scalar(out=yc[:], in0=yc[:], scalar1=gnv[:, 0:1], scalar2=gnv[:, 1:2], op0=ALU.mult, op1=ALU.add), reads=["yc", "gnv"], writes=["yc"])
        p.op("dve", lambda e, i=i: e.tensor_tensor(out=yc[:], in0=yc[:], in1=bt[i][:], op=ALU.add), reads=["yc", f"bt{i}"], writes=["yc"])
        p.op("dve", lambda e, i=i: e.tensor_tensor(out=ot[i][:], in0=yc[:], in1=gt[i][:], op=ALU.mult), reads=["yc", f"gt{i}"], writes=[f"ot{i}"])
        p.dma("sp", o_d[:, tsl], ot[i][:], reads=[f"ot{i}"])


def launch_rwkv(lc, I, T=S):
    sel = np.zeros((128, 128), np.float32)
    sel[0, :64] = 1
    sel[1, 64:] = 1
    blk = np.zeros((128, 128), np.float32)
    blk[:64, :64] = 1
    blk[64:, 64:] = 1
    in_maps = []
    nch = T // TCH
    for i in range(8):
        cq = slice(i * 128, (i + 1) * 128)
        q5 = np.stack([lc[i][n][:, :T] for n in ("wdec", "nkk", "kka", "kt", "rT")], 0)
        q5 = q5.reshape(5, 2, 64, nch, TCH).transpose(1, 3, 0, 4, 2)
        gnv = np.stack([I["gn_w"][0][cq], I["gn_b"][0][cq]], -1)
        in_maps.append({"bcin": np.ascontiguousarray(q5), "vT": np.ascontiguousarray(lc[i]["vrT"][:, :T]),
                        "g": np.ascontiguousarray(lc[i]["g"][:, :T]), "bonus": np.ascontiguousarray(lc[i]["bonus"][:, :T]),
                        "gnv": np.ascontiguousarray(gnv), "sel": sel, "blk": blk, "zer": np.zeros((126, 5, TCH, 64), np.float32)})
    res = _run(lambda nc, p, st: build_rwkv(nc, p, st, T), in_maps)
    return np.concatenate([r["o"].T for r in res], axis=1)


NEGB = 30000.0


def build_moba(nc, p, st, T=S):
    nb = T // 256
    nkt = T // 128
    qT_d = nc.dram_tensor("qT", [128, T], F32, kind="ExternalInput").ap()
    kT_d = nc.dram_tensor("kT", [128, T], F32, kind="ExternalInput").ap()
    v_d = nc.dram_tensor("v", [128, nkt, 128], F32, kind="ExternalInput").ap()
    E_d = nc.dram_tensor("E", [128, T], F32, kind="ExternalInput").ap()
    cm_d = nc.dram_tensor("cm", [128, 256], F32, kind="ExternalInput").ap()
    id_d = nc.dram_tensor("ident", [128, 128], F32, kind="ExternalInput").ap()
    on_d = nc.dram_tensor("ones", [128, 128], F32, kind="ExternalInput").ap()
    o_d = nc.dram_tensor("oT", [128, T], F32, kind="ExternalOutput").ap()
    r32 = lambda ap: ap.bitcast(F32R)
    qT = _sb(nc, st, "qT_sb", [128, T])
    kT = _sb(nc, st, "kT_sb", [128, T])
    va = _sb(nc, st, "va_sb", [128, nkt, 128])
    E = _sb(nc, st, "E_sb", [128, T])
    cm = _sb(nc, st, "cm_sb", [128, 256])
    ident = _sb(nc, st, "id_sb", [128, 128])
    ones = _sb(nc, st, "ones_sb", [128, 128])
    kmean = _sb(nc, st, "kmean", [128, 32])
    gsb = _sb(nc, st, "gsb", [128, 32])
    m8 = _sb(nc, st, "m8", [128, 8])
    bias = _sb(nc, st, "bias", [128, 128])
    biasT = [_sb(nc, st, f"biasT{i}", [128, 256]) for i in range(2)]
    pT = [_sb(nc, st, f"pT{i}", [128, 256]) for i in range(3)]
    osb = [_sb(nc, st, f"osb{i}", [128, 256]) for i in range(2)]
    rden = _sb(nc, st, "rden", [128, 256])
    s_ps = [_ps(nc, st, f"s_ps{i}") for i in range(2)]
    o_ps = [_ps(nc, st, f"o_ps{i}") for i in range(2)]
    d_ps = [_ps(nc, st, f"d_ps{i}") for i in range(2)]
    g_ps = _ps(nc, st, "g_ps")
    t_ps = _ps(nc, st, "t_ps")
    p.dma("pool", r32(cm[:]), r32(cm_d), writes=["cm"])
    p.dma("pool", r32(ident[:]), r32(id_d), writes=["ident"])
    p.dma("pool", r32(ones[:]), r32(on_d), writes=["ones"])
    PCS = 1024
    npc = max(1, T // PCS)
    pc = lambda tok: min(tok // PCS, npc - 1)
    for i_ in range(npc):
        tsl_ = slice(i_ * PCS, min(T, (i_ + 1) * PCS))
        ktl_ = slice(i_ * PCS // 128, min(T, (i_ + 1) * PCS) // 128)
        p.dma("pool", r32(qT[:, tsl_]), r32(qT_d[:, tsl_]), writes=[f"qT{i_}"])
        p.dma("pool", r32(kT[:, tsl_]), r32(kT_d[:, tsl_]), writes=[f"kT{i_}"])
        p.dma("pool", r32(va[:, ktl_, :]), r32(v_d[:, ktl_, :]), writes=[f"va{i_}"])
        p.dma("pool", r32(E[:, tsl_]), r32(E_d[:, tsl_]), writes=[f"E{i_}"])
        nb0 = i_ * PCS // 256
        nb1 = min(T, (i_ + 1) * PCS) // 256
        p.op("dve", lambda e, tsl_=tsl_, nb0=nb0, nb1=nb1: e.tensor_reduce(out=kmean[:, nb0:nb1], in_=kT[:, tsl_].bitcast(F32).rearrange("p (n k) -> p n k", k=256), axis=AX.X, op=ALU.add),
             reads=[f"kT{i_}"], writes=[f"kmean{i_}"])
        p.op("dve", lambda e, nb0=nb0, nb1=nb1: e.tensor_scalar(out=kmean[:, nb0:nb1], in0=kmean[:, nb0:nb1], scalar1=1.0 / 256, scalar2=None, op0=ALU.mult),
             reads=[f"kmean{i_}"], writes=[f"kmean{i_}"])
    kmkeys = lambda b_: [f"kmean{i_}" for i_ in range(pc(b_ * 256) + 1)]
    p.op("dve", lambda e: e.memset(gsb[:], -1e30), writes=["gsb"])
    p.op("dve", lambda e: e.memset(bias[:], 0.0), writes=["bias"])
    scale = 128 ** -0.5
    pti = 0
    si = 0
    for b in range(nb):
        bT = biasT[b % 2]
        bTk = f"biasT{b % 2}"
        for j in range(2):
            qs = slice(b * 256 + j * 128, b * 256 + (j + 1) * 128)
            if b > 3:
                p.op("pe", lambda e, qs=qs: e.matmul(g_ps[:, 0:nb], lhsT=qT[:, qs], rhs=kmean[:, 0:nb], start=True, stop=True),
                     reads=[f"qT{pc(b * 256)}"] + kmkeys(b), writes=["g_ps"])
                p.op("dve", lambda e, b=b: e.tensor_copy(out=gsb[:, 0:b], in_=g_ps[:, 0:b]), reads=["g_ps", "gsb"], writes=["gsb"])
                p.op("dve", lambda e: e.max(out=m8[:], in_=gsb[:]), reads=["gsb"], writes=["m8"])
                p.op("dve", lambda e: e.tensor_scalar(out=bias[:, 0:32], in0=gsb[:], scalar1=m8[:, 2:3], scalar2=None, op0=ALU.is_ge),
                     reads=["gsb", "m8", "bias"], writes=["bias"], force=True)
                p.op("dve", lambda e: e.tensor_scalar(out=bias[:, 0:32], in0=bias[:, 0:32], scalar1=NEGB, scalar2=-NEGB, op0=ALU.mult, op1=ALU.add),
                     reads=["bias"], writes=["bias"])
            else:
                p.op("dve", lambda e: e.memset(bias[:, 0:32], -NEGB), reads=["bias"], writes=["bias"])
                if b > 0:
                    p.op("dve", lambda e, b=b: e.memset(bias[:, 0:b], 0.0), reads=["bias"], writes=["bias"])
            p.op("dve", lambda e, b=b: e.memset(bias[:, b:b + 1], 0.0), reads=["bias"], writes=["bias"])
            p.op("pe", lambda e: e.transpose(t_ps[:, 0:128], bias[:], ident[:].bitcast(F32)), reads=["bias", "ident"], writes=["t_ps"])
            p.op("act", lambda e, bT=bT, j=j: e.copy(out=r32(bT[:, j * 128:(j + 1) * 128]), in_=t_ps[:, 0:128]), reads=["t_ps"], writes=[bTk])
        op_ = o_ps[b % 2]
        opk = f"o_ps{b % 2}"
        dp_ = d_ps[b % 2]
        dpk = f"d_ps{b % 2}"
        nkt_b = 2 * b + 2

        def qkm(kt, sp_, spk, b=b, bT=bT, bTk=bTk):
            ks = slice(kt * 128, (kt + 1) * 128)
            own = kt >= 2 * b
            p.op("pe", lambda e: e.matmul(sp_[:, 0:256], lhsT=r32(kT[:, ks]), rhs=r32(qT[:, b * 256:(b + 1) * 256]), start=True, stop=False),
                 reads=[f"kT{pc(kt * 128)}", f"qT{pc(b * 256)}"], writes=[spk])
            p.op("pe", lambda e: e.matmul(sp_[:, 0:256], lhsT=r32(E[:, ks]), rhs=r32(bT[:]), start=False, stop=(not own)),
                 reads=[f"E{pc(kt * 128)}", bTk], writes=[spk])
            if own:
                if kt == 2 * b:
                    p.op("pe", lambda e: e.matmul(sp_[:, 0:128], lhsT=ident[:].bitcast(F32), rhs=cm[:, 128:256].bitcast(F32), start=False, stop=True),
                         reads=["ident", "cm"], writes=[spk])
                else:
                    p.op("pe", lambda e: e.matmul(sp_[:, 0:256], lhsT=r32(ident[:]), rhs=r32(cm[:, 0:256]), start=False, stop=True),
                         reads=["ident", "cm"], writes=[spk])

        cur = (s_ps[si % 2], f"s_ps{si % 2}")
        si += 1
        qkm(0, *cur)
        for kt in range(nkt_b):
            nxt = None
            if kt + 1 < nkt_b:
                nxt = (s_ps[si % 2], f"s_ps{si % 2}")
                si += 1
                qkm(kt + 1, *nxt)
            sp_, spk = cur
            pt = pT[pti % 3]
            ptk = f"pT{pti % 3}"
            pti += 1
            p.op("act", lambda e, pt=pt, sp_=sp_: e.activation(out=r32(pt[:]), in_=sp_[:, 0:256], func=AF.Exp, scale=scale), reads=[spk], writes=[ptk])
            p.op("pe", lambda e, pt=pt, kt=kt, op_=op_, nkt_b=nkt_b: e.matmul(op_[:, 0:256], lhsT=r32(va[:, kt, :]), rhs=r32(pt[:]), start=(kt == 0), stop=(kt == nkt_b - 1)),
                 reads=[ptk, f"va{pc(kt * 128)}"], writes=[opk])
            p.op("pe", lambda e, pt=pt, kt=kt, dp_=dp_, nkt_b=nkt_b: e.matmul(dp_[:, 0:256], lhsT=r32(ones[:]), rhs=r32(pt[:]), start=(kt == 0), stop=(kt == nkt_b - 1)),
                 reads=[ptk, "ones"], writes=[dpk])
            cur = nxt
        ob_ = osb[b % 2]
        p.op("dve", lambda e, dp_=dp_: e.reciprocal(out=rden[:], in_=dp_[:, 0:256]), reads=[dpk], writes=["rden"])
        p.op("dve", lambda e, op_=op_, ob_=ob_: e.tensor_tensor(out=ob_[:], in0=op_[:, 0:256], in1=rden[:], op=ALU.mult), reads=[opk, "rden"], writes=[f"osb{b % 2}"])
        p.dma("sp", o_d[:, b * 256:(b + 1) * 256], ob_[:], reads=[f"osb{b % 2}"])


def launch_moba(lc, T=S):
    nkt = T // 128
    E = np.zeros((128, T), np.float32)
    for n in range(T // 256):
        E[n, n * 256:(n + 1) * 256] = 1
    kk = np.arange(128)
    cmc = np.where(kk[:, None] <= kk[None, :], 0.0, -NEGB).astype(np.float32)
    cm = np.concatenate([np.full((128, 128), -NEGB, np.float32), cmc], 1)
    ident = np.eye(128, dtype=np.float32)
    ones = np.ones((128, 128), np.float32)
    in_maps = []
    for i in range(8):
        v = lc[i]["vT"][:, :T].T.reshape(nkt, 128, 128).transpose(1, 0, 2)
        in_maps.append({"qT": np.ascontiguousarray(lc[i]["qT"][:, :T]), "kT": np.ascontiguousarray(lc[i]["kT"][:, :T]),
                        "v": np.ascontiguousarray(v), "E": E, "cm": cm, "ident": ident, "ones": ones})
    res = _run(lambda nc, p, st: build_moba(nc, p, st, T), in_maps)
    return np.concatenate([r["oT"].T for r in res], axis=1)


def build_merge(nc, p, st, nunit=2):
    TT = 512
    NT = nunit * TT
    hT_d = nc.dram_tensor("hT", [D, NT], F32, kind="ExternalInput").ap()
    oa_d = nc.dram_tensor("oaT", [1024, NT], F32, kind="ExternalInput").ap()
    or_d = nc.dram_tensor("orT", [1024, NT], F32, kind="ExternalInput").ap()
    wg_d = nc.dram_tensor("wg", [D, 4096], F32, kind="ExternalInput").ap()
    wua_d = nc.dram_tensor("wua", [1024, D], F32, kind="ExternalInput").ap()
    wur_d = nc.dram_tensor("wur", [1024, D], F32, kind="ExternalInput").ap()
    wo_d = nc.dram_tensor("wo", [D, D], F32, kind="ExternalInput").ap()
    y_d = nc.dram_tensor("yT", [D, NT], F32, kind="ExternalOutput").ap()
    r32 = lambda ap: ap.bitcast(F32R)
    hT = _sb(nc, st, "hT_sb", [128, 16, TT])
    oa = _sb(nc, st, "oa_sb", [128, 8, TT])
    orr = _sb(nc, st, "or_sb", [128, 8, TT])
    mix = _sb(nc, st, "mix_sb", [128, 16, TT])
    wb = [_sb(nc, st, f"wb{i}", [128, 48, 128]) for i in range(2)]
    wob = [_sb(nc, st, f"wob{i}", [128, 16, 128]) for i in range(2)]
    sg = [_sb(nc, st, f"sg{i}", [128, TT]) for i in range(2)]
    m12 = [_sb(nc, st, f"m12_{i}", [128, TT]) for i in range(2)]
    yo = [_sb(nc, st, f"yo{i}", [128, TT]) for i in range(2)]
    ps = [_ps(nc, st, f"ps{i}") for i in range(8)]
    wgv = wg_d.rearrange("(c p) n -> p c n", p=128)
    wuav = wua_d.rearrange("(c p) n -> p c n", p=128)
    wurv = wur_d.rearrange("(c p) n -> p c n", p=128)
    wov = wo_d.rearrange("(c p) n -> p c n", p=128)
    wcount = 0
    for u in range(nunit):
        ts_ = slice(u * TT, (u + 1) * TT)
        p.dma("pool", r32(hT[:]), r32(hT_d.rearrange("(c p) t -> p c t", p=128)[:, :, ts_]), writes=["hT"])
        p.dma("pool", r32(oa[:]), r32(oa_d.rearrange("(c p) t -> p c t", p=128)[:, :, ts_]), writes=["oa"])
        p.dma("pool", r32(orr[:]), r32(or_d.rearrange("(c p) t -> p c t", p=128)[:, :, ts_]), writes=["or"])
        for n in range(16):
            wi = wcount % 2
            wcount += 1
            W = wb[wi]
            wk = f"wb{wi}"
            ns = slice(n * 128, (n + 1) * 128)
            ns2 = slice(2048 + n * 128, 2048 + (n + 1) * 128)
            p.dma("pool", r32(W[:, 0:8, :]), r32(wgv[:, 0:8, ns]), writes=[wk + "a"])
            p.dma("pool", r32(W[:, 8:16, :]), r32(wgv[:, 8:16, ns]), writes=[wk + "b"])
            p.dma("pool", r32(W[:, 16:24, :]), r32(wgv[:, 0:8, ns2]), writes=[wk + "c"])
            p.dma("pool", r32(W[:, 24:32, :]), r32(wgv[:, 8:16, ns2]), writes=[wk + "d"])
            p.dma("pool", r32(W[:, 32:40, :]), r32(wuav[:, :, ns]), writes=[wk + "e"])
            p.dma("pool", r32(W[:, 40:48, :]), r32(wurv[:, :, ns]), writes=[wk + "f"])
            wkeys = [wk + x for x in "abcdef"]
            b0 = (n % 2) * 4
            pga, pgr, pua, pur = ps[b0], ps[b0 + 1], ps[b0 + 2], ps[b0 + 3]
            for c in range(16):
                p.op("pe", lambda e, c=c, W=W, pga=pga: e.matmul(pga[:], lhsT=r32(W[:, c, :]), rhs=r32(hT[:, c, :]), start=(c == 0), stop=(c == 15)),
                     reads=wkeys + ["hT"], writes=[f"ps{b0}"])
            for c in range(16):
                p.op("pe", lambda e, c=c, W=W, pgr=pgr: e.matmul(pgr[:], lhsT=r32(W[:, 16 + c, :]), rhs=r32(hT[:, c, :]), start=(c == 0), stop=(c == 15)),
                     reads=wkeys + ["hT"], writes=[f"ps{b0 + 1}"])
            for c in range(8):
                p.op("pe", lambda e, c=c, W=W, pua=pua: e.matmul(pua[:], lhsT=r32(W[:, 32 + c, :]), rhs=r32(oa[:, c, :]), start=(c == 0), stop=(c == 7)),
                     reads=wkeys + ["oa"], writes=[f"ps{b0 + 2}"])
            for c in range(8):
                p.op("pe", lambda e, c=c, W=W, pur=pur: e.matmul(pur[:], lhsT=r32(W[:, 40 + c, :]), rhs=r32(orr[:, c, :]), start=(c == 0), stop=(c == 7)),
                     reads=wkeys + ["or"], writes=[f"ps{b0 + 3}"])
            p.op("act", lambda e, pga=pga: e.activation(out=sg[0][:], in_=pga[:], func=AF.Sigmoid), reads=[f"ps{b0}"], writes=["sg0"])
            p.op("act", lambda e, pgr=pgr: e.activation(out=sg[1][:], in_=pgr[:], func=AF.Sigmoid), reads=[f"ps{b0 + 1}"], writes=["sg1"])
            p.op("dve", lambda e, pua=pua: e.tensor_tensor(out=m12[0][:], in0=pua[:], in1=sg[0][:], op=ALU.mult), reads=[f"ps{b0 + 2}", "sg0"], writes=["m0"])
            p.op("dve", lambda e, pur=pur: e.tensor_tensor(out=m12[1][:], in0=pur[:], in1=sg[1][:], op=ALU.mult), reads=[f"ps{b0 + 3}", "sg1"], writes=["m1"])
            p.op("dve", lambda e, n=n: e.tensor_tensor(out=r32(mix[:, n, :]), in0=m12[0][:], in1=m12[1][:], op=ALU.add), reads=["m0", "m1"], writes=[f"mix{n}"])
        for m in range(16):
            wi = m % 2
            ms = slice(m * 128, (m + 1) * 128)
            p.dma("pool", r32(wob[wi][:, 0:8, :]), r32(wov[:, 0:8, ms]), writes=[f"wob{wi}a"])
            p.dma("pool", r32(wob[wi][:, 8:16, :]), r32(wov[:, 8:16, ms]), writes=[f"wob{wi}b"])
            py = ps[m % 2]
            for c in range(16):
                p.op("pe", lambda e, c=c, wi=wi, py=py: e.matmul(py[:], lhsT=r32(wob[wi][:, c, :]), rhs=r32(mix[:, c, :]), start=(c == 0), stop=(c == 15)),
                     reads=[f"wob{wi}a", f"wob{wi}b", f"mix{c}"], writes=[f"ps{m % 2}"])
            p.op("act", lambda e, py=py, wi=wi: e.copy(out=yo[wi][:], in_=py[:]), reads=[f"ps{m % 2}"], writes=[f"yo{wi}"])
            p.dma("pool", y_d[ms, ts_], yo[wi][:], reads=[f"yo{wi}"])


def launch_merge(h1, o_att, o_rwkv, I):
    wg = np.ascontiguousarray(I["w_in"][0][:, 6144:10240])
    in_maps = []
    for i in range(8):
        ts_ = slice(i * 1024, (i + 1) * 1024)
        in_maps.append({"hT": np.ascontiguousarray(h1[ts_].T), "oaT": np.ascontiguousarray(o_att[ts_].T),
                        "orT": np.ascontiguousarray(o_rwkv[ts_].T), "wg": wg,
                        "wua": I["w_up_att"][0], "wur": I["w_up_rwkv"][0], "wo": I["w_o"][0]})
    res = _run(lambda nc, p, st: build_merge(nc, p, st, 2), in_maps)
    return np.concatenate([r["yT"].T for r in res], axis=0)


def build_router(nc, p, st, ntile=8):
    NT = ntile * 128
    hT_d = nc.dram_tensor("hT", [D, NT], F32, kind="ExternalInput").ap()
    wr_d = nc.dram_tensor("wr", [D, 72], F32, kind="ExternalInput").ap()
    br_d = nc.dram_tensor("br", [1, 72], F32, kind="ExternalInput").ap()
    o_d = nc.dram_tensor("o", [NT, 4], F32, kind="ExternalOutput").ap()
    hT = _sb(nc, st, "hT_sb", [128, 16, NT])
    wr = _sb(nc, st, "wr_sb", [128, 16, 72])
    br = _sb(nc, st, "br_sb", [128, 72])
    ps = [_ps(nc, st, f"ps{i}") for i in range(2)]
    p.dma("sp", hT[:], hT_d.rearrange("(c p) t -> p c t", p=128), writes=["hT"])
    p.dma("sp", wr[:], wr_d.rearrange("(c p) n -> p c n", p=128), writes=["wr"])
    p.dma("sp", br[:], br_d.partition_broadcast(128), writes=["br"])
    l_sb = _sb(nc, st, "l_sb", [128, 72])
    lem = _sb(nc, st, "lem", [128, 64])
    m8g = _sb(nc, st, "m8g", [128, 8])
    m8e = _sb(nc, st, "m8e", [128, 8])
    idx = _sb(nc, st, "idx", [128, 8], U32)
    sm = _sb(nc, st, "sm", [128, 8])
    junk = _sb(nc, st, "junk", [128, 8])
    pen = _sb(nc, st, "pen", [128, 8])
    res = [_sb(nc, st, f"res{i}", [128, 4]) for i in range(2)]
    for t in range(ntile):
        pt = ps[t % 2]
        ptk = f"ps{t % 2}"
        rs_ = res[t % 2]
        rk = f"res{t % 2}"
        for c in range(16):
            p.op("pe", lambda e, c=c, t=t, pt=pt: e.matmul(pt[:, 0:72], lhsT=hT[:, c, t * 128:(t + 1) * 128], rhs=wr[:, c, :], start=(c == 0), stop=(c == 15)),
                 reads=["hT", "wr"], writes=[ptk])
        p.op("dve", lambda e, pt=pt: e.tensor_tensor(out=l_sb[:], in0=pt[:, 0:72], in1=br[:], op=ALU.add), reads=[ptk, "br"], writes=["l"])
        p.op("dve", lambda e: e.max(out=m8g[:], in_=l_sb[:, 0:8]), reads=["l"], writes=["m8g"])
        p.op("dve", lambda e: e.tensor_scalar(out=sm[:, 0:1], in0=m8g[:, 0:1], scalar1=-1.0, scalar2=None, op0=ALU.mult), reads=["m8g"], writes=["sm0"])
        p.op("act", lambda e: e.activation(out=junk[:], in_=l_sb[:, 0:8], func=AF.Exp, bias=sm[:, 0:1], accum_out=sm[:, 1:2]), reads=["l", "sm0"], writes=["junk", "sm1"])
        p.op("dve", lambda e: e.reciprocal(out=sm[:, 2:3], in_=sm[:, 1:2]), reads=["sm1"], writes=["sm2"])
        p.op("dve", lambda e: e.tensor_scalar(out=pen[:], in0=l_sb[:, 0:8], scalar1=m8g[:, 0:1], scalar2=None, op0=ALU.is_ge), reads=["l", "m8g"], writes=["pen"], force=True)
        p.op("dve", lambda e: e.tensor_scalar(out=pen[:], in0=pen[:], scalar1=1e30, scalar2=-1e30, op0=ALU.mult, op1=ALU.add), reads=["pen"], writes=["pen"])
        for g in range(8):
            p.op("dve", lambda e, g=g: e.tensor_scalar(out=lem[:, g * 8:(g + 1) * 8], in0=l_sb[:, 8 + g * 8:16 + g * 8], scalar1=pen[:, g:g + 1], scalar2=None, op0=ALU.add),
                 reads=["l", "pen"], writes=["lem"], force=(g == 0))
        p.op("dve", lambda e: e.max(out=m8e[:], in_=lem[:]), reads=["lem"], writes=["m8e"])
        p.op("dve", lambda e: e.max_index(out=idx[:], in_max=m8e[:], in_values=lem[:]), reads=["m8e", "lem"], writes=["idx"], force=True)
        p.op("dve", lambda e, rs_=rs_: e.tensor_copy(out=rs_[:, 0:2], in_=idx[:, 0:2]), reads=["idx"], writes=[rk + "a"], force=True)
        p.op("dve", lambda e: e.tensor_tensor(out=sm[:, 3:4], in0=m8e[:, 0:1], in1=m8e[:, 1:2], op=ALU.subtract), reads=["m8e"], writes=["sm3"], force=True)
        p.op("act", lambda e: e.activation(out=sm[:, 4:5], in_=sm[:, 3:4], func=AF.Sigmoid), reads=["sm3"], writes=["sm4"])
        p.op("dve", lambda e, rs_=rs_: e.tensor_tensor(out=rs_[:, 2:3], in0=sm[:, 4:5], in1=sm[:, 2:3], op=ALU.mult), reads=["sm4", "sm2"], writes=[rk + "b"], force=True)
        p.op("dve", lambda e, rs_=rs_: e.tensor_tensor(out=rs_[:, 3:4], in0=sm[:, 2:3], in1=rs_[:, 2:3], op=ALU.subtract), reads=["sm2", rk + "b"], writes=[rk + "c"], force=True)
        p.dma("sp", o_d[t * 128:(t + 1) * 128, :], rs_[:], reads=[rk + "a", rk + "b", rk + "c"])


def launch_router(h2, I):
    wr = np.ascontiguousarray(np.concatenate([I["w_rg"][0], I["w_re"][0]], 1))
    br = np.ascontiguousarray(np.concatenate([I["b_rg"][0], I["b_re"][0]])[None, :])
    in_maps = [{"hT": np.ascontiguousarray(h2[i * 1024:(i + 1) * 1024].T), "wr": wr, "br": br} for i in range(8)]
    res = _run(lambda nc, p, st: build_router(nc, p, st, 8), in_maps)
    o = np.concatenate([r["o"] for r in res], axis=0)
    return o[:, 0:2].astype(np.int64), o[:, 2:4]


def build_experts(nc, p, st, cap):
    xT_d = nc.dram_tensor("xT", [8, D, cap], F32, kind="ExternalInput").ap()
    wg_d = nc.dram_tensor("wg", [8, D, 512], F32, kind="ExternalInput").ap()
    wu_d = nc.dram_tensor("wu", [8, D, 512], F32, kind="ExternalInput").ap()
    wd_d = nc.dram_tensor("wd", [8, 512, D], F32, kind="ExternalInput").ap()
    y_d = nc.dram_tensor("yT", [8, D, cap], F32, kind="ExternalOutput").ap()
    r32 = lambda ap: ap.bitcast(F32R)
    xT = _sb(nc, st, "xT_sb", [128, 16, cap])
    Wg = _sb(nc, st, "Wg_sb", [128, 16, 512])
    Wu = _sb(nc, st, "Wu_sb", [128, 16, 512])
    Wd = _sb(nc, st, "Wd_sb", [128, 4, D])
    hid = _sb(nc, st, "hid_sb", [128, 4, cap])
    sg = [_sb(nc, st, f"sg{i}", [128, cap]) for i in range(2)]
    yo = [_sb(nc, st, f"yo{i}", [128, cap]) for i in range(3)]
    ps = [_ps(nc, st, f"ps{i}") for i in range(8)]
    for ex in range(8):
        xv = xT_d[ex].rearrange("(c p) t -> p c t", p=128)
        for h in range(4):
            p.dma("pool", r32(xT[:, h * 4:(h + 1) * 4, :]), r32(xv[:, h * 4:(h + 1) * 4, :]), writes=[f"xT{h}"])
        gv = wg_d[ex].rearrange("(c p) n -> p c n", p=128)
        uv = wu_d[ex].rearrange("(c p) n -> p c n", p=128)
        dv = wd_d[ex].rearrange("(c p) n -> p c n", p=128)
        for h in range(8):
            p.dma("pool", r32(Wg[:, h * 2:(h + 1) * 2, :]), r32(gv[:, h * 2:(h + 1) * 2, :]), writes=[f"Wg{h}"])
        for h in range(8):
            p.dma("pool", r32(Wu[:, h * 2:(h + 1) * 2, :]), r32(uv[:, h * 2:(h + 1) * 2, :]), writes=[f"Wu{h}"])
        for h in range(4):
            p.dma("pool", r32(Wd[:, h, 0:1024]), r32(dv[:, h, 0:1024]), writes=[f"Wd{h}a"])
            p.dma("pool", r32(Wd[:, h, 1024:2048]), r32(dv[:, h, 1024:2048]), writes=[f"Wd{h}b"])
        for f in range(4):
            pg = ps[(f % 2) * 2]
            pu = ps[(f % 2) * 2 + 1]
            pgk = f"ps{(f % 2) * 2}"
            puk = f"ps{(f % 2) * 2 + 1}"
            fs = slice(f * 128, (f + 1) * 128)
            for c in range(16):
                p.op("pe", lambda e, c=c, pg=pg, fs=fs: e.matmul(pg[:, 0:cap], lhsT=r32(Wg[:, c, fs]), rhs=r32(xT[:, c, :]), start=(c == 0), stop=(c == 15)),
                     reads=[f"Wg{c // 2}", f"xT{c // 4}"], writes=[pgk])
            for c in range(16):
                p.op("pe", lambda e, c=c, pu=pu, fs=fs: e.matmul(pu[:, 0:cap], lhsT=r32(Wu[:, c, fs]), rhs=r32(xT[:, c, :]), start=(c == 0), stop=(c == 15)),
                     reads=[f"Wu{c // 2}", f"xT{c // 4}"], writes=[puk])
            s_ = sg[f % 2]
            p.op("act", lambda e, s_=s_, pg=pg: e.activation(out=s_[:], in_=pg[:, 0:cap], func=AF.Silu), reads=[pgk], writes=[f"sg{f % 2}"])
            p.op("dve", lambda e, s_=s_, pu=pu, f=f: e.tensor_tensor(out=r32(hid[:, f, :]), in0=pu[:, 0:cap], in1=s_[:], op=ALU.mult),
                 reads=[puk, f"sg{f % 2}"], writes=[f"hid{f}"])
        for d in range(16):
            py = ps[4 + d % 4]
            pyk = f"ps{4 + d % 4}"
            ds_ = slice(d * 128, (d + 1) * 128)
            for f in range(4):
                p.op("pe", lambda e, f=f, py=py, ds_=ds_: e.matmul(py[:, 0:cap], lhsT=r32(Wd[:, f, ds_]), rhs=r32(hid[:, f, :]), start=(f == 0), stop=(f == 3)),
                     reads=[f"Wd{f}a", f"Wd{f}b", f"hid{f}"], writes=[pyk])
            yb = yo[d % 3]
            p.op("act" if d % 2 else "dve", (lambda e, yb=yb, py=py: e.copy(out=yb[:], in_=py[:, 0:cap])) if d % 2 else (lambda e, yb=yb, py=py: e.tensor_copy(out=yb[:], in_=py[:, 0:cap])),
                 reads=[pyk], writes=[f"yo{d % 3}"])
            p.dma("sp", y_d[ex, ds_, :], yb[:], reads=[f"yo{d % 3}"])


def launch_experts(h2, eidx, I):
    N = h2.shape[0]
    flat_e = eidx.reshape(-1)
    flat_t = np.repeat(np.arange(N), 2)
    order = np.argsort(flat_e, kind="stable")
    counts = np.bincount(flat_e, minlength=64)
    cap = int(min(512, max(256, -(-counts.max() // 128) * 128)))
    nround = int(max(1, -(-counts.max() // cap)))
    starts = np.cumsum(counts) - counts
    xT = np.zeros((nround, 64, D, cap), np.float32)
    pos_of = np.zeros(2 * N, np.int64)
    rnd_of = np.zeros(2 * N, np.int64)
    for e in range(64):
        sl = order[starts[e]:starts[e] + counts[e]]
        pos = np.arange(counts[e])
        pos_of[sl] = pos % cap
        rnd_of[sl] = pos // cap
        for r in range(nround):
            sel = sl[(pos // cap) == r]
            xT[r, e, :, :len(sel)] = h2[flat_t[sel]].T
    yTs = []
    for r in range(nround):
        in_maps = [{"xT": np.ascontiguousarray(xT[r, g * 8:(g + 1) * 8]), "wg": np.ascontiguousarray(I["w_gate_e"][0][g * 8:(g + 1) * 8]),
                    "wu": np.ascontiguousarray(I["w_up_e"][0][g * 8:(g + 1) * 8]), "wd": np.ascontiguousarray(I["w_down_e"][0][g * 8:(g + 1) * 8])} for g in range(8)]
        res = _run(lambda nc, p, st: build_experts(nc, p, st, cap), in_maps)
        yTs.append(np.concatenate([r_["yT"] for r_ in res], axis=0))
    yT = np.stack(yTs, 0)
    yflat = yT[rnd_of, flat_e, :, pos_of]
    yflat = yflat.reshape(N, 2, D)
    return np.ascontiguousarray(yflat[:, 0]), np.ascontiguousarray(yflat[:, 1])


def build_combine(nc, p, st, ntile=8):
    n = ntile * 128
    ya_d = nc.dram_tensor("ya", [n, D], F32, kind="ExternalInput").ap()
    yb_d = nc.dram_tensor("yb", [n, D], F32, kind="ExternalInput").ap()
    w_d = nc.dram_tensor("w", [n, 2], F32, kind="ExternalInput").ap()
    g = nc.dram_tensor("g", [1, D], F32, kind="ExternalInput").ap()
    s = nc.dram_tensor("s", [1, D], F32, kind="ExternalInput").ap()
    base = nc.dram_tensor("base", [n, D], F32, kind="ExternalInput").ap()
    o = nc.dram_tensor("o", [n, D], F32, kind="ExternalOutput").ap()
    gb = _sb(nc, st, "gb", [128, D])
    A = _sb(nc, st, "A", [128, D])
    p.dma("sp", gb[:], g.partition_broadcast(128), writes=["gb"])
    p.dma("sp", A[:], s.partition_broadcast(128), writes=["A"])
    p.op("dve", lambda e: e.tensor_tensor(out=A[:], in0=A[:], in1=gb[:], op=ALU.mult), reads=["gb", "A"], writes=["A"])
    ya = [_sb(nc, st, f"ya{i}", [128, D]) for i in range(2)]
    yb = [_sb(nc, st, f"yb{i}", [128, D]) for i in range(2)]
    bt = [_sb(nc, st, f"bt{i}", [128, D]) for i in range(2)]
    wt = [_sb(nc, st, f"wt{i}", [128, 2]) for i in range(2)]
    junk = _sb(nc, st, "junk", [128, D])
    ot = [_sb(nc, st, f"ot{i}", [128, D]) for i in range(2)]
    ss = [_sb(nc, st, f"ss{i}", [128, 4]) for i in range(2)]
    for t in range(ntile):
        i = t % 2
        rows = slice(t * 128, (t + 1) * 128)
        p.dma("sp", ya[i][:], ya_d[rows, :], writes=[f"ya{i}"])
        p.dma("sp", yb[i][:], yb_d[rows, :], writes=[f"yb{i}"])
        p.dma("sp", bt[i][:], base[rows, :], writes=[f"bt{i}"])
        p.dma("sp", wt[i][:], w_d[rows, :], writes=[f"wt{i}"])
        p.op("dve", lambda e, i=i: e.tensor_scalar(out=ya[i][:], in0=ya[i][:], scalar1=wt[i][:, 0:1], scalar2=None, op0=ALU.mult),
             reads=[f"ya{i}", f"wt{i}"], writes=[f"ya{i}"])
        p.op("dve", lambda e, i=i: e.scalar_tensor_tensor(out=ya[i][:], in0=yb[i][:], scalar=wt[i][:, 1:2], in1=ya[i][:], op0=ALU.mult, op1=ALU.add),
             reads=[f"ya{i}", f"yb{i}", f"wt{i}"], writes=[f"ya{i}"])
        p.op("act", lambda e, i=i: e.activation(out=junk[:], in_=ya[i][:], func=AF.Square, accum_out=ss[i][:, 0:1]),
             reads=[f"ya{i}"], writes=["junk", f"ss{i}"])
        p.op("dve", lambda e, i=i: e.tensor_scalar(out=ss[i][:, 1:2], in0=ss[i][:, 0:1], scalar1=1.0 / D, scalar2=EPS, op0=ALU.mult, op1=ALU.add),
             reads=[f"ss{i}"], writes=[f"ss{i}"])
        p.op("act", lambda e, i=i: e.activation(out=ss[i][:, 2:3], in_=ss[i][:, 1:2], func=AF.Sqrt), reads=[f"ss{i}"], writes=[f"ss{i}"])
        p.op("dve", lambda e, i=i: e.reciprocal(out=ss[i][:, 3:4], in_=ss[i][:, 2:3]), reads=[f"ss{i}"], writes=[f"ss{i}"])
        p.op("dve", lambda e, i=i: e.scalar_tensor_tensor(out=ot[i][:], in0=ya[i][:], scalar=ss[i][:, 3:4], in1=A[:], op0=ALU.mult, op1=ALU.mult),
             reads=[f"ya{i}", f"ss{i}", "A"], writes=[f"ot{i}"], force=True)
        p.op("pool", lambda e, i=i: e.tensor_tensor(out=ot[i][:], in0=ot[i][:], in1=bt[i][:], op=ALU.add),
             reads=[f"ot{i}", f"bt{i}"], writes=[f"ot{i}"])
        p.dma("pool", o[rows, :], ot[i][:], reads=[f"ot{i}"])


def launch_combine(ya, yb, w, g, s, base):
    n = ya.shape[0] // 8
    in_maps = [{"ya": np.ascontiguousarray(ya[i * n:(i + 1) * n]), "yb": np.ascontiguousarray(yb[i * n:(i + 1) * n]),
                "w": np.ascontiguousarray(w[i * n:(i + 1) * n]), "g": np.ascontiguousarray(g[None, :]),
                "s": np.ascontiguousarray(s[None, :]), "base": np.ascontiguousarray(base[i * n:(i + 1) * n])} for i in range(8)]
    res = _run(lambda nc, p, st: build_combine(nc, p, st, n // 128), in_maps)
    return np.concatenate([r["o"] for r in res], axis=0)


def kernel(**inputs):
    I = {k: np.asarray(v) for k, v in inputs.items()}
    x = I["x"][0]
    ada = launch_ada(I["c"][0], I["w_ada"][0], I["b_ada"][0])
    sh1, sc1, gt1, sh2, sc2, gt2 = np.split(ada, 6)
    h1 = launch_norm(x, I["g_pre_mix"][0], sc1, 1.0, bv=sh1)
    lc = launch_inproj(h1, I)
    o_att = launch_moba(lc)
    o_rwkv = launch_rwkv_chunked(lc, I)
    y1 = launch_merge(h1, o_att, o_rwkv, I)
    x1 = launch_norm(y1, I["g_post_mix"][0], gt1, 0.0, base=x)
    h2 = launch_norm(x1, I["g_pre_ffn"][0], sc2, 1.0, bv=sh2)
    eidx, ew = launch_router(h2, I)
    ya, yb = launch_experts(h2, eidx, I)
    out = launch_combine(ya, yb, ew, I["g_post_ffn"][0], gt2, x1)
    return out[None].astype(np.float32)


CH_C = 64
SEG = 256
LOCK = 4


def build_rwkv_chunked(nc, p, st, T=S):
    nseg = T // SEG
    cps = SEG // CH_C
    F_d = nc.dram_tensor("F", [64, 6, 2, T], F32, kind="ExternalInput").ap()
    gb_d = nc.dram_tensor("gb", [64, 2, 2, T], F32, kind="ExternalInput").ap()
    gnv_d = nc.dram_tensor("gnv", [64, 2, 2], F32, kind="ExternalInput").ap()
    id_d = nc.dram_tensor("ident", [128, 128], F32, kind="ExternalInput").ap()
    msk_d = nc.dram_tensor("msk", [64, 10, 64], F32, kind="ExternalInput").ap()
    rm_d = nc.dram_tensor("rmask", [64, 2 * SEG], F32, kind="ExternalInput").ap()
    on_d = nc.dram_tensor("ones64", [64, 64], F32, kind="ExternalInput").ap()
    o_d = nc.dram_tensor("o", [64, 2, T], F32, kind="ExternalOutput").ap()

    ident = _sb(nc, st, "ident_sb", [128, 128])
    msk = _sb(nc, st, "msk_sb", [64, 10, 64])
    rmask = _sb(nc, st, "rmask_sb", [64, 2 * SEG])
    ones64 = _sb(nc, st, "ones64_sb", [64, 64])
    gnv = _sb(nc, st, "gnv_sb", [64, 2, 2])
    for dst, src, k in [(ident, id_d, "ident"), (msk, msk_d, "msk"),
                        (rmask, rm_d, "rmask"), (ones64, on_d, "ones64"), (gnv, gnv_d, "gnv")]:
        p.dma("sp", dst[:], src, writes=[k])
    Fin = [_sb(nc, st, f"Fin{i}", [64, 6, 2, SEG]) for i in range(2)]
    gbin = [_sb(nc, st, f"gbin{i}", [64, 2, 2, SEG]) for i in range(1)] * 2
    names = ["logw", "cum", "eg", "einv", "egm", "dte", "Af", "Bf", "Kf", "Rf", "Bh", "Kh", "Af32"]
    b16n = ("Af", "Bf", "Kf", "Rf")
    tmpn = ["logw", "cum", "eg", "einv", "egm", "dte"]
    Wtmp = {n: _sb(nc, st, f"wt_{n}", [64, 2, SEG]) for n in tmpn}
    W_ = []
    for i in range(2):
        d_ = dict(Wtmp)
        for n in names:
            if n not in tmpn:
                d_[n] = _sb(nc, st, f"w{i}_{n}", [64, 2, SEG], BF16 if n in b16n else F32)
        W_.append(d_)
    gC = [_sb(nc, st, f"gC{i}", [64, 2, cps]) for i in range(2)]
    NSL = 2 * LOCK
    TM = [_sb(nc, st, f"TM{i}", [64, 4, 128], BF16) for i in range(NSL)]
    MS = [_sb(nc, st, f"MS{i}", [64, 10, 64], BF16) for i in range(NSL)]
    MQ = [[_sb(nc, st, f"MQ{i}_{j}", [64, 4, 64], BF16) for j in range(2)] for i in range(NSL)]
    XW = [_sb(nc, st, f"XW{i}", [64, 2, 128], BF16) for i in range(NSL)]
    ident16 = _sb(nc, st, "ident16", [64, 64], BF16)
    p.op("dve", lambda e: e.tensor_copy(out=ident16[:], in_=ident[0:64, 0:64]), reads=["ident"], writes=["ident16"])
    NCH = 2 * LOCK + 2
    CHb = [_sb(nc, st, f"CHb{i}", [64, 4, 128]) for i in range(NCH)]
    Z = _sb(nc, st, "Zst", [64, 2, 64])
    Z2 = _sb(nc, st, "Zst2", [64, 2, 64])
    YT = [_sb(nc, st, f"YT{i}", [64, 2, SEG]) for i in range(2)]
    ps = [_ps(nc, st, f"ps{i}") for i in range(8)]
    psi = [0]

    def nb():
        i = psi[0] % 8
        psi[0] += 1
        return ps[i], f"ps{i}"

    p.op("dve", lambda e: e.memset(Z[:], 0.0), writes=["Z"])

    def load_seg(sg):
        i = sg % 2
        p.dma("sp", Fin[i][:], F_d[:, :, :, sg * SEG:(sg + 1) * SEG], writes=[f"Fin{i}"])

    def prep_seg(sg):
        i = sg % 2
        Fi = Fin[i]
        w = W_[i]
        fk = f"Fin{i}"
        k = lambda n: (f"wt_{n}" if n in tmpn else f"w{i}_{n}")
        fl = lambda ap: ap.rearrange("p h t -> p (h t)")
        p.op("act", lambda e: e.activation(out=fl(w["logw"][:]), in_=fl(Fi[:, 0]), func=AF.Ln), reads=[fk], writes=[k("logw")])
        p.op("dve", lambda e: e.tensor_tensor_scan(out=fl(w["cum"][:]), data0=rmask[:], data1=fl(w["logw"][:]), initial=0.0, op0=ALU.mult, op1=ALU.add),
             reads=["rmask", k("logw")], writes=[k("cum")])
        p.op("act", lambda e: e.activation(out=fl(w["eg"][:]), in_=fl(w["cum"][:]), func=AF.Exp), reads=[k("cum")], writes=[k("eg")])
        p.op("act", lambda e: e.activation(out=fl(w["einv"][:]), in_=fl(w["cum"][:]), func=AF.Exp, scale=-1.0), reads=[k("cum")], writes=[k("einv")])
        p.op("dve", lambda e: e.tensor_tensor(out=fl(w["egm"][:]), in0=fl(w["cum"][:]), in1=fl(w["logw"][:]), op=ALU.subtract), reads=[k("cum"), k("logw")], writes=[k("egm")])
        p.op("act", lambda e: e.activation(out=fl(w["egm"][:]), in_=fl(w["egm"][:]), func=AF.Exp), reads=[k("egm")], writes=[k("egm")])
        cumv = w["cum"][:].rearrange("p h (c t) -> p (h c) t", t=CH_C)
        p.op("dve", lambda e: e.tensor_tensor(out=w["dte"][:].rearrange("p h (c t) -> p (h c) t", t=CH_C), in0=cumv[:, :, CH_C - 1:CH_C].to_broadcast([64, 2 * cps, CH_C]), in1=cumv, op=ALU.subtract),
             reads=[k("cum")], writes=[k("dte")])
        p.op("act", lambda e: e.activation(out=fl(w["dte"][:]), in_=fl(w["dte"][:]), func=AF.Exp), reads=[k("dte")], writes=[k("dte")])
        for out_n, a_idx, b_n, eng in [("Af", 1, "egm", "dve"), ("Bf", 2, "einv", "pool"), ("Kf", 3, "einv", "dve"),
                                       ("Rf", 4, "eg", "pool"), ("Bh", 2, "dte", "dve"), ("Kh", 3, "dte", "pool"), ("Af32", 1, "egm", "pool")]:
            p.op(eng, lambda e, out_n=out_n, a_idx=a_idx, b_n=b_n: e.tensor_tensor(out=fl(w[out_n][:]), in0=fl(Fi[:, a_idx]), in1=fl(w[b_n][:]), op=ALU.mult),
                 reads=[fk, k(b_n)], writes=[k(out_n)])
        egC = w["eg"][:].rearrange("p h (c t) -> p h c t", t=CH_C)[:, :, :, CH_C - 1]
        p.op("act", lambda e: e.copy(out=gC[i][:], in_=egC), reads=[k("eg")], writes=[f"gC{i}"])

    def pre_stages(sg, cl, slot, chslot):
        i = sg % 2
        w = W_[i]
        Fi = Fin[i]
        k = lambda n: f"w{i}_{n}"
        cs = slice(cl * CH_C, (cl + 1) * CH_C)
        tm, ms, mq, xw, chb = TM[slot], MS[slot], MQ[slot], XW[slot], CHb[chslot]
        tmk, msk_, xwk, chk = f"TM{slot}", f"MS{slot}", f"XW{slot}", f"CHb{chslot}"
        stages = []

        def s1():
            b, bkey = nb()
            for q, (src, skey) in enumerate([(w["Af32"], k("Af32")), (w["Bh"], k("Bh")), (w["Kh"], k("Kh")), (None, f"Fin{i}")]):
                for h in range(2):
                    in_ap = Fi[:, 5, h, cs] if src is None else src[:, h, cs]
                    p.op("pe", lambda e, q=q, h=h, in_ap=in_ap: e.transpose(b[0:64, q * 128 + h * 64:q * 128 + (h + 1) * 64], in_ap, ident[0:64, 0:64]),
                         reads=[skey, "ident"], writes=[bkey])
            p.op("act", lambda e: e.copy(out=tm[:].rearrange("p a b -> p (a b)"), in_=b[0:64, :]), reads=[bkey], writes=[tmk])
        stages.append(s1)

        def s2a():
            b, bkey = nb()
            for h in range(2):
                pb = slice(h * 64, (h + 1) * 64)
                for col, (l, lk, r_, rk) in [(0 + h, (w["Bf"], k("Bf"), w["Af"], k("Af"))), (2 + h, (w["Kf"], k("Kf"), w["Af"], k("Af"))),
                                             (4 + h, (w["Af"], k("Af"), w["Bf"], k("Bf")))]:
                    p.op("pe", lambda e, col=col, l=l, r_=r_, h=h: e.matmul(b[0:64, col * 64:(col + 1) * 64], lhsT=l[:, h, cs], rhs=r_[:, h, cs], start=True, stop=True),
                         reads=[lk, rk], writes=[bkey])
            p.op("dve", lambda e: e.tensor_tensor(out=ms[:, 0:6, :].rearrange("p a b -> p (a b)"), in0=b[0:64, 0:384], in1=msk[:, 0:6, :].rearrange("p a b -> p (a b)"), op=ALU.mult),
                 reads=[bkey, "msk"], writes=[msk_ + "a"])
        stages.append(s2a)

        def s2b():
            b, bkey = nb()
            for h in range(2):
                pb = slice(h * 64, (h + 1) * 64)
                for col, (l, lk) in [(0 + h, (w["Bf"], k("Bf"))), (2 + h, (w["Kf"], k("Kf")))]:
                    p.op("pe", lambda e, col=col, l=l, h=h: e.matmul(b[0:64, col * 64:(col + 1) * 64], lhsT=l[:, h, cs], rhs=w["Rf"][:, h, cs], start=True, stop=True),
                         reads=[lk, k("Rf")], writes=[bkey])
            p.op("dve", lambda e: e.tensor_tensor(out=ms[:, 6:10, :].rearrange("p a b -> p (a b)"), in0=b[0:64, 0:256], in1=msk[:, 6:10, :].rearrange("p a b -> p (a b)"), op=ALU.mult),
                 reads=[bkey, "msk"], writes=[msk_ + "b"])
        stages.append(s2b)

        def s3():
            b, bkey = nb()
            for h in range(2):
                p.op("pe", lambda e, h=h: e.matmul(b[0:64, h * 64:(h + 1) * 64], lhsT=ms[:, 2 + h, :], rhs=tm[:, 3, h * 64:(h + 1) * 64], start=True, stop=True),
                     reads=[msk_ + "a", tmk], writes=[bkey])
            p.op("act", lambda e: e.copy(out=xw[:, :, 64:128], in_=b[0:64, 0:128].rearrange("p (h v) -> p h v", h=2)), reads=[bkey], writes=[xwk + "x"])
            p.op("act", lambda e: e.copy(out=xw[:, :, 0:64], in_=tm[:, 0, :].rearrange("p (h v) -> p h v", h=2)), reads=[tmk], writes=[xwk + "w"])
        stages.append(s3)

        def mk_level(j):
            def lv():
                if j == 0:
                    MT = [ms[:, 0, :], ms[:, 1, :]]
                    M = [ms[:, 4, :], ms[:, 5, :]]
                    mkey = msk_ + "a"
                else:
                    q = mq[j % 2]
                    MT = [q[:, 0, :], q[:, 1, :]]
                    M = [q[:, 2, :], q[:, 3, :]]
                    mkey = f"MQ{slot}_{j % 2}"
                b, bkey = nb()
                for h in range(2):
                    p.op("pe", lambda e, h=h: e.matmul(b[0:64, h * 128:(h + 1) * 128], lhsT=MT[h], rhs=xw[:, h, :], start=True, stop=True),
                         reads=[mkey, xwk + "x", xwk + "w"], writes=[bkey])
                if j < 5:
                    b2, b2key = nb()
                    for h in range(2):
                        p.op("pe", lambda e, h=h: e.matmul(b2[0:64, h * 64:(h + 1) * 64], lhsT=M[h], rhs=MT[h], start=True, stop=True), reads=[mkey], writes=[b2key])
                        p.op("pe", lambda e, h=h: e.matmul(b2[0:64, (2 + h) * 64:(3 + h) * 64], lhsT=MT[h], rhs=M[h], start=True, stop=True), reads=[mkey], writes=[b2key])
                p.op("dve", lambda e: e.tensor_tensor(out=xw[:].rearrange("p a b -> p (a b)"), in0=b[0:64, 0:256], in1=xw[:].rearrange("p a b -> p (a b)"), op=ALU.add),
                     reads=[bkey, xwk + "x", xwk + "w"], writes=[xwk + "x", xwk + "w"])
                if j < 5:
                    nq = mq[(j + 1) % 2]
                    p.op("act", lambda e: e.copy(out=nq[:].rearrange("p a b -> p (a b)"), in_=b2[0:64, 0:256]), reads=[b2key], writes=[f"MQ{slot}_{(j + 1) % 2}"])
            return lv
        for j in range(6):
            stages.append(mk_level(j))

        def s5():
            b, bkey = nb()
            xk = [xwk + "x", xwk + "w"]
            for h in range(2):
                pb = slice(h * 64, (h + 1) * 64)
                hs = slice(h * 64, (h + 1) * 64)
                Wh = xw[:, h, 0:64]
                Xh = xw[:, h, 64:128]
                p.op("pe", lambda e, Wh=Wh, hs=hs, h=h: e.matmul(b[0:64, h * 64:(h + 1) * 64], lhsT=Wh, rhs=tm[:, 1, hs], start=True, stop=True),
                     reads=xk + [tmk], writes=[bkey])
                p.op("pe", lambda e, Xh=Xh, hs=hs, h=h: e.matmul(b[0:64, 128 + h * 64:128 + (h + 1) * 64], lhsT=tm[:, 1, hs], rhs=Xh, start=True, stop=False),
                     reads=xk + [tmk], writes=[bkey])
                p.op("pe", lambda e, hs=hs, h=h: e.matmul(b[0:64, 128 + h * 64:128 + (h + 1) * 64], lhsT=tm[:, 2, hs], rhs=tm[:, 3, hs], start=False, stop=True),
                     reads=[tmk], writes=[bkey])
                p.op("pe", lambda e, Wh=Wh, h=h: e.matmul(b[0:64, 256 + h * 64:256 + (h + 1) * 64], lhsT=Wh, rhs=ms[:, 6 + h, :], start=True, stop=False),
                     reads=xk + [msk_ + "b"], writes=[bkey])
                p.op("pe", lambda e, h=h: e.matmul(b[0:64, 256 + h * 64:256 + (h + 1) * 64], lhsT=ident16[:], rhs=w["Rf"][:, h, cs], start=False, stop=True),
                     reads=["ident16", k("Rf")], writes=[bkey])
                p.op("pe", lambda e, Xh=Xh, h=h: e.matmul(b[0:64, 384 + h * 64:384 + (h + 1) * 64], lhsT=Xh, rhs=ms[:, 6 + h, :], start=True, stop=False),
                     reads=xk + [msk_ + "b"], writes=[bkey])
                p.op("pe", lambda e, hs=hs, h=h: e.matmul(b[0:64, 384 + h * 64:384 + (h + 1) * 64], lhsT=tm[:, 3, hs], rhs=ms[:, 8 + h, :], start=False, stop=True),
                     reads=[tmk, msk_ + "b"], writes=[bkey])
            p.op("act", lambda e: e.copy(out=chb[:].rearrange("p a b -> p (a b)"), in_=b[0:64, :]), reads=[bkey], writes=[chk])
        stages.append(s5)
        return stages

    def chain_step(sg, cl, chslot):
        i = sg % 2
        chb = CHb[chslot]
        chk = f"CHb{chslot}"
        yt = YT[i]
        b, bkey = nb()
        for h in range(2):
            hs = slice(h * 64, (h + 1) * 64)
            p.op("pe", lambda e, h=h, hs=hs: e.matmul(b[0:64, hs], lhsT=chb[:, 0, hs], rhs=Z[:, h, :], start=True, stop=True), reads=[chk, "Z"], writes=[bkey])
            p.op("pe", lambda e, h=h, hs=hs: e.matmul(b[0:64, 128 + h * 64:128 + (h + 1) * 64], lhsT=Z[:, h, :], rhs=chb[:, 2, hs], start=True, stop=True), reads=[chk, "Z"], writes=[bkey])
        p.op("dve", lambda e: e.tensor_tensor(out=yt[:, :, cl * CH_C:(cl + 1) * CH_C], in0=b[0:64, 128:256].rearrange("p (h t) -> p h t", h=2),
                                              in1=chb[:, 3, :].rearrange("p (h t) -> p h t", h=2), op=ALU.add),
             reads=[bkey, chk], writes=[f"YT{i}_{cl}"])
        for h in range(2):
            p.op("dve", lambda e, h=h: e.scalar_tensor_tensor(out=Z2[:, h, :], in0=Z[:, h, :], scalar=gC[i][:, h, cl:cl + 1], in1=b[0:64, h * 64:(h + 1) * 64], op0=ALU.mult, op1=ALU.add),
                 reads=["Z", f"gC{i}", bkey], writes=[f"Z2_{h}"])
        p.op("dve", lambda e: e.tensor_tensor(out=Z[:].rearrange("p a b -> p (a b)"), in0=Z2[:].rearrange("p a b -> p (a b)"), in1=chb[:, 1, :], op=ALU.add),
             reads=["Z2_0", "Z2_1", chk], writes=["Z"])

    yc = _sb(nc, st, "yc", [64, 2 * SEG])
    sq = _sb(nc, st, "sq", [64, 2 * SEG])
    rs = _sb(nc, st, "rs", [64, 2 * SEG])
    ot = [_sb(nc, st, f"ot{i}", [64, 2, SEG]) for i in range(1)] * 2

    def epilogue(sg):
        i = sg % 2
        yt = YT[i]
        ykeys = [f"YT{i}_{cl}" for cl in range(cps)]
        ytf = yt[:].rearrange("p h t -> p (h t)")
        p.dma("sp", gbin[0][:], gb_d[:, :, :, sg * SEG:(sg + 1) * SEG], writes=["gbin0"])
        for hh in range(2):
            b, bkey = nb()
            sl_ = slice(hh * SEG, (hh + 1) * SEG)
            p.op("pe", lambda e, sl_=sl_, b=b: e.matmul(b[0:64, 0:SEG], lhsT=ones64[:], rhs=ytf[:, sl_], start=True, stop=True), reads=["ones64"] + ykeys, writes=[bkey])
            p.op("dve", lambda e, sl_=sl_, b=b: e.scalar_tensor_tensor(out=yc[:, sl_], in0=b[0:64, 0:SEG], scalar=-1.0 / 64, in1=ytf[:, sl_], op0=ALU.mult, op1=ALU.add),
                 reads=[bkey] + ykeys, writes=[f"yc{hh}"])
            p.op("act", lambda e, sl_=sl_: e.activation(out=sq[:, sl_], in_=yc[:, sl_], func=AF.Square), reads=[f"yc{hh}"], writes=[f"sq{hh}"])
            b2, b2key = nb()
            p.op("pe", lambda e, sl_=sl_, b2=b2: e.matmul(b2[0:64, 0:SEG], lhsT=ones64[:], rhs=sq[:, sl_], start=True, stop=True), reads=["ones64", f"sq{hh}"], writes=[b2key])
            p.op("dve", lambda e, sl_=sl_, b2=b2: e.tensor_scalar(out=rs[:, sl_], in0=b2[0:64, 0:SEG], scalar1=1.0 / 64, scalar2=GN_EPS, op0=ALU.mult, op1=ALU.add), reads=[b2key], writes=[f"rs{hh}"])
            p.op("act", lambda e, sl_=sl_: e.activation(out=rs[:, sl_], in_=rs[:, sl_], func=AF.Sqrt), reads=[f"rs{hh}"], writes=[f"rs{hh}"])
            p.op("dve", lambda e, sl_=sl_: e.reciprocal(out=rs[:, sl_], in_=rs[:, sl_]), reads=[f"rs{hh}"], writes=[f"rs{hh}"])
            p.op("dve", lambda e, sl_=sl_: e.tensor_tensor(out=yc[:, sl_], in0=yc[:, sl_], in1=rs[:, sl_], op=ALU.mult), reads=[f"yc{hh}", f"rs{hh}"], writes=[f"yc{hh}"])
            p.op("dve", lambda e, sl_=sl_, hh=hh: e.tensor_scalar(out=yc[:, sl_], in0=yc[:, sl_], scalar1=gnv[:, hh, 0:1], scalar2=gnv[:, hh, 1:2], op0=ALU.mult, op1=ALU.add),
                 reads=[f"yc{hh}", "gnv"], writes=[f"yc{hh}"])
            p.op("pool", lambda e, sl_=sl_, hh=hh: e.tensor_tensor(out=yc[:, sl_], in0=yc[:, sl_], in1=gbin[0][:, 1, hh, :], op=ALU.add), reads=[f"yc{hh}", "gbin0"], writes=[f"yc{hh}"])
            p.op("pool", lambda e, sl_=sl_, hh=hh: e.tensor_tensor(out=ot[0][:, hh, :], in0=yc[:, sl_], in1=gbin[0][:, 0, hh, :], op=ALU.mult), reads=[f"yc{hh}", "gbin0"], writes=[f"ot0_{hh}"])
        p.dma("sp", o_d[:, :, sg * SEG:(sg + 1) * SEG], ot[0][:], reads=["ot0_0", "ot0_1"])

    load_seg(0)
    pending_chain = []
    slot_ctr = 0
    ch_ctr = 0
    for sg in range(nseg):
        if sg + 1 < nseg:
            load_seg(sg + 1)
        prep_seg(sg)
        for c0 in range(0, cps, LOCK):
            sts = []
            new_chain = []
            for gi in range(LOCK):
                sts.append(pre_stages(sg, c0 + gi, slot_ctr % NSL, ch_ctr % NCH))
                new_chain.append((sg, c0 + gi, ch_ctr % NCH))
                slot_ctr += 1
                ch_ctr += 1
            nst = len(sts[0])
            pop_at = set(range(1, nst, max(1, (nst - 1) // LOCK)))
            for si in range(nst):
                for gi in range(LOCK):
                    sts[gi][si]()
                if pending_chain and si in pop_at:
                    a = pending_chain.pop(0)
                    chain_step(*a)
                    if a[1] == cps - 1:
                        epilogue(a[0])
            pending_chain.extend(new_chain)
    while pending_chain:
        a = pending_chain.pop(0)
        chain_step(*a)
        if a[1] == cps - 1:
            epilogue(a[0])


def launch_rwkv_chunked(lc, I, T=S):
    C = CH_C
    su = np.triu(np.ones((C, C), np.float32), 1)
    sle = np.triu(np.ones((C, C), np.float32), 0)
    msk = np.stack([su, su, su, su, su.T, su.T, sle, sle, sle, sle], 0).transpose(1, 0, 2)
    ident = np.eye(128, dtype=np.float32)
    rmask = np.ones((64, 2 * SEG), np.float32)
    rmask[:, ::C] = 0
    ones64 = np.ones((64, 64), np.float32)
    in_maps = []
    for i in range(8):
        cq = slice(i * 128, (i + 1) * 128)
        F = np.stack([lc[i][n][:, :T].reshape(2, 64, T) for n in ("wdec", "nkk", "kka", "kt", "rT", "vrT")], 0)
        F = F.transpose(2, 0, 1, 3)
        gb = np.stack([lc[i]["g"][:, :T].reshape(2, 64, T), lc[i]["bonus"][:, :T].reshape(2, 64, T)], 0)
        gb = gb.transpose(2, 0, 1, 3)
        gnv = np.stack([I["gn_w"][0][cq].reshape(2, 64), I["gn_b"][0][cq].reshape(2, 64)], -1).transpose(1, 0, 2)
        in_maps.append({"F": np.ascontiguousarray(F), "gb": np.ascontiguousarray(gb), "gnv": np.ascontiguousarray(gnv),
                        "ident": ident, "msk": np.ascontiguousarray(msk), "rmask": rmask, "ones64": ones64})
    res = _run(lambda nc, p, st: build_rwkv_chunked(nc, p, st, T), in_maps)
    return np.concatenate([r["o"].transpose(2, 1, 0).reshape(T, 128) for r in res], axis=1)
```

```python
import numpy as np
import concourse.bass as bass
import concourse.mybir as mybir
from concourse.bass_utils import run_bass_kernel_spmd

F32 = mybir.dt.float32
F32R = mybir.dt.float32r
BF16 = mybir.dt.bfloat16
I32 = mybir.dt.int32
U32 = mybir.dt.uint32
AF = mybir.ActivationFunctionType
ALU = mybir.AluOpType
AX = mybir.AxisListType

NDMA_SLOTS = 6


class Prog:
    def __init__(self, nc):
        self.nc = nc
        self.ops = []
        self.last_w = {}
        self.readers = {}
        self.engs = {"pe": nc.tensor, "act": nc.scalar, "dve": nc.vector,
                     "pool": nc.gpsimd, "sp": nc.sync}

    def op(self, eng, fn, reads=(), writes=(), dma=False, force=False, inc=16):
        deps = set()
        raw = set()
        for k in reads:
            if k in self.last_w:
                deps.add(self.last_w[k])
                raw.add(self.last_w[k])
        for k in writes:
            if k in self.last_w:
                deps.add(self.last_w[k])
                raw.add(self.last_w[k])
            for r in self.readers.get(k, ()):
                deps.add(r)
        idx = len(self.ops)
        self.ops.append(dict(eng=eng, fn=fn, deps=deps, raw=raw, dma=dma, force=force, inc=inc))
        for k in reads:
            self.readers.setdefault(k, []).append(idx)
        for k in writes:
            self.last_w[k] = idx
            self.readers[k] = []
        return idx

    def dma(self, q, out, in_, reads=(), writes=(), **kw):
        return self.op(q, lambda e: e.dma_start(out=out, in_=in_, **kw), reads, writes, dma=True)

    def emit(self, stack):
        nc = self.nc
        ops = self.ops
        need = [False] * len(ops)
        for i, o in enumerate(ops):
            nd = set()
            for d in o["deps"]:
                od = ops[d]
                if od["dma"] or o["dma"] or o["force"] or od["eng"] != o["eng"] or (d in o["raw"] and o["eng"] != "pe"):
                    nd.add(d)
            o["xdeps"] = nd
            for d in nd:
                need[d] = True
        for i, o in enumerate(ops):
            if o["dma"]:
                need[i] = True
        esem = {e: stack.enter_context(nc.semaphore("es_" + e)) for e in self.engs}
        dsem = {e: [stack.enter_context(nc.semaphore(f"ds_{e}_{k}")) for k in range(NDMA_SLOTS)]
                for e in ("sp", "act", "pool")}
        ecount = {e: 0 for e in self.engs}
        dcount = {e: 0 for e in dsem}
        signal = [None] * len(ops)
        waited = {}
        nwaits = 0
        actions = {e: [] for e in self.engs}
        for i, o in enumerate(ops):
            e = o["eng"]
            wl = {}
            for d in o["xdeps"]:
                s_, v = signal[d]
                key = id(s_)
                if waited.get((e, key), 0) >= v:
                    continue
                if key not in wl or wl[key][1] < v:
                    wl[key] = (s_, v)
            if o["dma"]:
                j = dcount[e]
                slot = j % NDMA_SLOTS
                s_ = dsem[e][slot]
                prev = o.get("prev_total", None)
                prev = self._slot_total.get((e, slot), 0) if hasattr(self, "_slot_total") else 0
                if prev > 0 and waited.get((e, id(s_)), 0) < prev:
                    if id(s_) not in wl or wl[id(s_)][1] < prev:
                        wl[id(s_)] = (s_, prev)
            for key, (s_, v) in wl.items():
                waited[(e, key)] = v
                nwaits += 1
            sem = None
            inc = 0
            if o["dma"]:
                if not hasattr(self, "_slot_total"):
                    self._slot_total = {}
                j = dcount[e]
                dcount[e] += 1
                slot = j % NDMA_SLOTS
                sem = dsem[e][slot]
                inc = o.get("inc", 16)
                tot = self._slot_total.get((e, slot), 0) + inc
                self._slot_total[(e, slot)] = tot
                signal[i] = (sem, tot)
            elif need[i]:
                ecount[e] += 1
                sem = esem[e]
                inc = 1
                signal[i] = (sem, ecount[e])
            actions[e].append((list(wl.values()), o["fn"], sem, inc))
        finals = {e: [] for e in self.engs}
        for e in dsem:
            for slot in range(NDMA_SLOTS):
                tot = getattr(self, "_slot_total", {}).get((e, slot), 0)
                if tot > 0:
                    finals[e].append((dsem[e][slot], tot))
        bnames = {"pe": "tensor", "act": "scalar", "dve": "vector", "pool": "gpsimd", "sp": "sync"}
        with nc.Block() as block:
            for e in self.engs:
                if not actions[e] and not finals[e]:
                    continue

                def body(eng, e=e):
                    for waits, fn, sem, inc in actions[e]:
                        for s_, v in waits:
                            eng.wait_ge(s_, v)
                        inst = fn(eng)
                        if sem is not None:
                            inst.then_inc(sem, inc)
                    for s_, v in finals[e]:
                        eng.wait_ge(s_, v)
                getattr(block, bnames[e])(body)
        self.stats = dict(n_ops=len(ops), n_waits=nwaits, ecount=ecount, dcount=dcount)
        return self.stats


from contextlib import ExitStack

S = 8192
D = 2048
EPS = 1e-6
_TRACE = False


def _run(build, in_maps):
    nc = bass.Bass("TRN2", target_bir_lowering=False)
    with ExitStack() as st:
        p = Prog(nc)
        build(nc, p, st)
        p.emit(st)
    if _TRACE:
        r = run_bass_kernel_spmd(nc, in_maps, core_ids=list(range(8)), trace=True)
        print("EXEC_NS", getattr(build, "__name__", "?"), r.exec_time_ns, p.stats, flush=True)
    else:
        r = run_bass_kernel_spmd(nc, in_maps, core_ids=list(range(8)))
    return r.results


def _sb(nc, st, name, shape, dt=F32):
    return st.enter_context(nc.sbuf_tensor(name, shape, dt))


def _ps(nc, st, name, shape=(128, 512), dt=F32):
    return st.enter_context(nc.psum_tensor(name, list(shape), dt))


def build_ada(nc, p, st):
    w = nc.dram_tensor("w", [2048, 1536], F32, kind="ExternalInput").ap()
    c = nc.dram_tensor("c", [128, 16], F32, kind="ExternalInput").ap()
    b = nc.dram_tensor("b", [1, 1536], F32, kind="ExternalInput").ap()
    y = nc.dram_tensor("y", [1, 1536], F32, kind="ExternalOutput").ap()
    wt = [_sb(nc, st, f"wt{i}", [128, 1536]) for i in range(2)]
    ct = _sb(nc, st, "ct", [128, 16])
    bt = _sb(nc, st, "bt", [1, 1536])
    acc = _sb(nc, st, "acc", [128, 1536])
    ones = _sb(nc, st, "ones", [128, 1])
    res = _sb(nc, st, "res", [1, 1536])
    ps = [_ps(nc, st, f"ps{i}", (1, 512)) for i in range(2)]
    p.dma("sp", ct[:], c, writes=["ct"])
    p.dma("sp", bt[:], b, writes=["bt"])
    p.op("dve", lambda e: e.memset(ones[:], 1.0), writes=["ones"])
    for kc in range(16):
        i = kc % 2
        p.dma("sp", wt[i][:], w[kc * 128:(kc + 1) * 128, :], writes=[f"wt{i}"])
        if kc == 0:
            p.op("dve", lambda e, i=i, kc=kc: e.tensor_scalar(out=acc[:], in0=wt[i][:], scalar1=ct[:, kc:kc + 1], scalar2=None, op0=ALU.mult),
                 reads=[f"wt{i}", "ct"], writes=["acc"])
        else:
            p.op("dve", lambda e, i=i, kc=kc: e.scalar_tensor_tensor(out=acc[:], in0=wt[i][:], scalar=ct[:, kc:kc + 1], in1=acc[:], op0=ALU.mult, op1=ALU.add),
                 reads=[f"wt{i}", "ct", "acc"], writes=["acc"])
    for j in range(3):
        pj = ps[j % 2]
        p.op("pe", lambda e, j=j, pj=pj: e.matmul(pj[:], lhsT=ones[:], rhs=acc[:, j * 512:(j + 1) * 512], start=True, stop=True),
             reads=["acc", "ones"], writes=[f"ps{j%2}"])
        p.op("dve", lambda e, j=j, pj=pj: e.tensor_tensor(out=res[:, j * 512:(j + 1) * 512], in0=pj[:], in1=bt[:, j * 512:(j + 1) * 512], op=ALU.add),
             reads=[f"ps{j%2}", "bt"], writes=[f"res{j}"])
    p.dma("sp", y, res[:], reads=["res0", "res1", "res2"])


def launch_ada(c, w_ada, b_ada):
    in_maps = [{"w": np.ascontiguousarray(w_ada[:, i * 1536:(i + 1) * 1536]),
                "c": np.ascontiguousarray(c.reshape(16, 128).T),
                "b": np.ascontiguousarray(b_ada[None, i * 1536:(i + 1) * 1536])} for i in range(8)]
    res = _run(build_ada, in_maps)
    return np.concatenate([r["y"][0] for r in res])


def make_build_norm(add_one, has_b, has_base, ntile=8, second=False):
    def build(nc, p, st):
        n = ntile * 128
        y = nc.dram_tensor("y", [n, D], F32, kind="ExternalInput").ap()
        g = nc.dram_tensor("g", [1, D], F32, kind="ExternalInput").ap()
        s = nc.dram_tensor("s", [1, D], F32, kind="ExternalInput").ap()
        bv = nc.dram_tensor("bv", [1, D], F32, kind="ExternalInput").ap() if has_b else None
        base = nc.dram_tensor("base", [n, D], F32, kind="ExternalInput").ap() if has_base else None
        o = nc.dram_tensor("o", [n, D], F32, kind="ExternalOutput").ap()
        if second:
            g2 = nc.dram_tensor("g2", [1, D], F32, kind="ExternalInput").ap()
            s2 = nc.dram_tensor("s2", [1, D], F32, kind="ExternalInput").ap()
            bv2 = nc.dram_tensor("bv2", [1, D], F32, kind="ExternalInput").ap()
            o2 = nc.dram_tensor("o2", [n, D], F32, kind="ExternalOutput").ap()
            gb2 = _sb(nc, st, "gb2", [128, D])
            A2 = _sb(nc, st, "A2", [128, D])
            bb2 = _sb(nc, st, "bb2", [128, D])
            o2t = [_sb(nc, st, f"o2t{i}", [128, D]) for i in range(2)]
            ss2 = [_sb(nc, st, f"ss2_{i}", [128, 4]) for i in range(2)]
            p.dma("sp", gb2[:], g2.partition_broadcast(128), writes=["gb2"])
            p.dma("sp", A2[:], s2.partition_broadcast(128), writes=["A2"])
            p.dma("sp", bb2[:], bv2.partition_broadcast(128), writes=["bb2"])
            p.op("dve", lambda e: e.scalar_tensor_tensor(out=A2[:], in0=A2[:], scalar=1.0, in1=gb2[:], op0=ALU.add, op1=ALU.mult),
                 reads=["gb2", "A2"], writes=["A2"])
        gb = _sb(nc, st, "gb", [128, D])
        A = _sb(nc, st, "A", [128, D])
        bb = _sb(nc, st, "bb", [128, D]) if has_b else None
        p.dma("sp", gb[:], g.partition_broadcast(128), writes=["gb"])
        p.dma("sp", A[:], s.partition_broadcast(128), writes=["A"])
        if has_b:
            p.dma("sp", bb[:], bv.partition_broadcast(128), writes=["bb"])
        p.op("dve", lambda e: e.scalar_tensor_tensor(out=A[:], in0=A[:], scalar=float(add_one), in1=gb[:], op0=ALU.add, op1=ALU.mult),
             reads=["gb", "A"], writes=["A"])
        yt = [_sb(nc, st, f"yt{i}", [128, D]) for i in range(2)]
        bt = [_sb(nc, st, f"bt{i}", [128, D]) for i in range(2)] if has_base else None
        junk = _sb(nc, st, "junk", [128, D])
        ot = [_sb(nc, st, f"ot{i}", [128, D]) for i in range(2)]
        ss = [_sb(nc, st, f"ss{i}", [128, 4]) for i in range(2)]
        for t in range(ntile):
            i = t % 2
            rows = slice(t * 128, (t + 1) * 128)
            p.dma("sp", yt[i][:], y[rows, :], writes=[f"yt{i}"])
            if has_base:
                p.dma("sp", bt[i][:], base[rows, :], writes=[f"bt{i}"])
            p.op("act", lambda e, i=i: e.activation(out=junk[:], in_=yt[i][:], func=AF.Square, accum_out=ss[i][:, 0:1]),
                 reads=[f"yt{i}"], writes=["junk", f"ss{i}"])
            p.op("dve", lambda e, i=i: e.tensor_scalar(out=ss[i][:, 1:2], in0=ss[i][:, 0:1], scalar1=1.0 / D, scalar2=EPS, op0=ALU.mult, op1=ALU.add),
                 reads=[f"ss{i}"], writes=[f"ss{i}"])
            p.op("act", lambda e, i=i: e.activation(out=ss[i][:, 2:3], in_=ss[i][:, 1:2], func=AF.Sqrt),
                 reads=[f"ss{i}"], writes=[f"ss{i}"])
            p.op("dve", lambda e, i=i: e.reciprocal(out=ss[i][:, 3:4], in_=ss[i][:, 2:3]),
                 reads=[f"ss{i}"], writes=[f"ss{i}"])
            p.op("dve", lambda e, i=i: e.scalar_tensor_tensor(out=ot[i][:], in0=yt[i][:], scalar=ss[i][:, 3:4], in1=A[:], op0=ALU.mult, op1=ALU.mult),
                 reads=[f"yt{i}", f"ss{i}", "A"], writes=[f"ot{i}"], force=True)
            if has_b:
                p.op("pool", lambda e, i=i: e.tensor_tensor(out=ot[i][:], in0=ot[i][:], in1=bb[:], op=ALU.add),
                     reads=[f"ot{i}", "bb"], writes=[f"ot{i}"])
            if has_base:
                p.op("pool", lambda e, i=i: e.tensor_tensor(out=ot[i][:], in0=ot[i][:], in1=bt[i][:], op=ALU.add),
                     reads=[f"ot{i}", f"bt{i}"], writes=[f"ot{i}"])
            p.dma("pool", o[rows, :], ot[i][:], reads=[f"ot{i}"])
            if second:
                p.op("act", lambda e, i=i: e.activation(out=junk[:], in_=ot[i][:], func=AF.Square, accum_out=ss2[i][:, 0:1]),
                     reads=[f"ot{i}"], writes=["junk", f"ss2_{i}"])
                p.op("dve", lambda e, i=i: e.tensor_scalar(out=ss2[i][:, 1:2], in0=ss2[i][:, 0:1], scalar1=1.0 / D, scalar2=EPS, op0=ALU.mult, op1=ALU.add),
                     reads=[f"ss2_{i}"], writes=[f"ss2_{i}"])
                p.op("act", lambda e, i=i: e.activation(out=ss2[i][:, 2:3], in_=ss2[i][:, 1:2], func=AF.Sqrt),
                     reads=[f"ss2_{i}"], writes=[f"ss2_{i}"])
                p.op("dve", lambda e, i=i: e.reciprocal(out=ss2[i][:, 3:4], in_=ss2[i][:, 2:3]),
                     reads=[f"ss2_{i}"], writes=[f"ss2_{i}"])
                p.op("dve", lambda e, i=i: e.scalar_tensor_tensor(out=o2t[i][:], in0=ot[i][:], scalar=ss2[i][:, 3:4], in1=A2[:], op0=ALU.mult, op1=ALU.mult),
                     reads=[f"ot{i}", f"ss2_{i}", "A2"], writes=[f"o2t{i}"], force=True)
                p.op("pool", lambda e, i=i: e.tensor_tensor(out=o2t[i][:], in0=o2t[i][:], in1=bb2[:], op=ALU.add),
                     reads=[f"o2t{i}", "bb2"], writes=[f"o2t{i}"])
                p.dma("pool", o2[rows, :], o2t[i][:], reads=[f"o2t{i}"])
    return build


def launch_norm2(y, g, s, base, g2, s2, bv2):
    n = y.shape[0] // 8
    in_maps = []
    for i in range(8):
        in_maps.append({"y": np.ascontiguousarray(y[i * n:(i + 1) * n]), "g": np.ascontiguousarray(g[None, :]),
                        "s": np.ascontiguousarray(s[None, :]), "base": np.ascontiguousarray(base[i * n:(i + 1) * n]),
                        "g2": np.ascontiguousarray(g2[None, :]), "s2": np.ascontiguousarray(s2[None, :]),
                        "bv2": np.ascontiguousarray(bv2[None, :])})
    res = _run(make_build_norm(0.0, False, True, n // 128, second=True), in_maps)
    return np.concatenate([r["o"] for r in res], axis=0), np.concatenate([r["o2"] for r in res], axis=0)


def launch_norm(y, g, s, add_one, bv=None, base=None):
    n = y.shape[0] // 8
    in_maps = []
    for i in range(8):
        m = {"y": np.ascontiguousarray(y[i * n:(i + 1) * n]), "g": np.ascontiguousarray(g[None, :]),
             "s": np.ascontiguousarray(s[None, :])}
        if bv is not None:
            m["bv"] = np.ascontiguousarray(bv[None, :])
        if base is not None:
            m["base"] = np.ascontiguousarray(base[i * n:(i + 1) * n])
        in_maps.append(m)
    res = _run(make_build_norm(add_one, bv is not None, base is not None, n // 128), in_maps)
    return np.concatenate([r["o"] for r in res], axis=0)


LC_OUTS = ["qT", "kT", "vT", "rT", "krT", "vrT", "wdec", "nkk", "kka", "kt", "g", "bonus"]
WDECAY = 0.6065306597126334


def build_inproj(nc, p, st, ntt=16):
    T = ntt * 512
    hTp = nc.dram_tensor("hTp", [D, T + 1], F32, kind="ExternalInput").ap()
    ws_d = nc.dram_tensor("ws", [D, 448], F32, kind="ExternalInput").ap()
    wd_d = nc.dram_tensor("wd", [D, 384], F32, kind="ExternalInput").ap()
    mucol_d = nc.dram_tensor("mucol", [1, 384], F32, kind="ExternalInput").ap()
    mupp_d = nc.dram_tensor("mupp", [128, 3], F32, kind="ExternalInput").ap()
    wl_d = nc.dram_tensor("wl", [D, 448], F32, kind="ExternalInput").ap()
    murow_d = nc.dram_tensor("murow", [128, 16, 3], F32, kind="ExternalInput").ap()
    w2w_d = nc.dram_tensor("w2w", [96, 128], F32, kind="ExternalInput").ap()
    w2a_d = nc.dram_tensor("w2a", [96, 128], F32, kind="ExternalInput").ap()
    w2g_d = nc.dram_tensor("w2g", [128, 2, 128], F32, kind="ExternalInput").ap()
    vecs_d = nc.dram_tensor("vecs", [128, 5], F32, kind="ExternalInput").ap()
    cos_d = nc.dram_tensor("cos", [32, T], F32, kind="ExternalInput").ap()
    sin_d = nc.dram_tensor("sin", [32, T], F32, kind="ExternalInput").ap()
    blk_d = nc.dram_tensor("blk", [128, 128], F32, kind="ExternalInput").ap()
    outs = {n: nc.dram_tensor(n, [128, T], F32, kind="ExternalOutput").ap() for n in LC_OUTS}

    w0 = _sb(nc, st, "w0", [128, 16, 448])
    wa = _sb(nc, st, "wa", [128, 16, 448])
    wb = _sb(nc, st, "wb", [128, 16, 448])
    hb = [_sb(nc, st, f"hb{i}", [128, 16, 514]) for i in range(2)]
    mucol = _sb(nc, st, "mucol_sb", [128, 384])
    murow = _sb(nc, st, "murow_sb", [128, 16, 3])
    w2w = _sb(nc, st, "w2w_sb", [96, 128])
    w2a = _sb(nc, st, "w2a_sb", [96, 128])
    w2g = _sb(nc, st, "w2g_sb", [128, 2, 128])
    vecs = _sb(nc, st, "vecs_sb", [128, 5])
    blk = _sb(nc, st, "blk_sb", [128, 128])
    cs = [_sb(nc, st, f"cs{i}", [32, 2, 512]) for i in range(2)]
    mupp = _sb(nc, st, "mupp_sb", [128, 3])
    carry = _sb(nc, st, "carry_sb", [128, 3])
    Tb = [_sb(nc, st, f"Tb{i}", [128, 514]) for i in range(2)]
    ps = [_ps(nc, st, f"ps{i}") for i in range(8)]
    NOB = 6
    ob = [_sb(nc, st, f"ob{i}", [128, 512]) for i in range(NOB)]
    obi = [0]
    psi = [0]

    def nps():
        i = psi[0] % 8
        psi[0] += 1
        return ps[i], f"ps{i}"

    def nob():
        i = obi[0] % NOB
        obi[0] += 1
        return ob[i], f"ob{i}"

    hview = hTp.rearrange("(c p) t -> p c t", p=128)
    r32 = lambda ap: ap.bitcast(F32R)

    for dst, src, k in [(mucol[:], mucol_d.partition_broadcast(128), "mucol"), (murow[:], murow_d, "murow"),
                        (w2w[:], w2w_d, "w2w"), (w2a[:], w2a_d, "w2a"), (w2g[:], w2g_d, "w2g"),
                        (vecs[:], vecs_d, "vecs"), (blk[:], blk_d, "blk"), (mupp[:], mupp_d, "mupp")]:
        p.dma("sp", dst, src, writes=[k])

    def load_h(tt):
        i = tt % 2
        p.dma("pool", r32(hb[i][:, :, 0:513]), r32(hview[:, :, tt * 512:tt * 512 + 513]), writes=[f"hb{i}"])

    def gemm(tt, kind, co, M, pst, psk):
        i = tt % 2
        for c in range(16):
            cur = hb[i][:, c, 1:513]
            prev = hb[i][:, c, 0:512]
            if kind == "singleA":
                p.op("pe", lambda e, c=c, cur=cur: e.matmul(pst[:M, :], lhsT=r32(wa[:, c, co:co + M]), rhs=r32(cur), start=(c == 0), stop=(c == 15)),
                     reads=["wa", f"hb{i}"], writes=[psk])
            elif kind == "single":
                p.op("pe", lambda e, c=c, cur=cur: e.matmul(pst[:M, :], lhsT=r32(w0[:, c, co:co + M]), rhs=r32(cur), start=(c == 0), stop=(c == 15)),
                     reads=["w0", f"hb{i}"], writes=[psk])
            else:
                p.op("pe", lambda e, c=c, cur=cur: e.matmul(pst[:M, :], lhsT=r32(wa[:, c, co:co + M]), rhs=r32(cur), start=(c == 0), stop=False),
                     reads=["wa", f"hb{i}"], writes=[psk])
                p.op("pe", lambda e, c=c, prev=prev: e.matmul(pst[:M, :], lhsT=r32(wb[:, c, co:co + M]), rhs=r32(prev), start=False, stop=(c == 15)),
                     reads=["wb", f"hb{i}"], writes=[psk])

    p.dma("pool", r32(w0[:]), r32(ws_d.rearrange("(c p) n -> p c n", p=128)), writes=["w0"])
    p.dma("pool", r32(wa[:, :, 0:384]), r32(wd_d.rearrange("(c p) n -> p c n", p=128)), writes=["wa"])
    p.op("dve", lambda e: e.memset(carry[:], 0.0), writes=["carry0", "carry1", "carry2"])
    load_h(0)
    for tt in range(ntt):
        if tt + 1 < ntt:
            load_h(tt + 1)
        tsl = slice(tt * 512, (tt + 1) * 512)
        ci = tt % 2
        p.dma("sp", cs[ci][:, 0, :], cos_d[:, tsl], writes=[f"cs{ci}"])
        p.dma("sp", cs[ci][:, 1, :], sin_d[:, tsl], writes=[f"cs{ci}"])
        for name, co in (("qT", 0), ("kT", 160)):
            pq, pqk = nps()
            gemm(tt, "single", co, 128, pq, pqk)
            psw, pswk = nps()
            gemm(tt, "single", co + 128, 32, psw, pswk)
            o, ok = nob()
            p.op("act", lambda e, o=o, pq=pq: e.copy(out=o[:], in_=pq[:]), reads=[pqk], writes=[ok, ok + "hi"])
            t1, t1k = nob()
            p.op("dve", lambda e, t1=t1, psw=psw, ci=ci: e.tensor_tensor(out=t1[0:32, :], in0=psw[0:32, :], in1=cs[ci][:, 1, :], op=ALU.mult),
                 reads=[pswk, f"cs{ci}"], writes=[t1k])
            p.op("dve", lambda e, o=o, pq=pq, ci=ci: e.tensor_tensor(out=o[0:32, :], in0=pq[0:32, :], in1=cs[ci][:, 0, :], op=ALU.mult),
                 reads=[pqk, f"cs{ci}"], writes=[ok])
            p.op("dve", lambda e, o=o, t1=t1: e.tensor_tensor(out=o[0:32, :], in0=o[0:32, :], in1=t1[0:32, :], op=ALU.add),
                 reads=[ok, t1k], writes=[ok])
            p.dma("pool", outs[name][:, tsl], o[:], reads=[ok, ok + "hi"], writes=[f"d_{name}_{tt}"])
        pv, pvk = nps()
        gemm(tt, "single", 320, 128, pv, pvk)
        o, ok = nob()
        p.op("act", lambda e, o=o, pv=pv: e.copy(out=o[:], in_=pv[:]), reads=[pvk], writes=[ok, ok + "hi"])
        p.dma("pool", outs["vT"][:, tsl], o[:], reads=[ok, ok + "hi"], writes=[f"d_vT_{tt}"])
        for j, name in enumerate(("rT", "krT", "vrT")):
            pr, prk = nps()
            gemm(tt, "singleA", j * 128, 128, pr, prk)
            tb = Tb[(tt * 3 + j) % 2]
            tbk = f"Tb{(tt * 3 + j) % 2}"
            p.op("act", lambda e, tb=tb, j=j: e.copy(out=tb[:, 0:1], in_=carry[:, j:j + 1]), reads=[f"carry{j}"], writes=[tbk + "c"])
            p.op("act", lambda e, tb=tb, pr=pr: e.copy(out=tb[:, 1:513], in_=pr[:]), reads=[prk], writes=[tbk])
            p.op("act", lambda e, tb=tb, j=j: e.copy(out=carry[:, j:j + 1], in_=tb[:, 512:513]), reads=[tbk], writes=[f"carry{j}"])
            d_, dk = nob()
            p.op("dve", lambda e, tb=tb, d_=d_: e.tensor_tensor(out=d_[:], in0=tb[:, 0:512], in1=tb[:, 1:513], op=ALU.subtract), reads=[tbk, tbk + "c"], writes=[dk, dk + "hi"])
            o, ok = nob()
            p.op("dve", lambda e, tb=tb, d_=d_, o=o, j=j: e.scalar_tensor_tensor(out=o[:], in0=d_[:], scalar=mupp[:, j:j + 1], in1=tb[:, 1:513], op0=ALU.mult, op1=ALU.add),
                 reads=[dk, dk + "hi", tbk, "mupp"], writes=[ok, ok + "hi"])
            p.dma("pool", outs[name][:, tsl], o[:], reads=[ok, ok + "hi"], writes=[f"d_{name}_{tt}"])

    p.dma("pool", r32(w0[:]), r32(wl_d.rearrange("(c p) n -> p c n", p=128)), writes=["w0"])
    for c in range(16):
        for j, (lo, hi) in enumerate(((0, 96), (96, 192), (192, 448))):
            p.op("dve", lambda e, c=c, j=j, lo=lo, hi=hi: e.tensor_scalar(out=r32(wb[:, c, lo:hi]), in0=w0[:, c, lo:hi], scalar1=murow[:, c, j:j + 1], scalar2=None, op0=ALU.mult),
                 reads=["w0", "murow"], writes=["wb"])
    p.op("dve", lambda e: e.tensor_tensor(out=r32(wa[:]), in0=w0[:], in1=wb[:], op=ALU.subtract),
         reads=["w0", "wb"], writes=["wa"])
    tw = _sb(nc, st, "tw", [96, 512])
    ta = _sb(nc, st, "ta", [96, 512])
    tg = _sb(nc, st, "tg", [128, 2, 512])
    rin = [_sb(nc, st, f"rin{i}", [128, 3, 512]) for i in range(1)]
    tmp = {n: _sb(nc, st, "tmp_" + n, [128, 512]) for n in ["a", "kkr", "sq", "nrm", "rn", "u", "rk"]}
    load_h(0)
    for tt in range(ntt):
        if tt + 1 < ntt:
            load_h(tt + 1)
        tsl = slice(tt * 512, (tt + 1) * 512)
        ri = 0
        for j, name in enumerate(("rT", "krT", "vrT")):
            p.dma("sp", rin[ri][:, j, :], outs[name][:, tsl], reads=[f"d_{name}_{tt}"], writes=[f"rin{ri}_{j}"])
        R_, KR, VR = rin[ri][:, 0, :], rin[ri][:, 1, :], rin[ri][:, 2, :]
        rk_, krk, vrk = f"rin{ri}_0", f"rin{ri}_1", f"rin{ri}_2"
        pw, pwk = nps()
        gemm(tt, "dual", 0, 96, pw, pwk)
        p.op("act", lambda e, pw=pw: e.activation(out=tw[:], in_=pw[:96, :], func=AF.Tanh), reads=[pwk], writes=["tw"])
        pa, pak = nps()
        gemm(tt, "dual", 96, 96, pa, pak)
        p.op("act", lambda e, pa=pa: e.copy(out=ta[:], in_=pa[:96, :]), reads=[pak], writes=["ta"])
        for h in range(2):
            pg, pgk = nps()
            gemm(tt, "dual", 192 + h * 128, 128, pg, pgk)
            p.op("act", lambda e, pg=pg, h=h: e.activation(out=tg[:, h, :], in_=pg[:], func=AF.Sigmoid), reads=[pgk], writes=[f"tg{h}"])
        pd, pdk = nps()
        p.op("pe", lambda e, pd=pd: e.matmul(pd[:], lhsT=w2w[:], rhs=tw[:], start=True, stop=True), reads=["w2w", "tw"], writes=[pdk])
        o_w, o_wk = nob()
        p.op("act", lambda e, pd=pd: e.activation(out=tmp["sq"][:], in_=pd[:], func=AF.Sigmoid, bias=vecs[:, 0:1]), reads=[pdk, "vecs"], writes=["t_sq"])
        p.op("act", lambda e, o_w=o_w: e.activation(out=o_w[:], in_=tmp["sq"][:], func=AF.Exp, scale=-WDECAY), reads=["t_sq"], writes=[o_wk])
        p.dma("pool", outs["wdec"][:, tsl], o_w[:], reads=[o_wk])
        pa2, pa2k = nps()
        p.op("pe", lambda e, pa2=pa2: e.matmul(pa2[:], lhsT=w2a[:], rhs=ta[:], start=True, stop=True), reads=["w2a", "ta"], writes=[pa2k])
        p.op("act", lambda e, pa2=pa2: e.activation(out=tmp["a"][:], in_=pa2[:], func=AF.Sigmoid, bias=vecs[:, 1:2]), reads=[pa2k, "vecs"], writes=["t_a"])
        pg2, pg2k = nps()
        for h in range(2):
            p.op("pe", lambda e, pg2=pg2, h=h: e.matmul(pg2[:], lhsT=w2g[:, h, :], rhs=tg[:, h, :], start=(h == 0), stop=(h == 1)),
                 reads=["w2g", f"tg{h}"], writes=[pg2k])
        o_g, o_gk = nob()
        p.op("act", lambda e, o_g=o_g, pg2=pg2: e.copy(out=o_g[:], in_=pg2[:]), reads=[pg2k], writes=[o_gk])
        p.dma("pool", outs["g"][:, tsl], o_g[:], reads=[o_gk])
        p.op("dve", lambda e, KR=KR: e.tensor_scalar(out=tmp["kkr"][:], in0=KR, scalar1=vecs[:, 2:3], scalar2=None, op0=ALU.mult),
             reads=[krk, "vecs"], writes=["t_kkr"])
        p.op("pool", lambda e: e.tensor_tensor(out=tmp["sq"][:], in0=tmp["kkr"][:], in1=tmp["kkr"][:], op=ALU.mult),
             reads=["t_kkr", "t_sq"], writes=["t_sq"])
        pn, pnk = nps()
        p.op("pe", lambda e, pn=pn: e.matmul(pn[:], lhsT=blk[:], rhs=tmp["sq"][:], start=True, stop=True), reads=["blk", "t_sq"], writes=[pnk])
        p.op("act", lambda e, pn=pn: e.activation(out=tmp["nrm"][:], in_=pn[:], func=AF.Sqrt), reads=[pnk], writes=["t_nrm"])
        p.op("dve", lambda e: e.tensor_scalar(out=tmp["nrm"][:], in0=tmp["nrm"][:], scalar1=1e-12, scalar2=None, op0=ALU.max),
             reads=["t_nrm"], writes=["t_nrm"])
        p.op("dve", lambda e: e.reciprocal(out=tmp["rn"][:], in_=tmp["nrm"][:]), reads=["t_nrm"], writes=["t_rn"])
        o_n, o_nk = nob()
        p.op("dve", lambda e, o_n=o_n: e.scalar_tensor_tensor(out=o_n[:], in0=tmp["kkr"][:], scalar=-1.0, in1=tmp["rn"][:], op0=ALU.mult, op1=ALU.mult),
             reads=["t_kkr", "t_rn"], writes=[o_nk])
        p.dma("pool", outs["nkk"][:, tsl], o_n[:], reads=[o_nk])
        o_ka, o_kak = nob()
        p.op("dve", lambda e, o_n=o_n, o_ka=o_ka: e.scalar_tensor_tensor(out=o_ka[:], in0=o_n[:], scalar=-1.0, in1=tmp["a"][:], op0=ALU.mult, op1=ALU.mult),
             reads=[o_nk, "t_a"], writes=[o_kak])
        p.dma("pool", outs["kka"][:, tsl], o_ka[:], reads=[o_kak])
        p.op("dve", lambda e: e.tensor_scalar(out=tmp["u"][:], in0=tmp["a"][:], scalar1=-1.0, scalar2=vecs[:, 3:4], op0=ALU.add, op1=ALU.mult),
             reads=["t_a", "vecs"], writes=["t_u"])
        o_kt, o_ktk = nob()
        p.op("dve", lambda e, o_kt=o_kt, KR=KR: e.scalar_tensor_tensor(out=o_kt[:], in0=tmp["u"][:], scalar=1.0, in1=KR, op0=ALU.add, op1=ALU.mult),
             reads=["t_u", krk], writes=[o_ktk])
        p.dma("pool", outs["kt"][:, tsl], o_kt[:], reads=[o_ktk])
        p.op("dve", lambda e, o_kt=o_kt, R_=R_: e.scalar_tensor_tensor(out=tmp["rk"][:], in0=R_, scalar=vecs[:, 4:5], in1=o_kt[:], op0=ALU.mult, op1=ALU.mult),
             reads=[rk_, "vecs", o_ktk], writes=["t_rk"])
        pb, pbk = nps()
        p.op("pe", lambda e, pb=pb: e.matmul(pb[:], lhsT=blk[:], rhs=tmp["rk"][:], start=True, stop=True), reads=["blk", "t_rk"], writes=[pbk])
        o_b, o_bk = nob()
        p.op("dve", lambda e, o_b=o_b, pb=pb, VR=VR: e.tensor_tensor(out=o_b[:], in0=pb[:], in1=VR, op=ALU.mult),
             reads=[pbk, vrk], writes=[o_bk])
        p.dma("pool", outs["bonus"][:, tsl], o_b[:], reads=[o_bk])


def _rope_tables(T):
    half = 16
    inv = (500000.0 ** (-np.arange(half, dtype=np.float32) / half)).astype(np.float32)
    ang = np.arange(T, dtype=np.float32)[:, None] * inv[None, :]
    cos = np.cos(ang).astype(np.float32).T
    sin = np.sin(ang).astype(np.float32).T
    COS = np.concatenate([cos, cos], 0)
    SIN = np.concatenate([-sin, sin], 0)
    return np.ascontiguousarray(COS), np.ascontiguousarray(SIN)


def launch_inproj(h, I):
    T = h.shape[0]
    hTp = np.zeros((D, T + 1), np.float32)
    hTp[:, 1:] = h.T
    w_in = I["w_in"][0]
    COS, SIN = _rope_tables(T)
    swp = np.concatenate([np.arange(16, 32), np.arange(0, 16)])
    blk = np.zeros((128, 128), np.float32)
    blk[:64, :64] = 1
    blk[64:, 64:] = 1
    murow = np.stack([I["mu_w"][0], I["mu_a"][0], I["mu_g"][0]], -1).reshape(16, 128, 3).transpose(1, 0, 2)
    wl = np.concatenate([I["w_w1"][0], I["w_a1"][0], I["w_g1"][0]], 1)
    in_maps = []
    for i in range(8):
        cq = slice(i * 128, (i + 1) * 128)
        q = w_in[:, 0:1024][:, cq]
        k = w_in[:, 1024:2048][:, cq]
        v = w_in[:, 2048:3072][:, cq]
        ws = np.concatenate([q, q[:, swp], k, k[:, swp], v], 1)
        r = w_in[:, 3072:4096][:, cq]
        kr = w_in[:, 4096:5120][:, cq]
        vr = w_in[:, 5120:6144][:, cq]
        wd = np.concatenate([r, kr, vr], 1)
        mucol = np.concatenate([I["mu_r"][0][cq], I["mu_k"][0][cq], I["mu_v"][0][cq]])[None, :]
        vecs = np.stack([I["w0"][0][cq], I["a0"][0][cq], I["k_k"][0][cq], I["k_a"][0][cq], I["r_k"][0].reshape(-1)[cq]], -1)
        mupp = np.stack([I["mu_r"][0][cq], I["mu_k"][0][cq], I["mu_v"][0][cq]], -1)
        in_maps.append({
            "hTp": hTp, "ws": np.ascontiguousarray(ws), "wd": np.ascontiguousarray(wd), "mucol": np.ascontiguousarray(mucol),
            "wl": np.ascontiguousarray(wl), "murow": np.ascontiguousarray(murow),
            "w2w": np.ascontiguousarray(I["w_w2"][0][:, cq]), "w2a": np.ascontiguousarray(I["w_a2"][0][:, cq]),
            "w2g": np.ascontiguousarray(I["w_g2"][0][:, cq].reshape(2, 128, 128).transpose(1, 0, 2)),
            "vecs": np.ascontiguousarray(vecs), "cos": COS, "sin": SIN, "blk": blk, "mupp": np.ascontiguousarray(mupp)})
    ntt = T // 512
    res = _run(lambda nc, p, st: build_inproj(nc, p, st, ntt), in_maps)
    return res


GN_EPS = 64e-5
TCH = 32


def build_rwkv(nc, p, st, T=S):
    nch = T // TCH
    bcin_d = nc.dram_tensor("bcin", [2, nch, 5, TCH, 64], F32, kind="ExternalInput").ap()
    vT_d = nc.dram_tensor("vT", [128, T], F32, kind="ExternalInput").ap()
    g_d = nc.dram_tensor("g", [128, T], F32, kind="ExternalInput").ap()
    bonus_d = nc.dram_tensor("bonus", [128, T], F32, kind="ExternalInput").ap()
    gnv_d = nc.dram_tensor("gnv", [128, 2], F32, kind="ExternalInput").ap()
    sel_d = nc.dram_tensor("sel", [128, 128], F32, kind="ExternalInput").ap()
    blk_d = nc.dram_tensor("blk", [128, 128], F32, kind="ExternalInput").ap()
    o_d = nc.dram_tensor("o", [128, T], F32, kind="ExternalOutput").ap()
    r32 = lambda ap: ap.bitcast(F32R)

    vT = _sb(nc, st, "vT_sb", [128, T])
    yT = _sb(nc, st, "yT_sb", [128, T])
    Sst = _sb(nc, st, "S_sb", [128, 64])
    junk = _sb(nc, st, "junk", [128, 64])
    sa = _sb(nc, st, "sa", [128, 1])
    sel = _sb(nc, st, "sel_sb", [128, 128])
    blk = _sb(nc, st, "blk_sb", [128, 128])
    gnv = _sb(nc, st, "gnv_sb", [128, 2])
    bc = [_sb(nc, st, f"bc{i}", [128, 5, TCH, 64]) for i in range(2)]
    ps = [_ps(nc, st, f"ps{i}") for i in range(8)]
    p.dma("pool", r32(sel[:]), r32(sel_d), writes=["sel"])
    p.dma("sp", blk[:], blk_d, writes=["blk"])
    p.dma("sp", gnv[:], gnv_d, writes=["gnv"])
    p.dma("sp", vT[:], vT_d, writes=["vT"])
    p.op("dve", lambda e: e.memset(Sst[:], 0.0), writes=["S"])
    zer_d = nc.dram_tensor("zer", [126, 5, TCH, 64], F32, kind="ExternalInput").ap()
    for i in range(2):
        p.dma("pool", r32(bc[i][2:128]), r32(zer_d), writes=[f"bc{i}"])

    def load_bc(c):
        i = c % 2
        p.dma("pool", r32(bc[i][0:2]), r32(bcin_d[:, c]), writes=[f"bc{i}"])

    load_bc(0)
    grp = 0
    for c in range(nch):
        if c + 1 < nch:
            load_bc(c + 1)
        bi = c % 2
        for g4 in range(TCH // 4):
            base = (grp % 2) * 3
            grp += 1
            views = []
            for j in range(5):
                bank = ps[base + j // 2]
                bk = f"ps{base + j // 2}"
                half = bank[:, (j % 2) * 256:(j % 2) * 256 + 256]
                p.op("pe", lambda e, half=half, j=j, g4=g4, bi=bi: e.matmul(half, lhsT=r32(sel[:]), rhs=r32(bc[bi][:, j, g4 * 4:(g4 + 1) * 4, :]), start=True, stop=True),
                     reads=["sel", f"bc{bi}"], writes=[bk + f"h{j%2}"])
                views.append((half, bk + f"h{j%2}"))
            for tl in range(4):
                t = c * TCH + g4 * 4 + tl
                cs = slice(tl * 64, (tl + 1) * 64)
                wv, nv, kav, ktv, rv = [(v[0][:, cs], v[1]) for v in views]
                p.op("dve", lambda e, nv=nv: e.scalar_tensor_tensor(out=junk[:], in0=Sst[:], scalar=1.0, in1=nv[0], op0=ALU.mult, op1=ALU.mult, accum_out=sa[:]),
                     reads=["S", nv[1]], writes=["junk", "sa"])
                p.op("dve", lambda e, wv=wv: e.tensor_tensor(out=Sst[:], in0=Sst[:], in1=wv[0], op=ALU.mult),
                     reads=["S", wv[1]], writes=["S"])
                p.op("dve", lambda e, kav=kav: e.scalar_tensor_tensor(out=Sst[:], in0=kav[0], scalar=sa[:, 0:1], in1=Sst[:], op0=ALU.mult, op1=ALU.add),
                     reads=["S", "sa", kav[1]], writes=["S"], force=True)
                p.op("dve", lambda e, ktv=ktv, t=t: e.scalar_tensor_tensor(out=Sst[:], in0=ktv[0], scalar=vT[:, t:t + 1], in1=Sst[:], op0=ALU.mult, op1=ALU.add),
                     reads=["S", "vT", ktv[1]], writes=["S"])
                p.op("dve", lambda e, rv=rv, t=t: e.scalar_tensor_tensor(out=junk[:], in0=Sst[:], scalar=1.0, in1=rv[0], op0=ALU.mult, op1=ALU.mult, accum_out=yT[:, t:t + 1]),
                     reads=["S", rv[1]], writes=["junk", f"yT{t // 512}"])
    gt = [_sb(nc, st, f"g_sb{i}", [128, 512]) for i in range(2)]
    bt = [_sb(nc, st, f"b_sb{i}", [128, 512]) for i in range(2)]
    yc = _sb(nc, st, "yc", [128, 512])
    sq = _sb(nc, st, "sq", [128, 512])
    rs = _sb(nc, st, "rs", [128, 512])
    ot = [_sb(nc, st, f"ot{i}", [128, 512]) for i in range(2)]
    for tt in range(T // 512):
        i = tt % 2
        tsl = slice(tt * 512, (tt + 1) * 512)
        p.dma("sp", gt[i][:], g_d[:, tsl], writes=[f"gt{i}"])
        p.dma("sp", bt[i][:], bonus_d[:, tsl], writes=[f"bt{i}"])
        pm, pmk = ps[6], "ps6"
        p.op("pe", lambda e, tsl=tsl: e.matmul(ps[6][:], lhsT=blk[:], rhs=yT[:, tsl], start=True, stop=True), reads=["blk", f"yT{tt}"], writes=["ps6"])
        p.op("dve", lambda e, tsl=tsl: e.scalar_tensor_tensor(out=yc[:], in0=ps[6][:], scalar=-1.0 / 64, in1=yT[:, tsl], op0=ALU.mult, op1=ALU.add),
             reads=["ps6", f"yT{tt}"], writes=["yc"])
        p.op("act", lambda e: e.activation(out=sq[:], in_=yc[:], func=AF.Square), reads=["yc"], writes=["sq"])
        p.op("pe", lambda e: e.matmul(ps[7][:], lhsT=blk[:], rhs=sq[:], start=True, stop=True), reads=["blk", "sq"], writes=["ps7"])
        p.op("dve", lambda e: e.tensor_scalar(out=rs[:], in0=ps[7][:], scalar1=1.0 / 64, scalar2=GN_EPS, op0=ALU.mult, op1=ALU.add), reads=["ps7"], writes=["rs"])
        p.op("act", lambda e: e.activation(out=rs[:], in_=rs[:], func=AF.Sqrt), reads=["rs"], writes=["rs"])
        p.op("dve", lambda e: e.reciprocal(out=rs[:], in_=rs[:]), reads=["rs"], writes=["rs"])
        p.op("dve", lambda e: e.tensor_tensor(out=yc[:], in0=yc[:], in1=rs[:], op=ALU.mult), reads=["yc", "rs"], writes=["yc"])
        p.op("dve", lambda e: e.tensor_scalar(out=yc[:], in0=yc[:], scalar1=gnv[:, 0:1], scalar2=gnv[:, 1:2], op0=ALU.mult, op1=ALU.add), reads=["yc", "gnv"], writes=["yc"])
        p.op("dve", lambda e, i=i: e.tensor_tensor(out=yc[:], in0=yc[:], in1=bt[i][:], op=ALU.add), reads=["yc", f"bt{i}"], writes=["yc"])
        p.op("dve", lambda e, i=i: e.tensor_tensor(out=ot[i][:], in0=yc[:], in1=gt[i][:], op=ALU.mult), reads=["yc", f"gt{i}"], writes=[f"ot{i}"])
        p.dma("sp", o_d[:, tsl], ot[i][:], reads=[f"ot{i}"])


def launch_rwkv(lc, I, T=S):
    sel = np.zeros((128, 128), np.float32)
    sel[0, :64] = 1
    sel[1, 64:] = 1
    blk = np.zeros((128, 128), np.float32)
    blk[:64, :64] = 1
    blk[64:, 64:] = 1
    in_maps = []
    nch = T // TCH
    for i in range(8):
        cq = slice(i * 128, (i + 1) * 128)
        q5 = np.stack([lc[i][n][:, :T] for n in ("wdec", "nkk", "kka", "kt", "rT")], 0)
        q5 = q5.reshape(5, 2, 64, nch, TCH).transpose(1, 3, 0, 4, 2)
        gnv = np.stack([I["gn_w"][0][cq], I["gn_b"][0][cq]], -1)
        in_maps.append({"bcin": np.ascontiguousarray(q5), "vT": np.ascontiguousarray(lc[i]["vrT"][:, :T]),
                        "g": np.ascontiguousarray(lc[i]["g"][:, :T]), "bonus": np.ascontiguousarray(lc[i]["bonus"][:, :T]),
                        "gnv": np.ascontiguousarray(gnv), "sel": sel, "blk": blk, "zer": np.zeros((126, 5, TCH, 64), np.float32)})
    res = _run(lambda nc, p, st: build_rwkv(nc, p, st, T), in_maps)
    return np.concatenate([r["o"].T for r in res], axis=1)


NEGB = 30000.0


def build_moba(nc, p, st, T=S):
    nb = T // 256
    nkt = T // 128
    qT_d = nc.dram_tensor("qT", [128, T], F32, kind="ExternalInput").ap()
    kT_d = nc.dram_tensor("kT", [128, T], F32, kind="ExternalInput").ap()
    v_d = nc.dram_tensor("v", [128, nkt, 128], F32, kind="ExternalInput").ap()
    E_d = nc.dram_tensor("E", [128, T], F32, kind="ExternalInput").ap()
    cm_d = nc.dram_tensor("cm", [128, 256], F32, kind="ExternalInput").ap()
    id_d = nc.dram_tensor("ident", [128, 128], F32, kind="ExternalInput").ap()
    on_d = nc.dram_tensor("ones", [128, 128], F32, kind="ExternalInput").ap()
    o_d = nc.dram_tensor("oT", [128, T], F32, kind="ExternalOutput").ap()
    r32 = lambda ap: ap.bitcast(F32R)
    qT = _sb(nc, st, "qT_sb", [128, T])
    kT = _sb(nc, st, "kT_sb", [128, T])
    va = _sb(nc, st, "va_sb", [128, nkt, 128])
    E = _sb(nc, st, "E_sb", [128, T])
    cm = _sb(nc, st, "cm_sb", [128, 256])
    ident = _sb(nc, st, "id_sb", [128, 128])
    ones = _sb(nc, st, "ones_sb", [128, 128])
    kmean = _sb(nc, st, "kmean", [128, 32])
    gsb = _sb(nc, st, "gsb", [128, 32])
    m8 = _sb(nc, st, "m8", [128, 8])
    bias = _sb(nc, st, "bias", [128, 128])
    biasT = [_sb(nc, st, f"biasT{i}", [128, 256]) for i in range(2)]
    pT = [_sb(nc, st, f"pT{i}", [128, 256]) for i in range(3)]
    osb = [_sb(nc, st, f"osb{i}", [128, 256]) for i in range(2)]
    rden = _sb(nc, st, "rden", [128, 256])
    s_ps = [_ps(nc, st, f"s_ps{i}") for i in range(2)]
    o_ps = [_ps(nc, st, f"o_ps{i}") for i in range(2)]
    d_ps = [_ps(nc, st, f"d_ps{i}") for i in range(2)]
    g_ps = _ps(nc, st, "g_ps")
    t_ps = _ps(nc, st, "t_ps")
    p.dma("pool", r32(cm[:]), r32(cm_d), writes=["cm"])
    p.dma("pool", r32(ident[:]), r32(id_d), writes=["ident"])
    p.dma("pool", r32(ones[:]), r32(on_d), writes=["ones"])
    PCS = 1024
    npc = max(1, T // PCS)
    pc = lambda tok: min(tok // PCS, npc - 1)
    for i_ in range(npc):
        tsl_ = slice(i_ * PCS, min(T, (i_ + 1) * PCS))
        ktl_ = slice(i_ * PCS // 128, min(T, (i_ + 1) * PCS) // 128)
        p.dma("pool", r32(qT[:, tsl_]), r32(qT_d[:, tsl_]), writes=[f"qT{i_}"])
        p.dma("pool", r32(kT[:, tsl_]), r32(kT_d[:, tsl_]), writes=[f"kT{i_}"])
        p.dma("pool", r32(va[:, ktl_, :]), r32(v_d[:, ktl_, :]), writes=[f"va{i_}"])
        p.dma("pool", r32(E[:, tsl_]), r32(E_d[:, tsl_]), writes=[f"E{i_}"])
        nb0 = i_ * PCS // 256
        nb1 = min(T, (i_ + 1) * PCS) // 256
        p.op("dve", lambda e, tsl_=tsl_, nb0=nb0, nb1=nb1: e.tensor_reduce(out=kmean[:, nb0:nb1], in_=kT[:, tsl_].bitcast(F32).rearrange("p (n k) -> p n k", k=256), axis=AX.X, op=ALU.add),
             reads=[f"kT{i_}"], writes=[f"kmean{i_}"])
        p.op("dve", lambda e, nb0=nb0, nb1=nb1: e.tensor_scalar(out=kmean[:, nb0:nb1], in0=kmean[:, nb0:nb1], scalar1=1.0 / 256, scalar2=None, op0=ALU.mult),
             reads=[f"kmean{i_}"], writes=[f"kmean{i_}"])
    kmkeys = lambda b_: [f"kmean{i_}" for i_ in range(pc(b_ * 256) + 1)]
    p.op("dve", lambda e: e.memset(gsb[:], -1e30), writes=["gsb"])
    p.op("dve", lambda e: e.memset(bias[:], 0.0), writes=["bias"])
    scale = 128 ** -0.5
    pti = 0
    si = 0
    for b in range(nb):
        bT = biasT[b % 2]
        bTk = f"biasT{b % 2}"
        for j in range(2):
            qs = slice(b * 256 + j * 128, b * 256 + (j + 1) * 128)
            if b > 3:
                p.op("pe", lambda e, qs=qs: e.matmul(g_ps[:, 0:nb], lhsT=qT[:, qs], rhs=kmean[:, 0:nb], start=True, stop=True),
                     reads=[f"qT{pc(b * 256)}"] + kmkeys(b), writes=["g_ps"])
                p.op("dve", lambda e, b=b: e.tensor_copy(out=gsb[:, 0:b], in_=g_ps[:, 0:b]), reads=["g_ps", "gsb"], writes=["gsb"])
                p.op("dve", lambda e: e.max(out=m8[:], in_=gsb[:]), reads=["gsb"], writes=["m8"])
                p.op("dve", lambda e: e.tensor_scalar(out=bias[:, 0:32], in0=gsb[:], scalar1=m8[:, 2:3], scalar2=None, op0=ALU.is_ge),
                     reads=["gsb", "m8", "bias"], writes=["bias"], force=True)
                p.op("dve", lambda e: e.tensor_scalar(out=bias[:, 0:32], in0=bias[:, 0:32], scalar1=NEGB, scalar2=-NEGB, op0=ALU.mult, op1=ALU.add),
                     reads=["bias"], writes=["bias"])
            else:
                p.op("dve", lambda e: e.memset(bias[:, 0:32], -NEGB), reads=["bias"], writes=["bias"])
                if b > 0:
                    p.op("dve", lambda e, b=b: e.memset(bias[:, 0:b], 0.0), reads=["bias"], writes=["bias"])
            p.op("dve", lambda e, b=b: e.memset(bias[:, b:b + 1], 0.0), reads=["bias"], writes=["bias"])
            p.op("pe", lambda e: e.transpose(t_ps[:, 0:128], bias[:], ident[:].bitcast(F32)), reads=["bias", "ident"], writes=["t_ps"])
            p.op("act", lambda e, bT=bT, j=j: e.copy(out=r32(bT[:, j * 128:(j + 1) * 128]), in_=t_ps[:, 0:128]), reads=["t_ps"], writes=[bTk])
        op_ = o_ps[b % 2]
        opk = f"o_ps{b % 2}"
        dp_ = d_ps[b % 2]
        dpk = f"d_ps{b % 2}"
        nkt_b = 2 * b + 2

        def qkm(kt, sp_, spk, b=b, bT=bT, bTk=bTk):
            ks = slice(kt * 128, (kt + 1) * 128)
            own = kt >= 2 * b
            p.op("pe", lambda e: e.matmul(sp_[:, 0:256], lhsT=r32(kT[:, ks]), rhs=r32(qT[:, b * 256:(b + 1) * 256]), start=True, stop=False),
                 reads=[f"kT{pc(kt * 128)}", f"qT{pc(b * 256)}"], writes=[spk])
            p.op("pe", lambda e: e.matmul(sp_[:, 0:256], lhsT=r32(E[:, ks]), rhs=r32(bT[:]), start=False, stop=(not own)),
                 reads=[f"E{pc(kt * 128)}", bTk], writes=[spk])
            if own:
                if kt == 2 * b:
                    p.op("pe", lambda e: e.matmul(sp_[:, 0:128], lhsT=ident[:].bitcast(F32), rhs=cm[:, 128:256].bitcast(F32), start=False, stop=True),
                         reads=["ident", "cm"], writes=[spk])
                else:
                    p.op("pe", lambda e: e.matmul(sp_[:, 0:256], lhsT=r32(ident[:]), rhs=r32(cm[:, 0:256]), start=False, stop=True),
                         reads=["ident", "cm"], writes=[spk])

        cur = (s_ps[si % 2], f"s_ps{si % 2}")
        si += 1
        qkm(0, *cur)
        for kt in range(nkt_b):
            nxt = None
            if kt + 1 < nkt_b:
                nxt = (s_ps[si % 2], f"s_ps{si % 2}")
                si += 1
                qkm(kt + 1, *nxt)
            sp_, spk = cur
            pt = pT[pti % 3]
            ptk = f"pT{pti % 3}"
            pti += 1
            p.op("act", lambda e, pt=pt, sp_=sp_: e.activation(out=r32(pt[:]), in_=sp_[:, 0:256], func=AF.Exp, scale=scale), reads=[spk], writes=[ptk])
            p.op("pe", lambda e, pt=pt, kt=kt, op_=op_, nkt_b=nkt_b: e.matmul(op_[:, 0:256], lhsT=r32(va[:, kt, :]), rhs=r32(pt[:]), start=(kt == 0), stop=(kt == nkt_b - 1)),
                 reads=[ptk, f"va{pc(kt * 128)}"], writes=[opk])
            p.op("pe", lambda e, pt=pt, kt=kt, dp_=dp_, nkt_b=nkt_b: e.matmul(dp_[:, 0:256], lhsT=r32(ones[:]), rhs=r32(pt[:]), start=(kt == 0), stop=(kt == nkt_b - 1)),
                 reads=[ptk, "ones"], writes=[dpk])
            cur = nxt
        ob_ = osb[b % 2]
        p.op("dve", lambda e, dp_=dp_: e.reciprocal(out=rden[:], in_=dp_[:, 0:256]), reads=[dpk], writes=["rden"])
        p.op("dve", lambda e, op_=op_, ob_=ob_: e.tensor_tensor(out=ob_[:], in0=op_[:, 0:256], in1=rden[:], op=ALU.mult), reads=[opk, "rden"], writes=[f"osb{b % 2}"])
        p.dma("sp", o_d[:, b * 256:(b + 1) * 256], ob_[:], reads=[f"osb{b % 2}"])


def launch_moba(lc, T=S):
    nkt = T // 128
    E = np.zeros((128, T), np.float32)
    for n in range(T // 256):
        E[n, n * 256:(n + 1) * 256] = 1
    kk = np.arange(128)
    cmc = np.where(kk[:, None] <= kk[None, :], 0.0, -NEGB).astype(np.float32)
    cm = np.concatenate([np.full((128, 128), -NEGB, np.float32), cmc], 1)
    ident = np.eye(128, dtype=np.float32)
    ones = np.ones((128, 128), np.float32)
    in_maps = []
    for i in range(8):
        v = lc[i]["vT"][:, :T].T.reshape(nkt, 128, 128).transpose(1, 0, 2)
        in_maps.append({"qT": np.ascontiguousarray(lc[i]["qT"][:, :T]), "kT": np.ascontiguousarray(lc[i]["kT"][:, :T]),
                        "v": np.ascontiguousarray(v), "E": E, "cm": cm, "ident": ident, "ones": ones})
    res = _run(lambda nc, p, st: build_moba(nc, p, st, T), in_maps)
    return np.concatenate([r["oT"].T for r in res], axis=1)


def build_merge(nc, p, st, nunit=2):
    TT = 512
    NT = nunit * TT
    hT_d = nc.dram_tensor("hT", [D, NT], F32, kind="ExternalInput").ap()
    oa_d = nc.dram_tensor("oaT", [1024, NT], F32, kind="ExternalInput").ap()
    or_d = nc.dram_tensor("orT", [1024, NT], F32, kind="ExternalInput").ap()
    wg_d = nc.dram_tensor("wg", [D, 4096], F32, kind="ExternalInput").ap()
    wua_d = nc.dram_tensor("wua", [1024, D], F32, kind="ExternalInput").ap()
    wur_d = nc.dram_tensor("wur", [1024, D], F32, kind="ExternalInput").ap()
    wo_d = nc.dram_tensor("wo", [D, D], F32, kind="ExternalInput").ap()
    y_d = nc.dram_tensor("yT", [D, NT], F32, kind="ExternalOutput").ap()
    r32 = lambda ap: ap.bitcast(F32R)
    hT = _sb(nc, st, "hT_sb", [128, 16, TT])
    oa = _sb(nc, st, "oa_sb", [128, 8, TT])
    orr = _sb(nc, st, "or_sb", [128, 8, TT])
    mix = _sb(nc, st, "mix_sb", [128, 16, TT])
    wb = [_sb(nc, st, f"wb{i}", [128, 48, 128]) for i in range(2)]
    wob = [_sb(nc, st, f"wob{i}", [128, 16, 128]) for i in range(2)]
    sg = [_sb(nc, st, f"sg{i}", [128, TT]) for i in range(2)]
    m12 = [_sb(nc, st, f"m12_{i}", [128, TT]) for i in range(2)]
    yo = [_sb(nc, st, f"yo{i}", [128, TT]) for i in range(2)]
    ps = [_ps(nc, st, f"ps{i}") for i in range(8)]
    wgv = wg_d.rearrange("(c p) n -> p c n", p=128)
    wuav = wua_d.rearrange("(c p) n -> p c n", p=128)
    wurv = wur_d.rearrange("(c p) n -> p c n", p=128)
    wov = wo_d.rearrange("(c p) n -> p c n", p=128)
    wcount = 0
    for u in range(nunit):
        ts_ = slice(u * TT, (u + 1) * TT)
        p.dma("pool", r32(hT[:]), r32(hT_d.rearrange("(c p) t -> p c t", p=128)[:, :, ts_]), writes=["hT"])
        p.dma("pool", r32(oa[:]), r32(oa_d.rearrange("(c p) t -> p c t", p=128)[:, :, ts_]), writes=["oa"])
        p.dma("pool", r32(orr[:]), r32(or_d.rearrange("(c p) t -> p c t", p=128)[:, :, ts_]), writes=["or"])
        for n in range(16):
            wi = wcount % 2
            wcount += 1
            W = wb[wi]
            wk = f"wb{wi}"
            ns = slice(n * 128, (n + 1) * 128)
            ns2 = slice(2048 + n * 128, 2048 + (n + 1) * 128)
            p.dma("pool", r32(W[:, 0:8, :]), r32(wgv[:, 0:8, ns]), writes=[wk + "a"])
            p.dma("pool", r32(W[:, 8:16, :]), r32(wgv[:, 8:16, ns]), writes=[wk + "b"])
            p.dma("pool", r32(W[:, 16:24, :]), r32(wgv[:, 0:8, ns2]), writes=[wk + "c"])
            p.dma("pool", r32(W[:, 24:32, :]), r32(wgv[:, 8:16, ns2]), writes=[wk + "d"])
            p.dma("pool", r32(W[:, 32:40, :]), r32(wuav[:, :, ns]), writes=[wk + "e"])
            p.dma("pool", r32(W[:, 40:48, :]), r32(wurv[:, :, ns]), writes=[wk + "f"])
            wkeys = [wk + x for x in "abcdef"]
            b0 = (n % 2) * 4
            pga, pgr, pua, pur = ps[b0], ps[b0 + 1], ps[b0 + 2], ps[b0 + 3]
            for c in range(16):
                p.op("pe", lambda e, c=c, W=W, pga=pga: e.matmul(pga[:], lhsT=r32(W[:, c, :]), rhs=r32(hT[:, c, :]), start=(c == 0), stop=(c == 15)),
                     reads=wkeys + ["hT"], writes=[f"ps{b0}"])
            for c in range(16):
                p.op("pe", lambda e, c=c, W=W, pgr=pgr: e.matmul(pgr[:], lhsT=r32(W[:, 16 + c, :]), rhs=r32(hT[:, c, :]), start=(c == 0), stop=(c == 15)),
                     reads=wkeys + ["hT"], writes=[f"ps{b0 + 1}"])
            for c in range(8):
                p.op("pe", lambda e, c=c, W=W, pua=pua: e.matmul(pua[:], lhsT=r32(W[:, 32 + c, :]), rhs=r32(oa[:, c, :]), start=(c == 0), stop=(c == 7)),
                     reads=wkeys + ["oa"], writes=[f"ps{b0 + 2}"])
            for c in range(8):
                p.op("pe", lambda e, c=c, W=W, pur=pur: e.matmul(pur[:], lhsT=r32(W[:, 40 + c, :]), rhs=r32(orr[:, c, :]), start=(c == 0), stop=(c == 7)),
                     reads=wkeys + ["or"], writes=[f"ps{b0 + 3}"])
            p.op("act", lambda e, pga=pga: e.activation(out=sg[0][:], in_=pga[:], func=AF.Sigmoid), reads=[f"ps{b0}"], writes=["sg0"])
            p.op("act", lambda e, pgr=pgr: e.activation(out=sg[1][:], in_=pgr[:], func=AF.Sigmoid), reads=[f"ps{b0 + 1}"], writes=["sg1"])
            p.op("dve", lambda e, pua=pua: e.tensor_tensor(out=m12[0][:], in0=pua[:], in1=sg[0][:], op=ALU.mult), reads=[f"ps{b0 + 2}", "sg0"], writes=["m0"])
            p.op("dve", lambda e, pur=pur: e.tensor_tensor(out=m12[1][:], in0=pur[:], in1=sg[1][:], op=ALU.mult), reads=[f"ps{b0 + 3}", "sg1"], writes=["m1"])
            p.op("dve", lambda e, n=n: e.tensor_tensor(out=r32(mix[:, n, :]), in0=m12[0][:], in1=m12[1][:], op=ALU.add), reads=["m0", "m1"], writes=[f"mix{n}"])
        for m in range(16):
            wi = m % 2
            ms = slice(m * 128, (m + 1) * 128)
            p.dma("pool", r32(wob[wi][:, 0:8, :]), r32(wov[:, 0:8, ms]), writes=[f"wob{wi}a"])
            p.dma("pool", r32(wob[wi][:, 8:16, :]), r32(wov[:, 8:16, ms]), writes=[f"wob{wi}b"])
            py = ps[m % 2]
            for c in range(16):
                p.op("pe", lambda e, c=c, wi=wi, py=py: e.matmul(py[:], lhsT=r32(wob[wi][:, c, :]), rhs=r32(mix[:, c, :]), start=(c == 0), stop=(c == 15)),
                     reads=[f"wob{wi}a", f"wob{wi}b", f"mix{c}"], writes=[f"ps{m % 2}"])
            p.op("act", lambda e, py=py, wi=wi: e.copy(out=yo[wi][:], in_=py[:]), reads=[f"ps{m % 2}"], writes=[f"yo{wi}"])
            p.dma("pool", y_d[ms, ts_], yo[wi][:], reads=[f"yo{wi}"])


def launch_merge(h1, o_att, o_rwkv, I):
    wg = np.ascontiguousarray(I["w_in"][0][:, 6144:10240])
    in_maps = []
    for i in range(8):
        ts_ = slice(i * 1024, (i + 1) * 1024)
        in_maps.append({"hT": np.ascontiguousarray(h1[ts_].T), "oaT": np.ascontiguousarray(o_att[ts_].T),
                        "orT": np.ascontiguousarray(o_rwkv[ts_].T), "wg": wg,
                        "wua": I["w_up_att"][0], "wur": I["w_up_rwkv"][0], "wo": I["w_o"][0]})
    res = _run(lambda nc, p, st: build_merge(nc, p, st, 2), in_maps)
    return np.concatenate([r["yT"].T for r in res], axis=0)


def build_router(nc, p, st, ntile=8):
    NT = ntile * 128
    hT_d = nc.dram_tensor("hT", [D, NT], F32, kind="ExternalInput").ap()
    wr_d = nc.dram_tensor("wr", [D, 72], F32, kind="ExternalInput").ap()
    br_d = nc.dram_tensor("br", [1, 72], F32, kind="ExternalInput").ap()
    o_d = nc.dram_tensor("o", [NT, 4], F32, kind="ExternalOutput").ap()
    hT = _sb(nc, st, "hT_sb", [128, 16, NT])
    wr = _sb(nc, st, "wr_sb", [128, 16, 72])
    br = _sb(nc, st, "br_sb", [128, 72])
    ps = [_ps(nc, st, f"ps{i}") for i in range(2)]
    p.dma("sp", hT[:], hT_d.rearrange("(c p) t -> p c t", p=128), writes=["hT"])
    p.dma("sp", wr[:], wr_d.rearrange("(c p) n -> p c n", p=128), writes=["wr"])
    p.dma("sp", br[:], br_d.partition_broadcast(128), writes=["br"])
    l_sb = _sb(nc, st, "l_sb", [128, 72])
    lem = _sb(nc, st, "lem", [128, 64])
    m8g = _sb(nc, st, "m8g", [128, 8])
    m8e = _sb(nc, st, "m8e", [128, 8])
    idx = _sb(nc, st, "idx", [128, 8], U32)
    sm = _sb(nc, st, "sm", [128, 8])
    junk = _sb(nc, st, "junk", [128, 8])
    pen = _sb(nc, st, "pen", [128, 8])
    res = [_sb(nc, st, f"res{i}", [128, 4]) for i in range(2)]
    for t in range(ntile):
        pt = ps[t % 2]
        ptk = f"ps{t % 2}"
        rs_ = res[t % 2]
        rk = f"res{t % 2}"
        for c in range(16):
            p.op("pe", lambda e, c=c, t=t, pt=pt: e.matmul(pt[:, 0:72], lhsT=hT[:, c, t * 128:(t + 1) * 128], rhs=wr[:, c, :], start=(c == 0), stop=(c == 15)),
                 reads=["hT", "wr"], writes=[ptk])
        p.op("dve", lambda e, pt=pt: e.tensor_tensor(out=l_sb[:], in0=pt[:, 0:72], in1=br[:], op=ALU.add), reads=[ptk, "br"], writes=["l"])
        p.op("dve", lambda e: e.max(out=m8g[:], in_=l_sb[:, 0:8]), reads=["l"], writes=["m8g"])
        p.op("dve", lambda e: e.tensor_scalar(out=sm[:, 0:1], in0=m8g[:, 0:1], scalar1=-1.0, scalar2=None, op0=ALU.mult), reads=["m8g"], writes=["sm0"])
        p.op("act", lambda e: e.activation(out=junk[:], in_=l_sb[:, 0:8], func=AF.Exp, bias=sm[:, 0:1], accum_out=sm[:, 1:2]), reads=["l", "sm0"], writes=["junk", "sm1"])
        p.op("dve", lambda e: e.reciprocal(out=sm[:, 2:3], in_=sm[:, 1:2]), reads=["sm1"], writes=["sm2"])
        p.op("dve", lambda e: e.tensor_scalar(out=pen[:], in0=l_sb[:, 0:8], scalar1=m8g[:, 0:1], scalar2=None, op0=ALU.is_ge), reads=["l", "m8g"], writes=["pen"], force=True)
        p.op("dve", lambda e: e.tensor_scalar(out=pen[:], in0=pen[:], scalar1=1e30, scalar2=-1e30, op0=ALU.mult, op1=ALU.add), reads=["pen"], writes=["pen"])
        for g in range(8):
            p.op("dve", lambda e, g=g: e.tensor_scalar(out=lem[:, g * 8:(g + 1) * 8], in0=l_sb[:, 8 + g * 8:16 + g * 8], scalar1=pen[:, g:g + 1], scalar2=None, op0=ALU.add),
                 reads=["l", "pen"], writes=["lem"], force=(g == 0))
        p.op("dve", lambda e: e.max(out=m8e[:], in_=lem[:]), reads=["lem"], writes=["m8e"])
        p.op("dve", lambda e: e.max_index(out=idx[:], in_max=m8e[:], in_values=lem[:]), reads=["m8e", "lem"], writes=["idx"], force=True)
        p.op("dve", lambda e, rs_=rs_: e.tensor_copy(out=rs_[:, 0:2], in_=idx[:, 0:2]), reads=["idx"], writes=[rk + "a"], force=True)
        p.op("dve", lambda e: e.tensor_tensor(out=sm[:, 3:4], in0=m8e[:, 0:1], in1=m8e[:, 1:2], op=ALU.subtract), reads=["m8e"], writes=["sm3"], force=True)
        p.op("act", lambda e: e.activation(out=sm[:, 4:5], in_=sm[:, 3:4], func=AF.Sigmoid), reads=["sm3"], writes=["sm4"])
        p.op("dve", lambda e, rs_=rs_: e.tensor_tensor(out=rs_[:, 2:3], in0=sm[:, 4:5], in1=sm[:, 2:3], op=ALU.mult), reads=["sm4", "sm2"], writes=[rk + "b"], force=True)
        p.op("dve", lambda e, rs_=rs_: e.tensor_tensor(out=rs_[:, 3:4], in0=sm[:, 2:3], in1=rs_[:, 2:3], op=ALU.subtract), reads=["sm2", rk + "b"], writes=[rk + "c"], force=True)
        p.dma("sp", o_d[t * 128:(t + 1) * 128, :], rs_[:], reads=[rk + "a", rk + "b", rk + "c"])


def launch_router(h2, I):
    wr = np.ascontiguousarray(np.concatenate([I["w_rg"][0], I["w_re"][0]], 1))
    br = np.ascontiguousarray(np.concatenate([I["b_rg"][0], I["b_re"][0]])[None, :])
    in_maps = [{"hT": np.ascontiguousarray(h2[i * 1024:(i + 1) * 1024].T), "wr": wr, "br": br} for i in range(8)]
    res = _run(lambda nc, p, st: build_router(nc, p, st, 8), in_maps)
    o = np.concatenate([r["o"] for r in res], axis=0)
    return o[:, 0:2].astype(np.int64), o[:, 2:4]


def build_experts(nc, p, st, cap):
    xT_d = nc.dram_tensor("xT", [8, D, cap], F32, kind="ExternalInput").ap()
    wg_d = nc.dram_tensor("wg", [8, D, 512], F32, kind="ExternalInput").ap()
    wu_d = nc.dram_tensor("wu", [8, D, 512], F32, kind="ExternalInput").ap()
    wd_d = nc.dram_tensor("wd", [8, 512, D], F32, kind="ExternalInput").ap()
    y_d = nc.dram_tensor("yT", [8, D, cap], F32, kind="ExternalOutput").ap()
    r32 = lambda ap: ap.bitcast(F32R)
    xT = _sb(nc, st, "xT_sb", [128, 16, cap])
    Wg = _sb(nc, st, "Wg_sb", [128, 16, 512])
    Wu = _sb(nc, st, "Wu_sb", [128, 16, 512])
    Wd = _sb(nc, st, "Wd_sb", [128, 4, D])
    hid = _sb(nc, st, "hid_sb", [128, 4, cap])
    sg = [_sb(nc, st, f"sg{i}", [128, cap]) for i in range(2)]
    yo = [_sb(nc, st, f"yo{i}", [128, cap]) for i in range(3)]
    ps = [_ps(nc, st, f"ps{i}") for i in range(8)]
    for ex in range(8):
        xv = xT_d[ex].rearrange("(c p) t -> p c t", p=128)
        for h in range(4):
            p.dma("pool", r32(xT[:, h * 4:(h + 1) * 4, :]), r32(xv[:, h * 4:(h + 1) * 4, :]), writes=[f"xT{h}"])
        gv = wg_d[ex].rearrange("(c p) n -> p c n", p=128)
        uv = wu_d[ex].rearrange("(c p) n -> p c n", p=128)
        dv = wd_d[ex].rearrange("(c p) n -> p c n", p=128)
        for h in range(8):
            p.dma("pool", r32(Wg[:, h * 2:(h + 1) * 2, :]), r32(gv[:, h * 2:(h + 1) * 2, :]), writes=[f"Wg{h}"])
        for h in range(8):
            p.dma("pool", r32(Wu[:, h * 2:(h + 1) * 2, :]), r32(uv[:, h * 2:(h + 1) * 2, :]), writes=[f"Wu{h}"])
        for h in range(4):
            p.dma("pool", r32(Wd[:, h, 0:1024]), r32(dv[:, h, 0:1024]), writes=[f"Wd{h}a"])
            p.dma("pool", r32(Wd[:, h, 1024:2048]), r32(dv[:, h, 1024:2048]), writes=[f"Wd{h}b"])
        for f in range(4):
            pg = ps[(f % 2) * 2]
            pu = ps[(f % 2) * 2 + 1]
            pgk = f"ps{(f % 2) * 2}"
            puk = f"ps{(f % 2) * 2 + 1}"
            fs = slice(f * 128, (f + 1) * 128)
            for c in range(16):
                p.op("pe", lambda e, c=c, pg=pg, fs=fs: e.matmul(pg[:, 0:cap], lhsT=r32(Wg[:, c, fs]), rhs=r32(xT[:, c, :]), start=(c == 0), stop=(c == 15)),
                     reads=[f"Wg{c // 2}", f"xT{c // 4}"], writes=[pgk])
            for c in range(16):
                p.op("pe", lambda e, c=c, pu=pu, fs=fs: e.matmul(pu[:, 0:cap], lhsT=r32(Wu[:, c, fs]), rhs=r32(xT[:, c, :]), start=(c == 0), stop=(c == 15)),
                     reads=[f"Wu{c // 2}", f"xT{c // 4}"], writes=[puk])
            s_ = sg[f % 2]
            p.op("act", lambda e, s_=s_, pg=pg: e.activation(out=s_[:], in_=pg[:, 0:cap], func=AF.Silu), reads=[pgk], writes=[f"sg{f % 2}"])
            p.op("dve", lambda e, s_=s_, pu=pu, f=f: e.tensor_tensor(out=r32(hid[:, f, :]), in0=pu[:, 0:cap], in1=s_[:], op=ALU.mult),
                 reads=[puk, f"sg{f % 2}"], writes=[f"hid{f}"])
        for d in range(16):
            py = ps[4 + d % 4]
            pyk = f"ps{4 + d % 4}"
            ds_ = slice(d * 128, (d + 1) * 128)
            for f in range(4):
                p.op("pe", lambda e, f=f, py=py, ds_=ds_: e.matmul(py[:, 0:cap], lhsT=r32(Wd[:, f, ds_]), rhs=r32(hid[:, f, :]), start=(f == 0), stop=(f == 3)),
                     reads=[f"Wd{f}a", f"Wd{f}b", f"hid{f}"], writes=[pyk])
            yb = yo[d % 3]
            p.op("act" if d % 2 else "dve", (lambda e, yb=yb, py=py: e.copy(out=yb[:], in_=py[:, 0:cap])) if d % 2 else (lambda e, yb=yb, py=py: e.tensor_copy(out=yb[:], in_=py[:, 0:cap])),
                 reads=[pyk], writes=[f"yo{d % 3}"])
            p.dma("sp", y_d[ex, ds_, :], yb[:], reads=[f"yo{d % 3}"])


def launch_experts(h2, eidx, I):
    N = h2.shape[0]
    flat_e = eidx.reshape(-1)
    flat_t = np.repeat(np.arange(N), 2)
    order = np.argsort(flat_e, kind="stable")
    counts = np.bincount(flat_e, minlength=64)
    cap = int(min(512, max(256, -(-counts.max() // 128) * 128)))
    nround = int(max(1, -(-counts.max() // cap)))
    starts = np.cumsum(counts) - counts
    xT = np.zeros((nround, 64, D, cap), np.float32)
    pos_of = np.zeros(2 * N, np.int64)
    rnd_of = np.zeros(2 * N, np.int64)
    for e in range(64):
        sl = order[starts[e]:starts[e] + counts[e]]
        pos = np.arange(counts[e])
        pos_of[sl] = pos % cap
        rnd_of[sl] = pos // cap
        for r in range(nround):
            sel = sl[(pos // cap) == r]
            xT[r, e, :, :len(sel)] = h2[flat_t[sel]].T
    yTs = []
    for r in range(nround):
        in_maps = [{"xT": np.ascontiguousarray(xT[r, g * 8:(g + 1) * 8]), "wg": np.ascontiguousarray(I["w_gate_e"][0][g * 8:(g + 1) * 8]),
                    "wu": np.ascontiguousarray(I["w_up_e"][0][g * 8:(g + 1) * 8]), "wd": np.ascontiguousarray(I["w_down_e"][0][g * 8:(g + 1) * 8])} for g in range(8)]
        res = _run(lambda nc, p, st: build_experts(nc, p, st, cap), in_maps)
        yTs.append(np.concatenate([r_["yT"] for r_ in res], axis=0))
    yT = np.stack(yTs, 0)
    yflat = yT[rnd_of, flat_e, :, pos_of]
    yflat = yflat.reshape(N, 2, D)
    return np.ascontiguousarray(yflat[:, 0]), np.ascontiguousarray(yflat[:, 1])


def build_combine(nc, p, st, ntile=8):
    n = ntile * 128
    ya_d = nc.dram_tensor("ya", [n, D], F32, kind="ExternalInput").ap()
    yb_d = nc.dram_tensor("yb", [n, D], F32, kind="ExternalInput").ap()
    w_d = nc.dram_tensor("w", [n, 2], F32, kind="ExternalInput").ap()
    g = nc.dram_tensor("g", [1, D], F32, kind="ExternalInput").ap()
    s = nc.dram_tensor("s", [1, D], F32, kind="ExternalInput").ap()
    base = nc.dram_tensor("base", [n, D], F32, kind="ExternalInput").ap()
    o = nc.dram_tensor("o", [n, D], F32, kind="ExternalOutput").ap()
    gb = _sb(nc, st, "gb", [128, D])
    A = _sb(nc, st, "A", [128, D])
    p.dma("sp", gb[:], g.partition_broadcast(128), writes=["gb"])
    p.dma("sp", A[:], s.partition_broadcast(128), writes=["A"])
    p.op("dve", lambda e: e.tensor_tensor(out=A[:], in0=A[:], in1=gb[:], op=ALU.mult), reads=["gb", "A"], writes=["A"])
    ya = [_sb(nc, st, f"ya{i}", [128, D]) for i in range(2)]
    yb = [_sb(nc, st, f"yb{i}", [128, D]) for i in range(2)]
    bt = [_sb(nc, st, f"bt{i}", [128, D]) for i in range(2)]
    wt = [_sb(nc, st, f"wt{i}", [128, 2]) for i in range(2)]
    junk = _sb(nc, st, "junk", [128, D])
    ot = [_sb(nc, st, f"ot{i}", [128, D]) for i in range(2)]
    ss = [_sb(nc, st, f"ss{i}", [128, 4]) for i in range(2)]
    for t in range(ntile):
        i = t % 2
        rows = slice(t * 128, (t + 1) * 128)
        p.dma("sp", ya[i][:], ya_d[rows, :], writes=[f"ya{i}"])
        p.dma("sp", yb[i][:], yb_d[rows, :], writes=[f"yb{i}"])
        p.dma("sp", bt[i][:], base[rows, :], writes=[f"bt{i}"])
        p.dma("sp", wt[i][:], w_d[rows, :], writes=[f"wt{i}"])
        p.op("dve", lambda e, i=i: e.tensor_scalar(out=ya[i][:], in0=ya[i][:], scalar1=wt[i][:, 0:1], scalar2=None, op0=ALU.mult),
             reads=[f"ya{i}", f"wt{i}"], writes=[f"ya{i}"])
        p.op("dve", lambda e, i=i: e.scalar_tensor_tensor(out=ya[i][:], in0=yb[i][:], scalar=wt[i][:, 1:2], in1=ya[i][:], op0=ALU.mult, op1=ALU.add),
             reads=[f"ya{i}", f"yb{i}", f"wt{i}"], writes=[f"ya{i}"])
        p.op("act", lambda e, i=i: e.activation(out=junk[:], in_=ya[i][:], func=AF.Square, accum_out=ss[i][:, 0:1]),
             reads=[f"ya{i}"], writes=["junk", f"ss{i}"])
        p.op("dve", lambda e, i=i: e.tensor_scalar(out=ss[i][:, 1:2], in0=ss[i][:, 0:1], scalar1=1.0 / D, scalar2=EPS, op0=ALU.mult, op1=ALU.add),
             reads=[f"ss{i}"], writes=[f"ss{i}"])
        p.op("act", lambda e, i=i: e.activation(out=ss[i][:, 2:3], in_=ss[i][:, 1:2], func=AF.Sqrt), reads=[f"ss{i}"], writes=[f"ss{i}"])
        p.op("dve", lambda e, i=i: e.reciprocal(out=ss[i][:, 3:4], in_=ss[i][:, 2:3]), reads=[f"ss{i}"], writes=[f"ss{i}"])
        p.op("dve", lambda e, i=i: e.scalar_tensor_tensor(out=ot[i][:], in0=ya[i][:], scalar=ss[i][:, 3:4], in1=A[:], op0=ALU.mult, op1=ALU.mult),
             reads=[f"ya{i}", f"ss{i}", "A"], writes=[f"ot{i}"], force=True)
        p.op("pool", lambda e, i=i: e.tensor_tensor(out=ot[i][:], in0=ot[i][:], in1=bt[i][:], op=ALU.add),
             reads=[f"ot{i}", f"bt{i}"], writes=[f"ot{i}"])
        p.dma("pool", o[rows, :], ot[i][:], reads=[f"ot{i}"])


def launch_combine(ya, yb, w, g, s, base):
    n = ya.shape[0] // 8
    in_maps = [{"ya": np.ascontiguousarray(ya[i * n:(i + 1) * n]), "yb": np.ascontiguousarray(yb[i * n:(i + 1) * n]),
                "w": np.ascontiguousarray(w[i * n:(i + 1) * n]), "g": np.ascontiguousarray(g[None, :]),
                "s": np.ascontiguousarray(s[None, :]), "base": np.ascontiguousarray(base[i * n:(i + 1) * n])} for i in range(8)]
    res = _run(lambda nc, p, st: build_combine(nc, p, st, n // 128), in_maps)
    return np.concatenate([r["o"] for r in res], axis=0)


def kernel(**inputs):
    I = {k: np.asarray(v) for k, v in inputs.items()}
    x = I["x"][0]
    ada = launch_ada(I["c"][0], I["w_ada"][0], I["b_ada"][0])
    sh1, sc1, gt1, sh2, sc2, gt2 = np.split(ada, 6)
    h1 = launch_norm(x, I["g_pre_mix"][0], sc1, 1.0, bv=sh1)
    lc = launch_inproj(h1, I)
    o_att = launch_moba(lc)
    o_rwkv = launch_rwkv_chunked(lc, I)
    y1 = launch_merge(h1, o_att, o_rwkv, I)
    x1, h2 = launch_norm2(y1, I["g_post_mix"][0], gt1, x, I["g_pre_ffn"][0], sc2, sh2)
    eidx, ew = launch_router(h2, I)
    ya, yb = launch_experts(h2, eidx, I)
    out = launch_combine(ya, yb, ew, I["g_post_ffn"][0], gt2, x1)
    return out[None].astype(np.float32)


CH_C = 64
SEG = 256
LOCK = 4


def build_rwkv_chunked(nc, p, st, T=S):
    nseg = T // SEG
    cps = SEG // CH_C
    F_d = nc.dram_tensor("F", [64, 6, 2, T], F32, kind="ExternalInput").ap()
    gb_d = nc.dram_tensor("gb", [64, 2, 2, T], F32, kind="ExternalInput").ap()
    gnv_d = nc.dram_tensor("gnv", [64, 2, 2], F32, kind="ExternalInput").ap()
    id_d = nc.dram_tensor("ident", [128, 128], F32, kind="ExternalInput").ap()
    msk_d = nc.dram_tensor("msk", [64, 10, 64], F32, kind="ExternalInput").ap()
    rm_d = nc.dram_tensor("rmask", [64, 2 * SEG], F32, kind="ExternalInput").ap()
    on_d = nc.dram_tensor("ones64", [64, 64], F32, kind="ExternalInput").ap()
    o_d = nc.dram_tensor("o", [64, 2, T], F32, kind="ExternalOutput").ap()

    ident = _sb(nc, st, "ident_sb", [128, 128])
    msk = _sb(nc, st, "msk_sb", [64, 10, 64])
    rmask = _sb(nc, st, "rmask_sb", [64, 2 * SEG])
    ones64 = _sb(nc, st, "ones64_sb", [64, 64])
    gnv = _sb(nc, st, "gnv_sb", [64, 2, 2])
    for dst, src, k in [(ident, id_d, "ident"), (msk, msk_d, "msk"),
                        (rmask, rm_d, "rmask"), (ones64, on_d, "ones64"), (gnv, gnv_d, "gnv")]:
        p.dma("sp", dst[:], src, writes=[k])
    Fin = [_sb(nc, st, f"Fin{i}", [64, 6, 2, SEG]) for i in range(2)]
    gbin = [_sb(nc, st, f"gbin{i}", [64, 2, 2, SEG]) for i in range(1)] * 2
    names = ["logw", "cum", "eg", "einv", "egm", "dte", "Af", "Bf", "Kf", "Rf", "Bh", "Kh", "Af32"]
    b16n = ("Af", "Bf", "Kf", "Rf")
    tmpn = ["logw", "cum", "eg", "einv", "egm", "dte"]
    Wtmp = {n: _sb(nc, st, f"wt_{n}", [64, 2, SEG]) for n in tmpn}
    W_ = []
    for i in range(2):
        d_ = dict(Wtmp)
        for n in names:
            if n not in tmpn:
                d_[n] = _sb(nc, st, f"w{i}_{n}", [64, 2, SEG], BF16 if n in b16n else F32)
        W_.append(d_)
    gC = [_sb(nc, st, f"gC{i}", [64, 2, cps]) for i in range(2)]
    NSL = 2 * LOCK
    TM = [_sb(nc, st, f"TM{i}", [64, 4, 128], BF16) for i in range(NSL)]
    MS = [_sb(nc, st, f"MS{i}", [64, 10, 64], BF16) for i in range(NSL)]
    MQ = [[_sb(nc, st, f"MQ{i}_{j}", [64, 4, 64], BF16) for j in range(2)] for i in range(NSL)]
    XW = [_sb(nc, st, f"XW{i}", [64, 2, 128], BF16) for i in range(NSL)]
    ident16 = _sb(nc, st, "ident16", [64, 64], BF16)
    p.op("dve", lambda e: e.tensor_copy(out=ident16[:], in_=ident[0:64, 0:64]), reads=["ident"], writes=["ident16"])
    NCH = 2 * LOCK + 2
    CHb = [_sb(nc, st, f"CHb{i}", [64, 4, 128]) for i in range(NCH)]
    Z = _sb(nc, st, "Zst", [64, 2, 64])
    Z2 = _sb(nc, st, "Zst2", [64, 2, 64])
    YT = [_sb(nc, st, f"YT{i}", [64, 2, SEG]) for i in range(2)]
    ps = [_ps(nc, st, f"ps{i}") for i in range(8)]
    psi = [0]

    def nb():
        i = psi[0] % 8
        psi[0] += 1
        return ps[i], f"ps{i}"

    p.op("dve", lambda e: e.memset(Z[:], 0.0), writes=["Z"])

    def load_seg(sg):
        i = sg % 2
        p.dma("sp", Fin[i][:], F_d[:, :, :, sg * SEG:(sg + 1) * SEG], writes=[f"Fin{i}"])

    def prep_seg(sg):
        i = sg % 2
        Fi = Fin[i]
        w = W_[i]
        fk = f"Fin{i}"
        k = lambda n: (f"wt_{n}" if n in tmpn else f"w{i}_{n}")
        fl = lambda ap: ap.rearrange("p h t -> p (h t)")
        p.op("act", lambda e: e.activation(out=fl(w["logw"][:]), in_=fl(Fi[:, 0]), func=AF.Ln), reads=[fk], writes=[k("logw")])
        p.op("dve", lambda e: e.tensor_tensor_scan(out=fl(w["cum"][:]), data0=rmask[:], data1=fl(w["logw"][:]), initial=0.0, op0=ALU.mult, op1=ALU.add),
             reads=["rmask", k("logw")], writes=[k("cum")])
        p.op("act", lambda e: e.activation(out=fl(w["eg"][:]), in_=fl(w["cum"][:]), func=AF.Exp), reads=[k("cum")], writes=[k("eg")])
        p.op("act", lambda e: e.activation(out=fl(w["einv"][:]), in_=fl(w["cum"][:]), func=AF.Exp, scale=-1.0), reads=[k("cum")], writes=[k("einv")])
        p.op("dve", lambda e: e.tensor_tensor(out=fl(w["egm"][:]), in0=fl(w["cum"][:]), in1=fl(w["logw"][:]), op=ALU.subtract), reads=[k("cum"), k("logw")], writes=[k("egm")])
        p.op("act", lambda e: e.activation(out=fl(w["egm"][:]), in_=fl(w["egm"][:]), func=AF.Exp), reads=[k("egm")], writes=[k("egm")])
        cumv = w["cum"][:].rearrange("p h (c t) -> p (h c) t", t=CH_C)
        p.op("dve", lambda e: e.tensor_tensor(out=w["dte"][:].rearrange("p h (c t) -> p (h c) t", t=CH_C), in0=cumv[:, :, CH_C - 1:CH_C].to_broadcast([64, 2 * cps, CH_C]), in1=cumv, op=ALU.subtract),
             reads=[k("cum")], writes=[k("dte")])
        p.op("act", lambda e: e.activation(out=fl(w["dte"][:]), in_=fl(w["dte"][:]), func=AF.Exp), reads=[k("dte")], writes=[k("dte")])
        for out_n, a_idx, b_n, eng in [("Af", 1, "egm", "dve"), ("Bf", 2, "einv", "pool"), ("Kf", 3, "einv", "dve"),
                                       ("Rf", 4, "eg", "pool"), ("Bh", 2, "dte", "dve"), ("Kh", 3, "dte", "pool"), ("Af32", 1, "egm", "pool")]:
            p.op(eng, lambda e, out_n=out_n, a_idx=a_idx, b_n=b_n: e.tensor_tensor(out=fl(w[out_n][:]), in0=fl(Fi[:, a_idx]), in1=fl(w[b_n][:]), op=ALU.mult),
                 reads=[fk, k(b_n)], writes=[k(out_n)])
        egC = w["eg"][:].rearrange("p h (c t) -> p h c t", t=CH_C)[:, :, :, CH_C - 1]
        p.op("act", lambda e: e.copy(out=gC[i][:], in_=egC), reads=[k("eg")], writes=[f"gC{i}"])

    def pre_stages(sg, cl, slot, chslot):
        i = sg % 2
        w = W_[i]
        Fi = Fin[i]
        k = lambda n: f"w{i}_{n}"
        cs = slice(cl * CH_C, (cl + 1) * CH_C)
        tm, ms, mq, xw, chb = TM[slot], MS[slot], MQ[slot], XW[slot], CHb[chslot]
        tmk, msk_, xwk, chk = f"TM{slot}", f"MS{slot}", f"XW{slot}", f"CHb{chslot}"
        stages = []

        def s1():
            b, bkey = nb()
            for q, (src, skey) in enumerate([(w["Af32"], k("Af32")), (w["Bh"], k("Bh")), (w["Kh"], k("Kh")), (None, f"Fin{i}")]):
                for h in range(2):
                    in_ap = Fi[:, 5, h, cs] if src is None else src[:, h, cs]
                    p.op("pe", lambda e, q=q, h=h, in_ap=in_ap: e.transpose(b[0:64, q * 128 + h * 64:q * 128 + (h + 1) * 64], in_ap, ident[0:64, 0:64]),
                         reads=[skey, "ident"], writes=[bkey])
            p.op("act", lambda e: e.copy(out=tm[:].rearrange("p a b -> p (a b)"), in_=b[0:64, :]), reads=[bkey], writes=[tmk])
        stages.append(s1)

        def s2a():
            b, bkey = nb()
            for h in range(2):
                pb = slice(h * 64, (h + 1) * 64)
                for col, (l, lk, r_, rk) in [(0 + h, (w["Bf"], k("Bf"), w["Af"], k("Af"))), (2 + h, (w["Kf"], k("Kf"), w["Af"], k("Af"))),
                                             (4 + h, (w["Af"], k("Af"), w["Bf"], k("Bf")))]:
                    p.op("pe", lambda e, col=col, l=l, r_=r_, h=h: e.matmul(b[0:64, col * 64:(col + 1) * 64], lhsT=l[:, h, cs], rhs=r_[:, h, cs], start=True, stop=True),
                         reads=[lk, rk], writes=[bkey])
            p.op("dve", lambda e: e.tensor_tensor(out=ms[:, 0:6, :].rearrange("p a b -> p (a b)"), in0=b[0:64, 0:384], in1=msk[:, 0:6, :].rearrange("p a b -> p (a b)"), op=ALU.mult),
                 reads=[bkey, "msk"], writes=[msk_ + "a"])
        stages.append(s2a)

        def s2b():
            b, bkey = nb()
            for h in range(2):
                pb = slice(h * 64, (h + 1) * 64)
                for col, (l, lk) in [(0 + h, (w["Bf"], k("Bf"))), (2 + h, (w["Kf"], k("Kf")))]:
                    p.op("pe", lambda e, col=col, l=l, h=h: e.matmul(b[0:64, col * 64:(col + 1) * 64], lhsT=l[:, h, cs], rhs=w["Rf"][:, h, cs], start=True, stop=True),
                         reads=[lk, k("Rf")], writes=[bkey])
            p.op("dve", lambda e: e.tensor_tensor(out=ms[:, 6:10, :].rearrange("p a b -> p (a b)"), in0=b[0:64, 0:256], in1=msk[:, 6:10, :].rearrange("p a b -> p (a b)"), op=ALU.mult),
                 reads=[bkey, "msk"], writes=[msk_ + "b"])
        stages.append(s2b)

        def s3():
            b, bkey = nb()
            for h in range(2):
                p.op("pe", lambda e, h=h: e.matmul(b[0:64, h * 64:(h + 1) * 64], lhsT=ms[:, 2 + h, :], rhs=tm[:, 3, h * 64:(h + 1) * 64], start=True, stop=True),
                     reads=[msk_ + "a", tmk], writes=[bkey])
            p.op("act", lambda e: e.copy(out=xw[:, :, 64:128], in_=b[0:64, 0:128].rearrange("p (h v) -> p h v", h=2)), reads=[bkey], writes=[xwk + "x"])
            p.op("act", lambda e: e.copy(out=xw[:, :, 0:64], in_=tm[:, 0, :].rearrange("p (h v) -> p h v", h=2)), reads=[tmk], writes=[xwk + "w"])
        stages.append(s3)

        def mk_level(j):
            def lv():
                if j == 0:
                    MT = [ms[:, 0, :], ms[:, 1, :]]
                    M = [ms[:, 4, :], ms[:, 5, :]]
                    mkey = msk_ + "a"
                else:
                    q = mq[j % 2]
                    MT = [q[:, 0, :], q[:, 1, :]]
                    M = [q[:, 2, :], q[:, 3, :]]
                    mkey = f"MQ{slot}_{j % 2}"
                b, bkey = nb()
                for h in range(2):
                    p.op("pe", lambda e, h=h: e.matmul(b[0:64, h * 128:(h + 1) * 128], lhsT=MT[h], rhs=xw[:, h, :], start=True, stop=True),
                         reads=[mkey, xwk + "x", xwk + "w"], writes=[bkey])
                if j < 5:
                    b2, b2key = nb()
                    for h in range(2):
                        p.op("pe", lambda e, h=h: e.matmul(b2[0:64, h * 64:(h + 1) * 64], lhsT=M[h], rhs=MT[h], start=True, stop=True), reads=[mkey], writes=[b2key])
                        p.op("pe", lambda e, h=h: e.matmul(b2[0:64, (2 + h) * 64:(3 + h) * 64], lhsT=MT[h], rhs=M[h], start=True, stop=True), reads=[mkey], writes=[b2key])
                p.op("dve", lambda e: e.tensor_tensor(out=xw[:].rearrange("p a b -> p (a b)"), in0=b[0:64, 0:256], in1=xw[:].rearrange("p a b -> p (a b)"), op=ALU.add),
                     reads=[bkey, xwk + "x", xwk + "w"], writes=[xwk + "x", xwk + "w"])
                if j < 5:
                    nq = mq[(j + 1) % 2]
                    p.op("act", lambda e: e.copy(out=nq[:].rearrange("p a b -> p (a b)"), in_=b2[0:64, 0:256]), reads=[b2key], writes=[f"MQ{slot}_{(j + 1) % 2}"])
            return lv
        for j in range(6):
            stages.append(mk_level(j))

        def s5():
            b, bkey = nb()
            xk = [xwk + "x", xwk + "w"]
            for h in range(2):
                pb = slice(h * 64, (h + 1) * 64)
                hs = slice(h * 64, (h + 1) * 64)
                Wh = xw[:, h, 0:64]
                Xh = xw[:, h, 64:128]
                p.op("pe", lambda e, Wh=Wh, hs=hs, h=h: e.matmul(b[0:64, h * 64:(h + 1) * 64], lhsT=Wh, rhs=tm[:, 1, hs], start=True, stop=True),
                     reads=xk + [tmk], writes=[bkey])
                p.op("pe", lambda e, Xh=Xh, hs=hs, h=h: e.matmul(b[0:64, 128 + h * 64:128 + (h + 1) * 64], lhsT=tm[:, 1, hs], rhs=Xh, start=True, stop=False),
                     reads=xk + [tmk], writes=[bkey])
                p.op("pe", lambda e, hs=hs, h=h: e.matmul(b[0:64, 128 + h * 64:128 + (h + 1) * 64], lhsT=tm[:, 2, hs], rhs=tm[:, 3, hs], start=False, stop=True),
                     reads=[tmk], writes=[bkey])
                p.op("pe", lambda e, Wh=Wh, h=h: e.matmul(b[0:64, 256 + h * 64:256 + (h + 1) * 64], lhsT=Wh, rhs=ms[:, 6 + h, :], start=True, stop=False),
                     reads=xk + [msk_ + "b"], writes=[bkey])
                p.op("pe", lambda e, h=h: e.matmul(b[0:64, 256 + h * 64:256 + (h + 1) * 64], lhsT=ident16[:], rhs=w["Rf"][:, h, cs], start=False, stop=True),
                     reads=["ident16", k("Rf")], writes=[bkey])
                p.op("pe", lambda e, Xh=Xh, h=h: e.matmul(b[0:64, 384 + h * 64:384 + (h + 1) * 64], lhsT=Xh, rhs=ms[:, 6 + h, :], start=True, stop=False),
                     reads=xk + [msk_ + "b"], writes=[bkey])
                p.op("pe", lambda e, hs=hs, h=h: e.matmul(b[0:64, 384 + h * 64:384 + (h + 1) * 64], lhsT=tm[:, 3, hs], rhs=ms[:, 8 + h, :], start=False, stop=True),
                     reads=[tmk, msk_ + "b"], writes=[bkey])
            p.op("act", lambda e: e.copy(out=chb[:].rearrange("p a b -> p (a b)"), in_=b[0:64, :]), reads=[bkey], writes=[chk])
        stages.append(s5)
        return stages

    def chain_step(sg, cl, chslot):
        i = sg % 2
        chb = CHb[chslot]
        chk = f"CHb{chslot}"
        yt = YT[i]
        b, bkey = nb()
        for h in range(2):
            hs = slice(h * 64, (h + 1) * 64)
            p.op("pe", lambda e, h=h, hs=hs: e.matmul(b[0:64, hs], lhsT=chb[:, 0, hs], rhs=Z[:, h, :], start=True, stop=True), reads=[chk, "Z"], writes=[bkey])
            p.op("pe", lambda e, h=h, hs=hs: e.matmul(b[0:64, 128 + h * 64:128 + (h + 1) * 64], lhsT=Z[:, h, :], rhs=chb[:, 2, hs], start=True, stop=True), reads=[chk, "Z"], writes=[bkey])
        p.op("dve", lambda e: e.tensor_tensor(out=yt[:, :, cl * CH_C:(cl + 1) * CH_C], in0=b[0:64, 128:256].rearrange("p (h t) -> p h t", h=2),
                                              in1=chb[:, 3, :].rearrange("p (h t) -> p h t", h=2), op=ALU.add),
             reads=[bkey, chk], writes=[f"YT{i}_{cl}"])
        for h in range(2):
            p.op("dve", lambda e, h=h: e.scalar_tensor_tensor(out=Z2[:, h, :], in0=Z[:, h, :], scalar=gC[i][:, h, cl:cl + 1], in1=b[0:64, h * 64:(h + 1) * 64], op0=ALU.mult, op1=ALU.add),
                 reads=["Z", f"gC{i}", bkey], writes=[f"Z2_{h}"])
        p.op("dve", lambda e: e.tensor_tensor(out=Z[:].rearrange("p a b -> p (a b)"), in0=Z2[:].rearrange("p a b -> p (a b)"), in1=chb[:, 1, :], op=ALU.add),
             reads=["Z2_0", "Z2_1", chk], writes=["Z"])

    yc = _sb(nc, st, "yc", [64, 2 * SEG])
    sq = _sb(nc, st, "sq", [64, 2 * SEG])
    rs = _sb(nc, st, "rs", [64, 2 * SEG])
    ot = [_sb(nc, st, f"ot{i}", [64, 2, SEG]) for i in range(1)] * 2

    def epilogue(sg):
        i = sg % 2
        yt = YT[i]
        ykeys = [f"YT{i}_{cl}" for cl in range(cps)]
        ytf = yt[:].rearrange("p h t -> p (h t)")
        p.dma("sp", gbin[0][:], gb_d[:, :, :, sg * SEG:(sg + 1) * SEG], writes=["gbin0"])
        for hh in range(2):
            b, bkey = nb()
            sl_ = slice(hh * SEG, (hh + 1) * SEG)
            p.op("pe", lambda e, sl_=sl_, b=b: e.matmul(b[0:64, 0:SEG], lhsT=ones64[:], rhs=ytf[:, sl_], start=True, stop=True), reads=["ones64"] + ykeys, writes=[bkey])
            p.op("dve", lambda e, sl_=sl_, b=b: e.scalar_tensor_tensor(out=yc[:, sl_], in0=b[0:64, 0:SEG], scalar=-1.0 / 64, in1=ytf[:, sl_], op0=ALU.mult, op1=ALU.add),
                 reads=[bkey] + ykeys, writes=[f"yc{hh}"])
            p.op("act", lambda e, sl_=sl_: e.activation(out=sq[:, sl_], in_=yc[:, sl_], func=AF.Square), reads=[f"yc{hh}"], writes=[f"sq{hh}"])
            b2, b2key = nb()
            p.op("pe", lambda e, sl_=sl_, b2=b2: e.matmul(b2[0:64, 0:SEG], lhsT=ones64[:], rhs=sq[:, sl_], start=True, stop=True), reads=["ones64", f"sq{hh}"], writes=[b2key])
            p.op("dve", lambda e, sl_=sl_, b2=b2: e.tensor_scalar(out=rs[:, sl_], in0=b2[0:64, 0:SEG], scalar1=1.0 / 64, scalar2=GN_EPS, op0=ALU.mult, op1=ALU.add), reads=[b2key], writes=[f"rs{hh}"])
            p.op("act", lambda e, sl_=sl_: e.activation(out=rs[:, sl_], in_=rs[:, sl_], func=AF.Sqrt), reads=[f"rs{hh}"], writes=[f"rs{hh}"])
            p.op("dve", lambda e, sl_=sl_: e.reciprocal(out=rs[:, sl_], in_=rs[:, sl_]), reads=[f"rs{hh}"], writes=[f"rs{hh}"])
            p.op("dve", lambda e, sl_=sl_: e.tensor_tensor(out=yc[:, sl_], in0=yc[:, sl_], in1=rs[:, sl_], op=ALU.mult), reads=[f"yc{hh}", f"rs{hh}"], writes=[f"yc{hh}"])
            p.op("dve", lambda e, sl_=sl_, hh=hh: e.tensor_scalar(out=yc[:, sl_], in0=yc[:, sl_], scalar1=gnv[:, hh, 0:1], scalar2=gnv[:, hh, 1:2], op0=ALU.mult, op1=ALU.add),
                 reads=[f"yc{hh}", "gnv"], writes=[f"yc{hh}"])
            p.op("pool", lambda e, sl_=sl_, hh=hh: e.tensor_tensor(out=yc[:, sl_], in0=yc[:, sl_], in1=gbin[0][:, 1, hh, :], op=ALU.add), reads=[f"yc{hh}", "gbin0"], writes=[f"yc{hh}"])
            p.op("pool", lambda e, sl_=sl_, hh=hh: e.tensor_tensor(out=ot[0][:, hh, :], in0=yc[:, sl_], in1=gbin[0][:, 0, hh, :], op=ALU.mult), reads=[f"yc{hh}", "gbin0"], writes=[f"ot0_{hh}"])
        p.dma("sp", o_d[:, :, sg * SEG:(sg + 1) * SEG], ot[0][:], reads=["ot0_0", "ot0_1"])

    load_seg(0)
    pending_chain = []
    slot_ctr = 0
    ch_ctr = 0
    for sg in range(nseg):
        if sg + 1 < nseg:
            load_seg(sg + 1)
        prep_seg(sg)
        for c0 in range(0, cps, LOCK):
            sts = []
            new_chain = []
            for gi in range(LOCK):
                sts.append(pre_stages(sg, c0 + gi, slot_ctr % NSL, ch_ctr % NCH))
                new_chain.append((sg, c0 + gi, ch_ctr % NCH))
                slot_ctr += 1
                ch_ctr += 1
            nst = len(sts[0])
            pop_at = set(range(1, nst, max(1, (nst - 1) // LOCK)))
            for si in range(nst):
                for gi in range(LOCK):
                    sts[gi][si]()
                if pending_chain and si in pop_at:
                    a = pending_chain.pop(0)
                    chain_step(*a)
                    if a[1] == cps - 1:
                        epilogue(a[0])
            pending_chain.extend(new_chain)
    while pending_chain:
        a = pending_chain.pop(0)
        chain_step(*a)
        if a[1] == cps - 1:
            epilogue(a[0])


def launch_rwkv_chunked(lc, I, T=S):
    C = CH_C
    su = np.triu(np.ones((C, C), np.float32), 1)
    sle = np.triu(np.ones((C, C), np.float32), 0)
    msk = np.stack([su, su, su, su, su.T, su.T, sle, sle, sle, sle], 0).transpose(1, 0, 2)
    ident = np.eye(128, dtype=np.float32)
    rmask = np.ones((64, 2 * SEG), np.float32)
    rmask[:, ::C] = 0
    ones64 = np.ones((64, 64), np.float32)
    in_maps = []
    for i in range(8):
        cq = slice(i * 128, (i + 1) * 128)
        F = np.stack([lc[i][n][:, :T].reshape(2, 64, T) for n in ("wdec", "nkk", "kka", "kt", "rT", "vrT")], 0)
        F = F.transpose(2, 0, 1, 3)
        gb = np.stack([lc[i]["g"][:, :T].reshape(2, 64, T), lc[i]["bonus"][:, :T].reshape(2, 64, T)], 0)
        gb = gb.transpose(2, 0, 1, 3)
        gnv = np.stack([I["gn_w"][0][cq].reshape(2, 64), I["gn_b"][0][cq].reshape(2, 64)], -1).transpose(1, 0, 2)
        in_maps.append({"F": np.ascontiguousarray(F), "gb": np.ascontiguousarray(gb), "gnv": np.ascontiguousarray(gnv),
                        "ident": ident, "msk": np.ascontiguousarray(msk), "rmask": rmask, "ones64": ones64})
    res = _run(lambda nc, p, st: build_rwkv_chunked(nc, p, st, T), in_maps)
    return np.concatenate([r["o"].transpose(2, 1, 0).reshape(T, 128) for r in res], axis=1)
```
